# Optimizing a Trainium2 kernel written in Bass

```python
import math
import jax, jax.numpy as jnp
from jax import lax
import numpy as np

D_MODEL = 1024
BATCH = 2
SEQ = 8192
DEPTH = 1

D_MIX = D_MODEL
D_MLSTM = D_MIX // 2
D_S5 = D_MIX - D_MLSTM
MLSTM_HEADS = 4
MLSTM_HEAD_DIM = D_MLSTM // MLSTM_HEADS
MLSTM_CHUNK = 64
CONV_WIDTH = 4
S5_GROUP_CH = 16
S5_GROUPS = D_S5 // S5_GROUP_CH
S5_STATE = 64
D_IN_PROJ = 4 * D_MLSTM + 2 * MLSTM_HEADS + D_S5
N_EXPERT_GROUPS = 4
EXPERTS_PER_GROUP = 8
N_EXPERTS = N_EXPERT_GROUPS * EXPERTS_PER_GROUP
TOP_K_IN_GROUP = 2
D_EXPERT = 512
EPS = 1e-6
LAMBDA_RE_MAX = -1e-4

kernel_name = "hymba_mlstm_s5_hiermoe_block"


def rmsnorm(x, g):
    xf = x.astype(jnp.float32)
    y = xf * lax.rsqrt(jnp.mean(xf * xf, axis=-1, keepdims=True) + EPS) * g.astype(jnp.float32)
    return y.astype(x.dtype)


def causal_depthwise_conv(x, w, b):
    L = x.shape[1]
    W = w.shape[0]
    xp = jnp.pad(x, ((0, 0), (W - 1, 0), (0, 0)))
    out = b
    for j in range(W):
        out = out + xp[:, j:j + L, :] * w[j]
    return out


def mlstm_chunkwise(q, k, v, log_i, log_f):
    Bsz, H, L, d = q.shape
    nc = L // MLSTM_CHUNK

    def chunks(a):
        a = a.reshape(a.shape[:2] + (nc, MLSTM_CHUNK) + a.shape[3:])
        return jnp.moveaxis(a, 2, 0)

    mask = jnp.tril(jnp.ones((MLSTM_CHUNK, MLSTM_CHUNK), dtype=bool))

    def step(carry, inp):
        C, n, m = carry
        qc, kc, vc, li, lf = inp
        b = jnp.cumsum(lf, axis=-1)
        dmat = b[..., :, None] - b[..., None, :] + li[..., None, :]
        dmat = jnp.where(mask, dmat, -jnp.inf)
        inter = b + m[..., None]
        m_row = jnp.maximum(inter, jnp.max(dmat, axis=-1))
        w_intra = jnp.exp(dmat - m_row[..., None])
        w_inter = jnp.exp(inter - m_row)
        s = jnp.einsum('bhjd,bhsd->bhjs', qc, kc) * w_intra
        num = jnp.einsum('bhjs,bhsd->bhjd', s, vc) + w_inter[..., None] * jnp.einsum('bhvk,bhjk->bhjv', C, qc)
        den = jnp.sum(s, axis=-1) + w_inter * jnp.einsum('bhk,bhjk->bhj', n, qc)
        h = num / jnp.maximum(jnp.abs(den), jnp.exp(-m_row))[..., None]
        b_last = b[..., -1]
        d_state = b_last[..., None] - b + li
        m_new = jnp.maximum(b_last + m, jnp.max(d_state, axis=-1))
        w_s = jnp.exp(d_state - m_new[..., None])
        w_c = jnp.exp(b_last + m - m_new)
        C_new = w_c[..., None, None] * C + jnp.einsum('bhs,bhsv,bhsk->bhvk', w_s, vc, kc)
        n_new = w_c[..., None] * n + jnp.einsum('bhs,bhsk->bhk', w_s, kc)
        return (C_new, n_new, m_new), h

    init = (jnp.zeros((Bsz, H, d, d), jnp.float32),
            jnp.zeros((Bsz, H, d), jnp.float32),
            jnp.zeros((Bsz, H), jnp.float32))
    _, h = lax.scan(step, init, (chunks(q), chunks(k), chunks(v), chunks(log_i), chunks(log_f)))
    h = jnp.moveaxis(h, 0, 2)
    return h.reshape(Bsz, H, L, d)


def s5_combine(e1, e2):
    a1r, a1i, b1r, b1i = e1
    a2r, a2i, b2r, b2i = e2
    return (a2r * a1r - a2i * a1i,
            a2r * a1i + a2i * a1r,
            a2r * b1r - a2i * b1i + b2r,
            a2r * b1i + a2i * b1r + b2i)


def s5_group_mixer(u, lam_re, lam_im, log_dt, b_re, b_im, c_re, c_im, d_skip, glu_w, glu_b):
    f32 = jnp.float32
    lr = jnp.minimum(lam_re.astype(f32), LAMBDA_RE_MAX)
    li = lam_im.astype(f32)
    dt = jnp.exp(log_dt.astype(f32))[:, None]
    mag = jnp.exp(lr * dt)
    ar = mag * jnp.cos(li * dt)
    ai = mag * jnp.sin(li * dt)
    den = lr * lr + li * li
    cr = ((ar - 1.0) * lr + ai * li) / den
    ci = (ai * lr - (ar - 1.0) * li) / den
    br, bi = b_re.astype(f32), b_im.astype(f32)
    bbar_r = cr[..., None] * br - ci[..., None] * bi
    bbar_i = cr[..., None] * bi + ci[..., None] * br
    bu_r = jnp.einsum('blgh,gnh->blgn', u, bbar_r)
    bu_i = jnp.einsum('blgh,gnh->blgn', u, bbar_i)
    a_r = jnp.broadcast_to(ar, bu_r.shape)
    a_i = jnp.broadcast_to(ai, bu_i.shape)
    _, _, s_r, s_i = lax.associative_scan(s5_combine, (a_r, a_i, bu_r, bu_i), axis=1)
    y = (jnp.einsum('blgn,ghn->blgh', s_r, c_re.astype(f32))
         - jnp.einsum('blgn,ghn->blgh', s_i, c_im.astype(f32))
         + d_skip.astype(f32) * u)
    y = jax.nn.gelu(y)
    z = jnp.einsum('blgh,ghc->blgc', y, glu_w.astype(f32)) + glu_b.astype(f32)
    return z[..., :S5_GROUP_CH] * jax.nn.sigmoid(z[..., S5_GROUP_CH:])


def hybrid_mixer(h, w_in, conv_w, conv_b, i_bias, f_bias, mlstm_norm_g,
                 lam_re, lam_im, log_dt, b_re, b_im, c_re, c_im, d_skip, glu_w, glu_b, w_out):
    f32 = jnp.float32
    Bsz, L, _ = h.shape
    H, d = MLSTM_HEADS, MLSTM_HEAD_DIM
    proj = (h @ w_in).astype(f32)
    qk_raw = proj[..., :2 * D_MLSTM]
    v = proj[..., 2 * D_MLSTM:3 * D_MLSTM]
    o_pre = proj[..., 3 * D_MLSTM:4 * D_MLSTM]
    i_pre = proj[..., 4 * D_MLSTM:4 * D_MLSTM + H] + i_bias.astype(f32)
    f_pre = proj[..., 4 * D_MLSTM + H:4 * D_MLSTM + 2 * H] + f_bias.astype(f32)
    u = proj[..., 4 * D_MLSTM + 2 * H:]

    qk = jax.nn.silu(causal_depthwise_conv(qk_raw, conv_w.astype(f32), conv_b.astype(f32)))
    q, k = qk[..., :D_MLSTM], qk[..., D_MLSTM:]

    def heads(a):
        return a.reshape(Bsz, L, H, d).transpose(0, 2, 1, 3)

    log_i = i_pre.transpose(0, 2, 1)
    log_f = jax.nn.log_sigmoid(f_pre).transpose(0, 2, 1)
    h_m = mlstm_chunkwise(heads(q), heads(k) * (d ** -0.5), heads(v), log_i, log_f)
    h_m = h_m * lax.rsqrt(jnp.mean(h_m * h_m, axis=-1, keepdims=True) + EPS)
    h_m = h_m.transpose(0, 2, 1, 3).reshape(Bsz, L, D_MLSTM) * mlstm_norm_g.astype(f32)
    out_a = jax.nn.sigmoid(o_pre) * h_m

    out_b = s5_group_mixer(u.reshape(Bsz, L, S5_GROUPS, S5_GROUP_CH), lam_re, lam_im, log_dt,
                           b_re, b_im, c_re, c_im, d_skip, glu_w, glu_b).reshape(Bsz, L, D_S5)

    merged = jnp.concatenate([out_a, out_b], axis=-1).astype(h.dtype)
    return merged @ w_out


def hierarchical_moe(h, rg_w, rg_b, re_w, re_b, w_gate, w_up, w_down):
    f32 = jnp.float32
    Bsz, L, D = h.shape
    t = h.reshape(Bsz * L, D)
    T = t.shape[0]
    g_logits = (t @ rg_w).astype(f32) + rg_b.astype(f32)
    p_g = jax.nn.softmax(g_logits, axis=-1)
    g_w, g_idx = lax.top_k(p_g, 1)
    e_logits = ((t @ re_w).astype(f32) + re_b.astype(f32)).reshape(T, N_EXPERT_GROUPS, EXPERTS_PER_GROUP)
    e_logits = jnp.take_along_axis(e_logits, g_idx[:, :, None], axis=1)[:, 0]
    p_e = jax.nn.softmax(e_logits, axis=-1)
    e_w, e_idx = lax.top_k(p_e, TOP_K_IN_GROUP)
    e_w = e_w / jnp.sum(e_w, axis=-1, keepdims=True)
    weights = g_w * e_w
    expert_id = g_idx * EXPERTS_PER_GROUP + e_idx
    combine = jnp.einsum('tk,tke->te', weights, jax.nn.one_hot(expert_id, N_EXPERTS, dtype=f32))
    combine = combine.astype(h.dtype)
    y = jnp.zeros_like(t)
    for e in range(N_EXPERTS):
        ye = (jax.nn.silu(t @ w_gate[e]) * (t @ w_up[e])) @ w_down[e]
        y = y + combine[:, e:e + 1] * ye
    return y.reshape(Bsz, L, D)


def setup_inputs(seed: int = 0) -> dict:
    key = jax.random.key(seed)
    ks = jax.random.split(key, 32)
    f32 = jnp.float32
    nrm = lambda k, shape, s: jax.random.normal(k, shape, f32) * s
    H = MLSTM_HEADS
    G, N, C = S5_GROUPS, S5_STATE, S5_GROUP_CH
    x = jax.random.normal(ks[0], (BATCH, SEQ, D_MODEL), f32)
    norm_mix_g = 1.0 + nrm(ks[1], (DEPTH, D_MODEL), 0.02)
    w_in = nrm(ks[2], (DEPTH, D_MODEL, D_IN_PROJ), D_MODEL ** -0.5)
    conv_w = nrm(ks[3], (DEPTH, CONV_WIDTH, 2 * D_MLSTM), CONV_WIDTH ** -0.5)
    conv_b = nrm(ks[4], (DEPTH, 2 * D_MLSTM), 0.01)
    i_bias = nrm(ks[5], (DEPTH, H), 0.1)
    f_bias = jnp.linspace(3.0, 6.0, H, dtype=f32)[None, :] + nrm(ks[6], (DEPTH, H), 0.1)
    mlstm_norm_g = 1.0 + nrm(ks[7], (DEPTH, D_MLSTM), 0.02)
    s5_lambda_re = -0.5 + nrm(ks[8], (DEPTH, G, N), 0.01)
    s5_lambda_im = math.pi * jnp.arange(N, dtype=f32)[None, None, :] + nrm(ks[9], (DEPTH, G, N), 0.01)
    s5_log_dt = jax.random.uniform(ks[10], (DEPTH, G), f32, math.log(0.001), math.log(0.1))
    s5_b_re = nrm(ks[11], (DEPTH, G, N, C), (2.0 * C) ** -0.5)
    s5_b_im = nrm(ks[12], (DEPTH, G, N, C), (2.0 * C) ** -0.5)
    s5_c_re = nrm(ks[13], (DEPTH, G, C, N), (2.0 * N) ** -0.5)
    s5_c_im = nrm(ks[14], (DEPTH, G, C, N), (2.0 * N) ** -0.5)
    s5_d = nrm(ks[15], (DEPTH, G, C), 1.0)
    s5_glu_w = nrm(ks[16], (DEPTH, G, C, 2 * C), C ** -0.5)
    s5_glu_b = nrm(ks[17], (DEPTH, G, 2 * C), 0.01)
    w_out = nrm(ks[18], (DEPTH, D_MIX, D_MODEL), D_MIX ** -0.5)
    norm_ffn_g = 1.0 + nrm(ks[19], (DEPTH, D_MODEL), 0.02)
    router_group_w = nrm(ks[20], (DEPTH, D_MODEL, N_EXPERT_GROUPS), D_MODEL ** -0.5)
    router_group_b = nrm(ks[21], (DEPTH, N_EXPERT_GROUPS), 0.01)
    router_expert_w = nrm(ks[22], (DEPTH, D_MODEL, N_EXPERTS), D_MODEL ** -0.5)
    router_expert_b = nrm(ks[23], (DEPTH, N_EXPERTS), 0.01)
    expert_w_gate = nrm(ks[24], (DEPTH, N_EXPERTS, D_MODEL, D_EXPERT), D_MODEL ** -0.5)
    expert_w_up = nrm(ks[25], (DEPTH, N_EXPERTS, D_MODEL, D_EXPERT), D_MODEL ** -0.5)
    expert_w_down = nrm(ks[26], (DEPTH, N_EXPERTS, D_EXPERT, D_MODEL), D_EXPERT ** -0.5)
    norm_final_g = 1.0 + nrm(ks[27], (D_MODEL,), 0.02)
    return {"x": x, "norm_mix_g": norm_mix_g, "w_in": w_in, "conv_w": conv_w, "conv_b": conv_b,
            "i_bias": i_bias, "f_bias": f_bias, "mlstm_norm_g": mlstm_norm_g,
            "s5_lambda_re": s5_lambda_re, "s5_lambda_im": s5_lambda_im, "s5_log_dt": s5_log_dt,
            "s5_b_re": s5_b_re, "s5_b_im": s5_b_im, "s5_c_re": s5_c_re, "s5_c_im": s5_c_im,
            "s5_d": s5_d, "s5_glu_w": s5_glu_w, "s5_glu_b": s5_glu_b, "w_out": w_out,
            "norm_ffn_g": norm_ffn_g, "router_group_w": router_group_w, "router_group_b": router_group_b,
            "router_expert_w": router_expert_w, "router_expert_b": router_expert_b,
            "expert_w_gate": expert_w_gate, "expert_w_up": expert_w_up, "expert_w_down": expert_w_down,
            "norm_final_g": norm_final_g}


def reference(x, norm_mix_g, w_in, conv_w, conv_b, i_bias, f_bias, mlstm_norm_g,
              s5_lambda_re, s5_lambda_im, s5_log_dt, s5_b_re, s5_b_im, s5_c_re, s5_c_im,
              s5_d, s5_glu_w, s5_glu_b, w_out, norm_ffn_g, router_group_w, router_group_b,
              router_expert_w, router_expert_b, expert_w_gate, expert_w_up, expert_w_down,
              norm_final_g):
    for l in range(DEPTH):
        h = rmsnorm(x, norm_mix_g[l])
        mix = hybrid_mixer(h, w_in[l], conv_w[l], conv_b[l], i_bias[l], f_bias[l], mlstm_norm_g[l],
                           s5_lambda_re[l], s5_lambda_im[l], s5_log_dt[l], s5_b_re[l], s5_b_im[l],
                           s5_c_re[l], s5_c_im[l], s5_d[l], s5_glu_w[l], s5_glu_b[l], w_out[l])
        x = x + mix.astype(x.dtype)
        h = rmsnorm(x, norm_ffn_g[l])
        ffn = hierarchical_moe(h, router_group_w[l], router_group_b[l], router_expert_w[l], router_expert_b[l],
                               expert_w_gate[l], expert_w_up[l], expert_w_down[l])
        x = x + ffn.astype(x.dtype)
    return rmsnorm(x, norm_final_g)
```

```python
import numpy as np
from contextlib import ExitStack
import concourse.bass as bass
import concourse.mybir as mybir
from concourse.bass_utils import run_bass_kernel_spmd

F32 = mybir.dt.float32
BF16 = mybir.dt.bfloat16
AF = mybir.ActivationFunctionType
ALU = mybir.AluOpType

NTOK = 2048
NT = 16
NCH = 32
HALO = 32
D = 1024
EPS = 1e-6
NCORES = 8
GROUPS4 = [[0, 1, 2, 3], [4, 5, 6, 7]]


class Prog:
    def __init__(self, nc, es):
        self.nc = nc
        self.es = es
        self.queues = {e: [] for e in ("pe", "act", "dve", "pool", "sp")}
        self.esem = {}
        self.ecnt = {}
        for e in ("pe", "act", "dve", "pool"):
            self.esem[e] = es.enter_context(nc.semaphore("sem_" + e))
            self.ecnt[e] = 0
        self.dsem = {}
        self.dcnt = {}
        self.lastw = {}
        self.readers = {}
        self.known = {e: {} for e in self.queues}

    def _deps(self, reads, writes):
        deps = []
        for r in reads:
            if r in self.lastw:
                deps.append(self.lastw[r])
        for w in writes:
            if w in self.lastw:
                deps.append(self.lastw[w])
            deps.extend(self.readers.get(w, ()))
        return deps

    def _prune(self, eng, deps):
        need = {}
        for (s, v) in deps:
            if eng == "pe" and s is self.esem["pe"]:
                continue
            if v > need.get(s, 0):
                need[s] = v
        out = []
        kn = self.known[eng]
        for s, v in need.items():
            if kn.get(s, 0) >= v:
                continue
            kn[s] = v
            out.append((s, v))
        return out

    def _record(self, tok, reads, writes):
        for r in reads:
            self.readers.setdefault(r, []).append(tok)
        for w in writes:
            self.lastw[w] = tok
            self.readers[w] = []

    def op(self, eng, fn, reads=(), writes=()):
        reads = tuple(reads)
        writes = tuple(writes)
        waits = self._prune(eng, self._deps(reads, writes))
        self.ecnt[eng] += 1
        tok = (self.esem[eng], self.ecnt[eng])
        self.queues[eng].append((waits, fn, (self.esem[eng], 1)))
        self._record(tok, reads, writes)

    def seq(self, eng, fns, reads=(), writes=()):
        self._chain = getattr(self, "_chain", 0) + 1
        ck = f"__chain{self._chain}"
        for fn in fns:
            self.op(eng, fn, reads=tuple(reads) + (ck,), writes=tuple(writes) + (ck,))

    def dma(self, q, fn, stream, reads=(), writes=(), inc=16):
        reads = tuple(reads)
        writes = tuple(writes)
        if stream not in self.dsem:
            self.dsem[stream] = self.es.enter_context(self.nc.semaphore("dsem_" + stream))
            self.dcnt[stream] = 0
        waits = self._prune(q, self._deps(reads, writes))
        self.dcnt[stream] += inc
        tok = (self.dsem[stream], self.dcnt[stream])
        self.queues[q].append((waits, fn, (self.dsem[stream], inc)))
        self._record(tok, reads, writes)

    def bulk_done(self, stream):
        s = self.dsem[stream]
        fin = self.dcnt[stream]
        for k, (ss, v) in list(self.lastw.items()):
            if ss is s:
                self.lastw[k] = (s, fin)

    def barrier(self):
        allw = [(self.esem[e], self.ecnt[e]) for e in self.esem if self.ecnt[e] > 0]
        allw += [(self.dsem[s], self.dcnt[s]) for s in self.dsem]
        for e in self.queues:
            w = self._prune(e, allw)
            if w:
                self.queues[e].append((w, None, None))

    def final_wait(self, q, streams):
        waits = [(self.dsem[st], self.dcnt[st]) for st in streams if st in self.dsem]
        self.queues[q].append((waits, None, None))

    def replay(self, eng, h):
        for waits, fn, inc in self.queues[eng]:
            for (s, v) in waits:
                h.wait_ge(s, v)
            if fn is None:
                continue
            inst = fn(h)
            inst.then_inc(inc[0], inc[1])


def build(stage=99, dbg=()):
    nc = bass.Bass("TRN2", target_bir_lowering=False)
    din = lambda name, shape, dt=F32: nc.dram_tensor(name, shape, dt, kind="ExternalInput")
    x = din("x", [NTOK, D])
    xh = din("xh", [HALO, D])
    g_mix = din("g_mix", [1, D])
    w_in = din("w_in", [D, 2568])
    conv_wT = din("conv_wT", [128, 8, 4])
    conv_b = din("conv_b", [128, 8])
    i_bias = din("i_bias", [1, 4])
    f_bias = din("f_bias", [1, 4])
    g_ml = din("g_ml", [1, 512])
    pmask = din("pmask", [128, 4])
    lam2_re = din("lam2_re", [128, 32])
    lam2_im = din("lam2_im", [128, 32])
    logdt = din("logdt", [1, 32])
    X1d = din("X1d", [128, 32, 16])
    X2d = din("X2d", [128, 32, 16])
    CY1d = din("CY1d", [128, 32, 16])
    CY2d = din("CY2d", [128, 32, 16])
    s5d = din("s5d", [1, 512])
    WAd = din("WAd", [4, 128, 128])
    WBd = din("WBd", [4, 128, 128])
    gbad = din("gbad", [128, 4])
    gbbd = din("gbbd", [128, 4])
    w_out = din("w_out", [D, D])
    g_ffn = din("g_ffn", [1, D])
    w_rt = din("w_rt", [D, 36])
    b_rt = din("b_rt", [1, 36])
    if stage >= 5:
        w_gate = din("w_gate", [32, D, 512])
        w_up = din("w_up", [32, D, 512])
        w_down = din("w_down", [32, 512, D])
    g_fin = din("g_fin", [1, D])
    y = nc.dram_tensor("y", [NTOK, D], F32, kind="ExternalOutput")
    dbg_out = {}
    def dbgt(name, shape, dt=F32):
        dbg_out[name] = nc.dram_tensor("dbg_" + name, shape, dt, kind="ExternalOutput")
        return dbg_out[name]

    mergedT_scr = nc.dram_tensor("mergedT_scr", [1024, NTOK], BF16)
    cc_in = [nc.dram_tensor(f"cc_in{h}", [128, 130], F32) for h in range(4)]
    cc_out = [nc.dram_tensor(f"cc_out{h}", [512, 130], F32) for h in range(4)]
    u_scr = nc.dram_tensor("u_scr", [NTOK, 512], BF16)
    xmid_scr = nc.dram_tensor("xmid_scr", [NTOK, D], F32)
    ccs_in = [nc.dram_tensor(f"ccs_in{h}", [128, 8], F32) for h in range(4)]
    ccs_out = [nc.dram_tensor(f"ccs_out{h}", [512, 8], F32) for h in range(4)]

    w_in_v = w_in.ap().rearrange("(kc p) c -> p kc c", p=128)

    es = ExitStack()
    with es:
        P = Prog(nc, es)
        sb = lambda name, shape, dt: es.enter_context(nc.sbuf_tensor(name, shape, dt))
        ps = lambda name, shape, dt: es.enter_context(nc.psum_tensor(name, shape, dt))

        g_bc = sb("g_bc", [128, D], F32)
        ident = sb("ident", [128, 128], BF16)
        identf = sb("identf", [128, 128], F32)
        mslab = sb("mslab", [128, NTOK], BF16)
        esH = ExitStack()
        esH.__enter__()
        hT = esH.enter_context(nc.sbuf_tensor("hT", [128, 8, HALO + NTOK], BF16))
        esB = ExitStack()
        esB.__enter__()
        sb = lambda name, shape, dt: esB.enter_context(nc.sbuf_tensor(name, shape, dt))
        ps = lambda name, shape, dt: esB.enter_context(nc.psum_tensor(name, shape, dt))
        xt = [sb(f"xt{i}", [128, D], F32) for i in range(2)]
        junk = sb("junk", [128, D], BF16)
        hb = sb("hb", [128, D], BF16)
        ssq = sb("ssq", [128, NT + 1], F32)
        rstd = sb("rstd", [128, NT + 1], F32)
        wh = sb("wh", [128, 8, 512], BF16)
        wg = sb("wg", [128, 8, 33], BF16)
        raw = sb("raw", [128, NTOK + 3], F32)
        ctmp = sb("ctmp", [128, NTOK], F32)
        ctmp2 = sb("ctmp2", [128, NTOK], F32)
        qsT = sb("qsT", [128, NTOK], BF16)
        ksT = sb("ksT", [128, NTOK], BF16)
        cwt = sb("cwt", [128, 8, 4], F32)
        cbt = sb("cbt", [128, 8], F32)
        fbt = sb("fbt", [33, 4], F32)
        nfb = sb("nfb", [33, 4], F32)
        Gt = sb("Gt", [33, NTOK], F32)
        Gt2 = sb("Gt2", [33, NTOK], F32)
        cmask = sb("cmask", [33, NTOK], F32)
        sel0 = sb("sel0", [33, 128], F32)
        sel032 = sb("sel032", [33, 128], F32)
        expb = sb("expb", [128, NTOK], F32)
        e2bc = sb("e2bc", [128, NTOK], F32)
        v_ext = sb("v_ext", [64, NCH, 130], BF16)
        sigo = sb("sigo", [64, NCH, 128], BF16)
        so32 = sb("so32", [64, 128], F32)
        gA = sb("gA", [64, 512], F32)
        kstok = sb("kstok", [64, NCH, 128], BF16)
        CT = sb("CT", [128, 130], F32)
        CTt = sb("CTt", [128, 130], F32)
        CTb = sb("CTb", [128, 130], BF16)
        cg = sb("cg", [128, 4, 130], F32)
        hacc = sb("hacc", [128, 130], F32)
        hacc2 = sb("hacc2", [128, 130], F32)
        pm = sb("pm", [128, 4], F32)
        maskST = sb("maskST", [64, 64], F32)
        ST = sb("ST", [64, 64], BF16)
        dn = sb("dn", [64, 4], F32)
        hn = sb("hn", [64, 128], F32)
        oa = sb("oa", [64, 128], BF16)

        ps_big = [ps(f"ps_big{i}", [128, 512], F32) for i in range(2)]
        tp = ps("tp", [128, 8, 128], BF16)
        pv = ps("pv", [64, 256], F32)
        pa = ps("pa", [64, 64], F32)
        pc = ps("pc", [64, 130], F32)
        pu = ps("pu", [128, 130], F32)
        pt = ps("pt", [128, 128], BF16)

        P.dma("sp", lambda e: e.dma_start(out=g_bc[:, :], in_=g_mix[0:1, :].partition_broadcast(128)), "setup", writes=["g_bc"])
        P.dma("sp", lambda e: e.dma_start(out=cwt[:, :, :], in_=conv_wT[:, :, :]), "setup", writes=["cwt"])
        P.dma("sp", lambda e: e.dma_start(out=cbt[:, :], in_=conv_b[:, :]), "setup", writes=["cbt"])
        P.dma("sp", lambda e: e.dma_start(out=fbt[0:1, :], in_=f_bias[0:1, :]), "setup", writes=["fbt0"])
        P.dma("sp", lambda e: e.dma_start(out=fbt[32:33, :], in_=i_bias[0:1, :]), "setup", writes=["fbt32"])
        P.dma("sp", lambda e: e.dma_start(out=gA[:, :], in_=g_ml[0:1, :].partition_broadcast(64)), "setup", writes=["gA"])
        P.dma("sp", lambda e: e.dma_start(out=pm[:, :], in_=pmask[:, :]), "setup", writes=["pm"])
        P.bulk_done("setup")

        P.seq("pool", [lambda e: e.memset(identf[:, :], 0.0),
                       lambda e: e.affine_select(out=identf[:, :], in_=identf[:, :], pattern=[[-1, 128]], compare_op=ALU.not_equal,
                                                 fill=1.0, base=0, channel_multiplier=1)], writes=["identf"])
        P.op("dve", lambda e: e.tensor_copy(out=ident[:, :], in_=identf[:, :]), reads=["identf"], writes=["ident"])

        P.seq("pool", [lambda e: e.memset(cmask[:, :], 1.0),
                       lambda e: e.memset(cmask[:, 0:NTOK:64], 0.0),
                       lambda e: e.memset(sel0[:, :], 0.0),
                       lambda e: e.memset(sel0[0:1, :], 1.0),
                       lambda e: e.memset(sel032[:, :], 0.0),
                       lambda e: e.memset(sel032[0:1, :], 1.0),
                       lambda e: e.memset(sel032[32:33, :], 1.0),
                       lambda e: e.memset(Gt2[:, :], 0.0),
                       lambda e: e.memset(v_ext[:, :, 128:129], 1.0),
                       lambda e: e.memset(v_ext[:, :, 129:130], 0.0),
                       lambda e: e.memset(maskST[:, :], 1.0),
                       lambda e: e.affine_select(out=maskST[:, :], in_=maskST[:, :], pattern=[[1, 64]], compare_op=ALU.is_ge,
                                                 fill=0.0, base=0, channel_multiplier=-1)],
              writes=["cmask", "sel0", "sel032", "Gt2", "v_ext_c", "maskST"])
        P.op("pool", lambda e: e.tensor_scalar(out=nfb[0:1, :], in0=fbt[0:1, :], scalar1=-1.0, scalar2=None, op0=ALU.mult),
             reads=["fbt0"], writes=["nfb"])

        def norm_tile(src_ap, rows, slot, col0, idx, tag):
            s = slot
            P.dma("sp", lambda e: e.dma_start(out=xt[s][0:rows, :], in_=src_ap), f"xt{s}", writes=[f"xt{s}"])
            P.op("act", lambda e: e.activation(out=junk[0:rows, :], in_=xt[s][0:rows, :], func=AF.Square, accum_out=ssq[0:rows, idx:idx + 1]),
                 reads=[f"xt{s}"], writes=["junk", f"ssq{idx}"])
            P.op("act", lambda e: e.activation(out=ssq[0:rows, idx:idx + 1], in_=ssq[0:rows, idx:idx + 1], func=AF.Sqrt, scale=1.0 / D, bias=EPS),
                 reads=[f"ssq{idx}"], writes=[f"ssq{idx}"])
            P.op("dve", lambda e: e.reciprocal(out=rstd[0:rows, idx:idx + 1], in_=ssq[0:rows, idx:idx + 1]), reads=[f"ssq{idx}"], writes=[f"rstd{idx}"])
            P.op("dve", lambda e: e.scalar_tensor_tensor(out=hb[0:rows, :], in0=xt[s][0:rows, :], scalar=rstd[0:rows, idx:idx + 1], in1=g_bc[0:rows, :],
                                                         op0=ALU.mult, op1=ALU.mult),
                 reads=[f"xt{s}", f"rstd{idx}", "g_bc"], writes=["hb"])
            def f_tp(e):
                for k in range(8):
                    i = e.transpose(out=tp[:, k, 0:rows], in_=hb[0:rows, k * 128:(k + 1) * 128], identity=ident[0:rows, 0:rows])
                return i
            P.op("pe", f_tp, reads=["hb", "ident"], writes=["tp"])
            P.op("act", lambda e: e.copy(out=hT[:, :, col0:col0 + rows], in_=tp[:, :, 0:rows]), reads=["tp"], writes=[tag])

        norm_tile(xh[:, :], HALO, 0, 0, NT, "hT_h")
        for t in range(NT):
            norm_tile(x[t * 128:(t + 1) * 128, :], 128, (t + 1) % 2, HALO + t * 128, t, f"hT_{t // 4}")
        hT_all = ["hT_h"] + [f"hT_{i}" for i in range(4)]

        SC = float(128 ** -0.5)
        for hd in range(4 if stage >= 2 else 0):
            for j, c0 in enumerate((hd * 128, 512 + hd * 128, 1024 + hd * 128, 1536 + hd * 128)):
                P.dma("pool", lambda e, j=j, c0=c0: e.dma_start(out=wh[:, :, j * 128:(j + 1) * 128], in_=w_in_v[:, :, c0:c0 + 128]),
                      "wh", writes=[f"wh{j}"])
            P.op("pool", lambda e: e.memset(wg[:, :, :], 0.0), writes=["wg", "wg_a", "wg_b"])
            def ld_wg(e, dst, col):
                with nc.allow_non_contiguous_dma(reason="gate columns"):
                    return e.dma_start(out=wg[:, :, dst:dst + 1], in_=w_in_v[:, :, col:col + 1])
            P.dma("pool", lambda e, hd=hd: ld_wg(e, 0, 2052 + hd), "wg", reads=["wg"], writes=["wg_a"])
            P.dma("pool", lambda e, hd=hd: ld_wg(e, 32, 2048 + hd), "wg", reads=["wg"], writes=["wg_b"])

            for tb in range(4):
                pgt = ps_big[tb % 2]
                def f_g(e, tb=tb, pgt=pgt):
                    for k in range(8):
                        i = e.matmul(out=pgt[0:33, :], lhsT=wg[:, k, :], rhs=hT[:, k, HALO + tb * 512:HALO + (tb + 1) * 512], start=(k == 0), stop=(k == 7))
                    return i
                P.op("pe", f_g, reads=["wg", "wg_a", "wg_b", f"hT_{tb}"], writes=[f"psb{tb % 2}"])
                P.op("act", lambda e, tb=tb, pgt=pgt: e.copy(out=Gt[0:33, tb * 512:(tb + 1) * 512], in_=pgt[0:33, :]),
                     reads=[f"psb{tb % 2}"], writes=[f"Gt_{tb}"])
            Gt_all = [f"Gt_{tb}" for tb in range(4)]
            P.op("act", lambda e, hd=hd: e.activation(out=Gt[0:1, :], in_=Gt[0:1, :], func=AF.Exp, scale=-1.0, bias=nfb[0:1, hd:hd + 1]),
                 reads=Gt_all + ["nfb"], writes=["Gt_f"])
            P.op("act", lambda e: e.activation(out=Gt[0:1, :], in_=Gt[0:1, :], func=AF.Ln, bias=1.0), reads=["Gt_f"], writes=["Gt_f"])
            P.op("dve", lambda e: e.tensor_tensor_scan(out=Gt2[0:1, :], data0=cmask[0:1, :], data1=Gt[0:1, :], initial=0.0, op0=ALU.mult, op1=ALU.add),
                 reads=["Gt_f", "cmask"], writes=["Gt2_0"])
            P.op("act", lambda e, hd=hd: e.activation(out=Gt2[32:33, :], in_=Gt[32:33, :], func=AF.Identity, bias=fbt[32:33, hd:hd + 1]),
                 reads=Gt_all + ["fbt32", "Gt2"], writes=["Gt2_32"])
            for tb in range(4):
                pgt = ps_big[tb % 2]
                P.op("pe", lambda e, tb=tb, pgt=pgt: e.matmul(out=pgt[:, :], lhsT=sel0[0:33, :], rhs=Gt2[0:33, tb * 512:(tb + 1) * 512], start=True, stop=True),
                     reads=["sel0", "Gt2", "Gt2_0", "Gt2_32"], writes=[f"psb{tb % 2}"])
                P.op("act", lambda e, tb=tb, pgt=pgt: e.activation(out=expb[:, tb * 512:(tb + 1) * 512], in_=pgt[:, :], func=AF.Exp, scale=-1.0),
                     reads=[f"psb{tb % 2}"], writes=[f"expb_{tb}"])
            for tb in range(4):
                pgt = ps_big[tb % 2]
                P.op("pe", lambda e, tb=tb, pgt=pgt: e.matmul(out=pgt[:, :], lhsT=sel032[0:33, :], rhs=Gt2[0:33, tb * 512:(tb + 1) * 512], start=True, stop=True),
                     reads=["sel032", "Gt2", "Gt2_0", "Gt2_32"], writes=[f"psb{tb % 2}"])
                P.op("act", lambda e, tb=tb, pgt=pgt: e.activation(out=e2bc[:, tb * 512:(tb + 1) * 512], in_=pgt[:, :], func=AF.Exp),
                     reads=[f"psb{tb % 2}"], writes=[f"e2bc_{tb}"])
            expb_all = [f"expb_{tb}" for tb in range(4)]
            e2_all = [f"e2bc_{tb}" for tb in range(4)]

            for qi in range(2):
                cidx = qi * 4 + hd
                def f_halo(e, qi=qi):
                    for k in range(8):
                        i = e.matmul(out=ps_big[0][:, 0:HALO], lhsT=wh[:, k, qi * 128:(qi + 1) * 128], rhs=hT[:, k, 0:HALO], start=(k == 0), stop=(k == 7))
                    return i
                P.op("pe", f_halo, reads=[f"wh{qi}", "hT_h"], writes=["psb0"])
                P.op("act", lambda e: e.copy(out=raw[:, 0:3], in_=ps_big[0][:, HALO - 3:HALO]), reads=["psb0"], writes=["raw_h"])
                for tb in range(4):
                    pgt = ps_big[(tb + 1) % 2]
                    def f_q(e, tb=tb, qi=qi, pgt=pgt):
                        for k in range(8):
                            i = e.matmul(out=pgt[:, :], lhsT=wh[:, k, qi * 128:(qi + 1) * 128], rhs=hT[:, k, HALO + tb * 512:HALO + (tb + 1) * 512],
                                         start=(k == 0), stop=(k == 7))
                        return i
                    P.op("pe", f_q, reads=[f"wh{qi}", f"hT_{tb}"], writes=[f"psb{(tb + 1) % 2}"])
                    P.op("act", lambda e, tb=tb, pgt=pgt: e.copy(out=raw[:, 3 + tb * 512:3 + (tb + 1) * 512], in_=pgt[:, :]),
                         reads=[f"psb{(tb + 1) % 2}"], writes=[f"raw_{tb}"])
                raw_all = ["raw_h"] + [f"raw_{tb}" for tb in range(4)]
                fl = [lambda e, cidx=cidx: e.tensor_scalar(out=ctmp[:, :], in0=raw[:, 0:NTOK], scalar1=cwt[:, cidx, 0:1], scalar2=cbt[:, cidx:cidx + 1],
                                                           op0=ALU.mult, op1=ALU.add)]
                for j in (1, 2, 3):
                    fl.append(lambda e, cidx=cidx, j=j: e.scalar_tensor_tensor(out=ctmp[:, :], in0=raw[:, j:j + NTOK], scalar=cwt[:, cidx, j:j + 1],
                                                                               in1=ctmp[:, :], op0=ALU.mult, op1=ALU.add))
                P.seq("dve", fl, reads=raw_all + ["cwt", "cbt"], writes=["ctmp"])
                P.op("act", lambda e: e.activation(out=ctmp2[:, :], in_=ctmp[:, :], func=AF.Silu), reads=["ctmp"], writes=["ctmp2"])
                if qi == 0:
                    P.op("dve", lambda e: e.tensor_tensor(out=qsT[:, :], in0=ctmp2[:, :], in1=expb[:, :], op=ALU.mult),
                         reads=["ctmp2"] + expb_all, writes=["qsT"])
                else:
                    P.op("dve", lambda e: e.scalar_tensor_tensor(out=ksT[:, :], in0=ctmp2[:, :], scalar=SC, in1=e2bc[:, :], op0=ALU.mult, op1=ALU.mult),
                         reads=["ctmp2"] + e2_all, writes=["ksT"])

            for c in range(NCH):
                def f_v(e, c=c):
                    for k in range(8):
                        i = e.matmul(out=pv[:, :], lhsT=hT[:, k, HALO + c * 64:HALO + (c + 1) * 64], rhs=wh[:, k, 256:512], start=(k == 0), stop=(k == 7))
                    return i
                P.op("pe", f_v, reads=["wh2", "wh3", f"hT_{c // 8}"], writes=["pv"])
                P.op("act", lambda e, c=c: e.copy(out=v_ext[:, c, 0:128], in_=pv[:, 0:128]), reads=["pv"], writes=[f"v_{c}"])
                P.op("act", lambda e: e.activation(out=so32[:, :], in_=pv[:, 128:256], func=AF.Sigmoid), reads=["pv"], writes=["so32"])
                P.op("dve", lambda e, c=c, hd=hd: e.tensor_tensor(out=sigo[:, c, :], in0=so32[:, :], in1=gA[:, hd * 128:(hd + 1) * 128], op=ALU.mult),
                     reads=["so32", "gA"], writes=[f"sigo_{c}"])

            P.seq("pool", [lambda e: e.memset(CT[:, :], 0.0), lambda e: e.memset(CT[:, 129:130], 1.0)], writes=["CT"])

            def state_step(c, with_bf16):
                if f"kstok_{c}" not in P.lastw or True:
                    pass
                def f_u(e, c=c):
                    return e.matmul(out=pu[:, :], lhsT=kstok[:, c, :], rhs=v_ext[:, c, :], start=True, stop=True)
                P.op("pe", f_u, reads=[f"kstok_{c}", f"v_{c}", "v_ext_c"], writes=["pu"])
                P.op("dve", lambda e: e.tensor_tensor(out=CTt[:, :], in0=pu[:, :], in1=CT[:, :], op=ALU.add), reads=["pu", "CT"], writes=["CTt"])
                wc = expb[:, c * 64 + 63:c * 64 + 64]
                P.op("dve", lambda e: e.tensor_scalar(out=CT[:, :], in0=CTt[:, :], scalar1=wc, scalar2=None, op0=ALU.mult),
                     reads=["CTt", f"expb_{c // 8}"], writes=["CT"])
                if with_bf16:
                    P.op("act", lambda e: e.activation(out=CTb[:, :], in_=CTt[:, :], func=AF.Copy, scale=wc),
                         reads=["CTt", f"expb_{c // 8}"], writes=["CTb"])

            for c in range(NCH):
                P.op("pe", lambda e, c=c: e.transpose(out=pt[0:64, :], in_=ksT[:, c * 64:(c + 1) * 64], identity=ident[:, :]),
                     reads=["ksT", "ident"], writes=["pt"])
                P.op("act", lambda e, c=c: e.copy(out=kstok[:, c, :], in_=pt[0:64, :]), reads=["pt"], writes=[f"kstok_{c}"])
                state_step(c, False)

            P.dma("sp", lambda e, hd=hd: e.dma_start(out=cc_in[hd][:, :], in_=CT[:, :]), f"ccin{hd}", reads=["CT"], writes=[f"cc_in{hd}"])
            def f_cc(e, hd=hd):
                return e.collective_compute("AllGather", ALU.bypass, replica_groups=GROUPS4,
                                            ins=[cc_in[hd].ap().opt()], outs=[cc_out[hd].ap().opt()])
            P.dma("pool", f_cc, f"cc{hd}", reads=[f"cc_in{hd}"], writes=[f"cc_out{hd}"], inc=1)
            P.dma("sp", lambda e, hd=hd: e.dma_start(out=cg[:, :, :], in_=cc_out[hd].ap().rearrange("(r p) c -> p r c", p=128)),
                  f"ccld{hd}", reads=[f"cc_out{hd}"], writes=["cg"])
            P.op("pool", lambda e: e.memset(hacc[:, :], 0.0), writes=["hacc"])
            for pp in range(3):
                P.seq("dve", [
                    lambda e, pp=pp: e.scalar_tensor_tensor(out=hacc2[:, :], in0=hacc[:, :], scalar=cg[:, pp, 129:130], in1=cg[:, pp, :], op0=ALU.mult, op1=ALU.add),
                    lambda e: e.tensor_tensor(out=hacc2[:, :], in0=hacc2[:, :], in1=hacc[:, :], op=ALU.subtract),
                    lambda e, pp=pp: e.scalar_tensor_tensor(out=hacc[:, :], in0=hacc2[:, :], scalar=pm[:, pp:pp + 1], in1=hacc[:, :], op0=ALU.mult, op1=ALU.add)],
                    reads=["hacc", "cg", "pm"], writes=["hacc", "hacc2"])
            P.op("dve", lambda e: e.tensor_copy(out=CT[:, :], in_=hacc[:, :]), reads=["hacc"], writes=["CT"])
            P.op("act", lambda e: e.copy(out=CTb[:, :], in_=hacc[:, :]), reads=["hacc"], writes=["CTb"])

            for c in range(NCH):
                cs = slice(c * 64, (c + 1) * 64)
                P.op("pe", lambda e, cs=cs: e.matmul(out=pa[:, :], lhsT=ksT[:, cs], rhs=qsT[:, cs], start=True, stop=True),
                     reads=["ksT", "qsT"], writes=["pa"])
                P.op("dve", lambda e: e.tensor_tensor(out=ST[:, :], in0=pa[:, :], in1=maskST[:, :], op=ALU.mult), reads=["pa", "maskST"], writes=["ST"])
                def f_cn(e, c=c, cs=cs):
                    e.matmul(out=pc[:, :], lhsT=ST[:, :], rhs=v_ext[:, c, :], start=True, stop=False)
                    return e.matmul(out=pc[:, :], lhsT=qsT[:, cs], rhs=CTb[:, :], start=False, stop=True)
                P.op("pe", f_cn, reads=["ST", f"v_{c}", "v_ext_c", "qsT", "CTb"], writes=["pc"])
                P.seq("dve", [
                    lambda e: e.tensor_copy(out=dn[:, 1:2], in_=pc[:, 128:129]),
                    lambda e: e.scalar_tensor_tensor(out=dn[:, 0:1], in0=dn[:, 1:2], scalar=-1.0, in1=dn[:, 1:2], op0=ALU.mult, op1=ALU.max),
                    lambda e: e.tensor_scalar(out=dn[:, 0:1], in0=dn[:, 0:1], scalar1=1.0, scalar2=None, op0=ALU.max),
                    lambda e: e.reciprocal(out=dn[:, 1:2], in_=dn[:, 0:1]),
                    lambda e: e.tensor_scalar(out=hn[:, :], in0=pc[:, 0:128], scalar1=dn[:, 1:2], scalar2=None, op0=ALU.mult)],
                    reads=["pc"], writes=["dn", "hn"])
                P.seq("act", [
                    lambda e: e.activation(out=junk[0:64, 0:128], in_=hn[:, :], func=AF.Square, accum_out=dn[:, 2:3]),
                    lambda e: e.activation(out=dn[:, 2:3], in_=dn[:, 2:3], func=AF.Sqrt, scale=1.0 / 128, bias=EPS)],
                    reads=["hn", "dn"], writes=["junk", "dn2"])
                P.seq("dve", [
                    lambda e: e.reciprocal(out=dn[:, 3:4], in_=dn[:, 2:3]),
                    lambda e, c=c: e.scalar_tensor_tensor(out=oa[:, :], in0=hn[:, :], scalar=dn[:, 3:4], in1=sigo[:, c, :], op0=ALU.mult, op1=ALU.mult)],
                    reads=["dn2", "hn", f"sigo_{c}"], writes=["oa", "dn"])
                P.op("pe", lambda e: e.transpose(out=pt[:, 0:64], in_=oa[:, :], identity=ident[0:64, 0:64]), reads=["oa", "ident"], writes=["pt"])
                P.op("act", lambda e, cs=cs: e.copy(out=mslab[:, cs], in_=pt[:, 0:64]), reads=["pt"], writes=["mslab"])
                state_step(c, True)
            P.dma("sp", lambda e, hd=hd: e.dma_start(out=mergedT_scr[hd * 128:(hd + 1) * 128, :], in_=mslab[:, :]), "mscr", reads=["mslab"], writes=[f"mscr{hd}"])

        esB.close()
        P.barrier()
        if stage >= 3:
            esC1 = ExitStack()
            esC1.__enter__()
            sb = lambda name, shape, dt: esC1.enter_context(nc.sbuf_tensor(name, shape, dt))
            ps = lambda name, shape, dt: esC1.enter_context(nc.psum_tensor(name, shape, dt))
            wu = sb("wu", [128, 8, 512], BF16)
            utok = [sb(f"utok{i}", [128, 512], BF16) for i in range(2)]
            pu1 = [ps(f"pu1_{i}", [128, 512], F32) for i in range(2)]
            P.dma("pool", lambda e: e.dma_start(out=wu[:, :, :], in_=w_in_v[:, :, 2056:2568]), "wu", writes=["wu"])
            for t in range(NT):
                def f_u1(e, t=t):
                    for k in range(8):
                        i = e.matmul(out=pu1[t % 2][:, :], lhsT=hT[:, k, HALO + t * 128:HALO + (t + 1) * 128], rhs=wu[:, k, :], start=(k == 0), stop=(k == 7))
                    return i
                P.op("pe", f_u1, reads=["wu", f"hT_{t // 4}"], writes=[f"pu1_{t % 2}"])
                P.op("act", lambda e, t=t: e.copy(out=utok[t % 2][:, :], in_=pu1[t % 2][:, :]), reads=[f"pu1_{t % 2}"], writes=[f"utok{t % 2}"])
                P.dma("sp", lambda e, t=t: e.dma_start(out=u_scr[t * 128:(t + 1) * 128, :], in_=utok[t % 2][:, :]), f"uscr{t % 2}",
                      reads=[f"utok{t % 2}"], writes=[f"u_scr{t}"])
            esC1.close()
            esH.close()
            P.barrier()

            esC = ExitStack()
            esC.__enter__()
            sb = lambda name, shape, dt: esC.enter_context(nc.sbuf_tensor(name, shape, dt))
            ps = lambda name, shape, dt: esC.enter_context(nc.psum_tensor(name, shape, dt))
            G8 = 8
            lam_re_t = sb("lam_re_t", [128, 32], F32)
            lam_im_t = sb("lam_im_t", [128, 32], F32)
            logdt_t = sb("logdt_t", [128, 32], F32)
            X1t = sb("X1t", [128, 32, 16], F32)
            X2t = sb("X2t", [128, 32, 16], F32)
            CY1t = sb("CY1t", [128, 32, 16], F32)
            CY2t = sb("CY2t", [128, 32, 16], F32)
            sgn = sb("sgn", [128, 1], F32)
            d_bc = sb("d_bc", [32, 512], F32)
            WAt = sb("WAt", [128, 4, 128], BF16)
            WBt = sb("WBt", [128, 4, 128], BF16)
            gba = sb("gba", [128, 4], F32)
            gbb = sb("gbb", [128, 4], F32)
            SW = sb("SW", [128, 128], F32)
            rkt = sb("rkt", [128, 128], F32)
            send = sb("send", [128, G8], F32)
            maskG = sb("maskG", [128, 64, 16], F32)
            sc = {n: sb("sc_" + n, [128, G8], F32) for n in
                  ("lr", "dt", "lrdt", "th", "mag", "t1", "sn", "cs", "ar", "ai", "den", "am1", "cr", "ci", "ta", "tb", "scr", "nci", "im2", "bir", "bii", "nsq")}
            a5r = sb("a5r", [128, G8, 6], F32)
            a5i = sb("a5i", [128, G8, 6], F32)
            hpi = sb("hpi", [128, 1], F32)
            sqr = sb("sqr", [128, G8, 12], F32)
            sqi = sb("sqi", [128, G8, 12], F32)
            bqr = sb("bqr", [128, G8, 3], F32)
            bqi = sb("bqi", [128, G8, 3], F32)
            PWfr = sb("PWfr", [128, G8, 65], F32)
            PWfi = sb("PWfi", [128, G8, 65], F32)
            PWrr = sb("PWrr", [128, G8, 64], F32)
            PWri = sb("PWri", [128, G8, 64], F32)
            PWbr = sb("PWbr", [128, G8, 8], F32)
            PWbi = sb("PWbi", [128, G8, 8], F32)
            cta = sb("cta", [128, G8, 64], F32)
            ctb = sb("ctb", [128, G8, 64], F32)
            BB1 = sb("BB1", [128, G8, 16], F32)
            BB2 = sb("BB2", [128, G8, 16], F32)
            bbt = sb("bbt", [128, G8, 16], F32)
            PRs = sb("PRs", [128, G8, 65], F32)
            nPI = sb("nPI", [128, G8, 65], F32)
            tA = sb("tA", [128, 65, 16], F32)
            tB = sb("tB", [128, 65, 16], F32)
            ZTb = sb("ZTb", [128, 64, 16], BF16)
            XTb = sb("XTb", [128, 8, 16], BF16)
            Zt = sb("Zt", [128, G8, 8, 128], BF16)
            Gtab = sb("Gtab", [128, G8, 1024], BF16)
            Yt = sb("Yt", [128, G8, 65, 16], BF16)
            Rk = sb("Rk", [128, G8, 6, 128], F32)
            ucj = sb("ucj", [32, G8, 64, 16], BF16)
            ustack = sb("ustack", [128, G8, 8, 32], BF16)
            E32 = sb("E32", [128, G8, 32], F32)
            Xs = sb("Xs", [128, G8, 33], F32)
            Sprevb = sb("Sprevb", [128, G8, 32], BF16)
            sg_t = sb("sg_t", [128, 4, G8], F32)
            hs = sb("hs", [128, G8], F32)
            hs2 = sb("hs2", [128, G8], F32)
            du = sb("du", [32, 64, 16], F32)
            yv = sb("yv", [32, 1024], F32)
            y2 = sb("y2", [32, 1024], F32)
            ysg = sb("ysg", [32, 1024], F32)
            ygel = sb("ygel", [32, 64, 128], BF16)
            ygT = sb("ygT", [128, 64, 32], BF16)
            sbt = sb("sbt", [128, 512], F32)
            ps_z = [ps(f"ps_z{i}", [128, 512], F32) for i in range(2)]
            pT = ps("pT", [128, 64, 32], BF16)
            pE = ps("pE", [128, G8, 32], F32)
            pD = ps("pD", [128, G8, 33], F32)
            pY = ps("pY", [128, 1024], F32)

            def ld(dst, src, key, q="sp"):
                P.dma(q, lambda e: e.dma_start(out=dst, in_=src), "setupC", writes=[key])
            ld(lam_re_t[:, :], lam2_re[:, :], "lam_re_t")
            ld(lam_im_t[:, :], lam2_im[:, :], "lam_im_t")
            ld(logdt_t[:, :], logdt[0:1, :].partition_broadcast(128), "logdt_t")
            ld(X1t[:, :, :], X1d[:, :, :], "X1t")
            ld(X2t[:, :, :], X2d[:, :, :], "X2t")
            ld(CY1t[:, :, :], CY1d[:, :, :], "CY1t")
            ld(CY2t[:, :, :], CY2d[:, :, :], "CY2t")
            ld(d_bc[:, :], s5d[0:1, :].partition_broadcast(32), "d_bc")
            ld(gba[:, :], gbad[:, :], "gba")
            ld(gbb[:, :], gbbd[:, :], "gbb")
            ld(WAt[:, :, :], WAd.ap().rearrange("u p c -> p u c"), "WAt", q="pool")
            ld(WBt[:, :, :], WBd.ap().rearrange("u p c -> p u c"), "WBt", q="pool")
            P.bulk_done("setupC")
            P.seq("pool", [lambda e: e.memset(sgn[0:64, :], -1.0), lambda e: e.memset(sgn[64:128, :], 1.0)], writes=["sgn"])
            P.op("pool", lambda e: e.memset(hpi[:, :], float(np.pi / 2)), writes=["hpi"])
            P.seq("pool", [lambda e: e.memset(SW[:, :], 0.0),
                           lambda e: e.affine_select(out=SW[:, :], in_=SW[:, :], pattern=[[-1, 128]], compare_op=ALU.not_equal, fill=1.0, base=64, channel_multiplier=1),
                           lambda e: e.affine_select(out=SW[:, :], in_=SW[:, :], pattern=[[-1, 128]], compare_op=ALU.not_equal, fill=1.0, base=-64, channel_multiplier=1)],
                  writes=["SW"])
            P.seq("pool", [lambda e: e.memset(maskG[:, :, :], 1.0),
                           lambda e: e.affine_select(out=maskG[:, :, :], in_=maskG[:, :, :], pattern=[[16, 64], [0, 16]], compare_op=ALU.is_ge, fill=0.0,
                                                     base=15, channel_multiplier=-1)], writes=["maskG"])

            TT = lambda o, a, b, op: (lambda e: e.tensor_tensor(out=o, in0=a, in1=b, op=op))
            TS = lambda o, a, s1, s2, op0, op1=None: ((lambda e: e.tensor_scalar(out=o, in0=a, scalar1=s1, scalar2=s2, op0=op0, op1=op1)) if op1 is not None
                                                      else (lambda e: e.tensor_scalar(out=o, in0=a, scalar1=s1, scalar2=None, op0=op0)))
            ACT = lambda o, a, f, **kw: (lambda e: e.activation(out=o, in_=a, func=f, **kw))
            MUL, ADD, SUB = ALU.mult, ALU.add, ALU.subtract

            def cmul(o_r, o_i, a_r, a_i, s_r, s_i, t1, t2):
                return [TT(t1, a_r, s_r, MUL), TT(t2, a_i, s_i, MUL), TT(o_r, t1, t2, SUB),
                        TT(t1, a_r, s_i, MUL), TT(t2, a_i, s_r, MUL), TT(o_i, t1, t2, ADD)]

            PI = float(np.pi)
            for un in range(4):
                gs = slice(un * 8, (un + 1) * 8)
                S = {k: v[:, :] for k, v in sc.items()}
                ops = []
                ops.append(TS(S["lr"], lam_re_t[:, gs], -1e-4, None, ALU.min))
                P.seq("dve", ops, reads=["lam_re_t"], writes=["prep"]); ops = []
                P.op("act", ACT(S["dt"], logdt_t[:, gs], AF.Exp), reads=["logdt_t", "prep"], writes=["prep_dt"])
                ops += [TT(S["lrdt"], S["lr"], S["dt"], MUL), TT(S["th"], lam_im_t[:, gs], S["dt"], MUL)]
                P.seq("dve", ops, reads=["prep", "prep_dt", "lam_im_t"], writes=["prep"]); ops = []
                P.seq("act", [ACT(S["sn"], S["th"], AF.Sin, scale=1.0 / 32), ACT(S["cs"], S["th"], AF.Sin, scale=1.0 / 32, bias=hpi[:, 0:1]),
                              ACT(S["mag"], S["lrdt"], AF.Exp, scale=1.0 / 32), ACT(S["im2"], S["lrdt"], AF.Exp, scale=-2.0)],
                      reads=["prep", "hpi"], writes=["prep_cs"])
                li = lam_im_t[:, gs]
                ops += [TT(a5r[:, :, 0], S["mag"], S["cs"], MUL), TT(a5i[:, :, 0], S["mag"], S["sn"], MUL)]
                for e_ in range(5):
                    ops += cmul(a5r[:, :, e_ + 1], a5i[:, :, e_ + 1], a5r[:, :, e_], a5i[:, :, e_], a5r[:, :, e_], a5i[:, :, e_], S["ta"], S["nsq"])
                ops += [(lambda e: e.tensor_copy(out=S["ar"], in_=a5r[:, :, 5])), (lambda e: e.tensor_copy(out=S["ai"], in_=a5i[:, :, 5])),
                        TT(S["den"], S["lr"], S["lr"], MUL), TT(S["ta"], li, li, MUL), TT(S["den"], S["den"], S["ta"], ADD),
                        (lambda e: e.reciprocal(out=S["den"], in_=S["den"])),
                        TS(S["am1"], S["ar"], -1.0, None, ADD),
                        TT(S["ta"], S["am1"], S["lr"], MUL), TT(S["tb"], S["ai"], li, MUL), TT(S["ta"], S["ta"], S["tb"], ADD), TT(S["cr"], S["ta"], S["den"], MUL),
                        TT(S["ta"], S["ai"], S["lr"], MUL), TT(S["tb"], S["am1"], li, MUL), TT(S["ta"], S["ta"], S["tb"], SUB), TT(S["ci"], S["ta"], S["den"], MUL),
                        TS(S["scr"], S["cr"], sgn[:, 0:1], None, MUL),
                        TS(S["tb"], S["ci"], sgn[:, 0:1], None, MUL),
                        TS(S["nci"], S["ci"], -1.0, None, MUL),
                        TT(S["bir"], S["ar"], S["im2"], MUL), TT(S["bii"], S["ai"], S["im2"], MUL), TS(S["bii"], S["bii"], -1.0, None, MUL),
                        (lambda e: e.tensor_copy(out=sqr[:, :, 0], in_=S["ar"])), (lambda e: e.tensor_copy(out=sqi[:, :, 0], in_=S["ai"])),
                        (lambda e: e.tensor_copy(out=bqr[:, :, 0], in_=S["bir"])), (lambda e: e.tensor_copy(out=bqi[:, :, 0], in_=S["bii"]))]
                for e_ in range(11):
                    ops += cmul(sqr[:, :, e_ + 1], sqi[:, :, e_ + 1], sqr[:, :, e_], sqi[:, :, e_], sqr[:, :, e_], sqi[:, :, e_], S["ta"], S["nsq"])
                for e_ in range(2):
                    ops += cmul(bqr[:, :, e_ + 1], bqi[:, :, e_ + 1], bqr[:, :, e_], bqi[:, :, e_], bqr[:, :, e_], bqi[:, :, e_], S["ta"], S["nsq"])
                bc16 = lambda a: a.unsqueeze(2).to_broadcast([128, G8, 16])
                ops += [TT(BB1[:, :, :], X1t[:, gs, :], bc16(S["cr"]), MUL), TT(bbt[:, :, :], X2t[:, gs, :], bc16(S["tb"]), MUL), TT(BB1[:, :, :], BB1[:, :, :], bbt[:, :, :], ADD),
                        TT(BB2[:, :, :], X2t[:, gs, :], bc16(S["scr"]), MUL), TT(bbt[:, :, :], X1t[:, gs, :], bc16(S["nci"]), MUL), TT(BB2[:, :, :], BB2[:, :, :], bbt[:, :, :], ADD)]
                ops += [(lambda e: e.memset(PWfr[:, :, 0:1], 1.0)), (lambda e: e.memset(PWfi[:, :, 0:1], 0.0))]
                for k in range(6):
                    n = 1 << k
                    bcn = lambda a, n=n: a.to_broadcast([128, G8, n])
                    ops += cmul(PWfr[:, :, n:2 * n], PWfi[:, :, n:2 * n], PWfr[:, :, 0:n], PWfi[:, :, 0:n],
                                bcn(sqr[:, :, k:k + 1]), bcn(sqi[:, :, k:k + 1]), cta[:, :, 0:n], ctb[:, :, 0:n])
                ops += [(lambda e: e.tensor_copy(out=PWfr[:, :, 64:65], in_=sqr[:, :, 6:7])), (lambda e: e.tensor_copy(out=PWfi[:, :, 64:65], in_=sqi[:, :, 6:7]))]
                ops += [(lambda e: e.memset(PWrr[:, :, 63:64], 1.0)), (lambda e: e.memset(PWri[:, :, 63:64], 0.0))]
                for k in range(6):
                    n = 1 << k
                    bcn = lambda a, n=n: a.to_broadcast([128, G8, n])
                    ops += cmul(PWrr[:, :, 64 - 2 * n:64 - n], PWri[:, :, 64 - 2 * n:64 - n], PWrr[:, :, 64 - n:64], PWri[:, :, 64 - n:64],
                                bcn(sqr[:, :, k:k + 1]), bcn(sqi[:, :, k:k + 1]), cta[:, :, 0:n], ctb[:, :, 0:n])
                ops += [(lambda e: e.memset(PWbr[:, :, 0:1], 1.0)), (lambda e: e.memset(PWbi[:, :, 0:1], 0.0))]
                for k in range(3):
                    n = 1 << k
                    bcn = lambda a, n=n: a.to_broadcast([128, G8, n])
                    ops += cmul(PWbr[:, :, n:2 * n], PWbi[:, :, n:2 * n], PWbr[:, :, 0:n], PWbi[:, :, 0:n],
                                bcn(bqr[:, :, k:k + 1]), bcn(bqi[:, :, k:k + 1]), cta[:, :, 0:n], ctb[:, :, 0:n])
                ops += [TS(PRs[:, :, :], PWfr[:, :, :], sgn[:, 0:1], None, MUL), TS(PRs[:, :, :], PRs[:, :, :], -1.0, None, MUL), TS(nPI[:, :, :], PWfi[:, :, :], -1.0, None, MUL)]
                P.seq("dve", ops, reads=["prep", "prep_sn", "prep_cs", "sgn", "X1t", "X2t", "lam_im_t", "Rk_use", "tab_use"], writes=["prep", "prepT"]); ops = []

                for g in range(G8):
                    ga = un * 8 + g
                    bq = lambda a: a.unsqueeze(2).to_broadcast([128, 64, 16])
                    bh = lambda a, n: a.unsqueeze(1).to_broadcast([128, n, 16])
                    P.seq("dve", [TT(tA[:, 0:64, :], bq(PWrr[:, g, :]), bh(BB1[:, g, :], 64), MUL),
                                  TT(tB[:, 0:64, :], bq(PWri[:, g, :]), bh(BB2[:, g, :], 64), MUL),
                                  TT(ZTb[:, :, :], tA[:, 0:64, :], tB[:, 0:64, :], ADD)],
                          reads=["prepT", "ZTb_use"], writes=["tA", "tB", "ZTb"])
                    def f_zt(e):
                        for kc in range(8):
                            i = e.transpose(out=pT[:, kc * 4:(kc + 1) * 4, :], in_=ZTb[:, kc * 8:(kc + 1) * 8, :], identity=ident[:, :])
                        return i
                    P.op("pe", f_zt, reads=["ZTb", "ident"], writes=["pT"])
                    P.op("act", lambda e, g=g: e.copy(out=Zt[:, g, :, :], in_=pT[:, 0:32, :].rearrange("p (a b) c -> p a (b c)", b=4)), reads=["pT"], writes=["Zt", "ZTb_use"])
                    bq8 = lambda a: a.unsqueeze(2).to_broadcast([128, 8, 16])
                    P.seq("dve", [TT(tA[:, 0:8, :], bq8(PWbr[:, g, :]), bh(BB1[:, g, :], 8), MUL),
                                  TT(tB[:, 0:8, :], bq8(PWbi[:, g, :]), bh(BB2[:, g, :], 8), MUL),
                                  TT(XTb[:, :, :], tA[:, 0:8, :], tB[:, 0:8, :], ADD)],
                          reads=["prepT", "tA", "tB", "XTb_use"], writes=["tA", "tB", "XTb"])
                    bq65 = lambda a: a.unsqueeze(2).to_broadcast([128, 65, 16])
                    P.seq("dve", [TT(tA[:, :, :], bq65(PRs[:, g, :]), bh(CY1t[:, ga, :], 65), MUL),
                                  TT(tB[:, :, :], bq65(nPI[:, g, :]), bh(CY2t[:, ga, :], 65), MUL),
                                  TT(Yt[:, g, :, :], tA[:, :, :], tB[:, :, :], ADD)],
                          reads=["prepT", "tA", "tB", "CY1t", "CY2t", "tab_use"], writes=["tA", "tB", f"Yt{g}"])
                    def f_g(e, g=g):
                        e.matmul(out=pY[:, 0:512], lhsT=XTb[:, :, :], rhs=Yt[:, g, 0:32, :], start=True, stop=True)
                        return e.matmul(out=pY[:, 512:1024], lhsT=XTb[:, :, :], rhs=Yt[:, g, 32:64, :], start=True, stop=True)
                    P.op("pe", f_g, reads=["XTb", f"Yt{g}"], writes=["pY"])
                    P.op("dve", lambda e, g=g: e.tensor_tensor(out=Gtab[:, g, :], in0=pY[:, :], in1=maskG[:, :, :], op=MUL),
                         reads=["pY", "maskG", "tab_use"], writes=[f"Gtab{g}", "XTb_use"])
                    for k in range(6):
                        P.seq("pool", [lambda e, g=g, k=k: e.tensor_scalar(out=Rk[:, g, k, :], in0=identf[:, :], scalar1=sqr[:, g, 6 + k:7 + k], scalar2=None, op0=MUL),
                                       lambda e, g=g, k=k: e.tensor_scalar(out=rkt[:, :], in0=SW[:, :], scalar1=sqi[:, g, 6 + k:7 + k], scalar2=sgn[:, 0:1], op0=MUL, op1=MUL),
                                       lambda e, g=g, k=k: e.tensor_tensor(out=Rk[:, g, k, :], in0=Rk[:, g, k, :], in1=rkt[:, :], op=SUB)],
                              reads=["prepT", "identf", "SW", "sgn", "Rk_use"], writes=[f"Rk{g}", "rkt"])

                for g in range(G8):
                    P.dma("sp", lambda e, un=un, g=g: e.dma_start(out=ucj[:, g, :, :],
                                                                   in_=u_scr.ap().rearrange("(c j) n -> c j n", j=64)[:, :, un * 128 + g * 16:un * 128 + (g + 1) * 16]),
                          "ucj", reads=[f"u_scr{t}" for t in range(NT)] + ["ucj_use"], writes=[f"ucj{g}"])
                for g in range(G8):
                    def f_us(e, g=g):
                        for kc in range(8):
                            i = e.transpose(out=pT[:, 32 + kc, :], in_=ucj[:, g, kc * 8:(kc + 1) * 8, :], identity=ident[0:32, 0:32])
                        return i
                    P.op("pe", f_us, reads=[f"ucj{g}", "ident"], writes=["pT2"])
                    P.op("act", lambda e, g=g: e.copy(out=ustack[:, g, :, :], in_=pT[:, 32:40, :]), reads=["pT2"], writes=[f"ustack{g}"])
                    def f_E(e, g=g):
                        for kc in range(8):
                            i = e.matmul(out=pE[:, g, :], lhsT=Zt[:, g, kc, :], rhs=ustack[:, g, kc, :], start=(kc == 0), stop=(kc == 7))
                        return i
                    P.op("pe", f_E, reads=["Zt", f"ustack{g}"], writes=["pE"])
                P.op("act", lambda e: e.copy(out=E32[:, :, :], in_=pE[:, :, :]), reads=["pE"], writes=["E32"])

                def doubling(tag):
                    for k in range(6):
                        n = 33 - (1 << k)
                        def f_d(e, k=k, n=n):
                            for g in range(G8):
                                i = e.matmul(out=pD[:, g, 0:n], lhsT=Rk[:, g, k, :], rhs=Xs[:, g, 0:n], start=True, stop=True)
                            return i
                        P.op("pe", f_d, reads=["Xs"] + [f"Rk{g}" for g in range(G8)], writes=["pD"])
                        P.op("dve", lambda e, k=k, n=n: e.tensor_tensor(out=Xs[:, :, 1 << k:33], in0=Xs[:, :, 1 << k:33], in1=pD[:, :, 0:n], op=ADD),
                             reads=["pD", "Xs"], writes=["Xs"])
                P.seq("dve", [lambda e: e.memset(Xs[:, :, 0:1], 0.0), lambda e: e.tensor_copy(out=Xs[:, :, 1:33], in_=E32[:, :, :])], reads=["E32"], writes=["Xs"])
                doubling("loc")
                P.op("dve", lambda e: e.tensor_copy(out=send[:, :], in_=Xs[:, :, 32]), reads=["Xs"], writes=["send"])
                P.dma("sp", lambda e, un=un: e.dma_start(out=ccs_in[un][:, :], in_=send[:, :]), f"ccsin{un}", reads=["send"], writes=[f"ccs_in{un}"])
                P.dma("pool", lambda e, un=un: e.collective_compute("AllGather", ALU.bypass, replica_groups=GROUPS4,
                                                                     ins=[ccs_in[un].ap().opt()], outs=[ccs_out[un].ap().opt()]),
                      f"ccs{un}", reads=[f"ccs_in{un}"], writes=[f"ccs_out{un}"], inc=1)
                P.dma("sp", lambda e, un=un: e.dma_start(out=sg_t[:, :, :], in_=ccs_out[un].ap().rearrange("(r p) c -> p r c", p=128)),
                      f"ccsld{un}", reads=[f"ccs_out{un}"], writes=["sg_t"])
                P.op("dve", lambda e: e.memset(hs[:, :], 0.0), writes=["hs"])
                for pp in range(3):
                    def f_hr(e):
                        for g in range(G8):
                            i = e.matmul(out=pD[:, g, 0:1], lhsT=Rk[:, g, 5, :], rhs=hs[:, g:g + 1], start=True, stop=True)
                        return i
                    P.op("pe", f_hr, reads=["hs"] + [f"Rk{g}" for g in range(G8)], writes=["pD"])
                    P.seq("dve", [lambda e, pp=pp: e.tensor_tensor(out=hs2[:, :], in0=pD[:, :, 0], in1=sg_t[:, pp, :], op=ADD),
                                  lambda e: e.tensor_tensor(out=hs2[:, :], in0=hs2[:, :], in1=hs[:, :], op=SUB),
                                  lambda e, pp=pp: e.scalar_tensor_tensor(out=hs[:, :], in0=hs2[:, :], scalar=pm[:, pp:pp + 1], in1=hs[:, :], op0=MUL, op1=ADD)],
                          reads=["pD", "sg_t", "hs", "pm"], writes=["hs", "hs2"])
                P.seq("dve", [lambda e: e.tensor_copy(out=Xs[:, :, 0], in_=hs[:, :]), lambda e: e.tensor_copy(out=Xs[:, :, 1:33], in_=E32[:, :, :])],
                      reads=["hs", "E32"], writes=["Xs"])
                doubling("glob")
                P.op("act", lambda e: e.copy(out=Sprevb[:, :, :], in_=Xs[:, :, 0:32]), reads=["Xs"], writes=["Sprevb"])

                for g in range(G8):
                    def f_y(e, g=g):
                        Yf = Yt[:, g, :, :]
                        e.matmul(out=pY[0:32, 0:512], lhsT=Sprevb[:, g, :], rhs=Yt[:, g, 1:33, :], start=True, stop=False)
                        e.matmul(out=pY[0:32, 512:1024], lhsT=Sprevb[:, g, :], rhs=Yt[:, g, 33:65, :], start=True, stop=False)
                        i = None
                        for kc in range(8):
                            lo = 128 * kc
                            if lo < 512:
                                i = e.matmul(out=pY[0:32, lo:512], lhsT=ustack[:, g, kc, :], rhs=Gtab[:, g, 0:512 - lo], start=False, stop=(kc == 3))
                            b0 = max(512, lo)
                            i = e.matmul(out=pY[0:32, b0:1024], lhsT=ustack[:, g, kc, :], rhs=Gtab[:, g, b0 - lo:1024 - lo], start=False, stop=(kc == 7))
                        return i
                    P.op("pe", f_y, reads=["Sprevb", f"Yt{g}", f"Gtab{g}", f"ustack{g}"], writes=["pY"])
                    ga = un * 8 + g
                    P.op("pool", lambda e, g=g, ga=ga: e.tensor_tensor(out=du[:, :, :], in0=ucj[:, g, :, :],
                                                                       in1=d_bc[:, ga * 16:(ga + 1) * 16].unsqueeze(1).to_broadcast([32, 64, 16]), op=MUL),
                         reads=[f"ucj{g}", "d_bc"], writes=["du"])
                    yv_ = yv[:, :]
                    P.seq("dve", [lambda e: e.tensor_tensor(out=yv[:, :], in0=pY[0:32, :], in1=du[:, :, :], op=ADD),
                                  TT(y2[:, :], yv_, yv_, MUL), TS(y2[:, :], y2[:, :], 0.044715, 1.0, MUL, ADD), TT(y2[:, :], y2[:, :], yv_, MUL)],
                          reads=["pY", "du", "ysg"], writes=["yv", "y2"])
                    P.op("act", ACT(ysg[:, :], y2[:, :], AF.Sigmoid, scale=1.5957691216057308), reads=["y2"], writes=["ysg"])
                    P.op("dve", lambda e, g=g: e.tensor_tensor(out=ygel[:, :, g * 16:(g + 1) * 16], in0=yv[:, :], in1=ysg[:, :], op=MUL),
                         reads=["yv", "ysg", "ygel_use"], writes=[f"ygel{g}"])
                def f_gt(e):
                    for j in range(64):
                        i = e.transpose(out=pT[:, j, :], in_=ygel[:, j, :], identity=ident[0:32, 0:32])
                    return i
                P.op("pe", f_gt, reads=[f"ygel{g}" for g in range(G8)] + ["ident"], writes=["pT", "pT2"])
                P.op("act", lambda e: e.copy(out=ygT[:, :, :], in_=pT[:, :, :]), reads=["pT", "pT2"], writes=["ygT", "ygel_use"])
                ms_v = mslab[:, :].rearrange("p (c j) -> p j c", j=64)
                for nb in range(4):
                    P.op("pe", lambda e, nb=nb, un=un: e.matmul(out=ps_z[0][:, :], lhsT=WAt[:, un, :], rhs=ygT[:, nb * 16:(nb + 1) * 16, :], start=True, stop=True),
                         reads=["WAt", "ygT"], writes=["ps_z0"])
                    P.op("pe", lambda e, nb=nb, un=un: e.matmul(out=ps_z[1][:, :], lhsT=WBt[:, un, :], rhs=ygT[:, nb * 16:(nb + 1) * 16, :], start=True, stop=True),
                         reads=["WBt", "ygT"], writes=["ps_z1"])
                    P.op("act", lambda e, un=un: e.activation(out=sbt[:, :], in_=ps_z[1][:, :], func=AF.Sigmoid, bias=gbb[:, un:un + 1]),
                         reads=["ps_z1", "gbb"], writes=["sbt"])
                    P.op("dve", lambda e, nb=nb, un=un: e.scalar_tensor_tensor(out=ms_v[:, nb * 16:(nb + 1) * 16, :], in0=ps_z[0][:, :].rearrange("p (j c) -> p j c", c=32), scalar=gba[:, un:un + 1],
                                                                               in1=sbt[:, :].rearrange("p (j c) -> p j c", c=32), op0=ADD, op1=MUL),
                         reads=["ps_z0", "sbt", "gba"], writes=["mslab"])
                P.dma("sp", lambda e, un=un: e.dma_start(out=mergedT_scr[512 + un * 128:512 + (un + 1) * 128, :], in_=mslab[:, :]), "mscr",
                      reads=["mslab"], writes=[f"mscr{4 + un}"])
                P.op("pool", lambda e: e.memset(hs2[:, 0:1], 0.0), reads=["pY", "pD", "ygT", "Sprevb"] + [f"ustack{g}" for g in range(G8)], writes=["Rk_use", "tab_use", "ucj_use"] + [f"ucj{g}" for g in range(G8)])
            esC.close()
            P.barrier()
        else:
            esH.close()

        if stage >= 4:
            AX = mybir.AxisListType
            MUL, ADD, SUB = ALU.mult, ALU.add, ALU.subtract
            esDE = ExitStack()
            esDE.__enter__()
            sbp = lambda name, shape, dt: esDE.enter_context(nc.sbuf_tensor(name, shape, dt))
            h2T = sbp("h2T", [128, 8, NTOK], BF16)
            cw = sbp("cw", [128, NT, 32], F32)
            xt2 = [sbp(f"xt2_{i}", [128, D], F32) for i in range(2)]
            ssq2 = sbp("ssq2", [128, 2 * NT], F32)
            rstd2 = sbp("rstd2", [128, 2 * NT], F32)
            junk2 = sbp("junk2", [128, D], BF16)
            esD = ExitStack()
            esD.__enter__()
            sb = lambda name, shape, dt: esD.enter_context(nc.sbuf_tensor(name, shape, dt))
            ps = lambda name, shape, dt: esD.enter_context(nc.psum_tensor(name, shape, dt))
            mT = sb("mT", [128, 8, NTOK], BF16)
            Wout = sb("Wout", [128, 8, D], BF16)
            Wr = sb("Wr", [128, 8, 36], BF16)
            rb_bc = sb("rb_bc", [128, 36], F32)
            xm = [sb(f"xm{i}", [128, D], F32) for i in range(2)]
            hb2 = sb("hb2", [128, D], BF16)
            lg = sb("lg", [128, NT, 36], F32)
            r_a = sb("r_a", [128, NT, 4], F32)
            r_b = sb("r_b", [128, NT, 4], F32)
            r_gmax = sb("r_gmax", [128, NT], F32)
            r_gw = sb("r_gw", [128, NT], F32)
            r_el = sb("r_el", [128, NT, 32], F32)
            r_t = sb("r_t", [128, NT, 32], F32)
            r_oh1 = sb("r_oh1", [128, NT, 32], F32)
            r_oh2 = sb("r_oh2", [128, NT, 32], F32)
            r_m1 = sb("r_m1", [128, NT], F32)
            r_m2 = sb("r_m2", [128, NT], F32)
            r_w1 = sb("r_w1", [128, NT], F32)
            r_w2 = sb("r_w2", [128, NT], F32)
            po = [ps(f"po{i}", [128, 512], F32) for i in range(2)]
            tp2 = ps("tp2", [128, 8, 128], BF16)
            pr = ps("pr", [128, 36], F32)

            P.dma("sp", lambda e: e.dma_start(out=mT[:, :, :], in_=mergedT_scr.ap().rearrange("(kc p) t -> p kc t", p=128)), "mT",
                  reads=[f"mscr{i}" for i in range(8)], writes=["mT"])
            P.dma("pool", lambda e: e.dma_start(out=Wout[:, :, :], in_=w_out.ap().rearrange("(kc p) c -> p kc c", p=128)), "setupD", writes=["Wout"])
            P.dma("pool", lambda e: e.dma_start(out=Wr[:, :, :], in_=w_rt.ap().rearrange("(kc p) c -> p kc c", p=128)), "setupD", writes=["Wr"])
            P.dma("sp", lambda e: e.dma_start(out=rb_bc[:, :], in_=b_rt[0:1, :].partition_broadcast(128)), "setupD", writes=["rb_bc"])
            P.dma("sp", lambda e: e.dma_start(out=g_bc[:, :], in_=g_ffn[0:1, :].partition_broadcast(128)), "setupD", writes=["g_bc"])
            P.bulk_done("setupD")

            for t in range(NT):
                s_ = t % 2
                ts_ = slice(t * 128, (t + 1) * 128)
                P.dma("sp", lambda e, t=t, s_=s_: e.dma_start(out=xt2[s_][:, :], in_=x[t * 128:(t + 1) * 128, :]), f"xt2_{s_}", writes=[f"xt2_{s_}"])
                for hf in range(2):
                    def f_o(e, ts_=ts_, hf=hf):
                        for k in range(8):
                            i = e.matmul(out=po[hf][:, :], lhsT=mT[:, k, ts_], rhs=Wout[:, k, hf * 512:(hf + 1) * 512], start=(k == 0), stop=(k == 7))
                        return i
                    P.op("pe", f_o, reads=["mT", "Wout"], writes=[f"po{hf}"])
                    P.op("dve", lambda e, hf=hf, s_=s_: e.tensor_tensor(out=xm[s_][:, hf * 512:(hf + 1) * 512], in0=po[hf][:, :], in1=xt2[s_][:, hf * 512:(hf + 1) * 512], op=ADD),
                         reads=[f"po{hf}", f"xt2_{s_}"], writes=[f"xm{s_}_{hf}"])
                xk = [f"xm{s_}_0", f"xm{s_}_1"]
                P.dma("sp", lambda e, t=t, s_=s_: e.dma_start(out=xmid_scr[t * 128:(t + 1) * 128, :], in_=xm[s_][:, :]), f"xmid{s_}", reads=xk, writes=[f"xmid{t}"])
                P.seq("act", [lambda e, s_=s_, t=t: e.activation(out=junk2[:, :], in_=xm[s_][:, :], func=AF.Square, accum_out=ssq2[:, t:t + 1]),
                              lambda e, t=t: e.activation(out=ssq2[:, t:t + 1], in_=ssq2[:, t:t + 1], func=AF.Sqrt, scale=1.0 / D, bias=EPS)],
                      reads=xk, writes=["junk2", f"ssq2_{t}"])
                P.op("dve", lambda e, t=t: e.reciprocal(out=rstd2[:, t:t + 1], in_=ssq2[:, t:t + 1]), reads=[f"ssq2_{t}"], writes=[f"rstd2_{t}"])
                P.op("dve", lambda e, s_=s_, t=t: e.scalar_tensor_tensor(out=hb2[:, :], in0=xm[s_][:, :], scalar=rstd2[:, t:t + 1], in1=g_bc[:, :], op0=MUL, op1=MUL),
                     reads=xk + [f"rstd2_{t}", "g_bc"], writes=["hb2"])
                def f_tp2(e):
                    for k in range(8):
                        i = e.transpose(out=tp2[:, k, :], in_=hb2[:, k * 128:(k + 1) * 128], identity=ident[:, :])
                    return i
                P.op("pe", f_tp2, reads=["hb2", "ident"], writes=["tp2"])
                P.op("act", lambda e, ts_=ts_: e.copy(out=h2T[:, :, ts_], in_=tp2[:, :, :]), reads=["tp2"], writes=[f"h2T_{t // 4}"])
                def f_r(e, ts_=ts_):
                    for k in range(8):
                        i = e.matmul(out=pr[:, :], lhsT=h2T[:, k, ts_], rhs=Wr[:, k, :], start=(k == 0), stop=(k == 7))
                    return i
                P.op("pe", f_r, reads=[f"h2T_{t // 4}", "Wr"], writes=["pr"])
                P.op("dve", lambda e, t=t: e.tensor_tensor(out=lg[:, t, :], in0=pr[:, :], in1=rb_bc[:, :], op=ADD), reads=["pr", "rb_bc"], writes=["lg"])

            BIG = 1.0e9
            bc4 = lambda a: a.unsqueeze(2).to_broadcast([128, NT, 4])
            bc32 = lambda a: a.unsqueeze(2).to_broadcast([128, NT, 32])
            gl = lg[:, :, 0:4]
            el = lg[:, :, 4:36]
            P.seq("dve", [
                lambda e: e.tensor_reduce(out=r_gmax[:, :], in_=gl, axis=AX.X, op=ALU.max),
                lambda e: e.tensor_tensor(out=r_a[:, :, :], in0=gl, in1=bc4(r_gmax[:, :]), op=SUB)], reads=["lg"], writes=["r_a", "r_gmax"])
            P.op("act", lambda e: e.activation(out=r_b[:, :, :], in_=r_a[:, :, :], func=AF.Exp), reads=["r_a"], writes=["r_b"])
            P.seq("dve", [
                lambda e: e.tensor_reduce(out=r_gw[:, :], in_=r_b[:, :, :], axis=AX.X, op=ADD),
                lambda e: e.reciprocal(out=r_gw[:, :], in_=r_gw[:, :]),
                lambda e: e.tensor_tensor(out=r_a[:, :, :], in0=gl, in1=bc4(r_gmax[:, :]), op=ALU.is_equal),
                lambda e: e.tensor_scalar(out=r_a[:, :, :], in0=r_a[:, :, :], scalar1=-1.0, scalar2=BIG, op0=ADD, op1=MUL),
                lambda e: e.tensor_tensor(out=r_el[:, :, :].rearrange("p t (g k) -> p t g k", k=8), in0=el.rearrange("p t (g k) -> p t g k", k=8),
                                          in1=r_a[:, :, :].unsqueeze(3).to_broadcast([128, NT, 4, 8]), op=ADD),
                lambda e: e.tensor_reduce(out=r_m1[:, :], in_=r_el[:, :, :], axis=AX.X, op=ALU.max),
                lambda e: e.tensor_tensor(out=r_oh1[:, :, :], in0=r_el[:, :, :], in1=bc32(r_m1[:, :]), op=ALU.is_equal),
                lambda e: e.scalar_tensor_tensor(out=r_t[:, :, :], in0=r_oh1[:, :, :], scalar=-BIG, in1=r_el[:, :, :], op0=MUL, op1=ADD),
                lambda e: e.tensor_reduce(out=r_m2[:, :], in_=r_t[:, :, :], axis=AX.X, op=ALU.max),
                lambda e: e.tensor_tensor(out=r_oh2[:, :, :], in0=r_t[:, :, :], in1=bc32(r_m2[:, :]), op=ALU.is_equal),
                lambda e: e.tensor_tensor(out=r_w1[:, :], in0=r_m1[:, :], in1=r_m2[:, :], op=SUB)],
                reads=["lg", "r_b", "r_a"], writes=["r_a", "router1"])
            P.op("act", lambda e: e.activation(out=r_w1[:, :], in_=r_w1[:, :], func=AF.Sigmoid), reads=["router1"], writes=["r_w1"])
            P.seq("dve", [
                lambda e: e.tensor_scalar(out=r_w2[:, :], in0=r_w1[:, :], scalar1=-1.0, scalar2=1.0, op0=MUL, op1=ADD),
                lambda e: e.tensor_tensor(out=r_w1[:, :], in0=r_w1[:, :], in1=r_gw[:, :], op=MUL),
                lambda e: e.tensor_tensor(out=r_w2[:, :], in0=r_w2[:, :], in1=r_gw[:, :], op=MUL),
                lambda e: e.tensor_tensor(out=r_oh1[:, :, :], in0=r_oh1[:, :, :], in1=bc32(r_w1[:, :]), op=MUL),
                lambda e: e.tensor_tensor(out=r_oh2[:, :, :], in0=r_oh2[:, :, :], in1=bc32(r_w2[:, :]), op=MUL),
                lambda e: e.tensor_tensor(out=cw[:, :, :], in0=r_oh1[:, :, :], in1=r_oh2[:, :, :], op=ADD)],
                reads=["router1", "r_w1"], writes=["cw", "router1"])
            if "cw" in dbg:
                tcw = dbgt("cw", [128, NT, 32])
                P.dma("sp", lambda e: e.dma_start(out=tcw[:, :, :], in_=cw[:, :, :]), "out", reads=["cw"])
            esD.close()
            P.barrier()

            NE = 32 if stage >= 5 else 0
            esE = ExitStack()
            esE.__enter__()
            sb = lambda name, shape, dt: esE.enter_context(nc.sbuf_tensor(name, shape, dt))
            ps = lambda name, shape, dt: esE.enter_context(nc.psum_tensor(name, shape, dt))
            yacc = sb("yacc", [128, NT, D], F32)
            Wg = [sb(f"Wg{i}", [128, 8, 512], BF16) for i in range(2)]
            Wu = [sb(f"Wu{i}", [128, 8, 512], BF16) for i in range(2)]
            Wd = [sb(f"Wd{i}", [128, 4, D], BF16) for i in range(2)]
            actT = sb("actT", [128, 4, NTOK], BF16)
            sgt = [sb(f"sgt{i}", [128, 512], F32) for i in range(2)]
            pg = [ps(f"pg{i}", [128, 512], F32) for i in range(2)]
            pu2 = [ps(f"pu2_{i}", [128, 512], F32) for i in range(2)]
            pd = [ps(f"pd{i}", [128, 512], F32) for i in range(2)]
            P.op("pool", lambda e: e.memset(yacc[:, :, :], 0.0), writes=[f"yacc{t}_{hf}" for t in range(NT) for hf in range(2)])
            h2T_all = [f"h2T_{i}" for i in range(4)]
            for ex in range(NE):
                s_ = ex % 2
                P.dma("pool", lambda e, ex=ex, s_=s_: e.dma_start(out=Wg[s_][:, :, :], in_=w_gate[ex].rearrange("(kc p) c -> p kc c", p=128)), f"Wg{s_}", writes=[f"Wg{s_}"])
                P.dma("pool", lambda e, ex=ex, s_=s_: e.dma_start(out=Wu[s_][:, :, :], in_=w_up[ex].rearrange("(kc p) c -> p kc c", p=128)), f"Wu{s_}", writes=[f"Wu{s_}"])
                P.dma("pool", lambda e, ex=ex, s_=s_: e.dma_start(out=Wd[s_][:, :, :], in_=w_down[ex].rearrange("(kc p) c -> p kc c", p=128)), f"Wd{s_}", writes=[f"Wd{s_}"])
                it = 0
                for tb in range(4):
                    tbs = slice(tb * 512, (tb + 1) * 512)
                    for m in range(4):
                        b_ = it % 2
                        it += 1
                        def f_gu(e, s_=s_, m=m, tbs=tbs, b_=b_):
                            for k in range(8):
                                e.matmul(out=pg[b_][:, :], lhsT=Wg[s_][:, k, m * 128:(m + 1) * 128], rhs=h2T[:, k, tbs], start=(k == 0), stop=(k == 7))
                            for k in range(8):
                                i = e.matmul(out=pu2[b_][:, :], lhsT=Wu[s_][:, k, m * 128:(m + 1) * 128], rhs=h2T[:, k, tbs], start=(k == 0), stop=(k == 7))
                            return i
                        P.op("pe", f_gu, reads=[f"Wg{s_}", f"Wu{s_}", f"h2T_{tb}"], writes=[f"pg{b_}", f"pu2_{b_}"])
                        P.op("act", lambda e, b_=b_: e.activation(out=sgt[b_][:, :], in_=pg[b_][:, :], func=AF.Silu), reads=[f"pg{b_}"], writes=[f"sgt{b_}"])
                        P.op("dve", lambda e, b_=b_, m=m, tbs=tbs: e.tensor_tensor(out=actT[:, m, tbs], in0=sgt[b_][:, :], in1=pu2[b_][:, :], op=MUL),
                             reads=[f"sgt{b_}", f"pu2_{b_}"], writes=[f"actT_{tb}"])
                it = 0
                for t in range(NT):
                    ts_ = slice(t * 128, (t + 1) * 128)
                    for hf in range(2):
                        b_ = it % 2
                        it += 1
                        def f_d(e, s_=s_, ts_=ts_, hf=hf, b_=b_):
                            for m in range(4):
                                i = e.matmul(out=pd[b_][:, :], lhsT=actT[:, m, ts_], rhs=Wd[s_][:, m, hf * 512:(hf + 1) * 512], start=(m == 0), stop=(m == 3))
                            return i
                        P.op("pe", f_d, reads=[f"Wd{s_}", f"actT_{t // 4}"], writes=[f"pd{b_}"])
                        P.op("dve", lambda e, t=t, hf=hf, b_=b_, ex=ex: e.scalar_tensor_tensor(out=yacc[:, t, hf * 512:(hf + 1) * 512], in0=pd[b_][:, :], scalar=cw[:, t, ex:ex + 1],
                                                                                                in1=yacc[:, t, hf * 512:(hf + 1) * 512], op0=MUL, op1=ADD),
                             reads=[f"pd{b_}", "cw", f"yacc{t}_{hf}"], writes=[f"yacc{t}_{hf}"])

            P.dma("sp", lambda e: e.dma_start(out=g_bc[:, :], in_=g_fin[0:1, :].partition_broadcast(128)), "setupF", writes=["g_bc"])
            for t in range(NT):
                s_ = t % 2
                P.dma("sp", lambda e, t=t, s_=s_: e.dma_start(out=xt2[s_][:, :], in_=xmid_scr[t * 128:(t + 1) * 128, :]), f"xt2_{s_}", reads=[f"xmid{t}"], writes=[f"xt2_{s_}"])
                P.op("dve", lambda e, t=t, s_=s_: e.tensor_tensor(out=xt2[s_][:, :], in0=xt2[s_][:, :], in1=yacc[:, t, :], op=ADD),
                     reads=[f"xt2_{s_}", f"yacc{t}_0", f"yacc{t}_1"], writes=[f"xt2_{s_}"])
                P.seq("act", [lambda e, s_=s_, t=t: e.activation(out=junk2[:, :], in_=xt2[s_][:, :], func=AF.Square, accum_out=ssq2[:, NT + t:NT + t + 1]),
                              lambda e, t=t: e.activation(out=ssq2[:, NT + t:NT + t + 1], in_=ssq2[:, NT + t:NT + t + 1], func=AF.Sqrt, scale=1.0 / D, bias=EPS)],
                      reads=[f"xt2_{s_}"], writes=["junk2", f"ssq2_{NT + t}"])
                P.op("dve", lambda e, t=t: e.reciprocal(out=rstd2[:, NT + t:NT + t + 1], in_=ssq2[:, NT + t:NT + t + 1]), reads=[f"ssq2_{NT + t}"], writes=[f"rstd2_{NT + t}"])
                P.op("dve", lambda e, s_=s_, t=t: e.scalar_tensor_tensor(out=xt2[s_][:, :], in0=xt2[s_][:, :], scalar=rstd2[:, NT + t:NT + t + 1], in1=g_bc[:, :], op0=MUL, op1=MUL),
                     reads=[f"xt2_{s_}", f"rstd2_{NT + t}", "g_bc"], writes=[f"xt2_{s_}"])
                P.dma("sp", lambda e, t=t, s_=s_: e.dma_start(out=y[t * 128:(t + 1) * 128, :], in_=xt2[s_][:, :]), "yout", reads=[f"xt2_{s_}"], writes=[f"y{t}"])
            esE.close()
            esDE.close()

        if "merged" in dbg:
            t = dbgt("merged", [1024, NTOK], BF16)
            nrow = 1024 if stage >= 3 else 512
            P.dma("sp", lambda e: e.dma_start(out=t[0:nrow, :], in_=mergedT_scr[0:nrow, :]), "out", reads=[f"mscr{h}" for h in range(nrow // 128)])
        if "hT" in dbg:
            t2 = dbgt("hT", [128, 8, HALO + NTOK], BF16)
            P.dma("sp", lambda e: e.dma_start(out=t2[:, :, :], in_=hT[:, :, :]), "out", reads=hT_all)
        if "qk" in dbg:
            t3 = dbgt("qs", [128, NTOK], BF16)
            t4 = dbgt("ks", [128, NTOK], BF16)
            t5 = dbgt("expb", [128, NTOK], F32)
            t6 = dbgt("e2", [128, NTOK], F32)
            P.dma("sp", lambda e: e.dma_start(out=t3[:, :], in_=qsT[:, :]), "out", reads=["qsT"])
            P.dma("sp", lambda e: e.dma_start(out=t4[:, :], in_=ksT[:, :]), "out", reads=["ksT"])
            P.dma("sp", lambda e: e.dma_start(out=t5[:, :], in_=expb[:, :]), "out", reads=expb_all)
            P.dma("sp", lambda e: e.dma_start(out=t6[:, :], in_=e2bc[:, :]), "out", reads=e2_all)
        if "xmid" in dbg:
            txm = dbgt("xmid", [NTOK, D])
            P.dma("sp", lambda e: e.dma_start(out=txm[:, :], in_=xmid_scr[:, :]), "out", reads=[f"xmid{t}" for t in range(NT)])
        P.final_wait("sp", ["out", "yout"])

        with nc.Block() as block:
            @block.tensor
            def _(e): P.replay("pe", e)
            @block.scalar
            def _(e): P.replay("act", e)
            @block.vector
            def _(e): P.replay("dve", e)
            @block.gpsimd
            def _(e): P.replay("pool", e)
            @block.sync
            def _(e): P.replay("sp", e)
    return nc, dbg_out


def make_in_maps(inputs):
    f = lambda a: np.ascontiguousarray(a, dtype=np.float32)
    x = inputs["x"]
    common = {
        "g_mix": f(inputs["norm_mix_g"].reshape(1, D)),
        "w_in": f(inputs["w_in"].reshape(D, 2568)),
        "conv_wT": f(inputs["conv_w"].reshape(4, 8, 128).transpose(2, 1, 0)),
        "conv_b": f(inputs["conv_b"].reshape(8, 128).T),
        "i_bias": f(inputs["i_bias"].reshape(1, 4)),
        "f_bias": f(inputs["f_bias"].reshape(1, 4)),
        "g_ml": f(inputs["mlstm_norm_g"].reshape(1, 512)),
    }
    lre = inputs["s5_lambda_re"].reshape(32, 64).T
    lim = inputs["s5_lambda_im"].reshape(32, 64).T
    bre = inputs["s5_b_re"].reshape(32, 64, 16).transpose(1, 0, 2)
    bim = inputs["s5_b_im"].reshape(32, 64, 16).transpose(1, 0, 2)
    cre = inputs["s5_c_re"].reshape(32, 16, 64).transpose(2, 0, 1)
    cim = inputs["s5_c_im"].reshape(32, 16, 64).transpose(2, 0, 1)
    glw = inputs["s5_glu_w"].reshape(4, 8, 16, 32)
    WA = np.zeros((4, 128, 128), np.float32)
    WB = np.zeros((4, 128, 128), np.float32)
    for u in range(4):
        for g in range(8):
            WA[u, g * 16:(g + 1) * 16, g * 16:(g + 1) * 16] = glw[u, g, :, 0:16]
            WB[u, g * 16:(g + 1) * 16, g * 16:(g + 1) * 16] = glw[u, g, :, 16:32]
    glb = inputs["s5_glu_b"].reshape(4, 8, 32)
    common.update({
        "lam2_re": f(np.concatenate([lre, lre], 0)), "lam2_im": f(np.concatenate([lim, lim], 0)),
        "logdt": f(inputs["s5_log_dt"].reshape(1, 32)),
        "X1d": f(np.concatenate([bre, bim], 0)), "X2d": f(np.concatenate([bim, bre], 0)),
        "CY1d": f(np.concatenate([cre, cim], 0)), "CY2d": f(np.concatenate([cim, cre], 0)),
        "s5d": f(inputs["s5_d"].reshape(1, 512)),
        "WAd": WA, "WBd": WB,
        "w_out": f(inputs["w_out"].reshape(D, D)), "g_ffn": f(inputs["norm_ffn_g"].reshape(1, D)),
        "w_rt": f(np.concatenate([inputs["router_group_w"].reshape(D, 4), inputs["router_expert_w"].reshape(D, 32)], 1)),
        "b_rt": f(np.concatenate([inputs["router_group_b"].reshape(1, 4), inputs["router_expert_b"].reshape(1, 32)], 1)),
        "w_gate": f(inputs["expert_w_gate"].reshape(32, D, 512)), "w_up": f(inputs["expert_w_up"].reshape(32, D, 512)),
        "w_down": f(inputs["expert_w_down"].reshape(32, 512, D)), "g_fin": f(inputs["norm_final_g"].reshape(1, D)),
        "gbad": f(glb[:, :, 0:16].reshape(4, 128).T), "gbbd": f(glb[:, :, 16:32].reshape(4, 128).T),
    })
    maps = []
    for c in range(NCORES):
        b, p = c // 4, c % 4
        m = dict(common)
        m["x"] = f(x[b, p * NTOK:(p + 1) * NTOK])
        if p == 0:
            m["xh"] = np.zeros((HALO, D), np.float32)
        else:
            m["xh"] = f(x[b, p * NTOK - HALO:p * NTOK])
        pmk = np.zeros((128, 4), np.float32)
        pmk[:, :p] = 1.0
        m["pmask"] = pmk
        maps.append(m)
    return maps


_CACHE = {}


def kernel(**inputs):
    if "nc" not in _CACHE:
        _CACHE["nc"] = build()[0]
    nc = _CACHE["nc"]
    in_maps = make_in_maps(inputs)
    res = run_bass_kernel_spmd(nc, in_maps, core_ids=list(range(NCORES)))
    out = np.empty((2, 4 * NTOK, D), np.float32)
    for c in range(NCORES):
        b, p = c // 4, c % 4
        out[b, p * NTOK:(p + 1) * NTOK] = np.asarray(res.results[c]["y"], dtype=np.float32)
    return out
```

```python
import numpy as np
from contextlib import ExitStack
import concourse.bass as bass
import concourse.mybir as mybir
from concourse.bass_utils import run_bass_kernel_spmd

F32 = mybir.dt.float32
BF16 = mybir.dt.bfloat16
AF = mybir.ActivationFunctionType
ALU = mybir.AluOpType

NTOK = 2048
NT = 16
NCH = 32
HALO = 32
D = 1024
EPS = 1e-6
NCORES = 8
GROUPS4 = [[0, 1, 2, 3], [4, 5, 6, 7]]


class Prog:
    def __init__(self, nc, es):
        self.nc = nc
        self.es = es
        self.queues = {e: [] for e in ("pe", "act", "dve", "pool", "sp")}
        self.esem = {}
        self.ecnt = {}
        for e in ("pe", "act", "dve", "pool"):
            self.esem[e] = es.enter_context(nc.semaphore("sem_" + e))
            self.ecnt[e] = 0
        self.dsem = {}
        self.dcnt = {}
        self.lastw = {}
        self.readers = {}
        self.known = {e: {} for e in self.queues}

    def _deps(self, reads, writes):
        deps = []
        for r in reads:
            if r in self.lastw:
                deps.append(self.lastw[r])
        for w in writes:
            if w in self.lastw:
                deps.append(self.lastw[w])
            deps.extend(self.readers.get(w, ()))
        return deps

    def _prune(self, eng, deps):
        need = {}
        for (s, v) in deps:
            if eng == "pe" and s is self.esem["pe"]:
                continue
            if v > need.get(s, 0):
                need[s] = v
        out = []
        kn = self.known[eng]
        for s, v in need.items():
            if kn.get(s, 0) >= v:
                continue
            kn[s] = v
            out.append((s, v))
        return out

    def _record(self, tok, reads, writes):
        for r in reads:
            self.readers.setdefault(r, []).append(tok)
        for w in writes:
            self.lastw[w] = tok
            self.readers[w] = []

    def op(self, eng, fn, reads=(), writes=()):
        reads = tuple(reads)
        writes = tuple(writes)
        waits = self._prune(eng, self._deps(reads, writes))
        self.ecnt[eng] += 1
        tok = (self.esem[eng], self.ecnt[eng])
        self.queues[eng].append((waits, fn, (self.esem[eng], 1)))
        self._record(tok, reads, writes)

    def seq(self, eng, fns, reads=(), writes=()):
        self._chain = getattr(self, "_chain", 0) + 1
        ck = f"__chain{self._chain}"
        for fn in fns:
            self.op(eng, fn, reads=tuple(reads) + (ck,), writes=tuple(writes) + (ck,))

    def dma(self, q, fn, stream, reads=(), writes=(), inc=16):
        reads = tuple(reads)
        writes = tuple(writes)
        if stream not in self.dsem:
            self.dsem[stream] = self.es.enter_context(self.nc.semaphore("dsem_" + stream))
            self.dcnt[stream] = 0
        waits = self._prune(q, self._deps(reads, writes))
        self.dcnt[stream] += inc
        tok = (self.dsem[stream], self.dcnt[stream])
        self.queues[q].append((waits, fn, (self.dsem[stream], inc)))
        self._record(tok, reads, writes)

    def bulk_done(self, stream):
        s = self.dsem[stream]
        fin = self.dcnt[stream]
        for k, (ss, v) in list(self.lastw.items()):
            if ss is s:
                self.lastw[k] = (s, fin)

    def barrier(self):
        allw = [(self.esem[e], self.ecnt[e]) for e in self.esem if self.ecnt[e] > 0]
        allw += [(self.dsem[s], self.dcnt[s]) for s in self.dsem]
        for e in self.queues:
            w = self._prune(e, allw)
            if w:
                self.queues[e].append((w, None, None))

    def final_wait(self, q, streams):
        waits = [(self.dsem[st], self.dcnt[st]) for st in streams if st in self.dsem]
        self.queues[q].append((waits, None, None))

    def replay(self, eng, h):
        for waits, fn, inc in self.queues[eng]:
            for (s, v) in waits:
                h.wait_ge(s, v)
            if fn is None:
                continue
            inst = fn(h)
            inst.then_inc(inc[0], inc[1])


def build(stage=99, dbg=()):
    nc = bass.Bass("TRN2", target_bir_lowering=False)
    din = lambda name, shape, dt=F32: nc.dram_tensor(name, shape, dt, kind="ExternalInput")
    x = din("x", [NTOK, D])
    xh = din("xh", [HALO, D])
    g_mix = din("g_mix", [1, D])
    w_in = din("w_in", [D, 2568])
    conv_wT = din("conv_wT", [128, 8, 4])
    conv_b = din("conv_b", [128, 8])
    i_bias = din("i_bias", [1, 4])
    f_bias = din("f_bias", [1, 4])
    g_ml = din("g_ml", [1, 512])
    pmask = din("pmask", [128, 4])
    lam2_re = din("lam2_re", [128, 32])
    lam2_im = din("lam2_im", [128, 32])
    logdt = din("logdt", [1, 32])
    X1d = din("X1d", [128, 32, 16])
    X2d = din("X2d", [128, 32, 16])
    CY1d = din("CY1d", [128, 32, 16])
    CY2d = din("CY2d", [128, 32, 16])
    s5d = din("s5d", [1, 512])
    WAd = din("WAd", [4, 128, 128])
    WBd = din("WBd", [4, 128, 128])
    gbad = din("gbad", [128, 4])
    gbbd = din("gbbd", [128, 4])
    w_out = din("w_out", [D, D])
    g_ffn = din("g_ffn", [1, D])
    w_rt = din("w_rt", [D, 36])
    b_rt = din("b_rt", [1, 36])
    if stage >= 5:
        w_gate = din("w_gate", [32, D, 512])
        w_up = din("w_up", [32, D, 512])
        w_down = din("w_down", [32, 512, D])
    g_fin = din("g_fin", [1, D])
    y = nc.dram_tensor("y", [NTOK, D], F32, kind="ExternalOutput")
    dbg_out = {}
    def dbgt(name, shape, dt=F32):
        dbg_out[name] = nc.dram_tensor("dbg_" + name, shape, dt, kind="ExternalOutput")
        return dbg_out[name]

    mergedT_scr = nc.dram_tensor("mergedT_scr", [1024, NTOK], BF16)
    cc_in = [nc.dram_tensor(f"cc_in{h}", [128, 130], F32) for h in range(4)]
    cc_out = [nc.dram_tensor(f"cc_out{h}", [512, 130], F32) for h in range(4)]
    u_scr = nc.dram_tensor("u_scr", [NTOK, 512], BF16)
    xmid_scr = nc.dram_tensor("xmid_scr", [NTOK, D], F32)
    prep_scr = {n: nc.dram_tensor("prep_" + n, [128, 32, k], F32) for n, k in (("PWrr", 64), ("PWri", 64), ("PWbr", 8), ("PWbi", 8), ("BB1", 16), ("BB2", 16),
                                                                             ("PRs", 65), ("nPI", 65), ("sqr", 12), ("sqi", 12))}
    ccs_in = [nc.dram_tensor(f"ccs_in{h}", [128, 8], F32) for h in range(4)]
    ccs_out = [nc.dram_tensor(f"ccs_out{h}", [512, 8], F32) for h in range(4)]

    w_in_v = w_in.ap().rearrange("(kc p) c -> p kc c", p=128)

    es = ExitStack()
    with es:
        P = Prog(nc, es)
        sb = lambda name, shape, dt: es.enter_context(nc.sbuf_tensor(name, shape, dt))
        ps = lambda name, shape, dt: es.enter_context(nc.psum_tensor(name, shape, dt))

        g_bc = sb("g_bc", [128, D], F32)
        ident = sb("ident", [128, 128], BF16)
        identf = sb("identf", [128, 128], F32)
        mslab = sb("mslab", [128, NTOK], BF16)
        pm = sb("pm", [128, 4], F32)
        esH = ExitStack()
        esH.__enter__()
        hT = esH.enter_context(nc.sbuf_tensor("hT", [128, 8, HALO + NTOK], BF16))
        esB = ExitStack()
        esB.__enter__()
        sb = lambda name, shape, dt: esB.enter_context(nc.sbuf_tensor(name, shape, dt))
        ps = lambda name, shape, dt: esB.enter_context(nc.psum_tensor(name, shape, dt))
        xt = [sb(f"xt{i}", [128, D], F32) for i in range(2)]
        junk = sb("junk", [128, D], BF16)
        hb = sb("hb", [128, D], BF16)
        ssq = sb("ssq", [128, NT + 1], F32)
        rstd = sb("rstd", [128, NT + 1], F32)
        wh = sb("wh", [128, 8, 512], BF16)
        wg = sb("wg", [128, 8, 33], BF16)
        raw = sb("raw", [128, NTOK + 3], F32)
        ctmp = sb("ctmp", [128, NTOK], F32)
        qsT = sb("qsT", [128, NTOK], BF16)
        ksT = sb("ksT", [128, NTOK], BF16)
        cwt = sb("cwt", [128, 8, 4], F32)
        cbt = sb("cbt", [128, 8], F32)
        fbt = sb("fbt", [33, 4], F32)
        nfb = sb("nfb", [33, 4], F32)
        Gt = sb("Gt", [33, NTOK], F32)
        Gt2 = sb("Gt2", [33, NTOK], F32)
        cmask = sb("cmask", [33, NTOK], F32)
        sel0 = sb("sel0", [33, 128], F32)
        sel032 = sb("sel032", [33, 128], F32)
        expb = sb("expb", [128, NTOK], F32)
        e2bc = sb("e2bc", [128, NTOK], F32)
        v_ext = sb("v_ext", [64, NCH, 130], BF16)
        sigo = sb("sigo", [64, NCH, 128], BF16)
        gA = sb("gA", [64, 512], F32)
        kstok2 = sb("kstok2", [64, 2, 128], BF16)
        CTloc = sb("CTloc", [128, NCH + 1, 130], F32)
        CTball = sb("CTball", [128, NCH, 130], BF16)
        CTt = sb("CTt", [128, 130], F32)
        cg = sb("cg", [128, 4, 130], F32)
        hacc = sb("hacc", [128, 130], F32)
        hacc2 = sb("hacc2", [128, 130], F32)
        maskST = sb("maskST", [64, 64], F32)
        STb = sb("STb", [64, 4, 64], BF16)
        dnb = sb("dnb", [64, 4, 4], F32)
        hnb = sb("hnb", [64, 4, 128], F32)
        oab = sb("oab", [64, 4, 128], BF16)
        junkb = sb("junkb", [64, 4, 128], BF16)
        so32t = sb("so32t", [64, 2, 128], F32)
        so32b = [so32t[:, 0, :], so32t[:, 1, :]]

        Bk = [ps(f"bank{k}", [128, 512], F32) for k in range(8)]
        bfv = lambda k: Bk[k][:, :].bitcast(BF16)
        ps_big = [Bk[0], Bk[1]]
        tp = bfv(2).rearrange("p (a b) -> p a b", b=128)
        pvb = [Bk[3][0:64, 0:256], Bk[4][0:64, 0:256]]
        pab = [Bk[0][0:64, 0:64], Bk[1][0:64, 0:64]]
        pcb = [Bk[3][0:64, 0:130], Bk[4][0:64, 0:130]]
        pub = [Bk[5][:, 0:130], Bk[6][:, 0:130]]
        ptk = [bfv(2)[0:64, 0:128], bfv(7)[0:64, 0:128]]
        pto = [bfv(6)[:, 0:64], bfv(7)[:, 0:64]]
        KPV = ["bank3", "bank4"]; KPA = ["bank0", "bank1"]; KPC = ["bank3", "bank4"]; KPU = ["bank5", "bank6"]
        KPTK = ["bank2", "bank7"]; KPTO = ["bank6", "bank7"]

        P.dma("sp", lambda e: e.dma_start(out=g_bc[:, :], in_=g_mix[0:1, :].partition_broadcast(128)), "setup", writes=["g_bc"])
        P.dma("sp", lambda e: e.dma_start(out=cwt[:, :, :], in_=conv_wT[:, :, :]), "setup", writes=["cwt"])
        P.dma("sp", lambda e: e.dma_start(out=cbt[:, :], in_=conv_b[:, :]), "setup", writes=["cbt"])
        P.dma("sp", lambda e: e.dma_start(out=fbt[0:1, :], in_=f_bias[0:1, :]), "setup", writes=["fbt0"])
        P.dma("sp", lambda e: e.dma_start(out=fbt[32:33, :], in_=i_bias[0:1, :]), "setup", writes=["fbt32"])
        P.dma("sp", lambda e: e.dma_start(out=gA[:, :], in_=g_ml[0:1, :].partition_broadcast(64)), "setup", writes=["gA"])
        P.dma("sp", lambda e: e.dma_start(out=pm[:, :], in_=pmask[:, :]), "setup", writes=["pm"])
        P.bulk_done("setup")

        P.seq("pool", [lambda e: e.memset(identf[:, :], 0.0),
                       lambda e: e.affine_select(out=identf[:, :], in_=identf[:, :], pattern=[[-1, 128]], compare_op=ALU.not_equal,
                                                 fill=1.0, base=0, channel_multiplier=1)], writes=["identf"])
        P.op("dve", lambda e: e.tensor_copy(out=ident[:, :], in_=identf[:, :]), reads=["identf"], writes=["ident"])

        P.seq("pool", [lambda e: e.memset(cmask[:, :], 1.0),
                       lambda e: e.memset(cmask[:, 0:NTOK:64], 0.0),
                       lambda e: e.memset(sel0[:, :], 0.0),
                       lambda e: e.memset(sel0[0:1, :], 1.0),
                       lambda e: e.memset(sel032[:, :], 0.0),
                       lambda e: e.memset(sel032[0:1, :], 1.0),
                       lambda e: e.memset(sel032[32:33, :], 1.0),
                       lambda e: e.memset(Gt2[:, :], 0.0),
                       lambda e: e.memset(v_ext[:, :, 128:129], 1.0),
                       lambda e: e.memset(v_ext[:, :, 129:130], 0.0),
                       lambda e: e.memset(maskST[:, :], 1.0),
                       lambda e: e.affine_select(out=maskST[:, :], in_=maskST[:, :], pattern=[[1, 64]], compare_op=ALU.is_ge,
                                                 fill=0.0, base=0, channel_multiplier=-1)],
              writes=["cmask", "sel0", "sel032", "Gt2", "v_ext_c", "maskST"])
        P.op("pool", lambda e: e.tensor_scalar(out=nfb[0:1, :], in0=fbt[0:1, :], scalar1=-1.0, scalar2=None, op0=ALU.mult),
             reads=["fbt0"], writes=["nfb"])

        def norm_tile(src_ap, rows, slot, col0, idx, tag):
            s = slot
            P.dma("sp", lambda e: e.dma_start(out=xt[s][0:rows, :], in_=src_ap), f"xt{s}", writes=[f"xt{s}"])
            P.op("act", lambda e: e.activation(out=junk[0:rows, :], in_=xt[s][0:rows, :], func=AF.Square, accum_out=ssq[0:rows, idx:idx + 1]),
                 reads=[f"xt{s}"], writes=["junk", f"ssq{idx}"])
            P.op("act", lambda e: e.activation(out=ssq[0:rows, idx:idx + 1], in_=ssq[0:rows, idx:idx + 1], func=AF.Sqrt, scale=1.0 / D, bias=EPS),
                 reads=[f"ssq{idx}"], writes=[f"ssq{idx}"])
            P.op("dve", lambda e: e.reciprocal(out=rstd[0:rows, idx:idx + 1], in_=ssq[0:rows, idx:idx + 1]), reads=[f"ssq{idx}"], writes=[f"rstd{idx}"])
            P.op("dve", lambda e: e.scalar_tensor_tensor(out=hb[0:rows, :], in0=xt[s][0:rows, :], scalar=rstd[0:rows, idx:idx + 1], in1=g_bc[0:rows, :],
                                                         op0=ALU.mult, op1=ALU.mult),
                 reads=[f"xt{s}", f"rstd{idx}", "g_bc"], writes=["hb"])
            def f_tp(e):
                for k in range(8):
                    i = e.transpose(out=tp[:, k, 0:rows], in_=hb[0:rows, k * 128:(k + 1) * 128], identity=ident[0:rows, 0:rows])
                return i
            P.op("pe", f_tp, reads=["hb", "ident"], writes=["bank2"])
            P.op("act", lambda e: e.copy(out=hT[:, :, col0:col0 + rows], in_=tp[:, :, 0:rows]), reads=["bank2"], writes=[tag])

        norm_tile(xh[:, :], HALO, 0, 0, NT, "hT_h")
        for t in range(NT):
            norm_tile(x[t * 128:(t + 1) * 128, :], 128, (t + 1) % 2, HALO + t * 128, t, f"hT_{t // 4}")
        hT_all = ["hT_h"] + [f"hT_{i}" for i in range(4)]

        SC = float(128 ** -0.5)
        for hd in range(4 if stage >= 2 else 0):
            for j, c0 in enumerate((hd * 128, 512 + hd * 128, 1024 + hd * 128, 1536 + hd * 128)):
                P.dma("pool", lambda e, j=j, c0=c0: e.dma_start(out=wh[:, :, j * 128:(j + 1) * 128], in_=w_in_v[:, :, c0:c0 + 128]),
                      "wh", writes=[f"wh{j}"])
            P.bulk_done("wh")
            P.op("pool", lambda e: e.memset(wg[:, :, :], 0.0), writes=["wg", "wg_a", "wg_b"])
            def ld_wg(e, dst, col):
                with nc.allow_non_contiguous_dma(reason="gate columns"):
                    return e.dma_start(out=wg[:, :, dst:dst + 1], in_=w_in_v[:, :, col:col + 1])
            P.dma("pool", lambda e, hd=hd: ld_wg(e, 0, 2052 + hd), "wg", reads=["wg"], writes=["wg_a"])
            P.dma("pool", lambda e, hd=hd: ld_wg(e, 32, 2048 + hd), "wg", reads=["wg"], writes=["wg_b"])
            P.bulk_done("wg")

            for tb in range(4):
                pgt = ps_big[tb % 2]
                def f_g(e, tb=tb, pgt=pgt):
                    for k in range(8):
                        i = e.matmul(out=pgt[0:33, :], lhsT=wg[:, k, :], rhs=hT[:, k, HALO + tb * 512:HALO + (tb + 1) * 512], start=(k == 0), stop=(k == 7))
                    return i
                P.op("pe", f_g, reads=["wg", "wg_a", "wg_b", f"hT_{tb}"], writes=[f"bank{tb % 2}"])
                P.op("act", lambda e, tb=tb, pgt=pgt: e.copy(out=Gt[0:33, tb * 512:(tb + 1) * 512], in_=pgt[0:33, :]),
                     reads=[f"bank{tb % 2}"], writes=[f"Gt_{tb}"])
            Gt_all = [f"Gt_{tb}" for tb in range(4)]
            P.op("act", lambda e, hd=hd: e.activation(out=Gt[0:1, :], in_=Gt[0:1, :], func=AF.Exp, scale=-1.0, bias=nfb[0:1, hd:hd + 1]),
                 reads=Gt_all + ["nfb"], writes=["Gt_f"])
            P.op("act", lambda e: e.activation(out=Gt[0:1, :], in_=Gt[0:1, :], func=AF.Ln, bias=1.0), reads=["Gt_f"], writes=["Gt_f"])
            P.op("dve", lambda e: e.tensor_tensor_scan(out=Gt2[0:1, :], data0=cmask[0:1, :], data1=Gt[0:1, :], initial=0.0, op0=ALU.mult, op1=ALU.add),
                 reads=["Gt_f", "cmask"], writes=["Gt2_0"])
            P.op("act", lambda e, hd=hd: e.activation(out=Gt2[32:33, :], in_=Gt[32:33, :], func=AF.Identity, bias=fbt[32:33, hd:hd + 1]),
                 reads=Gt_all + ["fbt32", "Gt2"], writes=["Gt2_32"])
            for tb in range(4):
                pgt = ps_big[tb % 2]
                P.op("pe", lambda e, tb=tb, pgt=pgt: e.matmul(out=pgt[:, :], lhsT=sel0[0:33, :], rhs=Gt2[0:33, tb * 512:(tb + 1) * 512], start=True, stop=True),
                     reads=["sel0", "Gt2", "Gt2_0", "Gt2_32"], writes=[f"bank{tb % 2}"])
                P.op("act", lambda e, tb=tb, pgt=pgt: e.activation(out=expb[:, tb * 512:(tb + 1) * 512], in_=pgt[:, :], func=AF.Exp, scale=-1.0),
                     reads=[f"bank{tb % 2}"], writes=[f"expb_{tb}"])
            for tb in range(4):
                pgt = ps_big[tb % 2]
                P.op("pe", lambda e, tb=tb, pgt=pgt: e.matmul(out=pgt[:, :], lhsT=sel032[0:33, :], rhs=Gt2[0:33, tb * 512:(tb + 1) * 512], start=True, stop=True),
                     reads=["sel032", "Gt2", "Gt2_0", "Gt2_32"], writes=[f"bank{tb % 2}"])
                P.op("act", lambda e, tb=tb, pgt=pgt: e.activation(out=e2bc[:, tb * 512:(tb + 1) * 512], in_=pgt[:, :], func=AF.Exp),
                     reads=[f"bank{tb % 2}"], writes=[f"e2bc_{tb}"])
            expb_all = [f"expb_{tb}" for tb in range(4)]
            e2_all = [f"e2bc_{tb}" for tb in range(4)]

            for qi in range(2):
                cidx = qi * 4 + hd
                def f_halo(e, qi=qi):
                    for k in range(8):
                        i = e.matmul(out=ps_big[0][:, 0:HALO], lhsT=wh[:, k, qi * 128:(qi + 1) * 128], rhs=hT[:, k, 0:HALO], start=(k == 0), stop=(k == 7))
                    return i
                P.op("pe", f_halo, reads=[f"wh{qi}", "hT_h"], writes=["bank0"])
                P.op("act", lambda e: e.copy(out=raw[:, 0:3], in_=ps_big[0][:, HALO - 3:HALO]), reads=["bank0"], writes=["raw_h"])
                for tb in range(4):
                    pgt = ps_big[(tb + 1) % 2]
                    def f_q(e, tb=tb, qi=qi, pgt=pgt):
                        for k in range(8):
                            i = e.matmul(out=pgt[:, :], lhsT=wh[:, k, qi * 128:(qi + 1) * 128], rhs=hT[:, k, HALO + tb * 512:HALO + (tb + 1) * 512],
                                         start=(k == 0), stop=(k == 7))
                        return i
                    P.op("pe", f_q, reads=[f"wh{qi}", f"hT_{tb}"], writes=[f"bank{(tb + 1) % 2}"])
                    P.op("act", lambda e, tb=tb, pgt=pgt: e.copy(out=raw[:, 3 + tb * 512:3 + (tb + 1) * 512], in_=pgt[:, :]),
                         reads=[f"bank{(tb + 1) % 2}"], writes=[f"raw_{tb}"])
                raw_all = ["raw_h"] + [f"raw_{tb}" for tb in range(4)]
                fl = [lambda e, cidx=cidx: e.tensor_scalar(out=ctmp[:, :], in0=raw[:, 0:NTOK], scalar1=cwt[:, cidx, 0:1], scalar2=cbt[:, cidx:cidx + 1],
                                                           op0=ALU.mult, op1=ALU.add)]
                for j in (1, 2, 3):
                    fl.append(lambda e, cidx=cidx, j=j: e.scalar_tensor_tensor(out=ctmp[:, :], in0=raw[:, j:j + NTOK], scalar=cwt[:, cidx, j:j + 1],
                                                                               in1=ctmp[:, :], op0=ALU.mult, op1=ALU.add))
                P.seq("dve", fl, reads=raw_all + ["cwt", "cbt"], writes=["ctmp"])
                P.op("act", lambda e: e.activation(out=ctmp[:, :], in_=ctmp[:, :], func=AF.Silu), reads=["ctmp"], writes=["ctmp"])
                if qi == 0:
                    P.op("dve", lambda e: e.tensor_tensor(out=qsT[:, :], in0=ctmp[:, :], in1=expb[:, :], op=ALU.mult),
                         reads=["ctmp"] + expb_all, writes=["qsT"])
                else:
                    P.op("dve", lambda e: e.scalar_tensor_tensor(out=ksT[:, :], in0=ctmp[:, :], scalar=SC, in1=e2bc[:, :], op0=ALU.mult, op1=ALU.mult),
                         reads=["ctmp"] + e2_all, writes=["ksT"])

            for c0 in range(0, NCH, 2):
                for c in (c0, c0 + 1):
                    b = c % 2
                    def f_v(e, c=c, b=b):
                        for k in range(8):
                            i = e.matmul(out=pvb[b], lhsT=hT[:, k, HALO + c * 64:HALO + (c + 1) * 64], rhs=wh[:, k, 256:512], start=(k == 0), stop=(k == 7))
                        return i
                    P.op("pe", f_v, reads=["wh2", "wh3", f"hT_{c // 8}"], writes=[KPV[b]])
                for c in (c0, c0 + 1):
                    b = c % 2
                    P.op("act", lambda e, c=c, b=b: e.copy(out=v_ext[:, c, 0:128], in_=pvb[b][:, 0:128]), reads=[KPV[b]], writes=[f"v_{c}"])
                    P.op("act", lambda e, b=b: e.activation(out=so32b[b], in_=pvb[b][:, 128:256], func=AF.Sigmoid), reads=[KPV[b]], writes=[f"so32_{b}"])
                for c in (c0, c0 + 1):
                    b = c % 2
                    P.op("dve", lambda e, c=c, hd=hd, b=b: e.tensor_tensor(out=sigo[:, c, :], in0=so32b[b], in1=gA[:, hd * 128:(hd + 1) * 128], op=ALU.mult),
                         reads=[f"so32_{b}", "gA"], writes=[f"sigo_{c}"])

            P.seq("pool", [lambda e: e.memset(CTloc[:, 0, :], 0.0), lambda e: e.memset(CTloc[:, 0, 129:130], 1.0)], writes=["CTloc_0"])
            for c in range(NCH):
                b = c % 2
                P.op("pe", lambda e, c=c, b=b: e.transpose(out=ptk[b], in_=ksT[:, c * 64:(c + 1) * 64], identity=ident[:, :]),
                     reads=["ksT", "ident"], writes=[KPTK[b]])
                P.op("act", lambda e, b=b: e.copy(out=kstok2[:, b, :], in_=ptk[b]), reads=[KPTK[b]], writes=[f"kstok{b}"])
                P.op("pe", lambda e, c=c, b=b: e.matmul(out=pub[b], lhsT=kstok2[:, b, :], rhs=v_ext[:, c, :], start=True, stop=True),
                     reads=[f"kstok{b}", f"v_{c}", "v_ext_c"], writes=[KPU[b]])
                P.op("dve", lambda e, c=c, b=b: e.tensor_tensor(out=CTt[:, :], in0=pub[b], in1=CTloc[:, c, :], op=ALU.add),
                     reads=[KPU[b], f"CTloc_{c}"], writes=["CTt"])
                P.op("dve", lambda e, c=c: e.tensor_scalar(out=CTloc[:, c + 1, :], in0=CTt[:, :], scalar1=expb[:, c * 64 + 63:c * 64 + 64], scalar2=None, op0=ALU.mult),
                     reads=["CTt", f"expb_{c // 8}"], writes=[f"CTloc_{c + 1}"])

            P.dma("sp", lambda e, hd=hd: e.dma_start(out=cc_in[hd][:, :], in_=CTloc[:, NCH, :]), f"ccin{hd}", reads=[f"CTloc_{NCH}"], writes=[f"cc_in{hd}"])
            def f_cc(e, hd=hd):
                return e.collective_compute("AllGather", ALU.bypass, replica_groups=GROUPS4,
                                            ins=[cc_in[hd].ap().opt()], outs=[cc_out[hd].ap().opt()])
            P.dma("pool", f_cc, f"cc{hd}", reads=[f"cc_in{hd}"], writes=[f"cc_out{hd}"], inc=1)
            P.dma("sp", lambda e, hd=hd: e.dma_start(out=cg[:, :, :], in_=cc_out[hd].ap().rearrange("(r p) c -> p r c", p=128)),
                  f"ccld{hd}", reads=[f"cc_out{hd}"], writes=["cg"])
            P.op("pool", lambda e: e.memset(hacc[:, :], 0.0), writes=["hacc"])
            for pp in range(3):
                P.seq("dve", [
                    lambda e, pp=pp: e.scalar_tensor_tensor(out=hacc2[:, :], in0=hacc[:, :], scalar=cg[:, pp, 129:130], in1=cg[:, pp, :], op0=ALU.mult, op1=ALU.add),
                    lambda e: e.tensor_tensor(out=hacc2[:, :], in0=hacc2[:, :], in1=hacc[:, :], op=ALU.subtract),
                    lambda e, pp=pp: e.scalar_tensor_tensor(out=hacc[:, :], in0=hacc2[:, :], scalar=pm[:, pp:pp + 1], in1=hacc[:, :], op0=ALU.mult, op1=ALU.add)],
                    reads=["hacc", "cg", "pm"], writes=["hacc", "hacc2"])
            for c in range(NCH):
                eng = "dve"
                P.op(eng, lambda e, c=c: e.scalar_tensor_tensor(out=CTball[:, c, :], in0=hacc[:, :], scalar=CTloc[:, c, 129:130], in1=CTloc[:, c, :],
                                                                op0=ALU.mult, op1=ALU.add),
                     reads=["hacc", f"CTloc_{c}"], writes=[f"CTb_{c}"])

            NB = 2
            for c0 in range(0, NCH, NB):
                cl = list(range(c0, c0 + NB))
                for c in cl:
                    b = c % NB
                    cs = slice(c * 64, (c + 1) * 64)
                    P.op("pe", lambda e, cs=cs, b=b: e.matmul(out=pab[b], lhsT=ksT[:, cs], rhs=qsT[:, cs], start=True, stop=True),
                         reads=["ksT", "qsT"], writes=[KPA[b]])
                for c in cl:
                    b = c % NB
                    P.op("dve", lambda e, b=b: e.tensor_tensor(out=STb[:, b, :], in0=pab[b], in1=maskST[:, :], op=ALU.mult), reads=[KPA[b], "maskST"], writes=[f"ST{b}"])
                for c in cl:
                    b = c % NB
                    cs = slice(c * 64, (c + 1) * 64)
                    def f_cn(e, c=c, cs=cs, b=b):
                        e.matmul(out=pcb[b], lhsT=STb[:, b, :], rhs=v_ext[:, c, :], start=True, stop=False)
                        return e.matmul(out=pcb[b], lhsT=qsT[:, cs], rhs=CTball[:, c, :], start=False, stop=True)
                    P.op("pe", f_cn, reads=[f"ST{b}", f"v_{c}", "v_ext_c", "qsT", f"CTb_{c}"], writes=[KPC[b]])
                for c in cl:
                    b = c % NB
                    P.seq("dve", [
                        lambda e, b=b: e.tensor_copy(out=dnb[:, b, 1:2], in_=pcb[b][:, 128:129]),
                        lambda e, b=b: e.scalar_tensor_tensor(out=dnb[:, b, 0:1], in0=dnb[:, b, 1:2], scalar=-1.0, in1=dnb[:, b, 1:2], op0=ALU.mult, op1=ALU.max),
                        lambda e, b=b: e.tensor_scalar(out=dnb[:, b, 0:1], in0=dnb[:, b, 0:1], scalar1=1.0, scalar2=None, op0=ALU.max),
                        lambda e, b=b: e.reciprocal(out=dnb[:, b, 1:2], in_=dnb[:, b, 0:1]),
                        lambda e, b=b: e.tensor_scalar(out=hnb[:, b, :], in0=pcb[b][:, 0:128], scalar1=dnb[:, b, 1:2], scalar2=None, op0=ALU.mult)],
                        reads=[KPC[b]], writes=[f"dn{b}", f"hn{b}"])
                for c in cl:
                    b = c % NB
                    P.seq("act", [
                        lambda e, b=b: e.activation(out=junkb[:, b, :], in_=hnb[:, b, :], func=AF.Square, accum_out=dnb[:, b, 2:3]),
                        lambda e, b=b: e.activation(out=dnb[:, b, 2:3], in_=dnb[:, b, 2:3], func=AF.Sqrt, scale=1.0 / 128, bias=EPS)],
                        reads=[f"hn{b}", f"dn{b}"], writes=[f"junk{b}", f"dn2_{b}"])
                for c in cl:
                    b = c % NB
                    P.seq("dve", [
                        lambda e, b=b: e.reciprocal(out=dnb[:, b, 3:4], in_=dnb[:, b, 2:3]),
                        lambda e, c=c, b=b: e.scalar_tensor_tensor(out=oab[:, b, :], in0=hnb[:, b, :], scalar=dnb[:, b, 3:4], in1=sigo[:, c, :], op0=ALU.mult, op1=ALU.mult)],
                        reads=[f"dn2_{b}", f"hn{b}", f"sigo_{c}"], writes=[f"oa{b}", f"dn{b}"])
                for c in cl:
                    b = c % NB
                    P.op("pe", lambda e, b=b: e.transpose(out=pto[b], in_=oab[:, b, :], identity=ident[0:64, 0:64]), reads=[f"oa{b}", "ident"], writes=[KPTO[b]])
                for c in cl:
                    b = c % NB
                    cs = slice(c * 64, (c + 1) * 64)
                    P.op("act", lambda e, cs=cs, b=b: e.copy(out=mslab[:, cs], in_=pto[b]), reads=[KPTO[b]], writes=["mslab"])
            P.dma("sp", lambda e, hd=hd: e.dma_start(out=mergedT_scr[hd * 128:(hd + 1) * 128, :], in_=mslab[:, :]), "mscr", reads=["mslab"], writes=[f"mscr{hd}"])

        esB.close()
        P.barrier()
        if stage >= 3:
            esC1 = ExitStack()
            esC1.__enter__()
            sb = lambda name, shape, dt: esC1.enter_context(nc.sbuf_tensor(name, shape, dt))
            ps = lambda name, shape, dt: esC1.enter_context(nc.psum_tensor(name, shape, dt))
            wu = sb("wu", [128, 8, 512], BF16)
            utok = [sb(f"utok{i}", [128, 512], BF16) for i in range(2)]
            pu1 = [ps(f"pu1_{i}", [128, 512], F32) for i in range(2)]
            P.dma("pool", lambda e: e.dma_start(out=wu[:, :, :], in_=w_in_v[:, :, 2056:2568]), "wu", writes=["wu"])
            for t in range(NT):
                def f_u1(e, t=t):
                    for k in range(8):
                        i = e.matmul(out=pu1[t % 2][:, :], lhsT=hT[:, k, HALO + t * 128:HALO + (t + 1) * 128], rhs=wu[:, k, :], start=(k == 0), stop=(k == 7))
                    return i
                P.op("pe", f_u1, reads=["wu", f"hT_{t // 4}"], writes=[f"pu1_{t % 2}"])
                P.op("act", lambda e, t=t: e.copy(out=utok[t % 2][:, :], in_=pu1[t % 2][:, :]), reads=[f"pu1_{t % 2}"], writes=[f"utok{t % 2}"])
                P.dma("sp", lambda e, t=t: e.dma_start(out=u_scr[t * 128:(t + 1) * 128, :], in_=utok[t % 2][:, :]), f"uscr{t % 2}",
                      reads=[f"utok{t % 2}"], writes=[f"u_scr{t}"])
            esC1.close()
            esH.close()
            P.barrier()

            TT = lambda o, a, b, op: (lambda e: e.tensor_tensor(out=o, in0=a, in1=b, op=op))
            TS = lambda o, a, s1, s2, op0, op1=None: ((lambda e: e.tensor_scalar(out=o, in0=a, scalar1=s1, scalar2=s2, op0=op0, op1=op1)) if op1 is not None
                                                      else (lambda e: e.tensor_scalar(out=o, in0=a, scalar1=s1, scalar2=None, op0=op0)))
            ACT = lambda o, a, f, **kw: (lambda e: e.activation(out=o, in_=a, func=f, **kw))
            MUL, ADD, SUB = ALU.mult, ALU.add, ALU.subtract

            def cmul(o_r, o_i, a_r, a_i, s_r, s_i, t1, t2):
                return [TT(t1, a_r, s_r, MUL), TT(t2, a_i, s_i, MUL), TT(o_r, t1, t2, SUB),
                        TT(t1, a_r, s_i, MUL), TT(t2, a_i, s_r, MUL), TT(o_i, t1, t2, ADD)]

            def emit_prep(GP, gs, lam_re_t, lam_im_t, logdt_t, X1t, X2t, sgn, hpi, sc, sqr, sqi, bqr, bqi, a5r, a5i, PWfr, PWfi, PWrr, PWri, PWbr, PWbi, cta, ctb, BB1, BB2, bbt, PRs, nPI):
                ops = []
                S = {k: v[:, :] for k, v in sc.items()}
                ops = []
                ops.append(TS(S["lr"], lam_re_t[:, gs], -1e-4, None, ALU.min))
                P.seq("dve", ops, reads=["lam_re_t"], writes=["prep"]); ops = []
                P.op("act", ACT(S["dt"], logdt_t[:, gs], AF.Exp), reads=["logdt_t", "prep"], writes=["prep_dt"])
                ops += [TT(S["lrdt"], S["lr"], S["dt"], MUL), TT(S["th"], lam_im_t[:, gs], S["dt"], MUL)]
                P.seq("dve", ops, reads=["prep", "prep_dt", "lam_im_t"], writes=["prep"]); ops = []
                P.seq("act", [ACT(S["sn"], S["th"], AF.Sin, scale=1.0 / 32), ACT(S["cs"], S["th"], AF.Sin, scale=1.0 / 32, bias=hpi[:, 0:1]),
                              ACT(S["mag"], S["lrdt"], AF.Exp, scale=1.0 / 32), ACT(S["im2"], S["lrdt"], AF.Exp, scale=-2.0)],
                      reads=["prep", "hpi"], writes=["prep_cs"])
                li = lam_im_t[:, gs]
                ops += [TT(a5r[:, :, 0], S["mag"], S["cs"], MUL), TT(a5i[:, :, 0], S["mag"], S["sn"], MUL)]
                for e_ in range(5):
                    ops += cmul(a5r[:, :, e_ + 1], a5i[:, :, e_ + 1], a5r[:, :, e_], a5i[:, :, e_], a5r[:, :, e_], a5i[:, :, e_], S["ta"], S["nsq"])
                ops += [(lambda e: e.tensor_copy(out=S["ar"], in_=a5r[:, :, 5])), (lambda e: e.tensor_copy(out=S["ai"], in_=a5i[:, :, 5])),
                        TT(S["den"], S["lr"], S["lr"], MUL), TT(S["ta"], li, li, MUL), TT(S["den"], S["den"], S["ta"], ADD),
                        (lambda e: e.reciprocal(out=S["den"], in_=S["den"])),
                        TS(S["am1"], S["ar"], -1.0, None, ADD),
                        TT(S["ta"], S["am1"], S["lr"], MUL), TT(S["tb"], S["ai"], li, MUL), TT(S["ta"], S["ta"], S["tb"], ADD), TT(S["cr"], S["ta"], S["den"], MUL),
                        TT(S["ta"], S["ai"], S["lr"], MUL), TT(S["tb"], S["am1"], li, MUL), TT(S["ta"], S["ta"], S["tb"], SUB), TT(S["ci"], S["ta"], S["den"], MUL),
                        TS(S["scr"], S["cr"], sgn[:, 0:1], None, MUL),
                        TS(S["tb"], S["ci"], sgn[:, 0:1], None, MUL),
                        TS(S["nci"], S["ci"], -1.0, None, MUL),
                        TT(S["bir"], S["ar"], S["im2"], MUL), TT(S["bii"], S["ai"], S["im2"], MUL), TS(S["bii"], S["bii"], -1.0, None, MUL),
                        (lambda e: e.tensor_copy(out=sqr[:, :, 0], in_=S["ar"])), (lambda e: e.tensor_copy(out=sqi[:, :, 0], in_=S["ai"])),
                        (lambda e: e.tensor_copy(out=bqr[:, :, 0], in_=S["bir"])), (lambda e: e.tensor_copy(out=bqi[:, :, 0], in_=S["bii"]))]
                for e_ in range(11):
                    ops += cmul(sqr[:, :, e_ + 1], sqi[:, :, e_ + 1], sqr[:, :, e_], sqi[:, :, e_], sqr[:, :, e_], sqi[:, :, e_], S["ta"], S["nsq"])
                for e_ in range(2):
                    ops += cmul(bqr[:, :, e_ + 1], bqi[:, :, e_ + 1], bqr[:, :, e_], bqi[:, :, e_], bqr[:, :, e_], bqi[:, :, e_], S["ta"], S["nsq"])
                bc16 = lambda a: a.unsqueeze(2).to_broadcast([128, GP, 16])
                ops += [TT(BB1[:, :, :], X1t[:, gs, :], bc16(S["cr"]), MUL), TT(bbt[:, :, :], X2t[:, gs, :], bc16(S["tb"]), MUL), TT(BB1[:, :, :], BB1[:, :, :], bbt[:, :, :], ADD),
                        TT(BB2[:, :, :], X2t[:, gs, :], bc16(S["scr"]), MUL), TT(bbt[:, :, :], X1t[:, gs, :], bc16(S["nci"]), MUL), TT(BB2[:, :, :], BB2[:, :, :], bbt[:, :, :], ADD)]
                ops += [(lambda e: e.memset(PWfr[:, :, 0:1], 1.0)), (lambda e: e.memset(PWfi[:, :, 0:1], 0.0))]
                for k in range(6):
                    n = 1 << k
                    bcn = lambda a, n=n: a.to_broadcast([128, GP, n])
                    ops += cmul(PWfr[:, :, n:2 * n], PWfi[:, :, n:2 * n], PWfr[:, :, 0:n], PWfi[:, :, 0:n],
                                bcn(sqr[:, :, k:k + 1]), bcn(sqi[:, :, k:k + 1]), cta[:, :, 0:n], ctb[:, :, 0:n])
                ops += [(lambda e: e.tensor_copy(out=PWfr[:, :, 64:65], in_=sqr[:, :, 6:7])), (lambda e: e.tensor_copy(out=PWfi[:, :, 64:65], in_=sqi[:, :, 6:7]))]
                ops += [(lambda e: e.memset(PWrr[:, :, 63:64], 1.0)), (lambda e: e.memset(PWri[:, :, 63:64], 0.0))]
                for k in range(6):
                    n = 1 << k
                    bcn = lambda a, n=n: a.to_broadcast([128, GP, n])
                    ops += cmul(PWrr[:, :, 64 - 2 * n:64 - n], PWri[:, :, 64 - 2 * n:64 - n], PWrr[:, :, 64 - n:64], PWri[:, :, 64 - n:64],
                                bcn(sqr[:, :, k:k + 1]), bcn(sqi[:, :, k:k + 1]), cta[:, :, 0:n], ctb[:, :, 0:n])
                ops += [(lambda e: e.memset(PWbr[:, :, 0:1], 1.0)), (lambda e: e.memset(PWbi[:, :, 0:1], 0.0))]
                for k in range(3):
                    n = 1 << k
                    bcn = lambda a, n=n: a.to_broadcast([128, GP, n])
                    ops += cmul(PWbr[:, :, n:2 * n], PWbi[:, :, n:2 * n], PWbr[:, :, 0:n], PWbi[:, :, 0:n],
                                bcn(bqr[:, :, k:k + 1]), bcn(bqi[:, :, k:k + 1]), cta[:, :, 0:n], ctb[:, :, 0:n])
                ops += [TS(PRs[:, :, :], PWfr[:, :, :], sgn[:, 0:1], None, MUL), TS(PRs[:, :, :], PRs[:, :, :], -1.0, None, MUL), TS(nPI[:, :, :], PWfi[:, :, :], -1.0, None, MUL)]
                P.seq("dve", ops, reads=["prep", "prep_sn", "prep_cs", "sgn", "X1t", "X2t", "lam_im_t", "Rk_use", "tab_use"], writes=["prep", "prepT"]); ops = []

            esC0 = ExitStack()
            esC0.__enter__()
            sb0 = lambda name, shape, dt: esC0.enter_context(nc.sbuf_tensor(name, shape, dt))
            GA = 32
            i_lre = sb0("i_lre", [128, 32], F32); i_lim = sb0("i_lim", [128, 32], F32); i_ldt = sb0("i_ldt", [128, 32], F32)
            i_X1 = sb0("i_X1", [128, 32, 16], F32); i_X2 = sb0("i_X2", [128, 32, 16], F32)
            i_sgn = sb0("i_sgn", [128, 1], F32); i_hpi = sb0("i_hpi", [128, 1], F32)
            for dst_, src_ in ((i_lre[:, :], lam2_re[:, :]), (i_lim[:, :], lam2_im[:, :]), (i_ldt[:, :], logdt[0:1, :].partition_broadcast(128)),
                               (i_X1[:, :, :], X1d[:, :, :]), (i_X2[:, :, :], X2d[:, :, :])):
                P.dma("sp", lambda e, dst_=dst_, src_=src_: e.dma_start(out=dst_, in_=src_), "setupC0", writes=["c0in"])
            P.bulk_done("setupC0")
            P.seq("pool", [lambda e: e.memset(i_sgn[0:64, :], -1.0), lambda e: e.memset(i_sgn[64:128, :], 1.0), lambda e: e.memset(i_hpi[:, :], float(np.pi / 2))],
                  writes=["sgn", "hpi", "lam_re_t", "lam_im_t", "logdt_t", "X1t", "X2t"], reads=["c0in"])
            sc_names = ("lr", "dt", "lrdt", "th", "mag", "t1", "sn", "cs", "ar", "ai", "den", "am1", "cr", "ci", "ta", "tb", "scr", "nci", "im2", "bir", "bii", "nsq")
            sc0 = {n: sb0("sc0_" + n, [128, GA], F32) for n in sc_names}
            T0 = {n: sb0("p0_" + n, [128, GA, k], F32) for n, k in (("sqr", 12), ("sqi", 12), ("bqr", 3), ("bqi", 3), ("a5r", 6), ("a5i", 6), ("PWfr", 65), ("PWfi", 65),
                                                                     ("PWrr", 64), ("PWri", 64), ("PWbr", 8), ("PWbi", 8), ("cta", 64), ("ctb", 64), ("BB1", 16), ("BB2", 16),
                                                                     ("bbt", 16), ("PRs", 65), ("nPI", 65))}
            emit_prep(GA, slice(0, 32), i_lre, i_lim, i_ldt, i_X1, i_X2, i_sgn, i_hpi, sc0, *[T0[n] for n in ("sqr", "sqi", "bqr", "bqi", "a5r", "a5i", "PWfr", "PWfi", "PWrr", "PWri", "PWbr", "PWbi",
                                                                 "cta", "ctb", "BB1", "BB2", "bbt", "PRs", "nPI")])
            for nm_ in ("PWrr", "PWri", "PWbr", "PWbi", "BB1", "BB2", "PRs", "nPI", "sqr", "sqi"):
                P.dma("sp", lambda e, nm_=nm_: e.dma_start(out=prep_scr[nm_][:, :, :], in_=T0[nm_][:, :, :]), "prepst", reads=["prepT"], writes=["prep_scr"])
            P.bulk_done("prepst")
            esC0.close()
            P.barrier()
            esC = ExitStack()
            esC.__enter__()
            sb = lambda name, shape, dt: esC.enter_context(nc.sbuf_tensor(name, shape, dt))
            ps = lambda name, shape, dt: esC.enter_context(nc.psum_tensor(name, shape, dt))
            G8 = 8
            lam_re_t = sb("lam_re_t", [128, 32], F32)
            lam_im_t = sb("lam_im_t", [128, 32], F32)
            logdt_t = sb("logdt_t", [128, 32], F32)
            X1t = sb("X1t", [128, 32, 16], F32)
            X2t = sb("X2t", [128, 32, 16], F32)
            CY1t = sb("CY1t", [128, 32, 16], F32)
            CY2t = sb("CY2t", [128, 32, 16], F32)
            sgn = sb("sgn", [128, 1], F32)
            d_bc = sb("d_bc", [32, 512], F32)
            WAt = sb("WAt", [128, 4, 128], BF16)
            WBt = sb("WBt", [128, 4, 128], BF16)
            gba = sb("gba", [128, 4], F32)
            gbb = sb("gbb", [128, 4], F32)
            SW = sb("SW", [128, 128], F32)
            rkt8 = sb("rkt8", [128, G8, 128], F32)
            send = sb("send", [128, G8], F32)
            maskG = sb("maskG", [128, 64, 16], F32)
            sc = {n: sb("sc_" + n, [128, G8], F32) for n in
                  ("lr", "dt", "lrdt", "th", "mag", "t1", "sn", "cs", "ar", "ai", "den", "am1", "cr", "ci", "ta", "tb", "scr", "nci", "im2", "bir", "bii", "nsq")}
            a5r = sb("a5r", [128, G8, 6], F32)
            a5i = sb("a5i", [128, G8, 6], F32)
            hpi = sb("hpi", [128, 1], F32)
            sqr = sb("sqr", [128, G8, 12], F32)
            sqi = sb("sqi", [128, G8, 12], F32)
            bqr = sb("bqr", [128, G8, 3], F32)
            bqi = sb("bqi", [128, G8, 3], F32)
            PWfr = sb("PWfr", [128, G8, 65], F32)
            PWfi = sb("PWfi", [128, G8, 65], F32)
            PWrr = sb("PWrr", [128, G8, 64], F32)
            PWri = sb("PWri", [128, G8, 64], F32)
            PWbr = sb("PWbr", [128, G8, 8], F32)
            PWbi = sb("PWbi", [128, G8, 8], F32)
            cta = sb("cta", [128, G8, 64], F32)
            ctb = sb("ctb", [128, G8, 64], F32)
            BB1 = sb("BB1", [128, G8, 16], F32)
            BB2 = sb("BB2", [128, G8, 16], F32)
            bbt = sb("bbt", [128, G8, 16], F32)
            PRs = sb("PRs", [128, G8, 65], F32)
            nPI = sb("nPI", [128, G8, 65], F32)
            tA = sb("tA", [128, 65, 16], F32)
            tB = sb("tB", [128, 65, 16], F32)
            ZTb = sb("ZTb", [128, 64, 16], BF16)
            XTb = sb("XTb", [128, 8, 16], BF16)
            Zt = sb("Zt", [128, G8, 8, 128], BF16)
            Gtab = sb("Gtab", [128, G8, 1024], BF16)
            Yt = sb("Yt", [128, G8, 65, 16], BF16)
            Rk = sb("Rk", [128, G8, 6, 128], F32)
            ucj = sb("ucj", [32, G8, 64, 16], BF16)
            ustack = sb("ustack", [128, G8, 8, 32], BF16)
            E32 = sb("E32", [128, G8, 32], F32)
            Xs = sb("Xs", [128, G8, 33], F32)
            Sprevb = sb("Sprevb", [128, G8, 32], BF16)
            sg_t = sb("sg_t", [128, 4, G8], F32)
            hs = sb("hs", [128, G8], F32)
            hs2 = sb("hs2", [128, G8], F32)
            du = sb("du", [32, 64, 16], F32)
            yv = sb("yv", [32, 1024], F32)
            y2 = sb("y2", [32, 1024], F32)
            ysg = sb("ysg", [32, 1024], F32)
            ygel = sb("ygel", [32, 64, 128], BF16)
            ygT = sb("ygT", [128, 64, 32], BF16)
            sbt = sb("sbt", [128, 512], F32)
            ps_z = [ps(f"ps_z{i}", [128, 512], F32) for i in range(2)]
            pT = ps("pT", [128, 64, 32], BF16)
            pE = ps("pE", [128, G8, 32], F32)
            pD = ps("pD", [128, G8, 33], F32)
            pY = ps("pY", [128, 1024], F32)

            def ld(dst, src, key, q="sp"):
                P.dma(q, lambda e: e.dma_start(out=dst, in_=src), "setupC", writes=[key])
            ld(lam_re_t[:, :], lam2_re[:, :], "lam_re_t")
            ld(lam_im_t[:, :], lam2_im[:, :], "lam_im_t")
            ld(logdt_t[:, :], logdt[0:1, :].partition_broadcast(128), "logdt_t")
            ld(X1t[:, :, :], X1d[:, :, :], "X1t")
            ld(X2t[:, :, :], X2d[:, :, :], "X2t")
            ld(CY1t[:, :, :], CY1d[:, :, :], "CY1t")
            ld(CY2t[:, :, :], CY2d[:, :, :], "CY2t")
            ld(d_bc[:, :], s5d[0:1, :].partition_broadcast(32), "d_bc")
            ld(gba[:, :], gbad[:, :], "gba")
            ld(gbb[:, :], gbbd[:, :], "gbb")
            ld(WAt[:, :, :], WAd.ap().rearrange("u p c -> p u c"), "WAt", q="pool")
            ld(WBt[:, :, :], WBd.ap().rearrange("u p c -> p u c"), "WBt", q="pool")
            P.bulk_done("setupC")
            P.seq("pool", [lambda e: e.memset(sgn[0:64, :], -1.0), lambda e: e.memset(sgn[64:128, :], 1.0)], writes=["sgn"])
            P.op("pool", lambda e: e.memset(hpi[:, :], float(np.pi / 2)), writes=["hpi"])
            P.seq("pool", [lambda e: e.memset(SW[:, :], 0.0),
                           lambda e: e.affine_select(out=SW[:, :], in_=SW[:, :], pattern=[[-1, 128]], compare_op=ALU.not_equal, fill=1.0, base=64, channel_multiplier=1),
                           lambda e: e.affine_select(out=SW[:, :], in_=SW[:, :], pattern=[[-1, 128]], compare_op=ALU.not_equal, fill=1.0, base=-64, channel_multiplier=1)],
                  writes=["SW"])
            P.seq("pool", [lambda e: e.memset(maskG[:, :, :], 1.0),
                           lambda e: e.affine_select(out=maskG[:, :, :], in_=maskG[:, :, :], pattern=[[16, 64], [0, 16]], compare_op=ALU.is_ge, fill=0.0,
                                                     base=15, channel_multiplier=-1)], writes=["maskG"])

            PI = float(np.pi)
            for un in range(4):
                gs = slice(un * 8, (un + 1) * 8)
                for nm_, dst_ in (("PWrr", PWrr), ("PWri", PWri), ("PWbr", PWbr), ("PWbi", PWbi), ("BB1", BB1), ("BB2", BB2),
                                  ("PRs", PRs), ("nPI", nPI), ("sqr", sqr), ("sqi", sqi)):
                    P.dma("sp", lambda e, nm_=nm_, dst_=dst_, gs=gs: e.dma_start(out=dst_[:, :, :], in_=prep_scr[nm_][:, gs, :]), "prepld",
                          reads=["prep_scr", "Rk_use", "tab_use"], writes=["prepT"])
                P.bulk_done("prepld")

                for g in range(G8):
                    ga = un * 8 + g
                    bq = lambda a: a.unsqueeze(2).to_broadcast([128, 64, 16])
                    bh = lambda a, n: a.unsqueeze(1).to_broadcast([128, n, 16])
                    P.seq("dve", [TT(tA[:, 0:64, :], bq(PWrr[:, g, :]), bh(BB1[:, g, :], 64), MUL),
                                  TT(tB[:, 0:64, :], bq(PWri[:, g, :]), bh(BB2[:, g, :], 64), MUL),
                                  TT(ZTb[:, :, :], tA[:, 0:64, :], tB[:, 0:64, :], ADD)],
                          reads=["prepT", "ZTb_use"], writes=["tA", "tB", "ZTb"])
                    def f_zt(e):
                        for kc in range(8):
                            i = e.transpose(out=pT[:, kc * 4:(kc + 1) * 4, :], in_=ZTb[:, kc * 8:(kc + 1) * 8, :], identity=ident[:, :])
                        return i
                    P.op("pe", f_zt, reads=["ZTb", "ident"], writes=["pT"])
                    P.op("act", lambda e, g=g: e.copy(out=Zt[:, g, :, :], in_=pT[:, 0:32, :].rearrange("p (a b) c -> p a (b c)", b=4)), reads=["pT"], writes=["Zt", "ZTb_use"])
                for k in range(6):
                    P.seq("pool", [lambda e, k=k: e.tensor_tensor(out=Rk[:, :, k, :], in0=identf[:, :].unsqueeze(1).to_broadcast([128, G8, 128]),
                                                                  in1=sqr[:, :, 6 + k:7 + k].to_broadcast([128, G8, 128]), op=MUL),
                                   lambda e, k=k: e.tensor_tensor(out=rkt8[:, :, :], in0=SW[:, :].unsqueeze(1).to_broadcast([128, G8, 128]),
                                                                  in1=sqi[:, :, 6 + k:7 + k].to_broadcast([128, G8, 128]), op=MUL),
                                   lambda e: e.tensor_scalar(out=rkt8[:, :, :], in0=rkt8[:, :, :], scalar1=sgn[:, 0:1], scalar2=None, op0=MUL),
                                   lambda e, k=k: e.tensor_tensor(out=Rk[:, :, k, :], in0=Rk[:, :, k, :], in1=rkt8[:, :, :], op=SUB)],
                          reads=["prepT", "identf", "SW", "sgn", "Rk_use"], writes=[f"Rk{g}" for g in range(G8)] + ["rkt"])
                for g in range(G8):
                    P.dma("sp", lambda e, un=un, g=g: e.dma_start(out=ucj[:, g, :, :],
                                                                   in_=u_scr.ap().rearrange("(c j) n -> c j n", j=64)[:, :, un * 128 + g * 16:un * 128 + (g + 1) * 16]),
                          "ucj", reads=[f"u_scr{t}" for t in range(NT)] + ["ucj_use"], writes=[f"ucj{g}"])
                P.bulk_done("ucj")
                for g in range(G8):
                    def f_us(e, g=g):
                        for kc in range(8):
                            i = e.transpose(out=pT[:, 32 + kc, :], in_=ucj[:, g, kc * 8:(kc + 1) * 8, :], identity=ident[0:32, 0:32])
                        return i
                    P.op("pe", f_us, reads=[f"ucj{g}", "ident"], writes=["pT2"])
                    P.op("act", lambda e, g=g: e.copy(out=ustack[:, g, :, :], in_=pT[:, 32:40, :]), reads=["pT2"], writes=[f"ustack{g}"])
                    def f_E(e, g=g):
                        for kc in range(8):
                            i = e.matmul(out=pE[:, g, :], lhsT=Zt[:, g, kc, :], rhs=ustack[:, g, kc, :], start=(kc == 0), stop=(kc == 7))
                        return i
                    P.op("pe", f_E, reads=["Zt", f"ustack{g}"], writes=["pE"])
                P.op("act", lambda e: e.copy(out=E32[:, :, :], in_=pE[:, :, :]), reads=["pE"], writes=["E32"])

                def doubling(tag):
                    for k in range(6):
                        n = 33 - (1 << k)
                        def f_d(e, k=k, n=n):
                            for g in range(G8):
                                i = e.matmul(out=pD[:, g, 0:n], lhsT=Rk[:, g, k, :], rhs=Xs[:, g, 0:n], start=True, stop=True)
                            return i
                        P.op("pe", f_d, reads=["Xs"] + [f"Rk{g}" for g in range(G8)], writes=["pD"])
                        P.op("dve", lambda e, k=k, n=n: e.tensor_tensor(out=Xs[:, :, 1 << k:33], in0=Xs[:, :, 1 << k:33], in1=pD[:, :, 0:n], op=ADD),
                             reads=["pD", "Xs"], writes=["Xs"])
                P.seq("dve", [lambda e: e.memset(Xs[:, :, 0:1], 0.0), lambda e: e.tensor_copy(out=Xs[:, :, 1:33], in_=E32[:, :, :])], reads=["E32"], writes=["Xs"])
                doubling("loc")
                P.op("dve", lambda e: e.tensor_copy(out=send[:, :], in_=Xs[:, :, 32]), reads=["Xs"], writes=["send"])
                P.dma("sp", lambda e, un=un: e.dma_start(out=ccs_in[un][:, :], in_=send[:, :]), f"ccsin{un}", reads=["send"], writes=[f"ccs_in{un}"])
                P.dma("pool", lambda e, un=un: e.collective_compute("AllGather", ALU.bypass, replica_groups=GROUPS4,
                                                                     ins=[ccs_in[un].ap().opt()], outs=[ccs_out[un].ap().opt()]),
                      f"ccs{un}", reads=[f"ccs_in{un}"], writes=[f"ccs_out{un}"], inc=1)
                P.dma("sp", lambda e, un=un: e.dma_start(out=sg_t[:, :, :], in_=ccs_out[un].ap().rearrange("(r p) c -> p r c", p=128)),
                      f"ccsld{un}", reads=[f"ccs_out{un}"], writes=["sg_t"])
                for g in range(G8):
                    ga = un * 8 + g
                    bh = lambda a, n: a.unsqueeze(1).to_broadcast([128, n, 16])
                    bq8 = lambda a: a.unsqueeze(2).to_broadcast([128, 8, 16])
                    P.seq("dve", [TT(tA[:, 0:8, :], bq8(PWbr[:, g, :]), bh(BB1[:, g, :], 8), MUL),
                                  TT(tB[:, 0:8, :], bq8(PWbi[:, g, :]), bh(BB2[:, g, :], 8), MUL),
                                  TT(XTb[:, :, :], tA[:, 0:8, :], tB[:, 0:8, :], ADD)],
                          reads=["prepT", "tA", "tB", "XTb_use"], writes=["tA", "tB", "XTb"])
                    bq65 = lambda a: a.unsqueeze(2).to_broadcast([128, 65, 16])
                    P.seq("dve", [TT(tA[:, :, :], bq65(PRs[:, g, :]), bh(CY1t[:, ga, :], 65), MUL),
                                  TT(tB[:, :, :], bq65(nPI[:, g, :]), bh(CY2t[:, ga, :], 65), MUL),
                                  TT(Yt[:, g, :, :], tA[:, :, :], tB[:, :, :], ADD)],
                          reads=["prepT", "tA", "tB", "CY1t", "CY2t", "tab_use"], writes=["tA", "tB", f"Yt{g}"])
                    def f_g(e, g=g):
                        e.matmul(out=pY[:, 0:512], lhsT=XTb[:, :, :], rhs=Yt[:, g, 0:32, :], start=True, stop=True)
                        return e.matmul(out=pY[:, 512:1024], lhsT=XTb[:, :, :], rhs=Yt[:, g, 32:64, :], start=True, stop=True)
                    P.op("pe", f_g, reads=["XTb", f"Yt{g}"], writes=["pY"])
                    P.op("dve", lambda e, g=g: e.tensor_tensor(out=Gtab[:, g, :], in0=pY[:, :], in1=maskG[:, :, :], op=MUL),
                         reads=["pY", "maskG", "tab_use"], writes=[f"Gtab{g}", "XTb_use"])

                P.op("dve", lambda e: e.memset(hs[:, :], 0.0), writes=["hs"])
                for pp in range(3):
                    def f_hr(e):
                        for g in range(G8):
                            i = e.matmul(out=pD[:, g, 0:1], lhsT=Rk[:, g, 5, :], rhs=hs[:, g:g + 1], start=True, stop=True)
                        return i
                    P.op("pe", f_hr, reads=["hs"] + [f"Rk{g}" for g in range(G8)], writes=["pD"])
                    P.seq("dve", [lambda e, pp=pp: e.tensor_tensor(out=hs2[:, :], in0=pD[:, :, 0], in1=sg_t[:, pp, :], op=ADD),
                                  lambda e: e.tensor_tensor(out=hs2[:, :], in0=hs2[:, :], in1=hs[:, :], op=SUB),
                                  lambda e, pp=pp: e.scalar_tensor_tensor(out=hs[:, :], in0=hs2[:, :], scalar=pm[:, pp:pp + 1], in1=hs[:, :], op0=MUL, op1=ADD)],
                          reads=["pD", "sg_t", "hs", "pm"], writes=["hs", "hs2"])
                P.seq("dve", [lambda e: e.tensor_copy(out=Xs[:, :, 0], in_=hs[:, :]), lambda e: e.tensor_copy(out=Xs[:, :, 1:33], in_=E32[:, :, :])],
                      reads=["hs", "E32"], writes=["Xs"])
                doubling("glob")
                P.op("act", lambda e: e.copy(out=Sprevb[:, :, :], in_=Xs[:, :, 0:32]), reads=["Xs"], writes=["Sprevb"])

                for g in range(G8):
                    def f_y(e, g=g):
                        Yf = Yt[:, g, :, :]
                        e.matmul(out=pY[0:32, 0:512], lhsT=Sprevb[:, g, :], rhs=Yt[:, g, 1:33, :], start=True, stop=False)
                        e.matmul(out=pY[0:32, 512:1024], lhsT=Sprevb[:, g, :], rhs=Yt[:, g, 33:65, :], start=True, stop=False)
                        i = None
                        for kc in range(8):
                            lo = 128 * kc
                            if lo < 512:
                                i = e.matmul(out=pY[0:32, lo:512], lhsT=ustack[:, g, kc, :], rhs=Gtab[:, g, 0:512 - lo], start=False, stop=(kc == 3))
                            b0 = max(512, lo)
                            i = e.matmul(out=pY[0:32, b0:1024], lhsT=ustack[:, g, kc, :], rhs=Gtab[:, g, b0 - lo:1024 - lo], start=False, stop=(kc == 7))
                        return i
                    P.op("pe", f_y, reads=["Sprevb", f"Yt{g}", f"Gtab{g}", f"ustack{g}"], writes=["pY"])
                    ga = un * 8 + g
                    P.op("pool", lambda e, g=g, ga=ga: e.tensor_tensor(out=du[:, :, :], in0=ucj[:, g, :, :],
                                                                       in1=d_bc[:, ga * 16:(ga + 1) * 16].unsqueeze(1).to_broadcast([32, 64, 16]), op=MUL),
                         reads=[f"ucj{g}", "d_bc"], writes=["du"])
                    yv_ = yv[:, :]
                    P.seq("dve", [lambda e: e.tensor_tensor(out=yv[:, :], in0=pY[0:32, :], in1=du[:, :, :], op=ADD),
                                  TT(y2[:, :], yv_, yv_, MUL), TS(y2[:, :], y2[:, :], 0.044715, 1.0, MUL, ADD), TT(y2[:, :], y2[:, :], yv_, MUL)],
                          reads=["pY", "du", "ysg"], writes=["yv", "y2"])
                    P.op("act", ACT(ysg[:, :], y2[:, :], AF.Sigmoid, scale=1.5957691216057308), reads=["y2"], writes=["ysg"])
                    P.op("dve", lambda e, g=g: e.tensor_tensor(out=ygel[:, :, g * 16:(g + 1) * 16], in0=yv[:, :], in1=ysg[:, :], op=MUL),
                         reads=["yv", "ysg", "ygel_use"], writes=[f"ygel{g}"])
                def f_gt(e):
                    for j in range(64):
                        i = e.transpose(out=pT[:, j, :], in_=ygel[:, j, :], identity=ident[0:32, 0:32])
                    return i
                P.op("pe", f_gt, reads=[f"ygel{g}" for g in range(G8)] + ["ident"], writes=["pT", "pT2"])
                P.op("act", lambda e: e.copy(out=ygT[:, :, :], in_=pT[:, :, :]), reads=["pT", "pT2"], writes=["ygT", "ygel_use"])
                ms_v = mslab[:, :].rearrange("p (c j) -> p j c", j=64)
                for nb in range(4):
                    P.op("pe", lambda e, nb=nb, un=un: e.matmul(out=ps_z[0][:, :], lhsT=WAt[:, un, :], rhs=ygT[:, nb * 16:(nb + 1) * 16, :], start=True, stop=True),
                         reads=["WAt", "ygT"], writes=["ps_z0"])
                    P.op("pe", lambda e, nb=nb, un=un: e.matmul(out=ps_z[1][:, :], lhsT=WBt[:, un, :], rhs=ygT[:, nb * 16:(nb + 1) * 16, :], start=True, stop=True),
                         reads=["WBt", "ygT"], writes=["ps_z1"])
                    P.op("act", lambda e, un=un: e.activation(out=sbt[:, :], in_=ps_z[1][:, :], func=AF.Sigmoid, bias=gbb[:, un:un + 1]),
                         reads=["ps_z1", "gbb"], writes=["sbt"])
                    P.op("dve", lambda e, nb=nb, un=un: e.scalar_tensor_tensor(out=ms_v[:, nb * 16:(nb + 1) * 16, :], in0=ps_z[0][:, :].rearrange("p (j c) -> p j c", c=32), scalar=gba[:, un:un + 1],
                                                                               in1=sbt[:, :].rearrange("p (j c) -> p j c", c=32), op0=ADD, op1=MUL),
                         reads=["ps_z0", "sbt", "gba"], writes=["mslab"])
                P.dma("sp", lambda e, un=un: e.dma_start(out=mergedT_scr[512 + un * 128:512 + (un + 1) * 128, :], in_=mslab[:, :]), "mscr",
                      reads=["mslab"], writes=[f"mscr{4 + un}"])
            esC.close()
            P.barrier()
        else:
            esH.close()

        if stage >= 4:
            AX = mybir.AxisListType
            MUL, ADD, SUB = ALU.mult, ALU.add, ALU.subtract
            esDE = ExitStack()
            esDE.__enter__()
            sbp = lambda name, shape, dt: esDE.enter_context(nc.sbuf_tensor(name, shape, dt))
            h2T = sbp("h2T", [128, 8, NTOK], BF16)
            cw = sbp("cw", [128, NT, 32], F32)
            xt2 = [sbp(f"xt2_{i}", [128, D], F32) for i in range(2)]
            ssq2 = sbp("ssq2", [128, 2 * NT], F32)
            rstd2 = sbp("rstd2", [128, 2 * NT], F32)
            junk2 = sbp("junk2", [128, D], BF16)
            esD = ExitStack()
            esD.__enter__()
            sb = lambda name, shape, dt: esD.enter_context(nc.sbuf_tensor(name, shape, dt))
            ps = lambda name, shape, dt: esD.enter_context(nc.psum_tensor(name, shape, dt))
            mT = sb("mT", [128, 8, NTOK], BF16)
            Wout = sb("Wout", [128, 8, D], BF16)
            Wr = sb("Wr", [128, 8, 36], BF16)
            rb_bc = sb("rb_bc", [128, 36], F32)
            xm = [sb(f"xm{i}", [128, D], F32) for i in range(2)]
            hb2 = sb("hb2", [128, D], BF16)
            lg = sb("lg", [128, NT, 36], F32)
            r_a = sb("r_a", [128, NT, 4], F32)
            r_b = sb("r_b", [128, NT, 4], F32)
            r_gmax = sb("r_gmax", [128, NT], F32)
            r_gw = sb("r_gw", [128, NT], F32)
            r_el = sb("r_el", [128, NT, 32], F32)
            r_t = sb("r_t", [128, NT, 32], F32)
            r_oh1 = sb("r_oh1", [128, NT, 32], F32)
            r_oh2 = sb("r_oh2", [128, NT, 32], F32)
            r_m1 = sb("r_m1", [128, NT], F32)
            r_m2 = sb("r_m2", [128, NT], F32)
            r_w1 = sb("r_w1", [128, NT], F32)
            r_w2 = sb("r_w2", [128, NT], F32)
            po = [ps(f"po{i}", [128, 512], F32) for i in range(2)]
            tp2 = ps("tp2", [128, 8, 128], BF16)
            pr = ps("pr", [128, 36], F32)

            P.dma("sp", lambda e: e.dma_start(out=mT[:, :, :], in_=mergedT_scr.ap().rearrange("(kc p) t -> p kc t", p=128)), "mT",
                  reads=[f"mscr{i}" for i in range(8)], writes=["mT"])
            P.dma("pool", lambda e: e.dma_start(out=Wout[:, :, :], in_=w_out.ap().rearrange("(kc p) c -> p kc c", p=128)), "setupD", writes=["Wout"])
            P.dma("pool", lambda e: e.dma_start(out=Wr[:, :, :], in_=w_rt.ap().rearrange("(kc p) c -> p kc c", p=128)), "setupD", writes=["Wr"])
            P.dma("sp", lambda e: e.dma_start(out=rb_bc[:, :], in_=b_rt[0:1, :].partition_broadcast(128)), "setupD", writes=["rb_bc"])
            P.dma("sp", lambda e: e.dma_start(out=g_bc[:, :], in_=g_ffn[0:1, :].partition_broadcast(128)), "setupD", writes=["g_bc"])
            P.bulk_done("setupD")

            for t in range(NT):
                s_ = t % 2
                ts_ = slice(t * 128, (t + 1) * 128)
                P.dma("sp", lambda e, t=t, s_=s_: e.dma_start(out=xt2[s_][:, :], in_=x[t * 128:(t + 1) * 128, :]), f"xt2_{s_}", writes=[f"xt2_{s_}"])
                for hf in range(2):
                    def f_o(e, ts_=ts_, hf=hf):
                        for k in range(8):
                            i = e.matmul(out=po[hf][:, :], lhsT=mT[:, k, ts_], rhs=Wout[:, k, hf * 512:(hf + 1) * 512], start=(k == 0), stop=(k == 7))
                        return i
                    P.op("pe", f_o, reads=["mT", "Wout"], writes=[f"po{hf}"])
                    P.op("dve", lambda e, hf=hf, s_=s_: e.tensor_tensor(out=xm[s_][:, hf * 512:(hf + 1) * 512], in0=po[hf][:, :], in1=xt2[s_][:, hf * 512:(hf + 1) * 512], op=ADD),
                         reads=[f"po{hf}", f"xt2_{s_}"], writes=[f"xm{s_}_{hf}"])
                xk = [f"xm{s_}_0", f"xm{s_}_1"]
                P.dma("sp", lambda e, t=t, s_=s_: e.dma_start(out=xmid_scr[t * 128:(t + 1) * 128, :], in_=xm[s_][:, :]), f"xmid{s_}", reads=xk, writes=[f"xmid{t}"])
                P.seq("act", [lambda e, s_=s_, t=t: e.activation(out=junk2[:, :], in_=xm[s_][:, :], func=AF.Square, accum_out=ssq2[:, t:t + 1]),
                              lambda e, t=t: e.activation(out=ssq2[:, t:t + 1], in_=ssq2[:, t:t + 1], func=AF.Sqrt, scale=1.0 / D, bias=EPS)],
                      reads=xk, writes=["junk2", f"ssq2_{t}"])
                P.op("dve", lambda e, t=t: e.reciprocal(out=rstd2[:, t:t + 1], in_=ssq2[:, t:t + 1]), reads=[f"ssq2_{t}"], writes=[f"rstd2_{t}"])
                P.op("dve", lambda e, s_=s_, t=t: e.scalar_tensor_tensor(out=hb2[:, :], in0=xm[s_][:, :], scalar=rstd2[:, t:t + 1], in1=g_bc[:, :], op0=MUL, op1=MUL),
                     reads=xk + [f"rstd2_{t}", "g_bc"], writes=["hb2"])
                def f_tp2(e):
                    for k in range(8):
                        i = e.transpose(out=tp2[:, k, :], in_=hb2[:, k * 128:(k + 1) * 128], identity=ident[:, :])
                    return i
                P.op("pe", f_tp2, reads=["hb2", "ident"], writes=["tp2"])
                P.op("act", lambda e, ts_=ts_: e.copy(out=h2T[:, :, ts_], in_=tp2[:, :, :]), reads=["tp2"], writes=[f"h2T_{t // 4}"])
                def f_r(e, ts_=ts_):
                    for k in range(8):
                        i = e.matmul(out=pr[:, :], lhsT=h2T[:, k, ts_], rhs=Wr[:, k, :], start=(k == 0), stop=(k == 7))
                    return i
                P.op("pe", f_r, reads=[f"h2T_{t // 4}", "Wr"], writes=["pr"])
                P.op("dve", lambda e, t=t: e.tensor_tensor(out=lg[:, t, :], in0=pr[:, :], in1=rb_bc[:, :], op=ADD), reads=["pr", "rb_bc"], writes=["lg"])

            BIG = 1.0e9
            bc4 = lambda a: a.unsqueeze(2).to_broadcast([128, NT, 4])
            bc32 = lambda a: a.unsqueeze(2).to_broadcast([128, NT, 32])
            gl = lg[:, :, 0:4]
            el = lg[:, :, 4:36]
            P.seq("dve", [
                lambda e: e.tensor_reduce(out=r_gmax[:, :], in_=gl, axis=AX.X, op=ALU.max),
                lambda e: e.tensor_tensor(out=r_a[:, :, :], in0=gl, in1=bc4(r_gmax[:, :]), op=SUB)], reads=["lg"], writes=["r_a", "r_gmax"])
            P.op("act", lambda e: e.activation(out=r_b[:, :, :], in_=r_a[:, :, :], func=AF.Exp), reads=["r_a"], writes=["r_b"])
            P.seq("dve", [
                lambda e: e.tensor_reduce(out=r_gw[:, :], in_=r_b[:, :, :], axis=AX.X, op=ADD),
                lambda e: e.reciprocal(out=r_gw[:, :], in_=r_gw[:, :]),
                lambda e: e.tensor_tensor(out=r_a[:, :, :], in0=gl, in1=bc4(r_gmax[:, :]), op=ALU.is_equal),
                lambda e: e.tensor_scalar(out=r_a[:, :, :], in0=r_a[:, :, :], scalar1=-1.0, scalar2=BIG, op0=ADD, op1=MUL),
                lambda e: e.tensor_tensor(out=r_el[:, :, :].rearrange("p t (g k) -> p t g k", k=8), in0=el.rearrange("p t (g k) -> p t g k", k=8),
                                          in1=r_a[:, :, :].unsqueeze(3).to_broadcast([128, NT, 4, 8]), op=ADD),
                lambda e: e.tensor_reduce(out=r_m1[:, :], in_=r_el[:, :, :], axis=AX.X, op=ALU.max),
                lambda e: e.tensor_tensor(out=r_oh1[:, :, :], in0=r_el[:, :, :], in1=bc32(r_m1[:, :]), op=ALU.is_equal),
                lambda e: e.scalar_tensor_tensor(out=r_t[:, :, :], in0=r_oh1[:, :, :], scalar=-BIG, in1=r_el[:, :, :], op0=MUL, op1=ADD),
                lambda e: e.tensor_reduce(out=r_m2[:, :], in_=r_t[:, :, :], axis=AX.X, op=ALU.max),
                lambda e: e.tensor_tensor(out=r_oh2[:, :, :], in0=r_t[:, :, :], in1=bc32(r_m2[:, :]), op=ALU.is_equal),
                lambda e: e.tensor_tensor(out=r_w1[:, :], in0=r_m1[:, :], in1=r_m2[:, :], op=SUB)],
                reads=["lg", "r_b", "r_a"], writes=["r_a", "router1"])
            P.op("act", lambda e: e.activation(out=r_w1[:, :], in_=r_w1[:, :], func=AF.Sigmoid), reads=["router1"], writes=["r_w1"])
            P.seq("dve", [
                lambda e: e.tensor_scalar(out=r_w2[:, :], in0=r_w1[:, :], scalar1=-1.0, scalar2=1.0, op0=MUL, op1=ADD),
                lambda e: e.tensor_tensor(out=r_w1[:, :], in0=r_w1[:, :], in1=r_gw[:, :], op=MUL),
                lambda e: e.tensor_tensor(out=r_w2[:, :], in0=r_w2[:, :], in1=r_gw[:, :], op=MUL),
                lambda e: e.tensor_tensor(out=r_oh1[:, :, :], in0=r_oh1[:, :, :], in1=bc32(r_w1[:, :]), op=MUL),
                lambda e: e.tensor_tensor(out=r_oh2[:, :, :], in0=r_oh2[:, :, :], in1=bc32(r_w2[:, :]), op=MUL),
                lambda e: e.tensor_tensor(out=cw[:, :, :], in0=r_oh1[:, :, :], in1=r_oh2[:, :, :], op=ADD)],
                reads=["router1", "r_w1"], writes=["cw", "router1"])
            if "cw" in dbg:
                tcw = dbgt("cw", [128, NT, 32])
                P.dma("sp", lambda e: e.dma_start(out=tcw[:, :, :], in_=cw[:, :, :]), "out", reads=["cw"])
            esD.close()
            P.barrier()

            NE = 32 if stage >= 5 else 0
            esE = ExitStack()
            esE.__enter__()
            sb = lambda name, shape, dt: esE.enter_context(nc.sbuf_tensor(name, shape, dt))
            ps = lambda name, shape, dt: esE.enter_context(nc.psum_tensor(name, shape, dt))
            yacc = sb("yacc", [128, NT, D], F32)
            Wg = [sb(f"Wg{i}", [128, 8, 512], BF16) for i in range(2)]
            Wu = [sb(f"Wu{i}", [128, 8, 512], BF16) for i in range(2)]
            Wd = [sb(f"Wd{i}", [128, 4, D], BF16) for i in range(2)]
            actT = sb("actT", [128, 4, NTOK], BF16)
            sgt = [sb(f"sgt{i}", [128, 512], F32) for i in range(2)]
            pg = [ps(f"pg{i}", [128, 512], F32) for i in range(2)]
            pu2 = [ps(f"pu2_{i}", [128, 512], F32) for i in range(2)]
            pd = [ps(f"pd{i}", [128, 512], F32) for i in range(2)]
            P.op("pool", lambda e: e.memset(yacc[:, :, :], 0.0), writes=[f"yacc{t}_{hf}" for t in range(NT) for hf in range(2)])
            h2T_all = [f"h2T_{i}" for i in range(4)]
            for ex in range(NE):
                s_ = ex % 2
                P.dma("pool", lambda e, ex=ex, s_=s_: e.dma_start(out=Wg[s_][:, :, :], in_=w_gate[ex].rearrange("(kc p) c -> p kc c", p=128)), f"Wg{s_}", writes=[f"Wg{s_}"])
                P.dma("pool", lambda e, ex=ex, s_=s_: e.dma_start(out=Wu[s_][:, :, :], in_=w_up[ex].rearrange("(kc p) c -> p kc c", p=128)), f"Wu{s_}", writes=[f"Wu{s_}"])
                P.dma("pool", lambda e, ex=ex, s_=s_: e.dma_start(out=Wd[s_][:, :, :], in_=w_down[ex].rearrange("(kc p) c -> p kc c", p=128)), f"Wd{s_}", writes=[f"Wd{s_}"])
                it = 0
                for tb in range(4):
                    tbs = slice(tb * 512, (tb + 1) * 512)
                    for m in range(4):
                        b_ = it % 2
                        it += 1
                        def f_gu(e, s_=s_, m=m, tbs=tbs, b_=b_):
                            for k in range(8):
                                e.matmul(out=pg[b_][:, :], lhsT=Wg[s_][:, k, m * 128:(m + 1) * 128], rhs=h2T[:, k, tbs], start=(k == 0), stop=(k == 7))
                            for k in range(8):
                                i = e.matmul(out=pu2[b_][:, :], lhsT=Wu[s_][:, k, m * 128:(m + 1) * 128], rhs=h2T[:, k, tbs], start=(k == 0), stop=(k == 7))
                            return i
                        P.op("pe", f_gu, reads=[f"Wg{s_}", f"Wu{s_}", f"h2T_{tb}"], writes=[f"pg{b_}", f"pu2_{b_}"])
                        P.op("act", lambda e, b_=b_: e.activation(out=sgt[b_][:, :], in_=pg[b_][:, :], func=AF.Silu), reads=[f"pg{b_}"], writes=[f"sgt{b_}"])
                        P.op("dve", lambda e, b_=b_, m=m, tbs=tbs: e.tensor_tensor(out=actT[:, m, tbs], in0=sgt[b_][:, :], in1=pu2[b_][:, :], op=MUL),
                             reads=[f"sgt{b_}", f"pu2_{b_}"], writes=[f"actT_{tb}"])
                it = 0
                for t in range(NT):
                    ts_ = slice(t * 128, (t + 1) * 128)
                    for hf in range(2):
                        b_ = it % 2
                        it += 1
                        def f_d(e, s_=s_, ts_=ts_, hf=hf, b_=b_):
                            for m in range(4):
                                i = e.matmul(out=pd[b_][:, :], lhsT=actT[:, m, ts_], rhs=Wd[s_][:, m, hf * 512:(hf + 1) * 512], start=(m == 0), stop=(m == 3))
                            return i
                        P.op("pe", f_d, reads=[f"Wd{s_}", f"actT_{t // 4}"], writes=[f"pd{b_}"])
                        P.op("dve", lambda e, t=t, hf=hf, b_=b_, ex=ex: e.scalar_tensor_tensor(out=yacc[:, t, hf * 512:(hf + 1) * 512], in0=pd[b_][:, :], scalar=cw[:, t, ex:ex + 1],
                                                                                                in1=yacc[:, t, hf * 512:(hf + 1) * 512], op0=MUL, op1=ADD),
                             reads=[f"pd{b_}", "cw", f"yacc{t}_{hf}"], writes=[f"yacc{t}_{hf}"])

            P.dma("sp", lambda e: e.dma_start(out=g_bc[:, :], in_=g_fin[0:1, :].partition_broadcast(128)), "setupF", writes=["g_bc"])
            for t in range(NT):
                s_ = t % 2
                P.dma("sp", lambda e, t=t, s_=s_: e.dma_start(out=xt2[s_][:, :], in_=xmid_scr[t * 128:(t + 1) * 128, :]), f"xt2_{s_}", reads=[f"xmid{t}"], writes=[f"xt2_{s_}"])
                P.op("dve", lambda e, t=t, s_=s_: e.tensor_tensor(out=xt2[s_][:, :], in0=xt2[s_][:, :], in1=yacc[:, t, :], op=ADD),
                     reads=[f"xt2_{s_}", f"yacc{t}_0", f"yacc{t}_1"], writes=[f"xt2_{s_}"])
                P.seq("act", [lambda e, s_=s_, t=t: e.activation(out=junk2[:, :], in_=xt2[s_][:, :], func=AF.Square, accum_out=ssq2[:, NT + t:NT + t + 1]),
                              lambda e, t=t: e.activation(out=ssq2[:, NT + t:NT + t + 1], in_=ssq2[:, NT + t:NT + t + 1], func=AF.Sqrt, scale=1.0 / D, bias=EPS)],
                      reads=[f"xt2_{s_}"], writes=["junk2", f"ssq2_{NT + t}"])
                P.op("dve", lambda e, t=t: e.reciprocal(out=rstd2[:, NT + t:NT + t + 1], in_=ssq2[:, NT + t:NT + t + 1]), reads=[f"ssq2_{NT + t}"], writes=[f"rstd2_{NT + t}"])
                P.op("dve", lambda e, s_=s_, t=t: e.scalar_tensor_tensor(out=xt2[s_][:, :], in0=xt2[s_][:, :], scalar=rstd2[:, NT + t:NT + t + 1], in1=g_bc[:, :], op0=MUL, op1=MUL),
                     reads=[f"xt2_{s_}", f"rstd2_{NT + t}", "g_bc"], writes=[f"xt2_{s_}"])
                P.dma("sp", lambda e, t=t, s_=s_: e.dma_start(out=y[t * 128:(t + 1) * 128, :], in_=xt2[s_][:, :]), "yout", reads=[f"xt2_{s_}"], writes=[f"y{t}"])
            esE.close()
            esDE.close()

        if "merged" in dbg:
            t = dbgt("merged", [1024, NTOK], BF16)
            nrow = 1024 if stage >= 3 else 512
            P.dma("sp", lambda e: e.dma_start(out=t[0:nrow, :], in_=mergedT_scr[0:nrow, :]), "out", reads=[f"mscr{h}" for h in range(nrow // 128)])
        if "hT" in dbg:
            t2 = dbgt("hT", [128, 8, HALO + NTOK], BF16)
            P.dma("sp", lambda e: e.dma_start(out=t2[:, :, :], in_=hT[:, :, :]), "out", reads=hT_all)
        if "qk" in dbg:
            t3 = dbgt("qs", [128, NTOK], BF16)
            t4 = dbgt("ks", [128, NTOK], BF16)
            t5 = dbgt("expb", [128, NTOK], F32)
            t6 = dbgt("e2", [128, NTOK], F32)
            P.dma("sp", lambda e: e.dma_start(out=t3[:, :], in_=qsT[:, :]), "out", reads=["qsT"])
            P.dma("sp", lambda e: e.dma_start(out=t4[:, :], in_=ksT[:, :]), "out", reads=["ksT"])
            P.dma("sp", lambda e: e.dma_start(out=t5[:, :], in_=expb[:, :]), "out", reads=expb_all)
            P.dma("sp", lambda e: e.dma_start(out=t6[:, :], in_=e2bc[:, :]), "out", reads=e2_all)
        if "xmid" in dbg:
            txm = dbgt("xmid", [NTOK, D])
            P.dma("sp", lambda e: e.dma_start(out=txm[:, :], in_=xmid_scr[:, :]), "out", reads=[f"xmid{t}" for t in range(NT)])
        P.final_wait("sp", ["out", "yout"])

        with nc.Block() as block:
            @block.tensor
            def _(e): P.replay("pe", e)
            @block.scalar
            def _(e): P.replay("act", e)
            @block.vector
            def _(e): P.replay("dve", e)
            @block.gpsimd
            def _(e): P.replay("pool", e)
            @block.sync
            def _(e): P.replay("sp", e)
    return nc, dbg_out


def make_in_maps(inputs):
    f = lambda a: np.ascontiguousarray(a, dtype=np.float32)
    x = inputs["x"]
    common = {
        "g_mix": f(inputs["norm_mix_g"].reshape(1, D)),
        "w_in": f(inputs["w_in"].reshape(D, 2568)),
        "conv_wT": f(inputs["conv_w"].reshape(4, 8, 128).transpose(2, 1, 0)),
        "conv_b": f(inputs["conv_b"].reshape(8, 128).T),
        "i_bias": f(inputs["i_bias"].reshape(1, 4)),
        "f_bias": f(inputs["f_bias"].reshape(1, 4)),
        "g_ml": f(inputs["mlstm_norm_g"].reshape(1, 512)),
    }
    lre = inputs["s5_lambda_re"].reshape(32, 64).T
    lim = inputs["s5_lambda_im"].reshape(32, 64).T
    bre = inputs["s5_b_re"].reshape(32, 64, 16).transpose(1, 0, 2)
    bim = inputs["s5_b_im"].reshape(32, 64, 16).transpose(1, 0, 2)
    cre = inputs["s5_c_re"].reshape(32, 16, 64).transpose(2, 0, 1)
    cim = inputs["s5_c_im"].reshape(32, 16, 64).transpose(2, 0, 1)
    glw = inputs["s5_glu_w"].reshape(4, 8, 16, 32)
    WA = np.zeros((4, 128, 128), np.float32)
    WB = np.zeros((4, 128, 128), np.float32)
    for u in range(4):
        for g in range(8):
            WA[u, g * 16:(g + 1) * 16, g * 16:(g + 1) * 16] = glw[u, g, :, 0:16]
            WB[u, g * 16:(g + 1) * 16, g * 16:(g + 1) * 16] = glw[u, g, :, 16:32]
    glb = inputs["s5_glu_b"].reshape(4, 8, 32)
    common.update({
        "lam2_re": f(np.concatenate([lre, lre], 0)), "lam2_im": f(np.concatenate([lim, lim], 0)),
        "logdt": f(inputs["s5_log_dt"].reshape(1, 32)),
        "X1d": f(np.concatenate([bre, bim], 0)), "X2d": f(np.concatenate([bim, bre], 0)),
        "CY1d": f(np.concatenate([cre, cim], 0)), "CY2d": f(np.concatenate([cim, cre], 0)),
        "s5d": f(inputs["s5_d"].reshape(1, 512)),
        "WAd": WA, "WBd": WB,
        "w_out": f(inputs["w_out"].reshape(D, D)), "g_ffn": f(inputs["norm_ffn_g"].reshape(1, D)),
        "w_rt": f(np.concatenate([inputs["router_group_w"].reshape(D, 4), inputs["router_expert_w"].reshape(D, 32)], 1)),
        "b_rt": f(np.concatenate([inputs["router_group_b"].reshape(1, 4), inputs["router_expert_b"].reshape(1, 32)], 1)),
        "w_gate": f(inputs["expert_w_gate"].reshape(32, D, 512)), "w_up": f(inputs["expert_w_up"].reshape(32, D, 512)),
        "w_down": f(inputs["expert_w_down"].reshape(32, 512, D)), "g_fin": f(inputs["norm_final_g"].reshape(1, D)),
        "gbad": f(glb[:, :, 0:16].reshape(4, 128).T), "gbbd": f(glb[:, :, 16:32].reshape(4, 128).T),
    })
    maps = []
    for c in range(NCORES):
        b, p = c // 4, c % 4
        m = dict(common)
        m["x"] = f(x[b, p * NTOK:(p + 1) * NTOK])
        if p == 0:
            m["xh"] = np.zeros((HALO, D), np.float32)
        else:
            m["xh"] = f(x[b, p * NTOK - HALO:p * NTOK])
        pmk = np.zeros((128, 4), np.float32)
        pmk[:, :p] = 1.0
        m["pmask"] = pmk
        maps.append(m)
    return maps


_CACHE = {}


def kernel(**inputs):
    if "nc" not in _CACHE:
        _CACHE["nc"] = build()[0]
    nc = _CACHE["nc"]
    in_maps = make_in_maps(inputs)
    res = run_bass_kernel_spmd(nc, in_maps, core_ids=list(range(NCORES)))
    out = np.empty((2, 4 * NTOK, D), np.float32)
    for c in range(NCORES):
        b, p = c // 4, c % 4
        out[b, p * NTOK:(p + 1) * NTOK] = np.asarray(res.results[c]["y"], dtype=np.float32)
    return out
```

```python
import numpy as np
from contextlib import ExitStack
import concourse.bass as bass
import concourse.mybir as mybir
from concourse.bass_utils import run_bass_kernel_spmd

F32 = mybir.dt.float32
BF16 = mybir.dt.bfloat16
AF = mybir.ActivationFunctionType
ALU = mybir.AluOpType

NTOK = 2048
NT = 16
NCH = 32
HALO = 32
D = 1024
EPS = 1e-6
NCORES = 8
GROUPS4 = [[0, 1, 2, 3], [4, 5, 6, 7]]


class Prog:
    def __init__(self, nc, es):
        self.nc = nc
        self.es = es
        self.queues = {e: [] for e in ("pe", "act", "dve", "pool", "sp")}
        self.esem = {}
        self.ecnt = {}
        for e in ("pe", "act", "dve", "pool"):
            self.esem[e] = es.enter_context(nc.semaphore("sem_" + e))
            self.ecnt[e] = 0
        self.dsem = {}
        self.dcnt = {}
        self.lastw = {}
        self.readers = {}
        self.known = {e: {} for e in self.queues}

    def _deps(self, reads, writes):
        deps = []
        for r in reads:
            if r in self.lastw:
                deps.append(self.lastw[r])
        for w in writes:
            if w in self.lastw:
                deps.append(self.lastw[w])
            deps.extend(self.readers.get(w, ()))
        return deps

    def _prune(self, eng, deps):
        need = {}
        for (s, v) in deps:
            if eng == "pe" and s is self.esem["pe"]:
                continue
            if v > need.get(s, 0):
                need[s] = v
        out = []
        kn = self.known[eng]
        for s, v in need.items():
            if kn.get(s, 0) >= v:
                continue
            kn[s] = v
            out.append((s, v))
        return out

    def _record(self, tok, reads, writes):
        for r in reads:
            self.readers.setdefault(r, []).append(tok)
        for w in writes:
            self.lastw[w] = tok
            self.readers[w] = []

    def op(self, eng, fn, reads=(), writes=()):
        reads = tuple(reads)
        writes = tuple(writes)
        waits = self._prune(eng, self._deps(reads, writes))
        self.ecnt[eng] += 1
        tok = (self.esem[eng], self.ecnt[eng])
        self.queues[eng].append((waits, fn, (self.esem[eng], 1)))
        self._record(tok, reads, writes)

    def seq(self, eng, fns, reads=(), writes=()):
        self._chain = getattr(self, "_chain", 0) + 1
        ck = f"__chain{self._chain}"
        for fn in fns:
            self.op(eng, fn, reads=tuple(reads) + (ck,), writes=tuple(writes) + (ck,))

    def dma(self, q, fn, stream, reads=(), writes=(), inc=16):
        reads = tuple(reads)
        writes = tuple(writes)
        if stream not in self.dsem:
            self.dsem[stream] = self.es.enter_context(self.nc.semaphore("dsem_" + stream))
            self.dcnt[stream] = 0
        waits = self._prune(q, self._deps(reads, writes))
        self.dcnt[stream] += inc
        tok = (self.dsem[stream], self.dcnt[stream])
        self.queues[q].append((waits, fn, (self.dsem[stream], inc)))
        self._record(tok, reads, writes)

    def bulk_done(self, stream):
        s = self.dsem[stream]
        fin = self.dcnt[stream]
        for k, (ss, v) in list(self.lastw.items()):
            if ss is s:
                self.lastw[k] = (s, fin)

    def barrier(self):
        allw = [(self.esem[e], self.ecnt[e]) for e in self.esem if self.ecnt[e] > 0]
        allw += [(self.dsem[s], self.dcnt[s]) for s in self.dsem]
        for e in self.queues:
            w = self._prune(e, allw)
            if w:
                self.queues[e].append((w, None, None))

    def final_wait(self, q, streams):
        waits = [(self.dsem[st], self.dcnt[st]) for st in streams if st in self.dsem]
        self.queues[q].append((waits, None, None))

    def replay(self, eng, h):
        for waits, fn, inc in self.queues[eng]:
            for (s, v) in waits:
                h.wait_ge(s, v)
            if fn is None:
                continue
            inst = fn(h)
            inst.then_inc(inc[0], inc[1])


def build(stage=99, dbg=()):
    nc = bass.Bass("TRN2", target_bir_lowering=False)
    din = lambda name, shape, dt=F32: nc.dram_tensor(name, shape, dt, kind="ExternalInput")
    x = din("x", [NTOK, D])
    xh = din("xh", [HALO, D])
    g_mix = din("g_mix", [1, D])
    w_in = din("w_in", [D, 2568])
    conv_wT = din("conv_wT", [128, 8, 4])
    conv_b = din("conv_b", [128, 8])
    i_bias = din("i_bias", [1, 4])
    f_bias = din("f_bias", [1, 4])
    g_ml = din("g_ml", [1, 512])
    pmask = din("pmask", [128, 4])
    lam2_re = din("lam2_re", [128, 32])
    lam2_im = din("lam2_im", [128, 32])
    logdt = din("logdt", [1, 32])
    X1d = din("X1d", [128, 32, 16])
    X2d = din("X2d", [128, 32, 16])
    CY1d = din("CY1d", [128, 32, 16])
    CY2d = din("CY2d", [128, 32, 16])
    s5d = din("s5d", [1, 512])
    WAd = din("WAd", [4, 128, 128])
    WBd = din("WBd", [4, 128, 128])
    gbad = din("gbad", [128, 4])
    gbbd = din("gbbd", [128, 4])
    w_out = din("w_out", [D, D])
    g_ffn = din("g_ffn", [1, D])
    w_rt = din("w_rt", [D, 36])
    b_rt = din("b_rt", [1, 36])
    if stage >= 5:
        w_gate = din("w_gate", [32, D, 512])
        w_up = din("w_up", [32, D, 512])
        w_down = din("w_down", [32, 512, D])
    g_fin = din("g_fin", [1, D])
    y = nc.dram_tensor("y", [NTOK, D], F32, kind="ExternalOutput")
    dbg_out = {}
    def dbgt(name, shape, dt=F32):
        dbg_out[name] = nc.dram_tensor("dbg_" + name, shape, dt, kind="ExternalOutput")
        return dbg_out[name]

    mergedT_scr = nc.dram_tensor("mergedT_scr", [1024, NTOK], BF16)
    cc_in = [nc.dram_tensor(f"cc_in{h}", [128, 130], F32) for h in range(4)]
    cc_out = [nc.dram_tensor(f"cc_out{h}", [512, 130], F32) for h in range(4)]
    u_scr = nc.dram_tensor("u_scr", [NTOK, 512], BF16)
    xmid_scr = nc.dram_tensor("xmid_scr", [NTOK, D], F32)
    prep_scr = {n: nc.dram_tensor("prep_" + n, [128, 32, k], F32) for n, k in (("PWrr", 64), ("PWri", 64), ("PWbr", 8), ("PWbi", 8), ("BB1", 16), ("BB2", 16),
                                                                             ("PRs", 65), ("nPI", 65), ("sqr", 12), ("sqi", 12))}
    ccs_in = [nc.dram_tensor(f"ccs_in{h}", [128, 8], F32) for h in range(4)]
    ccs_out = [nc.dram_tensor(f"ccs_out{h}", [512, 8], F32) for h in range(4)]

    w_in_v = w_in.ap().rearrange("(kc p) c -> p kc c", p=128)

    es = ExitStack()
    with es:
        P = Prog(nc, es)
        sb = lambda name, shape, dt: es.enter_context(nc.sbuf_tensor(name, shape, dt))
        ps = lambda name, shape, dt: es.enter_context(nc.psum_tensor(name, shape, dt))

        g_bc = sb("g_bc", [128, D], F32)
        ident = sb("ident", [128, 128], BF16)
        identf = sb("identf", [128, 128], F32)
        mslab = sb("mslab", [128, NTOK], BF16)
        pm = sb("pm", [128, 4], F32)
        esH = ExitStack()
        esH.__enter__()
        hT = esH.enter_context(nc.sbuf_tensor("hT", [128, 8, HALO + NTOK], BF16))
        esB = ExitStack()
        esB.__enter__()
        sb = lambda name, shape, dt: esB.enter_context(nc.sbuf_tensor(name, shape, dt))
        ps = lambda name, shape, dt: esB.enter_context(nc.psum_tensor(name, shape, dt))
        xt = [sb(f"xt{i}", [128, D], F32) for i in range(2)]
        junk = sb("junk", [128, D], BF16)
        hb = sb("hb", [128, D], BF16)
        ssq = sb("ssq", [128, NT + 1], F32)
        rstd = sb("rstd", [128, NT + 1], F32)
        wh = sb("wh", [128, 8, 512], BF16)
        wg = sb("wg", [128, 8, 33], BF16)
        raw = sb("raw", [128, NTOK + 3], F32)
        ctmp = sb("ctmp", [128, NTOK], F32)
        qsT = sb("qsT", [128, NTOK], BF16)
        ksT = sb("ksT", [128, NTOK], BF16)
        cwt = sb("cwt", [128, 8, 4], F32)
        cbt = sb("cbt", [128, 8], F32)
        fbt = sb("fbt", [33, 4], F32)
        nfb = sb("nfb", [33, 4], F32)
        Gt = sb("Gt", [33, NTOK], F32)
        Gt2 = sb("Gt2", [33, NTOK], F32)
        cmask = sb("cmask", [33, NTOK], F32)
        sel0 = sb("sel0", [33, 128], F32)
        sel032 = sb("sel032", [33, 128], F32)
        expb = sb("expb", [128, NTOK], F32)
        e2bc = sb("e2bc", [128, NTOK], F32)
        v_ext = sb("v_ext", [64, NCH, 130], BF16)
        sigo = sb("sigo", [64, NCH, 128], BF16)
        gA = sb("gA", [64, 512], F32)
        kstok2 = sb("kstok2", [64, 2, 128], BF16)
        CTloc = sb("CTloc", [128, NCH + 1, 130], F32)
        CTball = sb("CTball", [128, NCH, 130], BF16)
        CTt = sb("CTt", [128, 130], F32)
        cg = sb("cg", [128, 4, 130], F32)
        hacc = sb("hacc", [128, 130], F32)
        hacc2 = sb("hacc2", [128, 130], F32)
        maskST = sb("maskST", [64, 64], F32)
        STb = sb("STb", [64, 4, 64], BF16)
        dnb = sb("dnb", [64, 4, 4], F32)
        hnb = sb("hnb", [64, 4, 128], F32)
        oab = sb("oab", [64, 4, 128], BF16)
        junkb = sb("junkb", [64, 4, 128], BF16)
        so32t = sb("so32t", [64, 2, 128], F32)
        so32b = [so32t[:, 0, :], so32t[:, 1, :]]

        Bk = [ps(f"bank{k}", [128, 512], F32) for k in range(8)]
        bfv = lambda k: Bk[k][:, :].bitcast(BF16)
        ps_big = [Bk[0], Bk[1]]
        tp = bfv(2).rearrange("p (a b) -> p a b", b=128)
        pvb = [Bk[3][0:64, 0:256], Bk[4][0:64, 0:256]]
        pab = [Bk[0][0:64, 0:64], Bk[1][0:64, 0:64]]
        pcb = [Bk[3][0:64, 0:130], Bk[4][0:64, 0:130]]
        pub = [Bk[5][:, 0:130], Bk[6][:, 0:130]]
        ptk = [bfv(2)[0:64, 0:128], bfv(7)[0:64, 0:128]]
        pto = [bfv(6)[:, 0:64], bfv(7)[:, 0:64]]
        KPV = ["bank3", "bank4"]; KPA = ["bank0", "bank1"]; KPC = ["bank3", "bank4"]; KPU = ["bank5", "bank6"]
        KPTK = ["bank2", "bank7"]; KPTO = ["bank6", "bank7"]

        P.dma("sp", lambda e: e.dma_start(out=g_bc[:, :], in_=g_mix[0:1, :].partition_broadcast(128)), "setup", writes=["g_bc"])
        P.dma("sp", lambda e: e.dma_start(out=cwt[:, :, :], in_=conv_wT[:, :, :]), "setup", writes=["cwt"])
        P.dma("sp", lambda e: e.dma_start(out=cbt[:, :], in_=conv_b[:, :]), "setup", writes=["cbt"])
        P.dma("sp", lambda e: e.dma_start(out=fbt[0:1, :], in_=f_bias[0:1, :]), "setup", writes=["fbt0"])
        P.dma("sp", lambda e: e.dma_start(out=fbt[32:33, :], in_=i_bias[0:1, :]), "setup", writes=["fbt32"])
        P.dma("sp", lambda e: e.dma_start(out=gA[:, :], in_=g_ml[0:1, :].partition_broadcast(64)), "setup", writes=["gA"])
        P.dma("sp", lambda e: e.dma_start(out=pm[:, :], in_=pmask[:, :]), "setup", writes=["pm"])
        P.bulk_done("setup")

        P.seq("pool", [lambda e: e.memset(identf[:, :], 0.0),
                       lambda e: e.affine_select(out=identf[:, :], in_=identf[:, :], pattern=[[-1, 128]], compare_op=ALU.not_equal,
                                                 fill=1.0, base=0, channel_multiplier=1)], writes=["identf"])
        P.op("dve", lambda e: e.tensor_copy(out=ident[:, :], in_=identf[:, :]), reads=["identf"], writes=["ident"])

        P.seq("pool", [lambda e: e.memset(cmask[:, :], 1.0),
                       lambda e: e.memset(cmask[:, 0:NTOK:64], 0.0),
                       lambda e: e.memset(sel0[:, :], 0.0),
                       lambda e: e.memset(sel0[0:1, :], 1.0),
                       lambda e: e.memset(sel032[:, :], 0.0),
                       lambda e: e.memset(sel032[0:1, :], 1.0),
                       lambda e: e.memset(sel032[32:33, :], 1.0),
                       lambda e: e.memset(Gt2[:, :], 0.0),
                       lambda e: e.memset(v_ext[:, :, 128:129], 1.0),
                       lambda e: e.memset(v_ext[:, :, 129:130], 0.0),
                       lambda e: e.memset(maskST[:, :], 1.0),
                       lambda e: e.affine_select(out=maskST[:, :], in_=maskST[:, :], pattern=[[1, 64]], compare_op=ALU.is_ge,
                                                 fill=0.0, base=0, channel_multiplier=-1)],
              writes=["cmask", "sel0", "sel032", "Gt2", "v_ext_c", "maskST"])
        P.op("pool", lambda e: e.tensor_scalar(out=nfb[0:1, :], in0=fbt[0:1, :], scalar1=-1.0, scalar2=None, op0=ALU.mult),
             reads=["fbt0"], writes=["nfb"])

        def norm_tile(src_ap, rows, slot, col0, idx, tag):
            s = slot
            P.dma("sp", lambda e: e.dma_start(out=xt[s][0:rows, :], in_=src_ap), f"xt{s}", writes=[f"xt{s}"])
            P.op("act", lambda e: e.activation(out=junk[0:rows, :], in_=xt[s][0:rows, :], func=AF.Square, accum_out=ssq[0:rows, idx:idx + 1]),
                 reads=[f"xt{s}"], writes=["junk", f"ssq{idx}"])
            P.op("act", lambda e: e.activation(out=ssq[0:rows, idx:idx + 1], in_=ssq[0:rows, idx:idx + 1], func=AF.Sqrt, scale=1.0 / D, bias=EPS),
                 reads=[f"ssq{idx}"], writes=[f"ssq{idx}"])
            P.op("dve", lambda e: e.reciprocal(out=rstd[0:rows, idx:idx + 1], in_=ssq[0:rows, idx:idx + 1]), reads=[f"ssq{idx}"], writes=[f"rstd{idx}"])
            P.op("dve", lambda e: e.scalar_tensor_tensor(out=hb[0:rows, :], in0=xt[s][0:rows, :], scalar=rstd[0:rows, idx:idx + 1], in1=g_bc[0:rows, :],
                                                         op0=ALU.mult, op1=ALU.mult),
                 reads=[f"xt{s}", f"rstd{idx}", "g_bc"], writes=["hb"])
            def f_tp(e):
                for k in range(8):
                    i = e.transpose(out=tp[:, k, 0:rows], in_=hb[0:rows, k * 128:(k + 1) * 128], identity=ident[0:rows, 0:rows])
                return i
            P.op("pe", f_tp, reads=["hb", "ident"], writes=["bank2"])
            P.op("act", lambda e: e.copy(out=hT[:, :, col0:col0 + rows], in_=tp[:, :, 0:rows]), reads=["bank2"], writes=[tag])

        norm_tile(xh[:, :], HALO, 0, 0, NT, "hT_h")
        for t in range(NT):
            norm_tile(x[t * 128:(t + 1) * 128, :], 128, (t + 1) % 2, HALO + t * 128, t, f"hT_{t // 4}")
        hT_all = ["hT_h"] + [f"hT_{i}" for i in range(4)]

        SC = float(128 ** -0.5)
        for hd in range(4 if stage >= 2 else 0):
            for j, c0 in enumerate((hd * 128, 512 + hd * 128, 1024 + hd * 128, 1536 + hd * 128)):
                P.dma("pool", lambda e, j=j, c0=c0: e.dma_start(out=wh[:, :, j * 128:(j + 1) * 128], in_=w_in_v[:, :, c0:c0 + 128]),
                      "wh", writes=[f"wh{j}"])
            P.bulk_done("wh")
            P.op("pool", lambda e: e.memset(wg[:, :, :], 0.0), writes=["wg", "wg_a", "wg_b"])
            def ld_wg(e, dst, col):
                with nc.allow_non_contiguous_dma(reason="gate columns"):
                    return e.dma_start(out=wg[:, :, dst:dst + 1], in_=w_in_v[:, :, col:col + 1])
            P.dma("pool", lambda e, hd=hd: ld_wg(e, 0, 2052 + hd), "wg", reads=["wg"], writes=["wg_a"])
            P.dma("pool", lambda e, hd=hd: ld_wg(e, 32, 2048 + hd), "wg", reads=["wg"], writes=["wg_b"])
            P.bulk_done("wg")

            for tb in range(4):
                pgt = ps_big[tb % 2]
                def f_g(e, tb=tb, pgt=pgt):
                    for k in range(8):
                        i = e.matmul(out=pgt[0:33, :], lhsT=wg[:, k, :], rhs=hT[:, k, HALO + tb * 512:HALO + (tb + 1) * 512], start=(k == 0), stop=(k == 7))
                    return i
                P.op("pe", f_g, reads=["wg", "wg_a", "wg_b", f"hT_{tb}"], writes=[f"bank{tb % 2}"])
                P.op("act", lambda e, tb=tb, pgt=pgt: e.copy(out=Gt[0:33, tb * 512:(tb + 1) * 512], in_=pgt[0:33, :]),
                     reads=[f"bank{tb % 2}"], writes=[f"Gt_{tb}"])
            Gt_all = [f"Gt_{tb}" for tb in range(4)]
            P.op("act", lambda e, hd=hd: e.activation(out=Gt[0:1, :], in_=Gt[0:1, :], func=AF.Exp, scale=-1.0, bias=nfb[0:1, hd:hd + 1]),
                 reads=Gt_all + ["nfb"], writes=["Gt_f"])
            P.op("act", lambda e: e.activation(out=Gt[0:1, :], in_=Gt[0:1, :], func=AF.Ln, bias=1.0), reads=["Gt_f"], writes=["Gt_f"])
            P.op("dve", lambda e: e.tensor_tensor_scan(out=Gt2[0:1, :], data0=cmask[0:1, :], data1=Gt[0:1, :], initial=0.0, op0=ALU.mult, op1=ALU.add),
                 reads=["Gt_f", "cmask"], writes=["Gt2_0"])
            P.op("act", lambda e, hd=hd: e.activation(out=Gt2[32:33, :], in_=Gt[32:33, :], func=AF.Identity, bias=fbt[32:33, hd:hd + 1]),
                 reads=Gt_all + ["fbt32", "Gt2"], writes=["Gt2_32"])
            for tb in range(4):
                pgt = ps_big[tb % 2]
                P.op("pe", lambda e, tb=tb, pgt=pgt: e.matmul(out=pgt[:, :], lhsT=sel0[0:33, :], rhs=Gt2[0:33, tb * 512:(tb + 1) * 512], start=True, stop=True),
                     reads=["sel0", "Gt2", "Gt2_0", "Gt2_32"], writes=[f"bank{tb % 2}"])
                P.op("act", lambda e, tb=tb, pgt=pgt: e.activation(out=expb[:, tb * 512:(tb + 1) * 512], in_=pgt[:, :], func=AF.Exp, scale=-1.0),
                     reads=[f"bank{tb % 2}"], writes=[f"expb_{tb}"])
            for tb in range(4):
                pgt = ps_big[tb % 2]
                P.op("pe", lambda e, tb=tb, pgt=pgt: e.matmul(out=pgt[:, :], lhsT=sel032[0:33, :], rhs=Gt2[0:33, tb * 512:(tb + 1) * 512], start=True, stop=True),
                     reads=["sel032", "Gt2", "Gt2_0", "Gt2_32"], writes=[f"bank{tb % 2}"])
                P.op("act", lambda e, tb=tb, pgt=pgt: e.activation(out=e2bc[:, tb * 512:(tb + 1) * 512], in_=pgt[:, :], func=AF.Exp),
                     reads=[f"bank{tb % 2}"], writes=[f"e2bc_{tb}"])
            expb_all = [f"expb_{tb}" for tb in range(4)]
            e2_all = [f"e2bc_{tb}" for tb in range(4)]

            for qi in range(2):
                cidx = qi * 4 + hd
                def f_halo(e, qi=qi):
                    for k in range(8):
                        i = e.matmul(out=ps_big[0][:, 0:HALO], lhsT=wh[:, k, qi * 128:(qi + 1) * 128], rhs=hT[:, k, 0:HALO], start=(k == 0), stop=(k == 7))
                    return i
                P.op("pe", f_halo, reads=[f"wh{qi}", "hT_h"], writes=["bank0"])
                P.op("act", lambda e: e.copy(out=raw[:, 0:3], in_=ps_big[0][:, HALO - 3:HALO]), reads=["bank0"], writes=["raw_h"])
                for tb in range(4):
                    pgt = ps_big[(tb + 1) % 2]
                    def f_q(e, tb=tb, qi=qi, pgt=pgt):
                        for k in range(8):
                            i = e.matmul(out=pgt[:, :], lhsT=wh[:, k, qi * 128:(qi + 1) * 128], rhs=hT[:, k, HALO + tb * 512:HALO + (tb + 1) * 512],
                                         start=(k == 0), stop=(k == 7))
                        return i
                    P.op("pe", f_q, reads=[f"wh{qi}", f"hT_{tb}"], writes=[f"bank{(tb + 1) % 2}"])
                    P.op("act", lambda e, tb=tb, pgt=pgt: e.copy(out=raw[:, 3 + tb * 512:3 + (tb + 1) * 512], in_=pgt[:, :]),
                         reads=[f"bank{(tb + 1) % 2}"], writes=[f"raw_{tb}"])
                raw_all = ["raw_h"] + [f"raw_{tb}" for tb in range(4)]
                fl = [lambda e, cidx=cidx: e.tensor_scalar(out=ctmp[:, :], in0=raw[:, 0:NTOK], scalar1=cwt[:, cidx, 0:1], scalar2=cbt[:, cidx:cidx + 1],
                                                           op0=ALU.mult, op1=ALU.add)]
                for j in (1, 2, 3):
                    fl.append(lambda e, cidx=cidx, j=j: e.scalar_tensor_tensor(out=ctmp[:, :], in0=raw[:, j:j + NTOK], scalar=cwt[:, cidx, j:j + 1],
                                                                               in1=ctmp[:, :], op0=ALU.mult, op1=ALU.add))
                P.seq("dve", fl, reads=raw_all + ["cwt", "cbt"], writes=["ctmp"])
                P.op("act", lambda e: e.activation(out=ctmp[:, :], in_=ctmp[:, :], func=AF.Silu), reads=["ctmp"], writes=["ctmp"])
                if qi == 0:
                    P.op("dve", lambda e: e.tensor_tensor(out=qsT[:, :], in0=ctmp[:, :], in1=expb[:, :], op=ALU.mult),
                         reads=["ctmp"] + expb_all, writes=["qsT"])
                else:
                    P.op("dve", lambda e: e.scalar_tensor_tensor(out=ksT[:, :], in0=ctmp[:, :], scalar=SC, in1=e2bc[:, :], op0=ALU.mult, op1=ALU.mult),
                         reads=["ctmp"] + e2_all, writes=["ksT"])

            for c0 in range(0, NCH, 2):
                for c in (c0, c0 + 1):
                    b = c % 2
                    def f_v(e, c=c, b=b):
                        for k in range(8):
                            i = e.matmul(out=pvb[b], lhsT=hT[:, k, HALO + c * 64:HALO + (c + 1) * 64], rhs=wh[:, k, 256:512], start=(k == 0), stop=(k == 7))
                        return i
                    P.op("pe", f_v, reads=["wh2", "wh3", f"hT_{c // 8}"], writes=[KPV[b]])
                for c in (c0, c0 + 1):
                    b = c % 2
                    P.op("act", lambda e, c=c, b=b: e.copy(out=v_ext[:, c, 0:128], in_=pvb[b][:, 0:128]), reads=[KPV[b]], writes=[f"v_{c}"])
                    P.op("act", lambda e, b=b: e.activation(out=so32b[b], in_=pvb[b][:, 128:256], func=AF.Sigmoid), reads=[KPV[b]], writes=[f"so32_{b}"])
                for c in (c0, c0 + 1):
                    b = c % 2
                    P.op("dve", lambda e, c=c, hd=hd, b=b: e.tensor_tensor(out=sigo[:, c, :], in0=so32b[b], in1=gA[:, hd * 128:(hd + 1) * 128], op=ALU.mult),
                         reads=[f"so32_{b}", "gA"], writes=[f"sigo_{c}"])

            P.seq("pool", [lambda e: e.memset(CTloc[:, 0, :], 0.0), lambda e: e.memset(CTloc[:, 0, 129:130], 1.0)], writes=["CTloc_0"])
            for c in range(NCH):
                b = c % 2
                P.op("pe", lambda e, c=c, b=b: e.transpose(out=ptk[b], in_=ksT[:, c * 64:(c + 1) * 64], identity=ident[:, :]),
                     reads=["ksT", "ident"], writes=[KPTK[b]])
                P.op("act", lambda e, b=b: e.copy(out=kstok2[:, b, :], in_=ptk[b]), reads=[KPTK[b]], writes=[f"kstok{b}"])
                P.op("pe", lambda e, c=c, b=b: e.matmul(out=pub[b], lhsT=kstok2[:, b, :], rhs=v_ext[:, c, :], start=True, stop=True),
                     reads=[f"kstok{b}", f"v_{c}", "v_ext_c"], writes=[KPU[b]])
                P.op("dve", lambda e, c=c, b=b: e.tensor_tensor(out=CTt[:, :], in0=pub[b], in1=CTloc[:, c, :], op=ALU.add),
                     reads=[KPU[b], f"CTloc_{c}"], writes=["CTt"])
                P.op("dve", lambda e, c=c: e.tensor_scalar(out=CTloc[:, c + 1, :], in0=CTt[:, :], scalar1=expb[:, c * 64 + 63:c * 64 + 64], scalar2=None, op0=ALU.mult),
                     reads=["CTt", f"expb_{c // 8}"], writes=[f"CTloc_{c + 1}"])

            P.dma("sp", lambda e, hd=hd: e.dma_start(out=cc_in[hd][:, :], in_=CTloc[:, NCH, :]), f"ccin{hd}", reads=[f"CTloc_{NCH}"], writes=[f"cc_in{hd}"])
            def f_cc(e, hd=hd):
                return e.collective_compute("AllGather", ALU.bypass, replica_groups=GROUPS4,
                                            ins=[cc_in[hd].ap().opt()], outs=[cc_out[hd].ap().opt()])
            P.dma("pool", f_cc, f"cc{hd}", reads=[f"cc_in{hd}"], writes=[f"cc_out{hd}"], inc=1)
            P.dma("sp", lambda e, hd=hd: e.dma_start(out=cg[:, :, :], in_=cc_out[hd].ap().rearrange("(r p) c -> p r c", p=128)),
                  f"ccld{hd}", reads=[f"cc_out{hd}"], writes=["cg"])
            P.op("pool", lambda e: e.memset(hacc[:, :], 0.0), writes=["hacc"])
            for pp in range(3):
                P.seq("dve", [
                    lambda e, pp=pp: e.scalar_tensor_tensor(out=hacc2[:, :], in0=hacc[:, :], scalar=cg[:, pp, 129:130], in1=cg[:, pp, :], op0=ALU.mult, op1=ALU.add),
                    lambda e: e.tensor_tensor(out=hacc2[:, :], in0=hacc2[:, :], in1=hacc[:, :], op=ALU.subtract),
                    lambda e, pp=pp: e.scalar_tensor_tensor(out=hacc[:, :], in0=hacc2[:, :], scalar=pm[:, pp:pp + 1], in1=hacc[:, :], op0=ALU.mult, op1=ALU.add)],
                    reads=["hacc", "cg", "pm"], writes=["hacc", "hacc2"])
            for c in range(NCH):
                eng = "dve"
                P.op(eng, lambda e, c=c: e.scalar_tensor_tensor(out=CTball[:, c, :], in0=hacc[:, :], scalar=CTloc[:, c, 129:130], in1=CTloc[:, c, :],
                                                                op0=ALU.mult, op1=ALU.add),
                     reads=["hacc", f"CTloc_{c}"], writes=[f"CTb_{c}"])

            NB = 2
            for c0 in range(0, NCH, NB):
                cl = list(range(c0, c0 + NB))
                for c in cl:
                    b = c % NB
                    cs = slice(c * 64, (c + 1) * 64)
                    P.op("pe", lambda e, cs=cs, b=b: e.matmul(out=pab[b], lhsT=ksT[:, cs], rhs=qsT[:, cs], start=True, stop=True),
                         reads=["ksT", "qsT"], writes=[KPA[b]])
                for c in cl:
                    b = c % NB
                    P.op("dve", lambda e, b=b: e.tensor_tensor(out=STb[:, b, :], in0=pab[b], in1=maskST[:, :], op=ALU.mult), reads=[KPA[b], "maskST"], writes=[f"ST{b}"])
                for c in cl:
                    b = c % NB
                    cs = slice(c * 64, (c + 1) * 64)
                    def f_cn(e, c=c, cs=cs, b=b):
                        e.matmul(out=pcb[b], lhsT=STb[:, b, :], rhs=v_ext[:, c, :], start=True, stop=False)
                        return e.matmul(out=pcb[b], lhsT=qsT[:, cs], rhs=CTball[:, c, :], start=False, stop=True)
                    P.op("pe", f_cn, reads=[f"ST{b}", f"v_{c}", "v_ext_c", "qsT", f"CTb_{c}"], writes=[KPC[b]])
                for c in cl:
                    b = c % NB
                    P.seq("dve", [
                        lambda e, b=b: e.tensor_copy(out=dnb[:, b, 1:2], in_=pcb[b][:, 128:129]),
                        lambda e, b=b: e.scalar_tensor_tensor(out=dnb[:, b, 0:1], in0=dnb[:, b, 1:2], scalar=-1.0, in1=dnb[:, b, 1:2], op0=ALU.mult, op1=ALU.max),
                        lambda e, b=b: e.tensor_scalar(out=dnb[:, b, 0:1], in0=dnb[:, b, 0:1], scalar1=1.0, scalar2=None, op0=ALU.max),
                        lambda e, b=b: e.reciprocal(out=dnb[:, b, 1:2], in_=dnb[:, b, 0:1]),
                        lambda e, b=b: e.tensor_scalar(out=hnb[:, b, :], in0=pcb[b][:, 0:128], scalar1=dnb[:, b, 1:2], scalar2=None, op0=ALU.mult)],
                        reads=[KPC[b]], writes=[f"dn{b}", f"hn{b}"])
                for c in cl:
                    b = c % NB
                    P.seq("act", [
                        lambda e, b=b: e.activation(out=junkb[:, b, :], in_=hnb[:, b, :], func=AF.Square, accum_out=dnb[:, b, 2:3]),
                        lambda e, b=b: e.activation(out=dnb[:, b, 2:3], in_=dnb[:, b, 2:3], func=AF.Sqrt, scale=1.0 / 128, bias=EPS)],
                        reads=[f"hn{b}", f"dn{b}"], writes=[f"junk{b}", f"dn2_{b}"])
                for c in cl:
                    b = c % NB
                    P.seq("dve", [
                        lambda e, b=b: e.reciprocal(out=dnb[:, b, 3:4], in_=dnb[:, b, 2:3]),
                        lambda e, c=c, b=b: e.scalar_tensor_tensor(out=oab[:, b, :], in0=hnb[:, b, :], scalar=dnb[:, b, 3:4], in1=sigo[:, c, :], op0=ALU.mult, op1=ALU.mult)],
                        reads=[f"dn2_{b}", f"hn{b}", f"sigo_{c}"], writes=[f"oa{b}", f"dn{b}"])
                for c in cl:
                    b = c % NB
                    P.op("pe", lambda e, b=b: e.transpose(out=pto[b], in_=oab[:, b, :], identity=ident[0:64, 0:64]), reads=[f"oa{b}", "ident"], writes=[KPTO[b]])
                for c in cl:
                    b = c % NB
                    cs = slice(c * 64, (c + 1) * 64)
                    P.op("act", lambda e, cs=cs, b=b: e.copy(out=mslab[:, cs], in_=pto[b]), reads=[KPTO[b]], writes=["mslab"])
            P.dma("sp", lambda e, hd=hd: e.dma_start(out=mergedT_scr[hd * 128:(hd + 1) * 128, :], in_=mslab[:, :]), "mscr", reads=["mslab"], writes=[f"mscr{hd}"])

        esB.close()
        P.barrier()
        if stage >= 3:
            esC1 = ExitStack()
            esC1.__enter__()
            sb = lambda name, shape, dt: esC1.enter_context(nc.sbuf_tensor(name, shape, dt))
            ps = lambda name, shape, dt: esC1.enter_context(nc.psum_tensor(name, shape, dt))
            wu = sb("wu", [128, 8, 512], BF16)
            utok = [sb(f"utok{i}", [128, 512], BF16) for i in range(2)]
            pu1 = [ps(f"pu1_{i}", [128, 512], F32) for i in range(2)]
            P.dma("pool", lambda e: e.dma_start(out=wu[:, :, :], in_=w_in_v[:, :, 2056:2568]), "wu", writes=["wu"])
            for t in range(NT):
                def f_u1(e, t=t):
                    for k in range(8):
                        i = e.matmul(out=pu1[t % 2][:, :], lhsT=hT[:, k, HALO + t * 128:HALO + (t + 1) * 128], rhs=wu[:, k, :], start=(k == 0), stop=(k == 7))
                    return i
                P.op("pe", f_u1, reads=["wu", f"hT_{t // 4}"], writes=[f"pu1_{t % 2}"])
                P.op("act", lambda e, t=t: e.copy(out=utok[t % 2][:, :], in_=pu1[t % 2][:, :]), reads=[f"pu1_{t % 2}"], writes=[f"utok{t % 2}"])
                P.dma("sp", lambda e, t=t: e.dma_start(out=u_scr[t * 128:(t + 1) * 128, :], in_=utok[t % 2][:, :]), f"uscr{t % 2}",
                      reads=[f"utok{t % 2}"], writes=[f"u_scr{t}"])
            esC1.close()
            esH.close()
            P.barrier()

            TT = lambda o, a, b, op: (lambda e: e.tensor_tensor(out=o, in0=a, in1=b, op=op))
            TS = lambda o, a, s1, s2, op0, op1=None: ((lambda e: e.tensor_scalar(out=o, in0=a, scalar1=s1, scalar2=s2, op0=op0, op1=op1)) if op1 is not None
                                                      else (lambda e: e.tensor_scalar(out=o, in0=a, scalar1=s1, scalar2=None, op0=op0)))
            ACT = lambda o, a, f, **kw: (lambda e: e.activation(out=o, in_=a, func=f, **kw))
            MUL, ADD, SUB = ALU.mult, ALU.add, ALU.subtract

            def cmul(o_r, o_i, a_r, a_i, s_r, s_i, t1, t2):
                return [TT(t1, a_r, s_r, MUL), TT(t2, a_i, s_i, MUL), TT(o_r, t1, t2, SUB),
                        TT(t1, a_r, s_i, MUL), TT(t2, a_i, s_r, MUL), TT(o_i, t1, t2, ADD)]

            def emit_prep(GP, gs, lam_re_t, lam_im_t, logdt_t, X1t, X2t, sgn, hpi, sc, sqr, sqi, bqr, bqi, a5r, a5i, PWfr, PWfi, PWrr, PWri, PWbr, PWbi, cta, ctb, BB1, BB2, bbt, PRs, nPI):
                ops = []
                S = {k: v[:, :] for k, v in sc.items()}
                ops = []
                ops.append(TS(S["lr"], lam_re_t[:, gs], -1e-4, None, ALU.min))
                P.seq("dve", ops, reads=["lam_re_t"], writes=["prep"]); ops = []
                P.op("act", ACT(S["dt"], logdt_t[:, gs], AF.Exp), reads=["logdt_t", "prep"], writes=["prep_dt"])
                ops += [TT(S["lrdt"], S["lr"], S["dt"], MUL), TT(S["th"], lam_im_t[:, gs], S["dt"], MUL)]
                P.seq("dve", ops, reads=["prep", "prep_dt", "lam_im_t"], writes=["prep"]); ops = []
                P.seq("act", [ACT(S["sn"], S["th"], AF.Sin, scale=1.0 / 32), ACT(S["cs"], S["th"], AF.Sin, scale=1.0 / 32, bias=hpi[:, 0:1]),
                              ACT(S["mag"], S["lrdt"], AF.Exp, scale=1.0 / 32), ACT(S["im2"], S["lrdt"], AF.Exp, scale=-2.0)],
                      reads=["prep", "hpi"], writes=["prep_cs"])
                li = lam_im_t[:, gs]
                ops += [TT(a5r[:, :, 0], S["mag"], S["cs"], MUL), TT(a5i[:, :, 0], S["mag"], S["sn"], MUL)]
                for e_ in range(5):
                    ops += cmul(a5r[:, :, e_ + 1], a5i[:, :, e_ + 1], a5r[:, :, e_], a5i[:, :, e_], a5r[:, :, e_], a5i[:, :, e_], S["ta"], S["nsq"])
                ops += [(lambda e: e.tensor_copy(out=S["ar"], in_=a5r[:, :, 5])), (lambda e: e.tensor_copy(out=S["ai"], in_=a5i[:, :, 5])),
                        TT(S["den"], S["lr"], S["lr"], MUL), TT(S["ta"], li, li, MUL), TT(S["den"], S["den"], S["ta"], ADD),
                        (lambda e: e.reciprocal(out=S["den"], in_=S["den"])),
                        TS(S["am1"], S["ar"], -1.0, None, ADD),
                        TT(S["ta"], S["am1"], S["lr"], MUL), TT(S["tb"], S["ai"], li, MUL), TT(S["ta"], S["ta"], S["tb"], ADD), TT(S["cr"], S["ta"], S["den"], MUL),
                        TT(S["ta"], S["ai"], S["lr"], MUL), TT(S["tb"], S["am1"], li, MUL), TT(S["ta"], S["ta"], S["tb"], SUB), TT(S["ci"], S["ta"], S["den"], MUL),
                        TS(S["scr"], S["cr"], sgn[:, 0:1], None, MUL),
                        TS(S["tb"], S["ci"], sgn[:, 0:1], None, MUL),
                        TS(S["nci"], S["ci"], -1.0, None, MUL),
                        TT(S["bir"], S["ar"], S["im2"], MUL), TT(S["bii"], S["ai"], S["im2"], MUL), TS(S["bii"], S["bii"], -1.0, None, MUL),
                        (lambda e: e.tensor_copy(out=sqr[:, :, 0], in_=S["ar"])), (lambda e: e.tensor_copy(out=sqi[:, :, 0], in_=S["ai"])),
                        (lambda e: e.tensor_copy(out=bqr[:, :, 0], in_=S["bir"])), (lambda e: e.tensor_copy(out=bqi[:, :, 0], in_=S["bii"]))]
                for e_ in range(11):
                    ops += cmul(sqr[:, :, e_ + 1], sqi[:, :, e_ + 1], sqr[:, :, e_], sqi[:, :, e_], sqr[:, :, e_], sqi[:, :, e_], S["ta"], S["nsq"])
                for e_ in range(2):
                    ops += cmul(bqr[:, :, e_ + 1], bqi[:, :, e_ + 1], bqr[:, :, e_], bqi[:, :, e_], bqr[:, :, e_], bqi[:, :, e_], S["ta"], S["nsq"])
                bc16 = lambda a: a.unsqueeze(2).to_broadcast([128, GP, 16])
                ops += [TT(BB1[:, :, :], X1t[:, gs, :], bc16(S["cr"]), MUL), TT(bbt[:, :, :], X2t[:, gs, :], bc16(S["tb"]), MUL), TT(BB1[:, :, :], BB1[:, :, :], bbt[:, :, :], ADD),
                        TT(BB2[:, :, :], X2t[:, gs, :], bc16(S["scr"]), MUL), TT(bbt[:, :, :], X1t[:, gs, :], bc16(S["nci"]), MUL), TT(BB2[:, :, :], BB2[:, :, :], bbt[:, :, :], ADD)]
                ops += [(lambda e: e.memset(PWfr[:, :, 0:1], 1.0)), (lambda e: e.memset(PWfi[:, :, 0:1], 0.0))]
                for k in range(6):
                    n = 1 << k
                    bcn = lambda a, n=n: a.to_broadcast([128, GP, n])
                    ops += cmul(PWfr[:, :, n:2 * n], PWfi[:, :, n:2 * n], PWfr[:, :, 0:n], PWfi[:, :, 0:n],
                                bcn(sqr[:, :, k:k + 1]), bcn(sqi[:, :, k:k + 1]), cta[:, :, 0:n], ctb[:, :, 0:n])
                ops += [(lambda e: e.tensor_copy(out=PWfr[:, :, 64:65], in_=sqr[:, :, 6:7])), (lambda e: e.tensor_copy(out=PWfi[:, :, 64:65], in_=sqi[:, :, 6:7]))]
                ops += [(lambda e: e.memset(PWrr[:, :, 63:64], 1.0)), (lambda e: e.memset(PWri[:, :, 63:64], 0.0))]
                for k in range(6):
                    n = 1 << k
                    bcn = lambda a, n=n: a.to_broadcast([128, GP, n])
                    ops += cmul(PWrr[:, :, 64 - 2 * n:64 - n], PWri[:, :, 64 - 2 * n:64 - n], PWrr[:, :, 64 - n:64], PWri[:, :, 64 - n:64],
                                bcn(sqr[:, :, k:k + 1]), bcn(sqi[:, :, k:k + 1]), cta[:, :, 0:n], ctb[:, :, 0:n])
                ops += [(lambda e: e.memset(PWbr[:, :, 0:1], 1.0)), (lambda e: e.memset(PWbi[:, :, 0:1], 0.0))]
                for k in range(3):
                    n = 1 << k
                    bcn = lambda a, n=n: a.to_broadcast([128, GP, n])
                    ops += cmul(PWbr[:, :, n:2 * n], PWbi[:, :, n:2 * n], PWbr[:, :, 0:n], PWbi[:, :, 0:n],
                                bcn(bqr[:, :, k:k + 1]), bcn(bqi[:, :, k:k + 1]), cta[:, :, 0:n], ctb[:, :, 0:n])
                ops += [TS(PRs[:, :, :], PWfr[:, :, :], sgn[:, 0:1], None, MUL), TS(PRs[:, :, :], PRs[:, :, :], -1.0, None, MUL), TS(nPI[:, :, :], PWfi[:, :, :], -1.0, None, MUL)]
                P.seq("dve", ops, reads=["prep", "prep_sn", "prep_cs", "sgn", "X1t", "X2t", "lam_im_t", "Rk_use", "tab_use"], writes=["prep", "prepT"]); ops = []

            esC0 = ExitStack()
            esC0.__enter__()
            sb0 = lambda name, shape, dt: esC0.enter_context(nc.sbuf_tensor(name, shape, dt))
            GA = 32
            i_lre = sb0("i_lre", [128, 32], F32); i_lim = sb0("i_lim", [128, 32], F32); i_ldt = sb0("i_ldt", [128, 32], F32)
            i_X1 = sb0("i_X1", [128, 32, 16], F32); i_X2 = sb0("i_X2", [128, 32, 16], F32)
            i_sgn = sb0("i_sgn", [128, 1], F32); i_hpi = sb0("i_hpi", [128, 1], F32)
            for dst_, src_ in ((i_lre[:, :], lam2_re[:, :]), (i_lim[:, :], lam2_im[:, :]), (i_ldt[:, :], logdt[0:1, :].partition_broadcast(128)),
                               (i_X1[:, :, :], X1d[:, :, :]), (i_X2[:, :, :], X2d[:, :, :])):
                P.dma("sp", lambda e, dst_=dst_, src_=src_: e.dma_start(out=dst_, in_=src_), "setupC0", writes=["c0in"])
            P.bulk_done("setupC0")
            P.seq("pool", [lambda e: e.memset(i_sgn[0:64, :], -1.0), lambda e: e.memset(i_sgn[64:128, :], 1.0), lambda e: e.memset(i_hpi[:, :], float(np.pi / 2))],
                  writes=["sgn", "hpi", "lam_re_t", "lam_im_t", "logdt_t", "X1t", "X2t"], reads=["c0in"])
            sc_names = ("lr", "dt", "lrdt", "th", "mag", "t1", "sn", "cs", "ar", "ai", "den", "am1", "cr", "ci", "ta", "tb", "scr", "nci", "im2", "bir", "bii", "nsq")
            sc0 = {n: sb0("sc0_" + n, [128, GA], F32) for n in sc_names}
            T0 = {n: sb0("p0_" + n, [128, GA, k], F32) for n, k in (("sqr", 12), ("sqi", 12), ("bqr", 3), ("bqi", 3), ("a5r", 6), ("a5i", 6), ("PWfr", 65), ("PWfi", 65),
                                                                     ("PWrr", 64), ("PWri", 64), ("PWbr", 8), ("PWbi", 8), ("cta", 64), ("ctb", 64), ("BB1", 16), ("BB2", 16),
                                                                     ("bbt", 16), ("PRs", 65), ("nPI", 65))}
            emit_prep(GA, slice(0, 32), i_lre, i_lim, i_ldt, i_X1, i_X2, i_sgn, i_hpi, sc0, *[T0[n] for n in ("sqr", "sqi", "bqr", "bqi", "a5r", "a5i", "PWfr", "PWfi", "PWrr", "PWri", "PWbr", "PWbi",
                                                                 "cta", "ctb", "BB1", "BB2", "bbt", "PRs", "nPI")])
            for nm_ in ("PWrr", "PWri", "PWbr", "PWbi", "BB1", "BB2", "PRs", "nPI", "sqr", "sqi"):
                P.dma("sp", lambda e, nm_=nm_: e.dma_start(out=prep_scr[nm_][:, :, :], in_=T0[nm_][:, :, :]), "prepst", reads=["prepT"], writes=["prep_scr"])
            P.bulk_done("prepst")
            esC0.close()
            P.barrier()
            esC = ExitStack()
            esC.__enter__()
            sb = lambda name, shape, dt: esC.enter_context(nc.sbuf_tensor(name, shape, dt))
            ps = lambda name, shape, dt: esC.enter_context(nc.psum_tensor(name, shape, dt))
            G8 = 8
            lam_re_t = sb("lam_re_t", [128, 32], F32)
            lam_im_t = sb("lam_im_t", [128, 32], F32)
            logdt_t = sb("logdt_t", [128, 32], F32)
            X1t = sb("X1t", [128, 32, 16], F32)
            X2t = sb("X2t", [128, 32, 16], F32)
            CY1t = sb("CY1t", [128, 32, 16], F32)
            CY2t = sb("CY2t", [128, 32, 16], F32)
            sgn = sb("sgn", [128, 1], F32)
            d_bc = sb("d_bc", [32, 512], F32)
            WAt = sb("WAt", [128, 4, 128], BF16)
            WBt = sb("WBt", [128, 4, 128], BF16)
            gba = sb("gba", [128, 4], F32)
            gbb = sb("gbb", [128, 4], F32)
            SW = sb("SW", [128, 128], F32)
            rkt8 = sb("rkt8", [128, G8, 128], F32)
            send = sb("send", [128, G8], F32)
            maskG = sb("maskG", [128, 64, 16], F32)
            sc = {n: sb("sc_" + n, [128, G8], F32) for n in
                  ("lr", "dt", "lrdt", "th", "mag", "t1", "sn", "cs", "ar", "ai", "den", "am1", "cr", "ci", "ta", "tb", "scr", "nci", "im2", "bir", "bii", "nsq")}
            a5r = sb("a5r", [128, G8, 6], F32)
            a5i = sb("a5i", [128, G8, 6], F32)
            hpi = sb("hpi", [128, 1], F32)
            sqr = sb("sqr", [128, G8, 12], F32)
            sqi = sb("sqi", [128, G8, 12], F32)
            bqr = sb("bqr", [128, G8, 3], F32)
            bqi = sb("bqi", [128, G8, 3], F32)
            PWrr = sb("PWrr", [128, G8, 64], F32)
            PWri = sb("PWri", [128, G8, 64], F32)
            PWbr = sb("PWbr", [128, G8, 8], F32)
            PWbi = sb("PWbi", [128, G8, 8], F32)
            BB1 = sb("BB1", [128, G8, 16], F32)
            BB2 = sb("BB2", [128, G8, 16], F32)
            bbt = sb("bbt", [128, G8, 16], F32)
            PRs = sb("PRs", [128, G8, 65], F32)
            nPI = sb("nPI", [128, G8, 65], F32)
            tA = sb("tA", [128, 65, 16], F32)
            tB = sb("tB", [128, 65, 16], F32)
            tC = sb("tC", [128, 65, 16], F32)
            tD = sb("tD", [128, 65, 16], F32)
            ZTb = sb("ZTb", [128, 64, 16], BF16)
            XTb = sb("XTb", [128, 8, 16], BF16)
            Zt = sb("Zt", [128, G8, 8, 128], BF16)
            Gtab = sb("Gtab", [128, G8, 1024], BF16)
            Yt = sb("Yt", [128, G8, 65, 16], BF16)
            Rk = sb("Rk", [128, G8, 6, 128], F32)
            ucj = sb("ucj", [32, G8, 64, 16], BF16)
            ustack = sb("ustack", [128, G8, 8, 32], BF16)
            E32 = sb("E32", [128, G8, 32], F32)
            Xs = sb("Xs", [128, G8, 33], F32)
            Sprevb = sb("Sprevb", [128, G8, 32], BF16)
            sg_t = sb("sg_t", [128, 4, G8], F32)
            hs = sb("hs", [128, G8], F32)
            hs2 = sb("hs2", [128, G8], F32)
            du = sb("du", [32, 64, 16], F32)
            yv = sb("yv", [32, 1024], F32)
            y2 = sb("y2", [32, 1024], F32)
            ysg = sb("ysg", [32, 1024], F32)
            ygel = sb("ygel", [32, 64, 128], BF16)
            ygT = sb("ygT", [128, 64, 32], BF16)
            sbt = sb("sbt", [128, 512], F32)
            ps_z = [ps(f"ps_z{i}", [128, 512], F32) for i in range(2)]
            pT = ps("pT", [128, 64, 32], BF16)
            pE = ps("pE", [128, G8, 32], F32)
            pD = ps("pD", [128, G8, 33], F32)
            pY = ps("pY", [128, 1024], F32)

            def ld(dst, src, key, q="sp"):
                P.dma(q, lambda e: e.dma_start(out=dst, in_=src), "setupC" if q == "sp" else "setupCp", writes=[key])
            ld(lam_re_t[:, :], lam2_re[:, :], "lam_re_t")
            ld(lam_im_t[:, :], lam2_im[:, :], "lam_im_t")
            ld(logdt_t[:, :], logdt[0:1, :].partition_broadcast(128), "logdt_t")
            ld(X1t[:, :, :], X1d[:, :, :], "X1t")
            ld(X2t[:, :, :], X2d[:, :, :], "X2t")
            ld(CY1t[:, :, :], CY1d[:, :, :], "CY1t")
            ld(CY2t[:, :, :], CY2d[:, :, :], "CY2t")
            ld(d_bc[:, :], s5d[0:1, :].partition_broadcast(32), "d_bc")
            ld(gba[:, :], gbad[:, :], "gba")
            ld(gbb[:, :], gbbd[:, :], "gbb")
            ld(WAt[:, :, :], WAd.ap().rearrange("u p c -> p u c"), "WAt", q="pool")
            ld(WBt[:, :, :], WBd.ap().rearrange("u p c -> p u c"), "WBt", q="pool")
            P.bulk_done("setupC")
            P.bulk_done("setupCp")
            P.seq("pool", [lambda e: e.memset(sgn[0:64, :], -1.0), lambda e: e.memset(sgn[64:128, :], 1.0)], writes=["sgn"])
            P.op("pool", lambda e: e.memset(hpi[:, :], float(np.pi / 2)), writes=["hpi"])
            P.seq("pool", [lambda e: e.memset(SW[:, :], 0.0),
                           lambda e: e.affine_select(out=SW[:, :], in_=SW[:, :], pattern=[[-1, 128]], compare_op=ALU.not_equal, fill=1.0, base=64, channel_multiplier=1),
                           lambda e: e.affine_select(out=SW[:, :], in_=SW[:, :], pattern=[[-1, 128]], compare_op=ALU.not_equal, fill=1.0, base=-64, channel_multiplier=1)],
                  writes=["SW"])
            P.op("pool", lambda e: e.tensor_scalar(out=SW[:, :], in0=SW[:, :], scalar1=sgn[:, 0:1], scalar2=None, op0=ALU.mult), reads=["SW", "sgn"], writes=["SW"])
            P.seq("pool", [lambda e: e.memset(maskG[:, :, :], 1.0),
                           lambda e: e.affine_select(out=maskG[:, :, :], in_=maskG[:, :, :], pattern=[[16, 64], [0, 16]], compare_op=ALU.is_ge, fill=0.0,
                                                     base=15, channel_multiplier=-1)], writes=["maskG"])

            PI = float(np.pi)
            for un in range(4):
                gs = slice(un * 8, (un + 1) * 8)
                for nm_, dst_ in (("PWrr", PWrr), ("PWri", PWri), ("PWbr", PWbr), ("PWbi", PWbi), ("BB1", BB1), ("BB2", BB2),
                                  ("PRs", PRs), ("nPI", nPI), ("sqr", sqr), ("sqi", sqi)):
                    P.dma("sp", lambda e, nm_=nm_, dst_=dst_, gs=gs: e.dma_start(out=dst_[:, :, :], in_=prep_scr[nm_][:, gs, :]), "prepld",
                          reads=["prep_scr", "Rk_use", "tab_use"], writes=["prepT"])
                P.bulk_done("prepld")

                for g in range(G8):
                    ga = un * 8 + g
                    bq = lambda a: a.unsqueeze(2).to_broadcast([128, 64, 16])
                    bh = lambda a, n: a.unsqueeze(1).to_broadcast([128, n, 16])
                    P.seq("dve", [TT(tA[:, 0:64, :], bq(PWrr[:, g, :]), bh(BB1[:, g, :], 64), MUL),
                                  TT(tB[:, 0:64, :], bq(PWri[:, g, :]), bh(BB2[:, g, :], 64), MUL),
                                  TT(ZTb[:, :, :], tA[:, 0:64, :], tB[:, 0:64, :], ADD)],
                          reads=["prepT", "ZTb_use"], writes=["tA", "tB", "ZTb"])
                    def f_zt(e):
                        for kc in range(8):
                            i = e.transpose(out=pT[:, kc * 4:(kc + 1) * 4, :], in_=ZTb[:, kc * 8:(kc + 1) * 8, :], identity=ident[:, :])
                        return i
                    P.op("pe", f_zt, reads=["ZTb", "ident"], writes=["pT"])
                    P.op("act", lambda e, g=g: e.copy(out=Zt[:, g, :, :], in_=pT[:, 0:32, :].rearrange("p (a b) c -> p a (b c)", b=4)), reads=["pT"], writes=["Zt", "ZTb_use"])
                for k in range(6):
                    P.seq("pool", [lambda e, k=k: e.tensor_tensor(out=Rk[:, :, k, :], in0=identf[:, :].unsqueeze(1).to_broadcast([128, G8, 128]),
                                                                  in1=sqr[:, :, 6 + k:7 + k].to_broadcast([128, G8, 128]), op=MUL),
                                   lambda e, k=k: e.tensor_tensor(out=rkt8[:, :, :], in0=SW[:, :].unsqueeze(1).to_broadcast([128, G8, 128]),
                                                                  in1=sqi[:, :, 6 + k:7 + k].to_broadcast([128, G8, 128]), op=MUL),
                                   lambda e, k=k: e.tensor_tensor(out=Rk[:, :, k, :], in0=Rk[:, :, k, :], in1=rkt8[:, :, :], op=SUB)],
                          reads=["prepT", "identf", "SW", "sgn", "Rk_use"], writes=[f"Rk{g}" for g in range(G8)] + ["rkt"])
                for g in range(G8):
                    P.dma("sp", lambda e, un=un, g=g: e.dma_start(out=ucj[:, g, :, :],
                                                                   in_=u_scr.ap().rearrange("(c j) n -> c j n", j=64)[:, :, un * 128 + g * 16:un * 128 + (g + 1) * 16]),
                          "ucj", reads=[f"u_scr{t}" for t in range(NT)] + ["ucj_use"], writes=[f"ucj{g}"])
                P.bulk_done("ucj")
                for g in range(G8):
                    def f_us(e, g=g):
                        for kc in range(8):
                            i = e.transpose(out=pT[:, 32 + kc, :], in_=ucj[:, g, kc * 8:(kc + 1) * 8, :], identity=ident[0:32, 0:32])
                        return i
                    P.op("pe", f_us, reads=[f"ucj{g}", "ident"], writes=["pT2"])
                    P.op("act", lambda e, g=g: e.copy(out=ustack[:, g, :, :], in_=pT[:, 32:40, :]), reads=["pT2"], writes=[f"ustack{g}"])
                    def f_E(e, g=g):
                        for kc in range(8):
                            i = e.matmul(out=pE[:, g, :], lhsT=Zt[:, g, kc, :], rhs=ustack[:, g, kc, :], start=(kc == 0), stop=(kc == 7))
                        return i
                    P.op("pe", f_E, reads=["Zt", f"ustack{g}"], writes=["pE"])
                P.op("act", lambda e: e.copy(out=E32[:, :, :], in_=pE[:, :, :]), reads=["pE"], writes=["E32"])

                def doubling(tag):
                    for k in range(6):
                        n = 33 - (1 << k)
                        def f_d(e, k=k, n=n):
                            for g in range(G8):
                                i = e.matmul(out=pD[:, g, 0:n], lhsT=Rk[:, g, k, :], rhs=Xs[:, g, 0:n], start=True, stop=True)
                            return i
                        P.op("pe", f_d, reads=["Xs"] + [f"Rk{g}" for g in range(G8)], writes=["pD"])
                        P.op("dve", lambda e, k=k, n=n: e.tensor_tensor(out=Xs[:, :, 1 << k:33], in0=Xs[:, :, 1 << k:33], in1=pD[:, :, 0:n], op=ADD),
                             reads=["pD", "Xs"], writes=["Xs"])
                P.seq("dve", [lambda e: e.memset(Xs[:, :, 0:1], 0.0), lambda e: e.tensor_copy(out=Xs[:, :, 1:33], in_=E32[:, :, :])], reads=["E32"], writes=["Xs"])
                doubling("loc")
                P.op("dve", lambda e: e.tensor_copy(out=send[:, :], in_=Xs[:, :, 32]), reads=["Xs"], writes=["send"])
                P.dma("sp", lambda e, un=un: e.dma_start(out=ccs_in[un][:, :], in_=send[:, :]), f"ccsin{un}", reads=["send"], writes=[f"ccs_in{un}"])
                P.dma("pool", lambda e, un=un: e.collective_compute("AllGather", ALU.bypass, replica_groups=GROUPS4,
                                                                     ins=[ccs_in[un].ap().opt()], outs=[ccs_out[un].ap().opt()]),
                      f"ccs{un}", reads=[f"ccs_in{un}"], writes=[f"ccs_out{un}"], inc=1)
                P.dma("sp", lambda e, un=un: e.dma_start(out=sg_t[:, :, :], in_=ccs_out[un].ap().rearrange("(r p) c -> p r c", p=128)),
                      f"ccsld{un}", reads=[f"ccs_out{un}"], writes=["sg_t"])
                for g in range(G8):
                    ga = un * 8 + g
                    bh = lambda a, n: a.unsqueeze(1).to_broadcast([128, n, 16])
                    bq8 = lambda a: a.unsqueeze(2).to_broadcast([128, 8, 16])
                    P.seq("dve", [TT(tA[:, 0:8, :], bq8(PWbr[:, g, :]), bh(BB1[:, g, :], 8), MUL),
                                  TT(tB[:, 0:8, :], bq8(PWbi[:, g, :]), bh(BB2[:, g, :], 8), MUL),
                                  TT(XTb[:, :, :], tA[:, 0:8, :], tB[:, 0:8, :], ADD)],
                          reads=["prepT", "tA", "tB", "XTb_use"], writes=["tA", "tB", "XTb"])
                    bq65 = lambda a: a.unsqueeze(2).to_broadcast([128, 65, 16])
                    P.seq("pool", [TT(tC[:, :, :], bq65(PRs[:, g, :]), bh(CY1t[:, ga, :], 65), MUL),
                                   TT(tD[:, :, :], bq65(nPI[:, g, :]), bh(CY2t[:, ga, :], 65), MUL),
                                   TT(Yt[:, g, :, :], tC[:, :, :], tD[:, :, :], ADD)],
                          reads=["prepT", "CY1t", "CY2t", "tab_use"], writes=["tC", "tD", f"Yt{g}"])
                    def f_g(e, g=g):
                        e.matmul(out=pY[:, 0:512], lhsT=XTb[:, :, :], rhs=Yt[:, g, 0:32, :], start=True, stop=True)
                        return e.matmul(out=pY[:, 512:1024], lhsT=XTb[:, :, :], rhs=Yt[:, g, 32:64, :], start=True, stop=True)
                    P.op("pe", f_g, reads=["XTb", f"Yt{g}"], writes=["pY"])
                    P.op("act", lambda e, g=g: e.copy(out=Gtab[:, g, :], in_=pY[:, :]), reads=["pY", "tab_use"], writes=[f"Gtab{g}", "XTb_use"])
                    P.op("pool", lambda e, g=g: e.affine_select(out=Gtab[:, g, :].rearrange("p (m h) -> p m h", h=16), in_=Gtab[:, g, :].rearrange("p (m h) -> p m h", h=16),
                                                                pattern=[[16, 64], [0, 16]], compare_op=ALU.is_ge, fill=0.0, base=15, channel_multiplier=-1),
                         reads=[f"Gtab{g}"], writes=[f"Gtab{g}"])

                P.op("dve", lambda e: e.memset(hs[:, :], 0.0), writes=["hs"])
                for pp in range(3):
                    def f_hr(e):
                        for g in range(G8):
                            i = e.matmul(out=pD[:, g, 0:1], lhsT=Rk[:, g, 5, :], rhs=hs[:, g:g + 1], start=True, stop=True)
                        return i
                    P.op("pe", f_hr, reads=["hs"] + [f"Rk{g}" for g in range(G8)], writes=["pD"])
                    P.seq("dve", [lambda e, pp=pp: e.tensor_tensor(out=hs2[:, :], in0=pD[:, :, 0], in1=sg_t[:, pp, :], op=ADD),
                                  lambda e: e.tensor_tensor(out=hs2[:, :], in0=hs2[:, :], in1=hs[:, :], op=SUB),
                                  lambda e, pp=pp: e.scalar_tensor_tensor(out=hs[:, :], in0=hs2[:, :], scalar=pm[:, pp:pp + 1], in1=hs[:, :], op0=MUL, op1=ADD)],
                          reads=["pD", "sg_t", "hs", "pm"], writes=["hs", "hs2"])
                P.seq("dve", [lambda e: e.tensor_copy(out=Xs[:, :, 0], in_=hs[:, :]), lambda e: e.tensor_copy(out=Xs[:, :, 1:33], in_=E32[:, :, :])],
                      reads=["hs", "E32"], writes=["Xs"])
                doubling("glob")
                P.op("act", lambda e: e.copy(out=Sprevb[:, :, :], in_=Xs[:, :, 0:32]), reads=["Xs"], writes=["Sprevb"])

                for g in range(G8):
                    def f_y(e, g=g):
                        Yf = Yt[:, g, :, :]
                        e.matmul(out=pY[0:32, 0:512], lhsT=Sprevb[:, g, :], rhs=Yt[:, g, 1:33, :], start=True, stop=False)
                        e.matmul(out=pY[0:32, 512:1024], lhsT=Sprevb[:, g, :], rhs=Yt[:, g, 33:65, :], start=True, stop=False)
                        i = None
                        for kc in range(8):
                            lo = 128 * kc
                            if lo < 512:
                                i = e.matmul(out=pY[0:32, lo:512], lhsT=ustack[:, g, kc, :], rhs=Gtab[:, g, 0:512 - lo], start=False, stop=(kc == 3))
                            b0 = max(512, lo)
                            i = e.matmul(out=pY[0:32, b0:1024], lhsT=ustack[:, g, kc, :], rhs=Gtab[:, g, b0 - lo:1024 - lo], start=False, stop=(kc == 7))
                        return i
                    P.op("pe", f_y, reads=["Sprevb", f"Yt{g}", f"Gtab{g}", f"ustack{g}"], writes=["pY"])
                    ga = un * 8 + g
                    P.op("pool", lambda e, g=g, ga=ga: e.tensor_tensor(out=du[:, :, :], in0=ucj[:, g, :, :],
                                                                       in1=d_bc[:, ga * 16:(ga + 1) * 16].unsqueeze(1).to_broadcast([32, 64, 16]), op=MUL),
                         reads=[f"ucj{g}", "d_bc"], writes=["du"])
                    yv_ = yv[:, :]
                    P.seq("dve", [lambda e: e.tensor_tensor(out=yv[:, :], in0=pY[0:32, :], in1=du[:, :, :], op=ADD),
                                  TT(y2[:, :], yv_, yv_, MUL), TS(y2[:, :], y2[:, :], 0.044715, 1.0, MUL, ADD), TT(y2[:, :], y2[:, :], yv_, MUL)],
                          reads=["pY", "du", "ysg"], writes=["yv", "y2"])
                    P.op("act", ACT(ysg[:, :], y2[:, :], AF.Sigmoid, scale=1.5957691216057308), reads=["y2"], writes=["ysg"])
                    P.op("dve", lambda e, g=g: e.tensor_tensor(out=ygel[:, :, g * 16:(g + 1) * 16], in0=yv[:, :], in1=ysg[:, :], op=MUL),
                         reads=["yv", "ysg", "ygel_use"], writes=[f"ygel{g}"])
                def f_gt(e):
                    for j in range(64):
                        i = e.transpose(out=pT[:, j, :], in_=ygel[:, j, :], identity=ident[0:32, 0:32])
                    return i
                P.op("pe", f_gt, reads=[f"ygel{g}" for g in range(G8)] + ["ident"], writes=["pT", "pT2"])
                P.op("act", lambda e: e.copy(out=ygT[:, :, :], in_=pT[:, :, :]), reads=["pT", "pT2"], writes=["ygT", "ygel_use"])
                ms_v = mslab[:, :].rearrange("p (c j) -> p j c", j=64)
                for nb in range(4):
                    P.op("pe", lambda e, nb=nb, un=un: e.matmul(out=ps_z[0][:, :], lhsT=WAt[:, un, :], rhs=ygT[:, nb * 16:(nb + 1) * 16, :], start=True, stop=True),
                         reads=["WAt", "ygT"], writes=["ps_z0"])
                    P.op("pe", lambda e, nb=nb, un=un: e.matmul(out=ps_z[1][:, :], lhsT=WBt[:, un, :], rhs=ygT[:, nb * 16:(nb + 1) * 16, :], start=True, stop=True),
                         reads=["WBt", "ygT"], writes=["ps_z1"])
                    P.op("act", lambda e, un=un: e.activation(out=sbt[:, :], in_=ps_z[1][:, :], func=AF.Sigmoid, bias=gbb[:, un:un + 1]),
                         reads=["ps_z1", "gbb"], writes=["sbt"])
                    P.op("dve", lambda e, nb=nb, un=un: e.scalar_tensor_tensor(out=ms_v[:, nb * 16:(nb + 1) * 16, :], in0=ps_z[0][:, :].rearrange("p (j c) -> p j c", c=32), scalar=gba[:, un:un + 1],
                                                                               in1=sbt[:, :].rearrange("p (j c) -> p j c", c=32), op0=ADD, op1=MUL),
                         reads=["ps_z0", "sbt", "gba"], writes=["mslab"])
                P.dma("sp", lambda e, un=un: e.dma_start(out=mergedT_scr[512 + un * 128:512 + (un + 1) * 128, :], in_=mslab[:, :]), "mscr",
                      reads=["mslab"], writes=[f"mscr{4 + un}"])
            esC.close()
            P.barrier()
        else:
            esH.close()

        if stage >= 4:
            AX = mybir.AxisListType
            MUL, ADD, SUB = ALU.mult, ALU.add, ALU.subtract
            esDE = ExitStack()
            esDE.__enter__()
            sbp = lambda name, shape, dt: esDE.enter_context(nc.sbuf_tensor(name, shape, dt))
            h2T = sbp("h2T", [128, 8, NTOK], BF16)
            cw = sbp("cw", [128, NT, 32], F32)
            xt2 = [sbp(f"xt2_{i}", [128, D], F32) for i in range(2)]
            ssq2 = sbp("ssq2", [128, 2 * NT], F32)
            rstd2 = sbp("rstd2", [128, 2 * NT], F32)
            junk2 = sbp("junk2", [128, D], BF16)
            esD = ExitStack()
            esD.__enter__()
            sb = lambda name, shape, dt: esD.enter_context(nc.sbuf_tensor(name, shape, dt))
            ps = lambda name, shape, dt: esD.enter_context(nc.psum_tensor(name, shape, dt))
            mT = sb("mT", [128, 8, NTOK], BF16)
            Wout = sb("Wout", [128, 8, D], BF16)
            Wr = sb("Wr", [128, 8, 36], BF16)
            rb_bc = sb("rb_bc", [128, 36], F32)
            xm = [sb(f"xm{i}", [128, D], F32) for i in range(2)]
            hb2 = [sb(f"hb2_{i}", [128, D], BF16) for i in range(2)]
            lg = sb("lg", [128, NT, 36], F32)
            r_a = sb("r_a", [128, NT, 4], F32)
            r_b = sb("r_b", [128, NT, 4], F32)
            r_gmax = sb("r_gmax", [128, NT], F32)
            r_gw = sb("r_gw", [128, NT], F32)
            r_el = sb("r_el", [128, NT, 32], F32)
            r_t = sb("r_t", [128, NT, 32], F32)
            r_oh1 = sb("r_oh1", [128, NT, 32], F32)
            r_oh2 = sb("r_oh2", [128, NT, 32], F32)
            r_m1 = sb("r_m1", [128, NT], F32)
            r_m2 = sb("r_m2", [128, NT], F32)
            r_w1 = sb("r_w1", [128, NT], F32)
            r_w2 = sb("r_w2", [128, NT], F32)
            po4 = [[ps(f"po{i}{j}", [128, 512], F32) for j in range(2)] for i in range(2)]
            tp2 = [ps(f"tp2_{i}", [128, 8, 128], BF16) for i in range(2)]
            pr2 = [ps(f"pr{i}", [128, 36], F32) for i in range(2)]

            P.dma("sp", lambda e: e.dma_start(out=mT[:, :, :], in_=mergedT_scr.ap().rearrange("(kc p) t -> p kc t", p=128)), "mT",
                  reads=[f"mscr{i}" for i in range(8)], writes=["mT"])
            P.dma("pool", lambda e: e.dma_start(out=Wout[:, :, :], in_=w_out.ap().rearrange("(kc p) c -> p kc c", p=128)), "setupDp", writes=["Wout"])
            P.dma("pool", lambda e: e.dma_start(out=Wr[:, :, :], in_=w_rt.ap().rearrange("(kc p) c -> p kc c", p=128)), "setupDp", writes=["Wr"])
            P.dma("sp", lambda e: e.dma_start(out=rb_bc[:, :], in_=b_rt[0:1, :].partition_broadcast(128)), "setupD", writes=["rb_bc"])
            P.dma("sp", lambda e: e.dma_start(out=g_bc[:, :], in_=g_ffn[0:1, :].partition_broadcast(128)), "setupD", writes=["g_bc"])
            P.bulk_done("setupD")
            P.bulk_done("setupDp")

            for t0 in range(0, NT, 2):
                tl = (t0, t0 + 1)
                for t in tl:
                    b = t % 2
                    P.dma("sp", lambda e, t=t, b=b: e.dma_start(out=xt2[b][:, :], in_=x[t * 128:(t + 1) * 128, :]), f"xt2_{b}", writes=[f"xt2_{b}"])
                for t in tl:
                    b = t % 2
                    ts_ = slice(t * 128, (t + 1) * 128)
                    for hf in range(2):
                        def f_o(e, ts_=ts_, hf=hf, b=b):
                            for k in range(8):
                                i = e.matmul(out=po4[b][hf][:, :], lhsT=mT[:, k, ts_], rhs=Wout[:, k, hf * 512:(hf + 1) * 512], start=(k == 0), stop=(k == 7))
                            return i
                        P.op("pe", f_o, reads=["mT", "Wout"], writes=[f"po{b}{hf}"])
                for t in tl:
                    b = t % 2
                    for hf in range(2):
                        P.op("dve", lambda e, hf=hf, b=b: e.tensor_tensor(out=xm[b][:, hf * 512:(hf + 1) * 512], in0=po4[b][hf][:, :], in1=xt2[b][:, hf * 512:(hf + 1) * 512], op=ADD),
                             reads=[f"po{b}{hf}", f"xt2_{b}"], writes=[f"xm{b}_{hf}"])
                for t in tl:
                    b = t % 2
                    xk = [f"xm{b}_0", f"xm{b}_1"]
                    P.dma("sp", lambda e, t=t, b=b: e.dma_start(out=xmid_scr[t * 128:(t + 1) * 128, :], in_=xm[b][:, :]), f"xmid{b}", reads=xk, writes=[f"xmid{t}"])
                    P.seq("act", [lambda e, b=b, t=t: e.activation(out=junk2[:, :], in_=xm[b][:, :], func=AF.Square, accum_out=ssq2[:, t:t + 1]),
                                  lambda e, t=t: e.activation(out=ssq2[:, t:t + 1], in_=ssq2[:, t:t + 1], func=AF.Sqrt, scale=1.0 / D, bias=EPS)],
                          reads=xk, writes=["junk2", f"ssq2_{t}"])
                for t in tl:
                    b = t % 2
                    xk = [f"xm{b}_0", f"xm{b}_1"]
                    P.op("dve", lambda e, t=t: e.reciprocal(out=rstd2[:, t:t + 1], in_=ssq2[:, t:t + 1]), reads=[f"ssq2_{t}"], writes=[f"rstd2_{t}"])
                    P.op("dve", lambda e, b=b, t=t: e.scalar_tensor_tensor(out=hb2[b][:, :], in0=xm[b][:, :], scalar=rstd2[:, t:t + 1], in1=g_bc[:, :], op0=MUL, op1=MUL),
                         reads=xk + [f"rstd2_{t}", "g_bc"], writes=[f"hb2_{b}"])
                for t in tl:
                    b = t % 2
                    def f_tp2(e, b=b):
                        for k in range(8):
                            i = e.transpose(out=tp2[b][:, k, :], in_=hb2[b][:, k * 128:(k + 1) * 128], identity=ident[:, :])
                        return i
                    P.op("pe", f_tp2, reads=[f"hb2_{b}", "ident"], writes=[f"tp2_{b}"])
                for t in tl:
                    b = t % 2
                    ts_ = slice(t * 128, (t + 1) * 128)
                    P.op("act", lambda e, ts_=ts_, b=b: e.copy(out=h2T[:, :, ts_], in_=tp2[b][:, :, :]), reads=[f"tp2_{b}"], writes=[f"h2T_{t // 4}"])
                for t in tl:
                    b = t % 2
                    ts_ = slice(t * 128, (t + 1) * 128)
                    def f_r(e, ts_=ts_, b=b):
                        for k in range(8):
                            i = e.matmul(out=pr2[b][:, :], lhsT=h2T[:, k, ts_], rhs=Wr[:, k, :], start=(k == 0), stop=(k == 7))
                        return i
                    P.op("pe", f_r, reads=[f"h2T_{t // 4}", "Wr"], writes=[f"pr{b}"])
                for t in tl:
                    b = t % 2
                    P.op("dve", lambda e, t=t, b=b: e.tensor_tensor(out=lg[:, t, :], in0=pr2[b][:, :], in1=rb_bc[:, :], op=ADD), reads=[f"pr{b}", "rb_bc"], writes=["lg"])

            BIG = 1.0e9
            bc4 = lambda a: a.unsqueeze(2).to_broadcast([128, NT, 4])
            bc32 = lambda a: a.unsqueeze(2).to_broadcast([128, NT, 32])
            gl = lg[:, :, 0:4]
            el = lg[:, :, 4:36]
            P.seq("dve", [
                lambda e: e.tensor_reduce(out=r_gmax[:, :], in_=gl, axis=AX.X, op=ALU.max),
                lambda e: e.tensor_tensor(out=r_a[:, :, :], in0=gl, in1=bc4(r_gmax[:, :]), op=SUB)], reads=["lg"], writes=["r_a", "r_gmax"])
            P.op("act", lambda e: e.activation(out=r_b[:, :, :], in_=r_a[:, :, :], func=AF.Exp), reads=["r_a"], writes=["r_b"])
            P.seq("dve", [
                lambda e: e.tensor_reduce(out=r_gw[:, :], in_=r_b[:, :, :], axis=AX.X, op=ADD),
                lambda e: e.reciprocal(out=r_gw[:, :], in_=r_gw[:, :]),
                lambda e: e.tensor_tensor(out=r_a[:, :, :], in0=gl, in1=bc4(r_gmax[:, :]), op=ALU.is_equal),
                lambda e: e.tensor_scalar(out=r_a[:, :, :], in0=r_a[:, :, :], scalar1=-1.0, scalar2=BIG, op0=ADD, op1=MUL),
                lambda e: e.tensor_tensor(out=r_el[:, :, :].rearrange("p t (g k) -> p t g k", k=8), in0=el.rearrange("p t (g k) -> p t g k", k=8),
                                          in1=r_a[:, :, :].unsqueeze(3).to_broadcast([128, NT, 4, 8]), op=ADD),
                lambda e: e.tensor_reduce(out=r_m1[:, :], in_=r_el[:, :, :], axis=AX.X, op=ALU.max),
                lambda e: e.tensor_tensor(out=r_oh1[:, :, :], in0=r_el[:, :, :], in1=bc32(r_m1[:, :]), op=ALU.is_equal),
                lambda e: e.scalar_tensor_tensor(out=r_t[:, :, :], in0=r_oh1[:, :, :], scalar=-BIG, in1=r_el[:, :, :], op0=MUL, op1=ADD),
                lambda e: e.tensor_reduce(out=r_m2[:, :], in_=r_t[:, :, :], axis=AX.X, op=ALU.max),
                lambda e: e.tensor_tensor(out=r_oh2[:, :, :], in0=r_t[:, :, :], in1=bc32(r_m2[:, :]), op=ALU.is_equal),
                lambda e: e.tensor_tensor(out=r_w1[:, :], in0=r_m1[:, :], in1=r_m2[:, :], op=SUB)],
                reads=["lg", "r_b", "r_a"], writes=["r_a", "router1"])
            P.op("act", lambda e: e.activation(out=r_w1[:, :], in_=r_w1[:, :], func=AF.Sigmoid), reads=["router1"], writes=["r_w1"])
            P.seq("dve", [
                lambda e: e.tensor_scalar(out=r_w2[:, :], in0=r_w1[:, :], scalar1=-1.0, scalar2=1.0, op0=MUL, op1=ADD),
                lambda e: e.tensor_tensor(out=r_w1[:, :], in0=r_w1[:, :], in1=r_gw[:, :], op=MUL),
                lambda e: e.tensor_tensor(out=r_w2[:, :], in0=r_w2[:, :], in1=r_gw[:, :], op=MUL),
                lambda e: e.tensor_tensor(out=r_oh1[:, :, :], in0=r_oh1[:, :, :], in1=bc32(r_w1[:, :]), op=MUL),
                lambda e: e.tensor_tensor(out=r_oh2[:, :, :], in0=r_oh2[:, :, :], in1=bc32(r_w2[:, :]), op=MUL),
                lambda e: e.tensor_tensor(out=cw[:, :, :], in0=r_oh1[:, :, :], in1=r_oh2[:, :, :], op=ADD)],
                reads=["router1", "r_w1"], writes=["cw", "router1"])
            if "cw" in dbg:
                tcw = dbgt("cw", [128, NT, 32])
                P.dma("sp", lambda e: e.dma_start(out=tcw[:, :, :], in_=cw[:, :, :]), "out", reads=["cw"])
            esD.close()
            P.barrier()

            NE = 32 if stage >= 5 else 0
            esE = ExitStack()
            esE.__enter__()
            sb = lambda name, shape, dt: esE.enter_context(nc.sbuf_tensor(name, shape, dt))
            ps = lambda name, shape, dt: esE.enter_context(nc.psum_tensor(name, shape, dt))
            yacc = sb("yacc", [128, NT, D], F32)
            Wg = [sb(f"Wg{i}", [128, 8, 512], BF16) for i in range(2)]
            Wu = [sb(f"Wu{i}", [128, 8, 512], BF16) for i in range(2)]
            Wd = [sb(f"Wd{i}", [128, 4, D], BF16) for i in range(2)]
            actT = sb("actT", [128, 4, NTOK], BF16)
            sgt = [sb(f"sgt{i}", [128, 512], F32) for i in range(2)]
            pg = [ps(f"pg{i}", [128, 512], F32) for i in range(2)]
            pu2 = [ps(f"pu2_{i}", [128, 512], F32) for i in range(2)]
            pd = [ps(f"pd{i}", [128, 512], F32) for i in range(2)]
            P.op("pool", lambda e: e.memset(yacc[:, :, :], 0.0), writes=[f"yacc{t}_{hf}" for t in range(NT) for hf in range(2)])
            h2T_all = [f"h2T_{i}" for i in range(4)]
            for ex in range(NE):
                s_ = ex % 2
                P.dma("pool", lambda e, ex=ex, s_=s_: e.dma_start(out=Wg[s_][:, :, :], in_=w_gate[ex].rearrange("(kc p) c -> p kc c", p=128)), f"Wg{s_}", writes=[f"Wg{s_}"])
                P.dma("pool", lambda e, ex=ex, s_=s_: e.dma_start(out=Wu[s_][:, :, :], in_=w_up[ex].rearrange("(kc p) c -> p kc c", p=128)), f"Wu{s_}", writes=[f"Wu{s_}"])
                P.dma("pool", lambda e, ex=ex, s_=s_: e.dma_start(out=Wd[s_][:, :, :], in_=w_down[ex].rearrange("(kc p) c -> p kc c", p=128)), f"Wd{s_}", writes=[f"Wd{s_}"])
                it = 0
                for tb in range(4):
                    tbs = slice(tb * 512, (tb + 1) * 512)
                    for m in range(4):
                        b_ = it % 2
                        it += 1
                        def f_gu(e, s_=s_, m=m, tbs=tbs, b_=b_):
                            for k in range(8):
                                e.matmul(out=pg[b_][:, :], lhsT=Wg[s_][:, k, m * 128:(m + 1) * 128], rhs=h2T[:, k, tbs], start=(k == 0), stop=(k == 7))
                            for k in range(8):
                                i = e.matmul(out=pu2[b_][:, :], lhsT=Wu[s_][:, k, m * 128:(m + 1) * 128], rhs=h2T[:, k, tbs], start=(k == 0), stop=(k == 7))
                            return i
                        P.op("pe", f_gu, reads=[f"Wg{s_}", f"Wu{s_}", f"h2T_{tb}"], writes=[f"pg{b_}", f"pu2_{b_}"])
                        P.op("act", lambda e, b_=b_: e.activation(out=sgt[b_][:, :], in_=pg[b_][:, :], func=AF.Silu), reads=[f"pg{b_}"], writes=[f"sgt{b_}"])
                        P.op("dve", lambda e, b_=b_, m=m, tbs=tbs: e.tensor_tensor(out=actT[:, m, tbs], in0=sgt[b_][:, :], in1=pu2[b_][:, :], op=MUL),
                             reads=[f"sgt{b_}", f"pu2_{b_}"], writes=[f"actT_{tb}"])
                it = 0
                for t in range(NT):
                    ts_ = slice(t * 128, (t + 1) * 128)
                    for hf in range(2):
                        b_ = it % 2
                        it += 1
                        def f_d(e, s_=s_, ts_=ts_, hf=hf, b_=b_):
                            for m in range(4):
                                i = e.matmul(out=pd[b_][:, :], lhsT=actT[:, m, ts_], rhs=Wd[s_][:, m, hf * 512:(hf + 1) * 512], start=(m == 0), stop=(m == 3))
                            return i
                        P.op("pe", f_d, reads=[f"Wd{s_}", f"actT_{t // 4}"], writes=[f"pd{b_}"])
                        P.op("dve", lambda e, t=t, hf=hf, b_=b_, ex=ex: e.scalar_tensor_tensor(out=yacc[:, t, hf * 512:(hf + 1) * 512], in0=pd[b_][:, :], scalar=cw[:, t, ex:ex + 1],
                                                                                                in1=yacc[:, t, hf * 512:(hf + 1) * 512], op0=MUL, op1=ADD),
                             reads=[f"pd{b_}", "cw", f"yacc{t}_{hf}"], writes=[f"yacc{t}_{hf}"])

            P.dma("sp", lambda e: e.dma_start(out=g_bc[:, :], in_=g_fin[0:1, :].partition_broadcast(128)), "setupF", writes=["g_bc"])
            for t in range(NT):
                s_ = t % 2
                P.dma("sp", lambda e, t=t, s_=s_: e.dma_start(out=xt2[s_][:, :], in_=xmid_scr[t * 128:(t + 1) * 128, :]), f"xt2_{s_}", reads=[f"xmid{t}"], writes=[f"xt2_{s_}"])
                P.op("dve", lambda e, t=t, s_=s_: e.tensor_tensor(out=xt2[s_][:, :], in0=xt2[s_][:, :], in1=yacc[:, t, :], op=ADD),
                     reads=[f"xt2_{s_}", f"yacc{t}_0", f"yacc{t}_1"], writes=[f"xt2_{s_}"])
                P.seq("act", [lambda e, s_=s_, t=t: e.activation(out=junk2[:, :], in_=xt2[s_][:, :], func=AF.Square, accum_out=ssq2[:, NT + t:NT + t + 1]),
                              lambda e, t=t: e.activation(out=ssq2[:, NT + t:NT + t + 1], in_=ssq2[:, NT + t:NT + t + 1], func=AF.Sqrt, scale=1.0 / D, bias=EPS)],
                      reads=[f"xt2_{s_}"], writes=["junk2", f"ssq2_{NT + t}"])
                P.op("dve", lambda e, t=t: e.reciprocal(out=rstd2[:, NT + t:NT + t + 1], in_=ssq2[:, NT + t:NT + t + 1]), reads=[f"ssq2_{NT + t}"], writes=[f"rstd2_{NT + t}"])
                P.op("dve", lambda e, s_=s_, t=t: e.scalar_tensor_tensor(out=xt2[s_][:, :], in0=xt2[s_][:, :], scalar=rstd2[:, NT + t:NT + t + 1], in1=g_bc[:, :], op0=MUL, op1=MUL),
                     reads=[f"xt2_{s_}", f"rstd2_{NT + t}", "g_bc"], writes=[f"xt2_{s_}"])
                P.dma("sp", lambda e, t=t, s_=s_: e.dma_start(out=y[t * 128:(t + 1) * 128, :], in_=xt2[s_][:, :]), "yout", reads=[f"xt2_{s_}"], writes=[f"y{t}"])
            esE.close()
            esDE.close()

        if "merged" in dbg:
            t = dbgt("merged", [1024, NTOK], BF16)
            nrow = 1024 if stage >= 3 else 512
            P.dma("sp", lambda e: e.dma_start(out=t[0:nrow, :], in_=mergedT_scr[0:nrow, :]), "out", reads=[f"mscr{h}" for h in range(nrow // 128)])
        if "hT" in dbg:
            t2 = dbgt("hT", [128, 8, HALO + NTOK], BF16)
            P.dma("sp", lambda e: e.dma_start(out=t2[:, :, :], in_=hT[:, :, :]), "out", reads=hT_all)
        if "qk" in dbg:
            t3 = dbgt("qs", [128, NTOK], BF16)
            t4 = dbgt("ks", [128, NTOK], BF16)
            t5 = dbgt("expb", [128, NTOK], F32)
            t6 = dbgt("e2", [128, NTOK], F32)
            P.dma("sp", lambda e: e.dma_start(out=t3[:, :], in_=qsT[:, :]), "out", reads=["qsT"])
            P.dma("sp", lambda e: e.dma_start(out=t4[:, :], in_=ksT[:, :]), "out", reads=["ksT"])
            P.dma("sp", lambda e: e.dma_start(out=t5[:, :], in_=expb[:, :]), "out", reads=expb_all)
            P.dma("sp", lambda e: e.dma_start(out=t6[:, :], in_=e2bc[:, :]), "out", reads=e2_all)
        if "xmid" in dbg:
            txm = dbgt("xmid", [NTOK, D])
            P.dma("sp", lambda e: e.dma_start(out=txm[:, :], in_=xmid_scr[:, :]), "out", reads=[f"xmid{t}" for t in range(NT)])
        P.final_wait("sp", ["out", "yout"])

        with nc.Block() as block:
            @block.tensor
            def _(e): P.replay("pe", e)
            @block.scalar
            def _(e): P.replay("act", e)
            @block.vector
            def _(e): P.replay("dve", e)
            @block.gpsimd
            def _(e): P.replay("pool", e)
            @block.sync
            def _(e): P.replay("sp", e)
    return nc, dbg_out


def make_in_maps(inputs):
    f = lambda a: np.ascontiguousarray(a, dtype=np.float32)
    x = inputs["x"]
    common = {
        "g_mix": f(inputs["norm_mix_g"].reshape(1, D)),
        "w_in": f(inputs["w_in"].reshape(D, 2568)),
        "conv_wT": f(inputs["conv_w"].reshape(4, 8, 128).transpose(2, 1, 0)),
        "conv_b": f(inputs["conv_b"].reshape(8, 128).T),
        "i_bias": f(inputs["i_bias"].reshape(1, 4)),
        "f_bias": f(inputs["f_bias"].reshape(1, 4)),
        "g_ml": f(inputs["mlstm_norm_g"].reshape(1, 512)),
    }
    lre = inputs["s5_lambda_re"].reshape(32, 64).T
    lim = inputs["s5_lambda_im"].reshape(32, 64).T
    bre = inputs["s5_b_re"].reshape(32, 64, 16).transpose(1, 0, 2)
    bim = inputs["s5_b_im"].reshape(32, 64, 16).transpose(1, 0, 2)
    cre = inputs["s5_c_re"].reshape(32, 16, 64).transpose(2, 0, 1)
    cim = inputs["s5_c_im"].reshape(32, 16, 64).transpose(2, 0, 1)
    glw = inputs["s5_glu_w"].reshape(4, 8, 16, 32)
    WA = np.zeros((4, 128, 128), np.float32)
    WB = np.zeros((4, 128, 128), np.float32)
    for u in range(4):
        for g in range(8):
            WA[u, g * 16:(g + 1) * 16, g * 16:(g + 1) * 16] = glw[u, g, :, 0:16]
            WB[u, g * 16:(g + 1) * 16, g * 16:(g + 1) * 16] = glw[u, g, :, 16:32]
    glb = inputs["s5_glu_b"].reshape(4, 8, 32)
    common.update({
        "lam2_re": f(np.concatenate([lre, lre], 0)), "lam2_im": f(np.concatenate([lim, lim], 0)),
        "logdt": f(inputs["s5_log_dt"].reshape(1, 32)),
        "X1d": f(np.concatenate([bre, bim], 0)), "X2d": f(np.concatenate([bim, bre], 0)),
        "CY1d": f(np.concatenate([cre, cim], 0)), "CY2d": f(np.concatenate([cim, cre], 0)),
        "s5d": f(inputs["s5_d"].reshape(1, 512)),
        "WAd": WA, "WBd": WB,
        "w_out": f(inputs["w_out"].reshape(D, D)), "g_ffn": f(inputs["norm_ffn_g"].reshape(1, D)),
        "w_rt": f(np.concatenate([inputs["router_group_w"].reshape(D, 4), inputs["router_expert_w"].reshape(D, 32)], 1)),
        "b_rt": f(np.concatenate([inputs["router_group_b"].reshape(1, 4), inputs["router_expert_b"].reshape(1, 32)], 1)),
        "w_gate": f(inputs["expert_w_gate"].reshape(32, D, 512)), "w_up": f(inputs["expert_w_up"].reshape(32, D, 512)),
        "w_down": f(inputs["expert_w_down"].reshape(32, 512, D)), "g_fin": f(inputs["norm_final_g"].reshape(1, D)),
        "gbad": f(glb[:, :, 0:16].reshape(4, 128).T), "gbbd": f(glb[:, :, 16:32].reshape(4, 128).T),
    })
    maps = []
    for c in range(NCORES):
        b, p = c // 4, c % 4
        m = dict(common)
        m["x"] = f(x[b, p * NTOK:(p + 1) * NTOK])
        if p == 0:
            m["xh"] = np.zeros((HALO, D), np.float32)
        else:
            m["xh"] = f(x[b, p * NTOK - HALO:p * NTOK])
        pmk = np.zeros((128, 4), np.float32)
        pmk[:, :p] = 1.0
        m["pmask"] = pmk
        maps.append(m)
    return maps


_CACHE = {}


def kernel(**inputs):
    if "nc" not in _CACHE:
        _CACHE["nc"] = build()[0]
    nc = _CACHE["nc"]
    in_maps = make_in_maps(inputs)
    res = run_bass_kernel_spmd(nc, in_maps, core_ids=list(range(NCORES)))
    out = np.empty((2, 4 * NTOK, D), np.float32)
    for c in range(NCORES):
        b, p = c // 4, c % 4
        out[b, p * NTOK:(p + 1) * NTOK] = np.asarray(res.results[c]["y"], dtype=np.float32)
    return out
```

```python
import numpy as np
from contextlib import ExitStack
import concourse.bass as bass
import concourse.mybir as mybir
from concourse.bass_utils import run_bass_kernel_spmd

F32 = mybir.dt.float32
BF16 = mybir.dt.bfloat16
AF = mybir.ActivationFunctionType
ALU = mybir.AluOpType

NTOK = 2048
NT = 16
NCH = 32
HALO = 32
D = 1024
EPS = 1e-6
NCORES = 8
GROUPS4 = [[0, 1, 2, 3], [4, 5, 6, 7]]
L5 = 32
LL = 5
NC5 = NTOK // L5
KC5 = L5 // 8
NLEV = 7


class Prog:
    def __init__(self, nc, es):
        self.nc = nc
        self.es = es
        self.queues = {e: [] for e in ("pe", "act", "dve", "pool", "sp")}
        self.esem = {}
        self.ecnt = {}
        for e in ("pe", "act", "dve", "pool"):
            self.esem[e] = es.enter_context(nc.semaphore("sem_" + e))
            self.ecnt[e] = 0
        self.dsem = {}
        self.dcnt = {}
        self.lastw = {}
        self.readers = {}
        self.known = {e: {} for e in self.queues}

    def _deps(self, reads, writes):
        deps = []
        for r in reads:
            if r in self.lastw:
                deps.append(self.lastw[r])
        for w in writes:
            if w in self.lastw:
                deps.append(self.lastw[w])
            deps.extend(self.readers.get(w, ()))
        return deps

    def _prune(self, eng, deps):
        need = {}
        for (s, v) in deps:
            if eng == "pe" and s is self.esem["pe"]:
                continue
            if v > need.get(s, 0):
                need[s] = v
        out = []
        kn = self.known[eng]
        for s, v in need.items():
            if kn.get(s, 0) >= v:
                continue
            kn[s] = v
            out.append((s, v))
        return out

    def _record(self, tok, reads, writes):
        for r in reads:
            self.readers.setdefault(r, []).append(tok)
        for w in writes:
            self.lastw[w] = tok
            self.readers[w] = []

    def op(self, eng, fn, reads=(), writes=()):
        reads = tuple(reads)
        writes = tuple(writes)
        waits = self._prune(eng, self._deps(reads, writes))
        self.ecnt[eng] += 1
        tok = (self.esem[eng], self.ecnt[eng])
        self.queues[eng].append((waits, fn, (self.esem[eng], 1)))
        self._record(tok, reads, writes)

    def seq(self, eng, fns, reads=(), writes=()):
        self._chain = getattr(self, "_chain", 0) + 1
        ck = f"__chain{self._chain}"
        for fn in fns:
            self.op(eng, fn, reads=tuple(reads) + (ck,), writes=tuple(writes) + (ck,))

    def dma(self, q, fn, stream, reads=(), writes=(), inc=16):
        reads = tuple(reads)
        writes = tuple(writes)
        if stream not in self.dsem:
            self.dsem[stream] = self.es.enter_context(self.nc.semaphore("dsem_" + stream))
            self.dcnt[stream] = 0
        waits = self._prune(q, self._deps(reads, writes))
        self.dcnt[stream] += inc
        tok = (self.dsem[stream], self.dcnt[stream])
        self.queues[q].append((waits, fn, (self.dsem[stream], inc)))
        self._record(tok, reads, writes)

    def bulk_done(self, stream):
        s = self.dsem[stream]
        fin = self.dcnt[stream]
        for k, (ss, v) in list(self.lastw.items()):
            if ss is s:
                self.lastw[k] = (s, fin)

    def barrier(self):
        allw = [(self.esem[e], self.ecnt[e]) for e in self.esem if self.ecnt[e] > 0]
        allw += [(self.dsem[s], self.dcnt[s]) for s in self.dsem]
        for e in self.queues:
            w = self._prune(e, allw)
            if w:
                self.queues[e].append((w, None, None))

    def final_wait(self, q, streams):
        waits = [(self.dsem[st], self.dcnt[st]) for st in streams if st in self.dsem]
        self.queues[q].append((waits, None, None))

    def replay(self, eng, h):
        for waits, fn, inc in self.queues[eng]:
            for (s, v) in waits:
                h.wait_ge(s, v)
            if fn is None:
                continue
            inst = fn(h)
            inst.then_inc(inc[0], inc[1])


def build(stage=99, dbg=()):
    nc = bass.Bass("TRN2", target_bir_lowering=False)
    din = lambda name, shape, dt=F32: nc.dram_tensor(name, shape, dt, kind="ExternalInput")
    x = din("x", [NTOK, D])
    xh = din("xh", [HALO, D])
    g_mix = din("g_mix", [1, D])
    w_in = din("w_in", [D, 2568])
    conv_wT = din("conv_wT", [128, 8, 4])
    conv_b = din("conv_b", [128, 8])
    i_bias = din("i_bias", [1, 4])
    f_bias = din("f_bias", [1, 4])
    g_ml = din("g_ml", [1, 512])
    pmask = din("pmask", [128, 4])
    lam2_re = din("lam2_re", [128, 32])
    lam2_im = din("lam2_im", [128, 32])
    logdt = din("logdt", [1, 32])
    X1d = din("X1d", [128, 32, 16])
    X2d = din("X2d", [128, 32, 16])
    CY1d = din("CY1d", [128, 32, 16])
    CY2d = din("CY2d", [128, 32, 16])
    s5d = din("s5d", [1, 512])
    WAd = din("WAd", [4, 128, 128])
    WBd = din("WBd", [4, 128, 128])
    gbad = din("gbad", [128, 4])
    gbbd = din("gbbd", [128, 4])
    w_out = din("w_out", [D, D])
    g_ffn = din("g_ffn", [1, D])
    w_rt = din("w_rt", [D, 36])
    b_rt = din("b_rt", [1, 36])
    if stage >= 5:
        w_gate = din("w_gate", [32, D, 512])
        w_up = din("w_up", [32, D, 512])
        w_down = din("w_down", [32, 512, D])
    g_fin = din("g_fin", [1, D])
    y = nc.dram_tensor("y", [NTOK, D], F32, kind="ExternalOutput")
    dbg_out = {}
    def dbgt(name, shape, dt=F32):
        dbg_out[name] = nc.dram_tensor("dbg_" + name, shape, dt, kind="ExternalOutput")
        return dbg_out[name]

    mergedT_scr = nc.dram_tensor("mergedT_scr", [1024, NTOK], BF16)
    cc_in = [nc.dram_tensor(f"cc_in{h}", [128, 130], F32) for h in range(4)]
    cc_out = [nc.dram_tensor(f"cc_out{h}", [512, 130], F32) for h in range(4)]
    u_scr = nc.dram_tensor("u_scr", [NTOK, 512], BF16)
    xmid_scr = nc.dram_tensor("xmid_scr", [NTOK, D], F32)
    prep_scr = {n: nc.dram_tensor("prep_" + n, [128, 32, k], F32) for n, k in (("PWrr", L5), ("PWri", L5), ("PWbr", 8), ("PWbi", 8), ("BB1", 16), ("BB2", 16),
                                                                             ("PRs", L5 + 1), ("nPI", L5 + 1), ("sqr", 12), ("sqi", 12))}
    ccs_in = [nc.dram_tensor(f"ccs_in{h}", [128, 8], F32) for h in range(4)]
    ccs_out = [nc.dram_tensor(f"ccs_out{h}", [512, 8], F32) for h in range(4)]

    w_in_v = w_in.ap().rearrange("(kc p) c -> p kc c", p=128)

    es = ExitStack()
    with es:
        P = Prog(nc, es)
        sb = lambda name, shape, dt: es.enter_context(nc.sbuf_tensor(name, shape, dt))
        ps = lambda name, shape, dt: es.enter_context(nc.psum_tensor(name, shape, dt))

        g_bc = sb("g_bc", [128, D], F32)
        ident = sb("ident", [128, 128], BF16)
        identf = sb("identf", [128, 128], F32)
        mslab = sb("mslab", [128, NTOK], BF16)
        pm = sb("pm", [128, 4], F32)
        esH = ExitStack()
        esH.__enter__()
        hT = esH.enter_context(nc.sbuf_tensor("hT", [128, 8, HALO + NTOK], BF16))
        esB = ExitStack()
        esB.__enter__()
        sb = lambda name, shape, dt: esB.enter_context(nc.sbuf_tensor(name, shape, dt))
        ps = lambda name, shape, dt: esB.enter_context(nc.psum_tensor(name, shape, dt))
        xt = [sb(f"xt{i}", [128, D], F32) for i in range(2)]
        junk = sb("junk", [128, D], BF16)
        hb = sb("hb", [128, D], BF16)
        ssq = sb("ssq", [128, NT + 1], F32)
        rstd = sb("rstd", [128, NT + 1], F32)
        wh = sb("wh", [128, 8, 512], BF16)
        wg = sb("wg", [128, 8, 33], BF16)
        raw = sb("raw", [128, NTOK + 3], F32)
        ctmp = sb("ctmp", [128, NTOK], F32)
        qsT = sb("qsT", [128, NTOK], BF16)
        ksT = sb("ksT", [128, NTOK], BF16)
        cwt = sb("cwt", [128, 8, 4], F32)
        cbt = sb("cbt", [128, 8], F32)
        fbt = sb("fbt", [33, 4], F32)
        nfb = sb("nfb", [33, 4], F32)
        Gt = sb("Gt", [33, NTOK], F32)
        Gt2 = sb("Gt2", [33, NTOK], F32)
        cmask = sb("cmask", [33, NTOK], F32)
        sel0 = sb("sel0", [33, 128], F32)
        sel032 = sb("sel032", [33, 128], F32)
        expb = sb("expb", [128, NTOK], F32)
        e2bc = sb("e2bc", [128, NTOK], F32)
        v_ext = sb("v_ext", [64, NCH, 130], BF16)
        sigo = sb("sigo", [64, NCH, 128], BF16)
        gA = sb("gA", [64, 512], F32)
        kstok2 = sb("kstok2", [64, 2, 128], BF16)
        CTloc = sb("CTloc", [128, NCH + 1, 130], F32)
        CTball = sb("CTball", [128, NCH, 130], BF16)
        CTt = sb("CTt", [128, 130], F32)
        cg = sb("cg", [128, 4, 130], F32)
        hacc = sb("hacc", [128, 130], F32)
        hacc2 = sb("hacc2", [128, 130], F32)
        maskST = sb("maskST", [64, 64], F32)
        STb = sb("STb", [64, 4, 64], BF16)
        dnb = sb("dnb", [64, 4, 4], F32)
        hnb = sb("hnb", [64, 4, 128], F32)
        oab = sb("oab", [64, 4, 128], BF16)
        junkb = sb("junkb", [64, 4, 128], BF16)
        so32t = sb("so32t", [64, 2, 128], F32)
        so32b = [so32t[:, 0, :], so32t[:, 1, :]]

        Bk = [ps(f"bank{k}", [128, 512], F32) for k in range(8)]
        bfv = lambda k: Bk[k][:, :].bitcast(BF16)
        ps_big = [Bk[0], Bk[1]]
        tp = bfv(2).rearrange("p (a b) -> p a b", b=128)
        pvb = [Bk[3][0:64, 0:256], Bk[4][0:64, 0:256]]
        pab = [Bk[0][0:64, 0:64], Bk[1][0:64, 0:64]]
        pcb = [Bk[3][0:64, 0:130], Bk[4][0:64, 0:130]]
        pub = [Bk[5][:, 0:130], Bk[6][:, 0:130]]
        ptk = [bfv(2)[0:64, 0:128], bfv(7)[0:64, 0:128]]
        pto = [bfv(6)[:, 0:64], bfv(7)[:, 0:64]]
        KPV = ["bank3", "bank4"]; KPA = ["bank0", "bank1"]; KPC = ["bank3", "bank4"]; KPU = ["bank5", "bank6"]
        KPTK = ["bank2", "bank7"]; KPTO = ["bank6", "bank7"]

        P.dma("sp", lambda e: e.dma_start(out=g_bc[:, :], in_=g_mix[0:1, :].partition_broadcast(128)), "setup", writes=["g_bc"])
        P.dma("sp", lambda e: e.dma_start(out=cwt[:, :, :], in_=conv_wT[:, :, :]), "setup", writes=["cwt"])
        P.dma("sp", lambda e: e.dma_start(out=cbt[:, :], in_=conv_b[:, :]), "setup", writes=["cbt"])
        P.dma("sp", lambda e: e.dma_start(out=fbt[0:1, :], in_=f_bias[0:1, :]), "setup", writes=["fbt0"])
        P.dma("sp", lambda e: e.dma_start(out=fbt[32:33, :], in_=i_bias[0:1, :]), "setup", writes=["fbt32"])
        P.dma("sp", lambda e: e.dma_start(out=gA[:, :], in_=g_ml[0:1, :].partition_broadcast(64)), "setup", writes=["gA"])
        P.dma("sp", lambda e: e.dma_start(out=pm[:, :], in_=pmask[:, :]), "setup", writes=["pm"])
        P.bulk_done("setup")

        P.seq("pool", [lambda e: e.memset(identf[:, :], 0.0),
                       lambda e: e.affine_select(out=identf[:, :], in_=identf[:, :], pattern=[[-1, 128]], compare_op=ALU.not_equal,
                                                 fill=1.0, base=0, channel_multiplier=1)], writes=["identf"])
        P.op("dve", lambda e: e.tensor_copy(out=ident[:, :], in_=identf[:, :]), reads=["identf"], writes=["ident"])

        P.seq("pool", [lambda e: e.memset(cmask[:, :], 1.0),
                       lambda e: e.memset(cmask[:, 0:NTOK:64], 0.0),
                       lambda e: e.memset(sel0[:, :], 0.0),
                       lambda e: e.memset(sel0[0:1, :], 1.0),
                       lambda e: e.memset(sel032[:, :], 0.0),
                       lambda e: e.memset(sel032[0:1, :], 1.0),
                       lambda e: e.memset(sel032[32:33, :], 1.0),
                       lambda e: e.memset(Gt2[:, :], 0.0),
                       lambda e: e.memset(v_ext[:, :, 128:129], 1.0),
                       lambda e: e.memset(v_ext[:, :, 129:130], 0.0),
                       lambda e: e.memset(maskST[:, :], 1.0),
                       lambda e: e.affine_select(out=maskST[:, :], in_=maskST[:, :], pattern=[[1, 64]], compare_op=ALU.is_ge,
                                                 fill=0.0, base=0, channel_multiplier=-1)],
              writes=["cmask", "sel0", "sel032", "Gt2", "v_ext_c", "maskST"])
        P.op("pool", lambda e: e.tensor_scalar(out=nfb[0:1, :], in0=fbt[0:1, :], scalar1=-1.0, scalar2=None, op0=ALU.mult),
             reads=["fbt0"], writes=["nfb"])

        def norm_tile(src_ap, rows, slot, col0, idx, tag):
            s = slot
            P.dma("sp", lambda e: e.dma_start(out=xt[s][0:rows, :], in_=src_ap), f"xt{s}", writes=[f"xt{s}"])
            P.op("act", lambda e: e.activation(out=junk[0:rows, :], in_=xt[s][0:rows, :], func=AF.Square, accum_out=ssq[0:rows, idx:idx + 1]),
                 reads=[f"xt{s}"], writes=["junk", f"ssq{idx}"])
            P.op("act", lambda e: e.activation(out=ssq[0:rows, idx:idx + 1], in_=ssq[0:rows, idx:idx + 1], func=AF.Sqrt, scale=1.0 / D, bias=EPS),
                 reads=[f"ssq{idx}"], writes=[f"ssq{idx}"])
            P.op("dve", lambda e: e.reciprocal(out=rstd[0:rows, idx:idx + 1], in_=ssq[0:rows, idx:idx + 1]), reads=[f"ssq{idx}"], writes=[f"rstd{idx}"])
            P.op("dve", lambda e: e.scalar_tensor_tensor(out=hb[0:rows, :], in0=xt[s][0:rows, :], scalar=rstd[0:rows, idx:idx + 1], in1=g_bc[0:rows, :],
                                                         op0=ALU.mult, op1=ALU.mult),
                 reads=[f"xt{s}", f"rstd{idx}", "g_bc"], writes=["hb"])
            def f_tp(e):
                for k in range(8):
                    i = e.transpose(out=tp[:, k, 0:rows], in_=hb[0:rows, k * 128:(k + 1) * 128], identity=ident[0:rows, 0:rows])
                return i
            P.op("pe", f_tp, reads=["hb", "ident"], writes=["bank2"])
            P.op("act", lambda e: e.copy(out=hT[:, :, col0:col0 + rows], in_=tp[:, :, 0:rows]), reads=["bank2"], writes=[tag])

        norm_tile(xh[:, :], HALO, 0, 0, NT, "hT_h")
        for t in range(NT):
            norm_tile(x[t * 128:(t + 1) * 128, :], 128, (t + 1) % 2, HALO + t * 128, t, f"hT_{t // 4}")
        hT_all = ["hT_h"] + [f"hT_{i}" for i in range(4)]

        SC = float(128 ** -0.5)
        for hd in range(4 if stage >= 2 else 0):
            for j, c0 in enumerate((hd * 128, 512 + hd * 128, 1024 + hd * 128, 1536 + hd * 128)):
                P.dma("pool", lambda e, j=j, c0=c0: e.dma_start(out=wh[:, :, j * 128:(j + 1) * 128], in_=w_in_v[:, :, c0:c0 + 128]),
                      "wh", writes=[f"wh{j}"])
            P.bulk_done("wh")
            P.op("pool", lambda e: e.memset(wg[:, :, :], 0.0), writes=["wg", "wg_a", "wg_b"])
            def ld_wg(e, dst, col):
                with nc.allow_non_contiguous_dma(reason="gate columns"):
                    return e.dma_start(out=wg[:, :, dst:dst + 1], in_=w_in_v[:, :, col:col + 1])
            P.dma("pool", lambda e, hd=hd: ld_wg(e, 0, 2052 + hd), "wg", reads=["wg"], writes=["wg_a"])
            P.dma("pool", lambda e, hd=hd: ld_wg(e, 32, 2048 + hd), "wg", reads=["wg"], writes=["wg_b"])
            P.bulk_done("wg")

            for tb in range(4):
                pgt = ps_big[tb % 2]
                def f_g(e, tb=tb, pgt=pgt):
                    for k in range(8):
                        i = e.matmul(out=pgt[0:33, :], lhsT=wg[:, k, :], rhs=hT[:, k, HALO + tb * 512:HALO + (tb + 1) * 512], start=(k == 0), stop=(k == 7))
                    return i
                P.op("pe", f_g, reads=["wg", "wg_a", "wg_b", f"hT_{tb}"], writes=[f"bank{tb % 2}"])
                P.op("act", lambda e, tb=tb, pgt=pgt: e.copy(out=Gt[0:33, tb * 512:(tb + 1) * 512], in_=pgt[0:33, :]),
                     reads=[f"bank{tb % 2}"], writes=[f"Gt_{tb}"])
            Gt_all = [f"Gt_{tb}" for tb in range(4)]
            P.op("act", lambda e, hd=hd: e.activation(out=Gt[0:1, :], in_=Gt[0:1, :], func=AF.Exp, scale=-1.0, bias=nfb[0:1, hd:hd + 1]),
                 reads=Gt_all + ["nfb"], writes=["Gt_f"])
            P.op("act", lambda e: e.activation(out=Gt[0:1, :], in_=Gt[0:1, :], func=AF.Ln, bias=1.0), reads=["Gt_f"], writes=["Gt_f"])
            P.op("dve", lambda e: e.tensor_tensor_scan(out=Gt2[0:1, :], data0=cmask[0:1, :], data1=Gt[0:1, :], initial=0.0, op0=ALU.mult, op1=ALU.add),
                 reads=["Gt_f", "cmask"], writes=["Gt2_0"])
            P.op("act", lambda e, hd=hd: e.activation(out=Gt2[32:33, :], in_=Gt[32:33, :], func=AF.Identity, bias=fbt[32:33, hd:hd + 1]),
                 reads=Gt_all + ["fbt32", "Gt2"], writes=["Gt2_32"])
            for tb in range(4):
                pgt = ps_big[tb % 2]
                P.op("pe", lambda e, tb=tb, pgt=pgt: e.matmul(out=pgt[:, :], lhsT=sel0[0:33, :], rhs=Gt2[0:33, tb * 512:(tb + 1) * 512], start=True, stop=True),
                     reads=["sel0", "Gt2", "Gt2_0", "Gt2_32"], writes=[f"bank{tb % 2}"])
                P.op("act", lambda e, tb=tb, pgt=pgt: e.activation(out=expb[:, tb * 512:(tb + 1) * 512], in_=pgt[:, :], func=AF.Exp, scale=-1.0),
                     reads=[f"bank{tb % 2}"], writes=[f"expb_{tb}"])
            for tb in range(4):
                pgt = ps_big[tb % 2]
                P.op("pe", lambda e, tb=tb, pgt=pgt: e.matmul(out=pgt[:, :], lhsT=sel032[0:33, :], rhs=Gt2[0:33, tb * 512:(tb + 1) * 512], start=True, stop=True),
                     reads=["sel032", "Gt2", "Gt2_0", "Gt2_32"], writes=[f"bank{tb % 2}"])
                P.op("act", lambda e, tb=tb, pgt=pgt: e.activation(out=e2bc[:, tb * 512:(tb + 1) * 512], in_=pgt[:, :], func=AF.Exp),
                     reads=[f"bank{tb % 2}"], writes=[f"e2bc_{tb}"])
            expb_all = [f"expb_{tb}" for tb in range(4)]
            e2_all = [f"e2bc_{tb}" for tb in range(4)]

            for qi in range(2):
                cidx = qi * 4 + hd
                def f_halo(e, qi=qi):
                    for k in range(8):
                        i = e.matmul(out=ps_big[0][:, 0:HALO], lhsT=wh[:, k, qi * 128:(qi + 1) * 128], rhs=hT[:, k, 0:HALO], start=(k == 0), stop=(k == 7))
                    return i
                P.op("pe", f_halo, reads=[f"wh{qi}", "hT_h"], writes=["bank0"])
                P.op("act", lambda e: e.copy(out=raw[:, 0:3], in_=ps_big[0][:, HALO - 3:HALO]), reads=["bank0"], writes=["raw_h"])
                for tb in range(4):
                    pgt = ps_big[(tb + 1) % 2]
                    def f_q(e, tb=tb, qi=qi, pgt=pgt):
                        for k in range(8):
                            i = e.matmul(out=pgt[:, :], lhsT=wh[:, k, qi * 128:(qi + 1) * 128], rhs=hT[:, k, HALO + tb * 512:HALO + (tb + 1) * 512],
                                         start=(k == 0), stop=(k == 7))
                        return i
                    P.op("pe", f_q, reads=[f"wh{qi}", f"hT_{tb}"], writes=[f"bank{(tb + 1) % 2}"])
                    P.op("act", lambda e, tb=tb, pgt=pgt: e.copy(out=raw[:, 3 + tb * 512:3 + (tb + 1) * 512], in_=pgt[:, :]),
                         reads=[f"bank{(tb + 1) % 2}"], writes=[f"raw_{tb}"])
                raw_all = ["raw_h"] + [f"raw_{tb}" for tb in range(4)]
                fl = [lambda e, cidx=cidx: e.tensor_scalar(out=ctmp[:, :], in0=raw[:, 0:NTOK], scalar1=cwt[:, cidx, 0:1], scalar2=cbt[:, cidx:cidx + 1],
                                                           op0=ALU.mult, op1=ALU.add)]
                for j in (1, 2, 3):
                    fl.append(lambda e, cidx=cidx, j=j: e.scalar_tensor_tensor(out=ctmp[:, :], in0=raw[:, j:j + NTOK], scalar=cwt[:, cidx, j:j + 1],
                                                                               in1=ctmp[:, :], op0=ALU.mult, op1=ALU.add))
                P.seq("dve", fl, reads=raw_all + ["cwt", "cbt"], writes=["ctmp"])
                P.op("act", lambda e: e.activation(out=ctmp[:, :], in_=ctmp[:, :], func=AF.Silu), reads=["ctmp"], writes=["ctmp"])
                if qi == 0:
                    P.op("dve", lambda e: e.tensor_tensor(out=qsT[:, :], in0=ctmp[:, :], in1=expb[:, :], op=ALU.mult),
                         reads=["ctmp"] + expb_all, writes=["qsT"])
                else:
                    P.op("dve", lambda e: e.scalar_tensor_tensor(out=ksT[:, :], in0=ctmp[:, :], scalar=SC, in1=e2bc[:, :], op0=ALU.mult, op1=ALU.mult),
                         reads=["ctmp"] + e2_all, writes=["ksT"])

            for c0 in range(0, NCH, 2):
                for c in (c0, c0 + 1):
                    b = c % 2
                    def f_v(e, c=c, b=b):
                        for k in range(8):
                            i = e.matmul(out=pvb[b], lhsT=hT[:, k, HALO + c * 64:HALO + (c + 1) * 64], rhs=wh[:, k, 256:512], start=(k == 0), stop=(k == 7))
                        return i
                    P.op("pe", f_v, reads=["wh2", "wh3", f"hT_{c // 8}"], writes=[KPV[b]])
                for c in (c0, c0 + 1):
                    b = c % 2
                    P.op("act", lambda e, c=c, b=b: e.copy(out=v_ext[:, c, 0:128], in_=pvb[b][:, 0:128]), reads=[KPV[b]], writes=[f"v_{c}"])
                    P.op("act", lambda e, b=b: e.activation(out=so32b[b], in_=pvb[b][:, 128:256], func=AF.Sigmoid), reads=[KPV[b]], writes=[f"so32_{b}"])
                for c in (c0, c0 + 1):
                    b = c % 2
                    P.op("dve", lambda e, c=c, hd=hd, b=b: e.tensor_tensor(out=sigo[:, c, :], in0=so32b[b], in1=gA[:, hd * 128:(hd + 1) * 128], op=ALU.mult),
                         reads=[f"so32_{b}", "gA"], writes=[f"sigo_{c}"])

            P.seq("pool", [lambda e: e.memset(CTloc[:, 0, :], 0.0), lambda e: e.memset(CTloc[:, 0, 129:130], 1.0)], writes=["CTloc_0"])
            for c in range(NCH):
                b = c % 2
                P.op("pe", lambda e, c=c, b=b: e.transpose(out=ptk[b], in_=ksT[:, c * 64:(c + 1) * 64], identity=ident[:, :]),
                     reads=["ksT", "ident"], writes=[KPTK[b]])
                P.op("act", lambda e, b=b: e.copy(out=kstok2[:, b, :], in_=ptk[b]), reads=[KPTK[b]], writes=[f"kstok{b}"])
                P.op("pe", lambda e, c=c, b=b: e.matmul(out=pub[b], lhsT=kstok2[:, b, :], rhs=v_ext[:, c, :], start=True, stop=True),
                     reads=[f"kstok{b}", f"v_{c}", "v_ext_c"], writes=[KPU[b]])
                P.op("dve", lambda e, c=c, b=b: e.tensor_tensor(out=CTt[:, :], in0=pub[b], in1=CTloc[:, c, :], op=ALU.add),
                     reads=[KPU[b], f"CTloc_{c}"], writes=["CTt"])
                P.op("dve", lambda e, c=c: e.tensor_scalar(out=CTloc[:, c + 1, :], in0=CTt[:, :], scalar1=expb[:, c * 64 + 63:c * 64 + 64], scalar2=None, op0=ALU.mult),
                     reads=["CTt", f"expb_{c // 8}"], writes=[f"CTloc_{c + 1}"])

            P.dma("sp", lambda e, hd=hd: e.dma_start(out=cc_in[hd][:, :], in_=CTloc[:, NCH, :]), f"ccin{hd}", reads=[f"CTloc_{NCH}"], writes=[f"cc_in{hd}"])
            def f_cc(e, hd=hd):
                return e.collective_compute("AllGather", ALU.bypass, replica_groups=GROUPS4,
                                            ins=[cc_in[hd].ap().opt()], outs=[cc_out[hd].ap().opt()])
            P.dma("pool", f_cc, f"cc{hd}", reads=[f"cc_in{hd}"], writes=[f"cc_out{hd}"], inc=1)
            P.dma("sp", lambda e, hd=hd: e.dma_start(out=cg[:, :, :], in_=cc_out[hd].ap().rearrange("(r p) c -> p r c", p=128)),
                  f"ccld{hd}", reads=[f"cc_out{hd}"], writes=["cg"])
            P.op("pool", lambda e: e.memset(hacc[:, :], 0.0), writes=["hacc"])
            for pp in range(3):
                P.seq("dve", [
                    lambda e, pp=pp: e.scalar_tensor_tensor(out=hacc2[:, :], in0=hacc[:, :], scalar=cg[:, pp, 129:130], in1=cg[:, pp, :], op0=ALU.mult, op1=ALU.add),
                    lambda e: e.tensor_tensor(out=hacc2[:, :], in0=hacc2[:, :], in1=hacc[:, :], op=ALU.subtract),
                    lambda e, pp=pp: e.scalar_tensor_tensor(out=hacc[:, :], in0=hacc2[:, :], scalar=pm[:, pp:pp + 1], in1=hacc[:, :], op0=ALU.mult, op1=ALU.add)],
                    reads=["hacc", "cg", "pm"], writes=["hacc", "hacc2"])
            for c in range(NCH):
                eng = "dve"
                P.op(eng, lambda e, c=c: e.scalar_tensor_tensor(out=CTball[:, c, :], in0=hacc[:, :], scalar=CTloc[:, c, 129:130], in1=CTloc[:, c, :],
                                                                op0=ALU.mult, op1=ALU.add),
                     reads=["hacc", f"CTloc_{c}"], writes=[f"CTb_{c}"])

            NB = 2
            for c0 in range(0, NCH, NB):
                cl = list(range(c0, c0 + NB))
                for c in cl:
                    b = c % NB
                    cs = slice(c * 64, (c + 1) * 64)
                    P.op("pe", lambda e, cs=cs, b=b: e.matmul(out=pab[b], lhsT=ksT[:, cs], rhs=qsT[:, cs], start=True, stop=True),
                         reads=["ksT", "qsT"], writes=[KPA[b]])
                for c in cl:
                    b = c % NB
                    P.op("dve", lambda e, b=b: e.tensor_tensor(out=STb[:, b, :], in0=pab[b], in1=maskST[:, :], op=ALU.mult), reads=[KPA[b], "maskST"], writes=[f"ST{b}"])
                for c in cl:
                    b = c % NB
                    cs = slice(c * 64, (c + 1) * 64)
                    def f_cn(e, c=c, cs=cs, b=b):
                        e.matmul(out=pcb[b], lhsT=STb[:, b, :], rhs=v_ext[:, c, :], start=True, stop=False)
                        return e.matmul(out=pcb[b], lhsT=qsT[:, cs], rhs=CTball[:, c, :], start=False, stop=True)
                    P.op("pe", f_cn, reads=[f"ST{b}", f"v_{c}", "v_ext_c", "qsT", f"CTb_{c}"], writes=[KPC[b]])
                for c in cl:
                    b = c % NB
                    P.seq("dve", [
                        lambda e, b=b: e.tensor_copy(out=dnb[:, b, 1:2], in_=pcb[b][:, 128:129]),
                        lambda e, b=b: e.scalar_tensor_tensor(out=dnb[:, b, 0:1], in0=dnb[:, b, 1:2], scalar=-1.0, in1=dnb[:, b, 1:2], op0=ALU.mult, op1=ALU.max),
                        lambda e, b=b: e.tensor_scalar(out=dnb[:, b, 0:1], in0=dnb[:, b, 0:1], scalar1=1.0, scalar2=None, op0=ALU.max),
                        lambda e, b=b: e.reciprocal(out=dnb[:, b, 1:2], in_=dnb[:, b, 0:1]),
                        lambda e, b=b: e.tensor_scalar(out=hnb[:, b, :], in0=pcb[b][:, 0:128], scalar1=dnb[:, b, 1:2], scalar2=None, op0=ALU.mult)],
                        reads=[KPC[b]], writes=[f"dn{b}", f"hn{b}"])
                for c in cl:
                    b = c % NB
                    P.seq("act", [
                        lambda e, b=b: e.activation(out=junkb[:, b, :], in_=hnb[:, b, :], func=AF.Square, accum_out=dnb[:, b, 2:3]),
                        lambda e, b=b: e.activation(out=dnb[:, b, 2:3], in_=dnb[:, b, 2:3], func=AF.Sqrt, scale=1.0 / 128, bias=EPS)],
                        reads=[f"hn{b}", f"dn{b}"], writes=[f"junk{b}", f"dn2_{b}"])
                for c in cl:
                    b = c % NB
                    P.seq("dve", [
                        lambda e, b=b: e.reciprocal(out=dnb[:, b, 3:4], in_=dnb[:, b, 2:3]),
                        lambda e, c=c, b=b: e.scalar_tensor_tensor(out=oab[:, b, :], in0=hnb[:, b, :], scalar=dnb[:, b, 3:4], in1=sigo[:, c, :], op0=ALU.mult, op1=ALU.mult)],
                        reads=[f"dn2_{b}", f"hn{b}", f"sigo_{c}"], writes=[f"oa{b}", f"dn{b}"])
                for c in cl:
                    b = c % NB
                    P.op("pe", lambda e, b=b: e.transpose(out=pto[b], in_=oab[:, b, :], identity=ident[0:64, 0:64]), reads=[f"oa{b}", "ident"], writes=[KPTO[b]])
                for c in cl:
                    b = c % NB
                    cs = slice(c * 64, (c + 1) * 64)
                    P.op("act", lambda e, cs=cs, b=b: e.copy(out=mslab[:, cs], in_=pto[b]), reads=[KPTO[b]], writes=["mslab"])
            P.dma("sp", lambda e, hd=hd: e.dma_start(out=mergedT_scr[hd * 128:(hd + 1) * 128, :], in_=mslab[:, :]), "mscr", reads=["mslab"], writes=[f"mscr{hd}"])

        esB.close()
        P.barrier()
        if stage >= 3:
            esC1 = ExitStack()
            esC1.__enter__()
            sb = lambda name, shape, dt: esC1.enter_context(nc.sbuf_tensor(name, shape, dt))
            ps = lambda name, shape, dt: esC1.enter_context(nc.psum_tensor(name, shape, dt))
            wu = sb("wu", [128, 8, 512], BF16)
            utok = [sb(f"utok{i}", [128, 512], BF16) for i in range(2)]
            pu1 = [ps(f"pu1_{i}", [128, 512], F32) for i in range(2)]
            P.dma("pool", lambda e: e.dma_start(out=wu[:, :, :], in_=w_in_v[:, :, 2056:2568]), "wu", writes=["wu"])
            for t in range(NT):
                def f_u1(e, t=t):
                    for k in range(8):
                        i = e.matmul(out=pu1[t % 2][:, :], lhsT=hT[:, k, HALO + t * 128:HALO + (t + 1) * 128], rhs=wu[:, k, :], start=(k == 0), stop=(k == 7))
                    return i
                P.op("pe", f_u1, reads=["wu", f"hT_{t // 4}"], writes=[f"pu1_{t % 2}"])
                P.op("act", lambda e, t=t: e.copy(out=utok[t % 2][:, :], in_=pu1[t % 2][:, :]), reads=[f"pu1_{t % 2}"], writes=[f"utok{t % 2}"])
                P.dma("sp", lambda e, t=t: e.dma_start(out=u_scr[t * 128:(t + 1) * 128, :], in_=utok[t % 2][:, :]), f"uscr{t % 2}",
                      reads=[f"utok{t % 2}"], writes=[f"u_scr{t}"])
            esC1.close()
            esH.close()
            P.barrier()

            TT = lambda o, a, b, op: (lambda e: e.tensor_tensor(out=o, in0=a, in1=b, op=op))
            TS = lambda o, a, s1, s2, op0, op1=None: ((lambda e: e.tensor_scalar(out=o, in0=a, scalar1=s1, scalar2=s2, op0=op0, op1=op1)) if op1 is not None
                                                      else (lambda e: e.tensor_scalar(out=o, in0=a, scalar1=s1, scalar2=None, op0=op0)))
            ACT = lambda o, a, f, **kw: (lambda e: e.activation(out=o, in_=a, func=f, **kw))
            MUL, ADD, SUB = ALU.mult, ALU.add, ALU.subtract

            def cmul(o_r, o_i, a_r, a_i, s_r, s_i, t1, t2):
                return [TT(t1, a_r, s_r, MUL), TT(t2, a_i, s_i, MUL), TT(o_r, t1, t2, SUB),
                        TT(t1, a_r, s_i, MUL), TT(t2, a_i, s_r, MUL), TT(o_i, t1, t2, ADD)]

            def emit_prep(GP, gs, lam_re_t, lam_im_t, logdt_t, X1t, X2t, sgn, hpi, sc, sqr, sqi, bqr, bqi, a5r, a5i, PWfr, PWfi, PWrr, PWri, PWbr, PWbi, cta, ctb, BB1, BB2, bbt, PRs, nPI):
                ops = []
                S = {k: v[:, :] for k, v in sc.items()}
                ops = []
                ops.append(TS(S["lr"], lam_re_t[:, gs], -1e-4, None, ALU.min))
                P.seq("dve", ops, reads=["lam_re_t"], writes=["prep"]); ops = []
                P.op("act", ACT(S["dt"], logdt_t[:, gs], AF.Exp), reads=["logdt_t", "prep"], writes=["prep_dt"])
                ops += [TT(S["lrdt"], S["lr"], S["dt"], MUL), TT(S["th"], lam_im_t[:, gs], S["dt"], MUL)]
                P.seq("dve", ops, reads=["prep", "prep_dt", "lam_im_t"], writes=["prep"]); ops = []
                P.seq("act", [ACT(S["sn"], S["th"], AF.Sin, scale=1.0 / 32), ACT(S["cs"], S["th"], AF.Sin, scale=1.0 / 32, bias=hpi[:, 0:1]),
                              ACT(S["mag"], S["lrdt"], AF.Exp, scale=1.0 / 32), ACT(S["im2"], S["lrdt"], AF.Exp, scale=-2.0)],
                      reads=["prep", "hpi"], writes=["prep_cs"])
                li = lam_im_t[:, gs]
                ops += [TT(a5r[:, :, 0], S["mag"], S["cs"], MUL), TT(a5i[:, :, 0], S["mag"], S["sn"], MUL)]
                for e_ in range(5):
                    ops += cmul(a5r[:, :, e_ + 1], a5i[:, :, e_ + 1], a5r[:, :, e_], a5i[:, :, e_], a5r[:, :, e_], a5i[:, :, e_], S["ta"], S["nsq"])
                ops += [(lambda e: e.tensor_copy(out=S["ar"], in_=a5r[:, :, 5])), (lambda e: e.tensor_copy(out=S["ai"], in_=a5i[:, :, 5])),
                        TT(S["den"], S["lr"], S["lr"], MUL), TT(S["ta"], li, li, MUL), TT(S["den"], S["den"], S["ta"], ADD),
                        (lambda e: e.reciprocal(out=S["den"], in_=S["den"])),
                        TS(S["am1"], S["ar"], -1.0, None, ADD),
                        TT(S["ta"], S["am1"], S["lr"], MUL), TT(S["tb"], S["ai"], li, MUL), TT(S["ta"], S["ta"], S["tb"], ADD), TT(S["cr"], S["ta"], S["den"], MUL),
                        TT(S["ta"], S["ai"], S["lr"], MUL), TT(S["tb"], S["am1"], li, MUL), TT(S["ta"], S["ta"], S["tb"], SUB), TT(S["ci"], S["ta"], S["den"], MUL),
                        TS(S["scr"], S["cr"], sgn[:, 0:1], None, MUL),
                        TS(S["tb"], S["ci"], sgn[:, 0:1], None, MUL),
                        TS(S["nci"], S["ci"], -1.0, None, MUL),
                        TT(S["bir"], S["ar"], S["im2"], MUL), TT(S["bii"], S["ai"], S["im2"], MUL), TS(S["bii"], S["bii"], -1.0, None, MUL),
                        (lambda e: e.tensor_copy(out=sqr[:, :, 0], in_=S["ar"])), (lambda e: e.tensor_copy(out=sqi[:, :, 0], in_=S["ai"])),
                        (lambda e: e.tensor_copy(out=bqr[:, :, 0], in_=S["bir"])), (lambda e: e.tensor_copy(out=bqi[:, :, 0], in_=S["bii"]))]
                for e_ in range(11):
                    ops += cmul(sqr[:, :, e_ + 1], sqi[:, :, e_ + 1], sqr[:, :, e_], sqi[:, :, e_], sqr[:, :, e_], sqi[:, :, e_], S["ta"], S["nsq"])
                for e_ in range(2):
                    ops += cmul(bqr[:, :, e_ + 1], bqi[:, :, e_ + 1], bqr[:, :, e_], bqi[:, :, e_], bqr[:, :, e_], bqi[:, :, e_], S["ta"], S["nsq"])
                bc16 = lambda a: a.unsqueeze(2).to_broadcast([128, GP, 16])
                ops += [TT(BB1[:, :, :], X1t[:, gs, :], bc16(S["cr"]), MUL), TT(bbt[:, :, :], X2t[:, gs, :], bc16(S["tb"]), MUL), TT(BB1[:, :, :], BB1[:, :, :], bbt[:, :, :], ADD),
                        TT(BB2[:, :, :], X2t[:, gs, :], bc16(S["scr"]), MUL), TT(bbt[:, :, :], X1t[:, gs, :], bc16(S["nci"]), MUL), TT(BB2[:, :, :], BB2[:, :, :], bbt[:, :, :], ADD)]
                ops += [(lambda e: e.memset(PWfr[:, :, 0:1], 1.0)), (lambda e: e.memset(PWfi[:, :, 0:1], 0.0))]
                for k in range(LL):
                    n = 1 << k
                    bcn = lambda a, n=n: a.to_broadcast([128, GP, n])
                    ops += cmul(PWfr[:, :, n:2 * n], PWfi[:, :, n:2 * n], PWfr[:, :, 0:n], PWfi[:, :, 0:n],
                                bcn(sqr[:, :, k:k + 1]), bcn(sqi[:, :, k:k + 1]), cta[:, :, 0:n], ctb[:, :, 0:n])
                ops += [(lambda e: e.tensor_copy(out=PWfr[:, :, L5:L5 + 1], in_=sqr[:, :, LL:LL + 1])), (lambda e: e.tensor_copy(out=PWfi[:, :, L5:L5 + 1], in_=sqi[:, :, LL:LL + 1]))]
                ops += [(lambda e: e.memset(PWrr[:, :, L5 - 1:L5], 1.0)), (lambda e: e.memset(PWri[:, :, L5 - 1:L5], 0.0))]
                for k in range(LL):
                    n = 1 << k
                    bcn = lambda a, n=n: a.to_broadcast([128, GP, n])
                    ops += cmul(PWrr[:, :, L5 - 2 * n:L5 - n], PWri[:, :, L5 - 2 * n:L5 - n], PWrr[:, :, L5 - n:L5], PWri[:, :, L5 - n:L5],
                                bcn(sqr[:, :, k:k + 1]), bcn(sqi[:, :, k:k + 1]), cta[:, :, 0:n], ctb[:, :, 0:n])
                ops += [(lambda e: e.memset(PWbr[:, :, 0:1], 1.0)), (lambda e: e.memset(PWbi[:, :, 0:1], 0.0))]
                for k in range(3):
                    n = 1 << k
                    bcn = lambda a, n=n: a.to_broadcast([128, GP, n])
                    ops += cmul(PWbr[:, :, n:2 * n], PWbi[:, :, n:2 * n], PWbr[:, :, 0:n], PWbi[:, :, 0:n],
                                bcn(bqr[:, :, k:k + 1]), bcn(bqi[:, :, k:k + 1]), cta[:, :, 0:n], ctb[:, :, 0:n])
                ops += [TS(PRs[:, :, :], PWfr[:, :, :], sgn[:, 0:1], None, MUL), TS(PRs[:, :, :], PRs[:, :, :], -1.0, None, MUL), TS(nPI[:, :, :], PWfi[:, :, :], -1.0, None, MUL)]
                P.seq("dve", ops, reads=["prep", "prep_sn", "prep_cs", "sgn", "X1t", "X2t", "lam_im_t", "Rk_use", "tab_use"], writes=["prep", "prepT"]); ops = []

            esC0 = ExitStack()
            esC0.__enter__()
            sb0 = lambda name, shape, dt: esC0.enter_context(nc.sbuf_tensor(name, shape, dt))
            GA = 32
            i_lre = sb0("i_lre", [128, 32], F32); i_lim = sb0("i_lim", [128, 32], F32); i_ldt = sb0("i_ldt", [128, 32], F32)
            i_X1 = sb0("i_X1", [128, 32, 16], F32); i_X2 = sb0("i_X2", [128, 32, 16], F32)
            i_sgn = sb0("i_sgn", [128, 1], F32); i_hpi = sb0("i_hpi", [128, 1], F32)
            for dst_, src_ in ((i_lre[:, :], lam2_re[:, :]), (i_lim[:, :], lam2_im[:, :]), (i_ldt[:, :], logdt[0:1, :].partition_broadcast(128)),
                               (i_X1[:, :, :], X1d[:, :, :]), (i_X2[:, :, :], X2d[:, :, :])):
                P.dma("sp", lambda e, dst_=dst_, src_=src_: e.dma_start(out=dst_, in_=src_), "setupC0", writes=["c0in"])
            P.bulk_done("setupC0")
            P.seq("pool", [lambda e: e.memset(i_sgn[0:64, :], -1.0), lambda e: e.memset(i_sgn[64:128, :], 1.0), lambda e: e.memset(i_hpi[:, :], float(np.pi / 2))],
                  writes=["sgn", "hpi", "lam_re_t", "lam_im_t", "logdt_t", "X1t", "X2t"], reads=["c0in"])
            sc_names = ("lr", "dt", "lrdt", "th", "mag", "t1", "sn", "cs", "ar", "ai", "den", "am1", "cr", "ci", "ta", "tb", "scr", "nci", "im2", "bir", "bii", "nsq")
            sc0 = {n: sb0("sc0_" + n, [128, GA], F32) for n in sc_names}
            T0 = {n: sb0("p0_" + n, [128, GA, k], F32) for n, k in (("sqr", 12), ("sqi", 12), ("bqr", 3), ("bqi", 3), ("a5r", 6), ("a5i", 6), ("PWfr", L5 + 1), ("PWfi", L5 + 1),
                                                                     ("PWrr", L5), ("PWri", L5), ("PWbr", 8), ("PWbi", 8), ("cta", 64), ("ctb", 64), ("BB1", 16), ("BB2", 16),
                                                                     ("bbt", 16), ("PRs", L5 + 1), ("nPI", L5 + 1))}
            emit_prep(GA, slice(0, 32), i_lre, i_lim, i_ldt, i_X1, i_X2, i_sgn, i_hpi, sc0, *[T0[n] for n in ("sqr", "sqi", "bqr", "bqi", "a5r", "a5i", "PWfr", "PWfi", "PWrr", "PWri", "PWbr", "PWbi",
                                                                 "cta", "ctb", "BB1", "BB2", "bbt", "PRs", "nPI")])
            for nm_ in ("PWrr", "PWri", "PWbr", "PWbi", "BB1", "BB2", "PRs", "nPI", "sqr", "sqi"):
                P.dma("sp", lambda e, nm_=nm_: e.dma_start(out=prep_scr[nm_][:, :, :], in_=T0[nm_][:, :, :]), "prepst", reads=["prepT"], writes=["prep_scr"])
            P.bulk_done("prepst")
            esC0.close()
            P.barrier()
            esC = ExitStack()
            esC.__enter__()
            sb = lambda name, shape, dt: esC.enter_context(nc.sbuf_tensor(name, shape, dt))
            ps = lambda name, shape, dt: esC.enter_context(nc.psum_tensor(name, shape, dt))
            G8 = 8
            lam_re_t = sb("lam_re_t", [128, 32], F32)
            lam_im_t = sb("lam_im_t", [128, 32], F32)
            logdt_t = sb("logdt_t", [128, 32], F32)
            X1t = sb("X1t", [128, 32, 16], F32)
            X2t = sb("X2t", [128, 32, 16], F32)
            CY1t = sb("CY1t", [128, 32, 16], F32)
            CY2t = sb("CY2t", [128, 32, 16], F32)
            sgn = sb("sgn", [128, 1], F32)
            d_bc = sb("d_bc", [NC5, 512], F32)
            WAt = sb("WAt", [128, 4, 128], BF16)
            WBt = sb("WBt", [128, 4, 128], BF16)
            gba = sb("gba", [128, 4], F32)
            gbb = sb("gbb", [128, 4], F32)
            SW = sb("SW", [128, 128], F32)
            rkt8 = sb("rkt8", [128, G8, 128], F32)
            send = sb("send", [128, G8], F32)
            sc = {n: sb("sc_" + n, [128, G8], F32) for n in
                  ("lr", "dt", "lrdt", "th", "mag", "t1", "sn", "cs", "ar", "ai", "den", "am1", "cr", "ci", "ta", "tb", "scr", "nci", "im2", "bir", "bii", "nsq")}
            a5r = sb("a5r", [128, G8, 6], F32)
            a5i = sb("a5i", [128, G8, 6], F32)
            hpi = sb("hpi", [128, 1], F32)
            sqr = sb("sqr", [128, G8, 12], F32)
            sqi = sb("sqi", [128, G8, 12], F32)
            bqr = sb("bqr", [128, G8, 3], F32)
            bqi = sb("bqi", [128, G8, 3], F32)
            PWrr = sb("PWrr", [128, G8, L5], F32)
            PWri = sb("PWri", [128, G8, L5], F32)
            PWbr = sb("PWbr", [128, G8, 8], F32)
            PWbi = sb("PWbi", [128, G8, 8], F32)
            BB1 = sb("BB1", [128, G8, 16], F32)
            BB2 = sb("BB2", [128, G8, 16], F32)
            bbt = sb("bbt", [128, G8, 16], F32)
            PRs = sb("PRs", [128, G8, L5 + 1], F32)
            nPI = sb("nPI", [128, G8, L5 + 1], F32)
            tA = sb("tA", [128, L5 + 1, 16], F32)
            tB = sb("tB", [128, L5 + 1, 16], F32)
            tC = sb("tC", [128, L5 + 1, 16], F32)
            tD = sb("tD", [128, L5 + 1, 16], F32)
            ZTb = sb("ZTb", [128, L5, 16], BF16)
            XTb = sb("XTb", [128, 8, 16], BF16)
            Zt = sb("Zt", [128, G8, KC5, 128], BF16)
            Gtab = sb("Gtab", [128, G8, L5 * 16], BF16)
            Yt = sb("Yt", [128, G8, L5 + 1, 16], BF16)
            Rk = sb("Rk", [128, G8, NLEV, 128], F32)
            ucj = sb("ucj", [NC5, G8, L5, 16], BF16)
            ustack = sb("ustack", [128, G8, KC5, NC5], BF16)
            E32 = sb("E32", [128, G8, NC5], F32)
            Xs = sb("Xs", [128, G8, NC5 + 1], F32)
            Sprevb = sb("Sprevb", [128, G8, NC5], BF16)
            sg_t = sb("sg_t", [128, 4, G8], F32)
            hs = sb("hs", [128, G8], F32)
            hs2 = sb("hs2", [128, G8], F32)
            du = sb("du", [NC5, L5, 16], F32)
            yv = sb("yv", [NC5, L5 * 16], F32)
            y2 = sb("y2", [NC5, L5 * 16], F32)
            ysg = sb("ysg", [NC5, L5 * 16], F32)
            ygel = sb("ygel", [NC5, L5, 128], BF16)
            ygT = sb("ygT", [128, L5, NC5], BF16)
            sbt = sb("sbt", [128, 512], F32)
            ps_z = [ps(f"ps_z{i}", [128, 512], F32) for i in range(2)]
            pT = ps("pT", [128, 2048], BF16)
            pE = ps("pE", [128, G8, NC5], F32)
            pD = ps("pD", [128, G8, NC5], F32)
            pY = ps("pY", [128, 512], F32)

            def ld(dst, src, key, q="sp"):
                P.dma(q, lambda e: e.dma_start(out=dst, in_=src), "setupC" if q == "sp" else "setupCp", writes=[key])
            ld(lam_re_t[:, :], lam2_re[:, :], "lam_re_t")
            ld(lam_im_t[:, :], lam2_im[:, :], "lam_im_t")
            ld(logdt_t[:, :], logdt[0:1, :].partition_broadcast(128), "logdt_t")
            ld(X1t[:, :, :], X1d[:, :, :], "X1t")
            ld(X2t[:, :, :], X2d[:, :, :], "X2t")
            ld(CY1t[:, :, :], CY1d[:, :, :], "CY1t")
            ld(CY2t[:, :, :], CY2d[:, :, :], "CY2t")
            ld(d_bc[:, :], s5d[0:1, :].partition_broadcast(NC5), "d_bc")
            ld(gba[:, :], gbad[:, :], "gba")
            ld(gbb[:, :], gbbd[:, :], "gbb")
            ld(WAt[:, :, :], WAd.ap().rearrange("u p c -> p u c"), "WAt", q="pool")
            ld(WBt[:, :, :], WBd.ap().rearrange("u p c -> p u c"), "WBt", q="pool")
            P.bulk_done("setupC")
            P.bulk_done("setupCp")
            P.seq("pool", [lambda e: e.memset(sgn[0:64, :], -1.0), lambda e: e.memset(sgn[64:128, :], 1.0)], writes=["sgn"])
            P.op("pool", lambda e: e.memset(hpi[:, :], float(np.pi / 2)), writes=["hpi"])
            P.seq("pool", [lambda e: e.memset(SW[:, :], 0.0),
                           lambda e: e.affine_select(out=SW[:, :], in_=SW[:, :], pattern=[[-1, 128]], compare_op=ALU.not_equal, fill=1.0, base=64, channel_multiplier=1),
                           lambda e: e.affine_select(out=SW[:, :], in_=SW[:, :], pattern=[[-1, 128]], compare_op=ALU.not_equal, fill=1.0, base=-64, channel_multiplier=1)],
                  writes=["SW"])
            P.op("pool", lambda e: e.tensor_scalar(out=SW[:, :], in0=SW[:, :], scalar1=sgn[:, 0:1], scalar2=None, op0=ALU.mult), reads=["SW", "sgn"], writes=["SW"])

            PI = float(np.pi)
            for un in range(4):
                gs = slice(un * 8, (un + 1) * 8)
                for nm_, dst_ in (("PWrr", PWrr), ("PWri", PWri), ("PWbr", PWbr), ("PWbi", PWbi), ("BB1", BB1), ("BB2", BB2),
                                  ("PRs", PRs), ("nPI", nPI), ("sqr", sqr), ("sqi", sqi)):
                    P.dma("sp", lambda e, nm_=nm_, dst_=dst_, gs=gs: e.dma_start(out=dst_[:, :, :], in_=prep_scr[nm_][:, gs, :]), "prepld",
                          reads=["prep_scr", "Rk_use", "tab_use"], writes=["prepT"])
                P.bulk_done("prepld")

                bh = lambda a, n: a.unsqueeze(1).to_broadcast([128, n, 16])
                for g in range(G8):
                    bq = lambda a: a.unsqueeze(2).to_broadcast([128, L5, 16])
                    P.seq("dve", [TT(tA[:, 0:L5, :], bq(PWrr[:, g, :]), bh(BB1[:, g, :], L5), MUL),
                                  TT(tB[:, 0:L5, :], bq(PWri[:, g, :]), bh(BB2[:, g, :], L5), MUL),
                                  TT(ZTb[:, :, :], tA[:, 0:L5, :], tB[:, 0:L5, :], ADD)],
                          reads=["prepT", "ZTb_use"], writes=["tA", "tB", "ZTb"])
                    def f_zt(e):
                        for kc in range(KC5):
                            i = e.transpose(out=pT[:, kc * 128:(kc + 1) * 128], in_=ZTb[:, kc * 8:(kc + 1) * 8, :], identity=ident[:, :])
                        return i
                    P.op("pe", f_zt, reads=["ZTb", "ident"], writes=["pT"])
                    P.op("act", lambda e, g=g: e.copy(out=Zt[:, g, :, :], in_=pT[:, 0:KC5 * 128].rearrange("p (a b) -> p a b", b=128)), reads=["pT"], writes=["Zt", "ZTb_use"])
                for k in range(NLEV):
                    P.seq("pool", [lambda e, k=k: e.tensor_tensor(out=Rk[:, :, k, :], in0=identf[:, :].unsqueeze(1).to_broadcast([128, G8, 128]),
                                                                  in1=sqr[:, :, LL + k:LL + k + 1].to_broadcast([128, G8, 128]), op=MUL),
                                   lambda e, k=k: e.tensor_tensor(out=rkt8[:, :, :], in0=SW[:, :].unsqueeze(1).to_broadcast([128, G8, 128]),
                                                                  in1=sqi[:, :, LL + k:LL + k + 1].to_broadcast([128, G8, 128]), op=MUL),
                                   lambda e, k=k: e.tensor_tensor(out=Rk[:, :, k, :], in0=Rk[:, :, k, :], in1=rkt8[:, :, :], op=SUB)],
                          reads=["prepT", "identf", "SW", "sgn", "Rk_use"], writes=[f"Rk{g}" for g in range(G8)] + ["rkt"])
                for g in range(G8):
                    P.dma("sp", lambda e, un=un, g=g: e.dma_start(out=ucj[:, g, :, :],
                                                                   in_=u_scr.ap().rearrange("(c j) n -> c j n", j=L5)[:, :, un * 128 + g * 16:un * 128 + (g + 1) * 16]),
                          "ucj", reads=[f"u_scr{t}" for t in range(NT)] + ["ucj_use"], writes=[f"ucj{g}"])
                P.bulk_done("ucj")
                US0 = 1024
                for g in range(G8):
                    def f_us(e, g=g):
                        for kc in range(KC5):
                            i = e.transpose(out=pT[:, US0 + kc * NC5:US0 + (kc + 1) * NC5], in_=ucj[:, g, kc * 8:(kc + 1) * 8, :], identity=ident[0:NC5, 0:NC5])
                        return i
                    P.op("pe", f_us, reads=[f"ucj{g}", "ident"], writes=["pT2"])
                    P.op("act", lambda e, g=g: e.copy(out=ustack[:, g, :, :], in_=pT[:, US0:US0 + KC5 * NC5].rearrange("p (a b) -> p a b", b=NC5)), reads=["pT2"], writes=[f"ustack{g}"])
                    def f_E(e, g=g):
                        for kc in range(KC5):
                            i = e.matmul(out=pE[:, g, :], lhsT=Zt[:, g, kc, :], rhs=ustack[:, g, kc, :], start=(kc == 0), stop=(kc == KC5 - 1))
                        return i
                    P.op("pe", f_E, reads=["Zt", f"ustack{g}"], writes=["pE"])
                P.op("act", lambda e: e.copy(out=E32[:, :, :], in_=pE[:, :, :]), reads=["pE"], writes=["E32"])

                NX = NC5 + 1
                def doubling(tag):
                    for k in range(NLEV):
                        n = NX - (1 << k)
                        def f_d(e, k=k, n=n):
                            for g in range(G8):
                                i = e.matmul(out=pD[:, g, 0:n], lhsT=Rk[:, g, k, :], rhs=Xs[:, g, 0:n], start=True, stop=True)
                            return i
                        P.op("pe", f_d, reads=["Xs"] + [f"Rk{g}" for g in range(G8)], writes=["pD"])
                        P.op("dve", lambda e, k=k, n=n: e.tensor_tensor(out=Xs[:, :, 1 << k:NX], in0=Xs[:, :, 1 << k:NX], in1=pD[:, :, 0:n], op=ADD),
                             reads=["pD", "Xs"], writes=["Xs"])
                P.seq("dve", [lambda e: e.memset(Xs[:, :, 0:1], 0.0), lambda e: e.tensor_copy(out=Xs[:, :, 1:NX], in_=E32[:, :, :])], reads=["E32"], writes=["Xs"])
                doubling("loc")
                P.op("dve", lambda e: e.tensor_copy(out=send[:, :], in_=Xs[:, :, NC5]), reads=["Xs"], writes=["send"])
                P.dma("sp", lambda e, un=un: e.dma_start(out=ccs_in[un][:, :], in_=send[:, :]), f"ccsin{un}", reads=["send"], writes=[f"ccs_in{un}"])
                P.dma("pool", lambda e, un=un: e.collective_compute("AllGather", ALU.bypass, replica_groups=GROUPS4,
                                                                     ins=[ccs_in[un].ap().opt()], outs=[ccs_out[un].ap().opt()]),
                      f"ccs{un}", reads=[f"ccs_in{un}"], writes=[f"ccs_out{un}"], inc=1)
                P.dma("sp", lambda e, un=un: e.dma_start(out=sg_t[:, :, :], in_=ccs_out[un].ap().rearrange("(r p) c -> p r c", p=128)),
                      f"ccsld{un}", reads=[f"ccs_out{un}"], writes=["sg_t"])
                for g in range(G8):
                    ga = un * 8 + g
                    bq8 = lambda a: a.unsqueeze(2).to_broadcast([128, 8, 16])
                    P.seq("dve", [TT(tA[:, 0:8, :], bq8(PWbr[:, g, :]), bh(BB1[:, g, :], 8), MUL),
                                  TT(tB[:, 0:8, :], bq8(PWbi[:, g, :]), bh(BB2[:, g, :], 8), MUL),
                                  TT(XTb[:, :, :], tA[:, 0:8, :], tB[:, 0:8, :], ADD)],
                          reads=["prepT", "tA", "tB", "XTb_use"], writes=["tA", "tB", "XTb"])
                    bqY = lambda a: a.unsqueeze(2).to_broadcast([128, L5 + 1, 16])
                    P.seq("pool", [TT(tC[:, :, :], bqY(PRs[:, g, :]), bh(CY1t[:, ga, :], L5 + 1), MUL),
                                   TT(tD[:, :, :], bqY(nPI[:, g, :]), bh(CY2t[:, ga, :], L5 + 1), MUL),
                                   TT(Yt[:, g, :, :], tC[:, :, :], tD[:, :, :], ADD)],
                          reads=["prepT", "CY1t", "CY2t", "tab_use"], writes=["tC", "tD", f"Yt{g}"])
                    P.op("pe", lambda e, g=g: e.matmul(out=pY[:, :], lhsT=XTb[:, :, :], rhs=Yt[:, g, 0:L5, :], start=True, stop=True), reads=["XTb", f"Yt{g}"], writes=["pY"])
                    P.op("act", lambda e, g=g: e.copy(out=Gtab[:, g, :], in_=pY[:, :]), reads=["pY", "tab_use"], writes=[f"Gtab{g}", "XTb_use"])
                    P.op("pool", lambda e, g=g: e.affine_select(out=Gtab[:, g, :].rearrange("p (m h) -> p m h", h=16), in_=Gtab[:, g, :].rearrange("p (m h) -> p m h", h=16),
                                                                pattern=[[16, L5], [0, 16]], compare_op=ALU.is_ge, fill=0.0, base=15, channel_multiplier=-1),
                         reads=[f"Gtab{g}"], writes=[f"Gtab{g}"])

                P.op("dve", lambda e: e.memset(hs[:, :], 0.0), writes=["hs"])
                for pp in range(3):
                    def f_hr(e):
                        for g in range(G8):
                            i = e.matmul(out=pD[:, g, 0:1], lhsT=Rk[:, g, NLEV - 1, :], rhs=hs[:, g:g + 1], start=True, stop=True)
                        return i
                    P.op("pe", f_hr, reads=["hs"] + [f"Rk{g}" for g in range(G8)], writes=["pD"])
                    P.seq("dve", [lambda e, pp=pp: e.tensor_tensor(out=hs2[:, :], in0=pD[:, :, 0], in1=sg_t[:, pp, :], op=ADD),
                                  lambda e: e.tensor_tensor(out=hs2[:, :], in0=hs2[:, :], in1=hs[:, :], op=SUB),
                                  lambda e, pp=pp: e.scalar_tensor_tensor(out=hs[:, :], in0=hs2[:, :], scalar=pm[:, pp:pp + 1], in1=hs[:, :], op0=MUL, op1=ADD)],
                          reads=["pD", "sg_t", "hs", "pm"], writes=["hs", "hs2"])
                P.seq("dve", [lambda e: e.tensor_copy(out=Xs[:, :, 0], in_=hs[:, :]), lambda e: e.tensor_copy(out=Xs[:, :, 1:NX], in_=E32[:, :, :])],
                      reads=["hs", "E32"], writes=["Xs"])
                doubling("glob")
                P.op("act", lambda e: e.copy(out=Sprevb[:, :, :], in_=Xs[:, :, 0:NC5]), reads=["Xs"], writes=["Sprevb"])

                for g in range(G8):
                    def f_y(e, g=g):
                        e.matmul(out=pY[0:NC5, :], lhsT=Sprevb[:, g, :], rhs=Yt[:, g, 1:L5 + 1, :], start=True, stop=False)
                        for kc in range(KC5):
                            lo = 128 * kc
                            i = e.matmul(out=pY[0:NC5, lo:512], lhsT=ustack[:, g, kc, :], rhs=Gtab[:, g, 0:512 - lo], start=False, stop=(kc == KC5 - 1))
                        return i
                    P.op("pe", f_y, reads=["Sprevb", f"Yt{g}", f"Gtab{g}", f"ustack{g}"], writes=["pY"])
                    ga = un * 8 + g
                    P.op("pool", lambda e, g=g, ga=ga: e.tensor_tensor(out=du[:, :, :], in0=ucj[:, g, :, :],
                                                                       in1=d_bc[:, ga * 16:(ga + 1) * 16].unsqueeze(1).to_broadcast([NC5, L5, 16]), op=MUL),
                         reads=[f"ucj{g}", "d_bc"], writes=["du"])
                    yv_ = yv[:, :]
                    P.seq("dve", [lambda e: e.tensor_tensor(out=yv[:, :], in0=pY[0:NC5, :], in1=du[:, :, :].rearrange("p a b -> p (a b)"), op=ADD),
                                  TT(y2[:, :], yv_, yv_, MUL), TS(y2[:, :], y2[:, :], 0.044715, 1.0, MUL, ADD), TT(y2[:, :], y2[:, :], yv_, MUL)],
                          reads=["pY", "du", "ysg"], writes=["yv", "y2"])
                    P.op("act", ACT(ysg[:, :], y2[:, :], AF.Sigmoid, scale=1.5957691216057308), reads=["y2"], writes=["ysg"])
                    P.op("dve", lambda e, g=g: e.tensor_tensor(out=ygel[:, :, g * 16:(g + 1) * 16], in0=yv[:, :].rearrange("p (a b) -> p a b", b=16),
                                                               in1=ysg[:, :].rearrange("p (a b) -> p a b", b=16), op=MUL),
                         reads=["yv", "ysg", "ygel_use"], writes=[f"ygel{g}"])
                def f_gt(e):
                    for j in range(L5):
                        i = e.transpose(out=pT[:, j * NC5:(j + 1) * NC5], in_=ygel[:, j, :], identity=ident[0:NC5, 0:NC5])
                    return i
                P.op("pe", f_gt, reads=[f"ygel{g}" for g in range(G8)] + ["ident"], writes=["pT", "pT2"])
                P.op("act", lambda e: e.copy(out=ygT[:, :, :], in_=pT[:, :].rearrange("p (a b) -> p a b", b=NC5)), reads=["pT", "pT2"], writes=["ygT", "ygel_use"])
                ms_v = mslab[:, :].rearrange("p (c j) -> p j c", j=L5)
                JB = 512 // NC5
                for nb in range(4):
                    P.op("pe", lambda e, nb=nb, un=un: e.matmul(out=ps_z[0][:, :], lhsT=WAt[:, un, :], rhs=ygT[:, nb * JB:(nb + 1) * JB, :], start=True, stop=True),
                         reads=["WAt", "ygT"], writes=["ps_z0"])
                    P.op("pe", lambda e, nb=nb, un=un: e.matmul(out=ps_z[1][:, :], lhsT=WBt[:, un, :], rhs=ygT[:, nb * JB:(nb + 1) * JB, :], start=True, stop=True),
                         reads=["WBt", "ygT"], writes=["ps_z1"])
                    P.op("act", lambda e, un=un: e.activation(out=sbt[:, :], in_=ps_z[1][:, :], func=AF.Sigmoid, bias=gbb[:, un:un + 1]),
                         reads=["ps_z1", "gbb"], writes=["sbt"])
                    P.op("dve", lambda e, nb=nb, un=un: e.scalar_tensor_tensor(out=ms_v[:, nb * JB:(nb + 1) * JB, :], in0=ps_z[0][:, :].rearrange("p (j c) -> p j c", c=NC5), scalar=gba[:, un:un + 1],
                                                                               in1=sbt[:, :].rearrange("p (j c) -> p j c", c=NC5), op0=ADD, op1=MUL),
                         reads=["ps_z0", "sbt", "gba"], writes=["mslab"])
                P.dma("sp", lambda e, un=un: e.dma_start(out=mergedT_scr[512 + un * 128:512 + (un + 1) * 128, :], in_=mslab[:, :]), "mscr",
                      reads=["mslab"], writes=[f"mscr{4 + un}"])
            esC.close()
            P.barrier()
        else:
            esH.close()

        if stage >= 4:
            AX = mybir.AxisListType
            MUL, ADD, SUB = ALU.mult, ALU.add, ALU.subtract
            esDE = ExitStack()
            esDE.__enter__()
            sbp = lambda name, shape, dt: esDE.enter_context(nc.sbuf_tensor(name, shape, dt))
            h2T = sbp("h2T", [128, 8, NTOK], BF16)
            cw = sbp("cw", [128, NT, 32], F32)
            xt2 = [sbp(f"xt2_{i}", [128, D], F32) for i in range(2)]
            ssq2 = sbp("ssq2", [128, 2 * NT], F32)
            rstd2 = sbp("rstd2", [128, 2 * NT], F32)
            junk2 = sbp("junk2", [128, D], BF16)
            esD = ExitStack()
            esD.__enter__()
            sb = lambda name, shape, dt: esD.enter_context(nc.sbuf_tensor(name, shape, dt))
            ps = lambda name, shape, dt: esD.enter_context(nc.psum_tensor(name, shape, dt))
            mT = sb("mT", [128, 8, NTOK], BF16)
            Wout = sb("Wout", [128, 8, D], BF16)
            Wr = sb("Wr", [128, 8, 36], BF16)
            rb_bc = sb("rb_bc", [128, 36], F32)
            xm = [sb(f"xm{i}", [128, D], F32) for i in range(2)]
            hb2 = [sb(f"hb2_{i}", [128, D], BF16) for i in range(2)]
            lg = sb("lg", [128, NT, 36], F32)
            r_a = sb("r_a", [128, NT, 4], F32)
            r_b = sb("r_b", [128, NT, 4], F32)
            r_gmax = sb("r_gmax", [128, NT], F32)
            r_gw = sb("r_gw", [128, NT], F32)
            r_el = sb("r_el", [128, NT, 32], F32)
            r_t = sb("r_t", [128, NT, 32], F32)
            r_oh1 = sb("r_oh1", [128, NT, 32], F32)
            r_oh2 = sb("r_oh2", [128, NT, 32], F32)
            r_m1 = sb("r_m1", [128, NT], F32)
            r_m2 = sb("r_m2", [128, NT], F32)
            r_w1 = sb("r_w1", [128, NT], F32)
            r_w2 = sb("r_w2", [128, NT], F32)
            po4 = [[ps(f"po{i}{j}", [128, 512], F32) for j in range(2)] for i in range(2)]
            tp2 = [ps(f"tp2_{i}", [128, 8, 128], BF16) for i in range(2)]
            pr2 = [ps(f"pr{i}", [128, 36], F32) for i in range(2)]

            P.dma("sp", lambda e: e.dma_start(out=mT[:, :, :], in_=mergedT_scr.ap().rearrange("(kc p) t -> p kc t", p=128)), "mT",
                  reads=[f"mscr{i}" for i in range(8)], writes=["mT"])
            P.dma("pool", lambda e: e.dma_start(out=Wout[:, :, :], in_=w_out.ap().rearrange("(kc p) c -> p kc c", p=128)), "setupDp", writes=["Wout"])
            P.dma("pool", lambda e: e.dma_start(out=Wr[:, :, :], in_=w_rt.ap().rearrange("(kc p) c -> p kc c", p=128)), "setupDp", writes=["Wr"])
            P.dma("sp", lambda e: e.dma_start(out=rb_bc[:, :], in_=b_rt[0:1, :].partition_broadcast(128)), "setupD", writes=["rb_bc"])
            P.dma("sp", lambda e: e.dma_start(out=g_bc[:, :], in_=g_ffn[0:1, :].partition_broadcast(128)), "setupD", writes=["g_bc"])
            P.bulk_done("setupD")
            P.bulk_done("setupDp")

            for t0 in range(0, NT, 2):
                tl = (t0, t0 + 1)
                for t in tl:
                    b = t % 2
                    P.dma("sp", lambda e, t=t, b=b: e.dma_start(out=xt2[b][:, :], in_=x[t * 128:(t + 1) * 128, :]), f"xt2_{b}", writes=[f"xt2_{b}"])
                for t in tl:
                    b = t % 2
                    ts_ = slice(t * 128, (t + 1) * 128)
                    for hf in range(2):
                        def f_o(e, ts_=ts_, hf=hf, b=b):
                            for k in range(8):
                                i = e.matmul(out=po4[b][hf][:, :], lhsT=mT[:, k, ts_], rhs=Wout[:, k, hf * 512:(hf + 1) * 512], start=(k == 0), stop=(k == 7))
                            return i
                        P.op("pe", f_o, reads=["mT", "Wout"], writes=[f"po{b}{hf}"])
                for t in tl:
                    b = t % 2
                    for hf in range(2):
                        P.op("dve", lambda e, hf=hf, b=b: e.tensor_tensor(out=xm[b][:, hf * 512:(hf + 1) * 512], in0=po4[b][hf][:, :], in1=xt2[b][:, hf * 512:(hf + 1) * 512], op=ADD),
                             reads=[f"po{b}{hf}", f"xt2_{b}"], writes=[f"xm{b}_{hf}"])
                for t in tl:
                    b = t % 2
                    xk = [f"xm{b}_0", f"xm{b}_1"]
                    P.dma("sp", lambda e, t=t, b=b: e.dma_start(out=xmid_scr[t * 128:(t + 1) * 128, :], in_=xm[b][:, :]), f"xmid{b}", reads=xk, writes=[f"xmid{t}"])
                    P.seq("act", [lambda e, b=b, t=t: e.activation(out=junk2[:, :], in_=xm[b][:, :], func=AF.Square, accum_out=ssq2[:, t:t + 1]),
                                  lambda e, t=t: e.activation(out=ssq2[:, t:t + 1], in_=ssq2[:, t:t + 1], func=AF.Sqrt, scale=1.0 / D, bias=EPS)],
                          reads=xk, writes=["junk2", f"ssq2_{t}"])
                for t in tl:
                    b = t % 2
                    xk = [f"xm{b}_0", f"xm{b}_1"]
                    P.op("dve", lambda e, t=t: e.reciprocal(out=rstd2[:, t:t + 1], in_=ssq2[:, t:t + 1]), reads=[f"ssq2_{t}"], writes=[f"rstd2_{t}"])
                    P.op("dve", lambda e, b=b, t=t: e.scalar_tensor_tensor(out=hb2[b][:, :], in0=xm[b][:, :], scalar=rstd2[:, t:t + 1], in1=g_bc[:, :], op0=MUL, op1=MUL),
                         reads=xk + [f"rstd2_{t}", "g_bc"], writes=[f"hb2_{b}"])
                for t in tl:
                    b = t % 2
                    def f_tp2(e, b=b):
                        for k in range(8):
                            i = e.transpose(out=tp2[b][:, k, :], in_=hb2[b][:, k * 128:(k + 1) * 128], identity=ident[:, :])
                        return i
                    P.op("pe", f_tp2, reads=[f"hb2_{b}", "ident"], writes=[f"tp2_{b}"])
                for t in tl:
                    b = t % 2
                    ts_ = slice(t * 128, (t + 1) * 128)
                    P.op("act", lambda e, ts_=ts_, b=b: e.copy(out=h2T[:, :, ts_], in_=tp2[b][:, :, :]), reads=[f"tp2_{b}"], writes=[f"h2T_{t // 4}"])
                for t in tl:
                    b = t % 2
                    ts_ = slice(t * 128, (t + 1) * 128)
                    def f_r(e, ts_=ts_, b=b):
                        for k in range(8):
                            i = e.matmul(out=pr2[b][:, :], lhsT=h2T[:, k, ts_], rhs=Wr[:, k, :], start=(k == 0), stop=(k == 7))
                        return i
                    P.op("pe", f_r, reads=[f"h2T_{t // 4}", "Wr"], writes=[f"pr{b}"])
                for t in tl:
                    b = t % 2
                    P.op("dve", lambda e, t=t, b=b: e.tensor_tensor(out=lg[:, t, :], in0=pr2[b][:, :], in1=rb_bc[:, :], op=ADD), reads=[f"pr{b}", "rb_bc"], writes=["lg"])

            BIG = 1.0e9
            bc4 = lambda a: a.unsqueeze(2).to_broadcast([128, NT, 4])
            bc32 = lambda a: a.unsqueeze(2).to_broadcast([128, NT, 32])
            gl = lg[:, :, 0:4]
            el = lg[:, :, 4:36]
            P.seq("dve", [
                lambda e: e.tensor_reduce(out=r_gmax[:, :], in_=gl, axis=AX.X, op=ALU.max),
                lambda e: e.tensor_tensor(out=r_a[:, :, :], in0=gl, in1=bc4(r_gmax[:, :]), op=SUB)], reads=["lg"], writes=["r_a", "r_gmax"])
            P.op("act", lambda e: e.activation(out=r_b[:, :, :], in_=r_a[:, :, :], func=AF.Exp), reads=["r_a"], writes=["r_b"])
            P.seq("dve", [
                lambda e: e.tensor_reduce(out=r_gw[:, :], in_=r_b[:, :, :], axis=AX.X, op=ADD),
                lambda e: e.reciprocal(out=r_gw[:, :], in_=r_gw[:, :]),
                lambda e: e.tensor_tensor(out=r_a[:, :, :], in0=gl, in1=bc4(r_gmax[:, :]), op=ALU.is_equal),
                lambda e: e.tensor_scalar(out=r_a[:, :, :], in0=r_a[:, :, :], scalar1=-1.0, scalar2=BIG, op0=ADD, op1=MUL),
                lambda e: e.tensor_tensor(out=r_el[:, :, :].rearrange("p t (g k) -> p t g k", k=8), in0=el.rearrange("p t (g k) -> p t g k", k=8),
                                          in1=r_a[:, :, :].unsqueeze(3).to_broadcast([128, NT, 4, 8]), op=ADD),
                lambda e: e.tensor_reduce(out=r_m1[:, :], in_=r_el[:, :, :], axis=AX.X, op=ALU.max),
                lambda e: e.tensor_tensor(out=r_oh1[:, :, :], in0=r_el[:, :, :], in1=bc32(r_m1[:, :]), op=ALU.is_equal),
                lambda e: e.scalar_tensor_tensor(out=r_t[:, :, :], in0=r_oh1[:, :, :], scalar=-BIG, in1=r_el[:, :, :], op0=MUL, op1=ADD),
                lambda e: e.tensor_reduce(out=r_m2[:, :], in_=r_t[:, :, :], axis=AX.X, op=ALU.max),
                lambda e: e.tensor_tensor(out=r_oh2[:, :, :], in0=r_t[:, :, :], in1=bc32(r_m2[:, :]), op=ALU.is_equal),
                lambda e: e.tensor_tensor(out=r_w1[:, :], in0=r_m1[:, :], in1=r_m2[:, :], op=SUB)],
                reads=["lg", "r_b", "r_a"], writes=["r_a", "router1"])
            P.op("act", lambda e: e.activation(out=r_w1[:, :], in_=r_w1[:, :], func=AF.Sigmoid), reads=["router1"], writes=["r_w1"])
            P.seq("dve", [
                lambda e: e.tensor_scalar(out=r_w2[:, :], in0=r_w1[:, :], scalar1=-1.0, scalar2=1.0, op0=MUL, op1=ADD),
                lambda e: e.tensor_tensor(out=r_w1[:, :], in0=r_w1[:, :], in1=r_gw[:, :], op=MUL),
                lambda e: e.tensor_tensor(out=r_w2[:, :], in0=r_w2[:, :], in1=r_gw[:, :], op=MUL),
                lambda e: e.tensor_tensor(out=r_oh1[:, :, :], in0=r_oh1[:, :, :], in1=bc32(r_w1[:, :]), op=MUL),
                lambda e: e.tensor_tensor(out=r_oh2[:, :, :], in0=r_oh2[:, :, :], in1=bc32(r_w2[:, :]), op=MUL),
                lambda e: e.tensor_tensor(out=cw[:, :, :], in0=r_oh1[:, :, :], in1=r_oh2[:, :, :], op=ADD)],
                reads=["router1", "r_w1"], writes=["cw", "router1"])
            if "cw" in dbg:
                tcw = dbgt("cw", [128, NT, 32])
                P.dma("sp", lambda e: e.dma_start(out=tcw[:, :, :], in_=cw[:, :, :]), "out", reads=["cw"])
            esD.close()
            P.barrier()

            NE = 32 if stage >= 5 else 0
            esE = ExitStack()
            esE.__enter__()
            sb = lambda name, shape, dt: esE.enter_context(nc.sbuf_tensor(name, shape, dt))
            ps = lambda name, shape, dt: esE.enter_context(nc.psum_tensor(name, shape, dt))
            yacc = sb("yacc", [128, NT, D], F32)
            Wg = [sb(f"Wg{i}", [128, 8, 512], BF16) for i in range(2)]
            Wu = [sb(f"Wu{i}", [128, 8, 512], BF16) for i in range(2)]
            Wd = [sb(f"Wd{i}", [128, 4, D], BF16) for i in range(2)]
            actT = sb("actT", [128, 4, NTOK], BF16)
            sgt = [sb(f"sgt{i}", [128, 512], F32) for i in range(2)]
            pg = [ps(f"pg{i}", [128, 512], F32) for i in range(2)]
            pu2 = [ps(f"pu2_{i}", [128, 512], F32) for i in range(2)]
            pd = [ps(f"pd{i}", [128, 512], F32) for i in range(2)]
            P.op("pool", lambda e: e.memset(yacc[:, :, :], 0.0), writes=[f"yacc{t}_{hf}" for t in range(NT) for hf in range(2)])
            h2T_all = [f"h2T_{i}" for i in range(4)]
            for ex in range(NE):
                s_ = ex % 2
                P.dma("pool", lambda e, ex=ex, s_=s_: e.dma_start(out=Wg[s_][:, :, :], in_=w_gate[ex].rearrange("(kc p) c -> p kc c", p=128)), f"Wg{s_}", writes=[f"Wg{s_}"])
                P.dma("pool", lambda e, ex=ex, s_=s_: e.dma_start(out=Wu[s_][:, :, :], in_=w_up[ex].rearrange("(kc p) c -> p kc c", p=128)), f"Wu{s_}", writes=[f"Wu{s_}"])
                P.dma("pool", lambda e, ex=ex, s_=s_: e.dma_start(out=Wd[s_][:, :, :], in_=w_down[ex].rearrange("(kc p) c -> p kc c", p=128)), f"Wd{s_}", writes=[f"Wd{s_}"])
                it = 0
                for tb in range(4):
                    tbs = slice(tb * 512, (tb + 1) * 512)
                    for m in range(4):
                        b_ = it % 2
                        it += 1
                        def f_gu(e, s_=s_, m=m, tbs=tbs, b_=b_):
                            for k in range(8):
                                e.matmul(out=pg[b_][:, :], lhsT=Wg[s_][:, k, m * 128:(m + 1) * 128], rhs=h2T[:, k, tbs], start=(k == 0), stop=(k == 7))
                            for k in range(8):
                                i = e.matmul(out=pu2[b_][:, :], lhsT=Wu[s_][:, k, m * 128:(m + 1) * 128], rhs=h2T[:, k, tbs], start=(k == 0), stop=(k == 7))
                            return i
                        P.op("pe", f_gu, reads=[f"Wg{s_}", f"Wu{s_}", f"h2T_{tb}"], writes=[f"pg{b_}", f"pu2_{b_}"])
                        P.op("act", lambda e, b_=b_: e.activation(out=sgt[b_][:, :], in_=pg[b_][:, :], func=AF.Silu), reads=[f"pg{b_}"], writes=[f"sgt{b_}"])
                        P.op("dve", lambda e, b_=b_, m=m, tbs=tbs: e.tensor_tensor(out=actT[:, m, tbs], in0=sgt[b_][:, :], in1=pu2[b_][:, :], op=MUL),
                             reads=[f"sgt{b_}", f"pu2_{b_}"], writes=[f"actT_{tb}"])
                it = 0
                for t in range(NT):
                    ts_ = slice(t * 128, (t + 1) * 128)
                    for hf in range(2):
                        b_ = it % 2
                        it += 1
                        def f_d(e, s_=s_, ts_=ts_, hf=hf, b_=b_):
                            for m in range(4):
                                i = e.matmul(out=pd[b_][:, :], lhsT=actT[:, m, ts_], rhs=Wd[s_][:, m, hf * 512:(hf + 1) * 512], start=(m == 0), stop=(m == 3))
                            return i
                        P.op("pe", f_d, reads=[f"Wd{s_}", f"actT_{t // 4}"], writes=[f"pd{b_}"])
                        P.op("dve", lambda e, t=t, hf=hf, b_=b_, ex=ex: e.scalar_tensor_tensor(out=yacc[:, t, hf * 512:(hf + 1) * 512], in0=pd[b_][:, :], scalar=cw[:, t, ex:ex + 1],
                                                                                                in1=yacc[:, t, hf * 512:(hf + 1) * 512], op0=MUL, op1=ADD),
                             reads=[f"pd{b_}", "cw", f"yacc{t}_{hf}"], writes=[f"yacc{t}_{hf}"])

            P.dma("sp", lambda e: e.dma_start(out=g_bc[:, :], in_=g_fin[0:1, :].partition_broadcast(128)), "setupF", writes=["g_bc"])
            for t in range(NT):
                s_ = t % 2
                P.dma("sp", lambda e, t=t, s_=s_: e.dma_start(out=xt2[s_][:, :], in_=xmid_scr[t * 128:(t + 1) * 128, :]), f"xt2_{s_}", reads=[f"xmid{t}"], writes=[f"xt2_{s_}"])
                P.op("dve", lambda e, t=t, s_=s_: e.tensor_tensor(out=xt2[s_][:, :], in0=xt2[s_][:, :], in1=yacc[:, t, :], op=ADD),
                     reads=[f"xt2_{s_}", f"yacc{t}_0", f"yacc{t}_1"], writes=[f"xt2_{s_}"])
                P.seq("act", [lambda e, s_=s_, t=t: e.activation(out=junk2[:, :], in_=xt2[s_][:, :], func=AF.Square, accum_out=ssq2[:, NT + t:NT + t + 1]),
                              lambda e, t=t: e.activation(out=ssq2[:, NT + t:NT + t + 1], in_=ssq2[:, NT + t:NT + t + 1], func=AF.Sqrt, scale=1.0 / D, bias=EPS)],
                      reads=[f"xt2_{s_}"], writes=["junk2", f"ssq2_{NT + t}"])
                P.op("dve", lambda e, t=t: e.reciprocal(out=rstd2[:, NT + t:NT + t + 1], in_=ssq2[:, NT + t:NT + t + 1]), reads=[f"ssq2_{NT + t}"], writes=[f"rstd2_{NT + t}"])
                P.op("dve", lambda e, s_=s_, t=t: e.scalar_tensor_tensor(out=xt2[s_][:, :], in0=xt2[s_][:, :], scalar=rstd2[:, NT + t:NT + t + 1], in1=g_bc[:, :], op0=MUL, op1=MUL),
                     reads=[f"xt2_{s_}", f"rstd2_{NT + t}", "g_bc"], writes=[f"xt2_{s_}"])
                P.dma("sp", lambda e, t=t, s_=s_: e.dma_start(out=y[t * 128:(t + 1) * 128, :], in_=xt2[s_][:, :]), "yout", reads=[f"xt2_{s_}"], writes=[f"y{t}"])
            esE.close()
            esDE.close()

        if "merged" in dbg:
            t = dbgt("merged", [1024, NTOK], BF16)
            nrow = 1024 if stage >= 3 else 512
            P.dma("sp", lambda e: e.dma_start(out=t[0:nrow, :], in_=mergedT_scr[0:nrow, :]), "out", reads=[f"mscr{h}" for h in range(nrow // 128)])
        if "hT" in dbg:
            t2 = dbgt("hT", [128, 8, HALO + NTOK], BF16)
            P.dma("sp", lambda e: e.dma_start(out=t2[:, :, :], in_=hT[:, :, :]), "out", reads=hT_all)
        if "qk" in dbg:
            t3 = dbgt("qs", [128, NTOK], BF16)
            t4 = dbgt("ks", [128, NTOK], BF16)
            t5 = dbgt("expb", [128, NTOK], F32)
            t6 = dbgt("e2", [128, NTOK], F32)
            P.dma("sp", lambda e: e.dma_start(out=t3[:, :], in_=qsT[:, :]), "out", reads=["qsT"])
            P.dma("sp", lambda e: e.dma_start(out=t4[:, :], in_=ksT[:, :]), "out", reads=["ksT"])
            P.dma("sp", lambda e: e.dma_start(out=t5[:, :], in_=expb[:, :]), "out", reads=expb_all)
            P.dma("sp", lambda e: e.dma_start(out=t6[:, :], in_=e2bc[:, :]), "out", reads=e2_all)
        if "xmid" in dbg:
            txm = dbgt("xmid", [NTOK, D])
            P.dma("sp", lambda e: e.dma_start(out=txm[:, :], in_=xmid_scr[:, :]), "out", reads=[f"xmid{t}" for t in range(NT)])
        P.final_wait("sp", ["out", "yout"])

        with nc.Block() as block:
            @block.tensor
            def _(e): P.replay("pe", e)
            @block.scalar
            def _(e): P.replay("act", e)
            @block.vector
            def _(e): P.replay("dve", e)
            @block.gpsimd
            def _(e): P.replay("pool", e)
            @block.sync
            def _(e): P.replay("sp", e)
    return nc, dbg_out


def make_in_maps(inputs):
    f = lambda a: np.ascontiguousarray(a, dtype=np.float32)
    x = inputs["x"]
    common = {
        "g_mix": f(inputs["norm_mix_g"].reshape(1, D)),
        "w_in": f(inputs["w_in"].reshape(D, 2568)),
        "conv_wT": f(inputs["conv_w"].reshape(4, 8, 128).transpose(2, 1, 0)),
        "conv_b": f(inputs["conv_b"].reshape(8, 128).T),
        "i_bias": f(inputs["i_bias"].reshape(1, 4)),
        "f_bias": f(inputs["f_bias"].reshape(1, 4)),
        "g_ml": f(inputs["mlstm_norm_g"].reshape(1, 512)),
    }
    lre = inputs["s5_lambda_re"].reshape(32, 64).T
    lim = inputs["s5_lambda_im"].reshape(32, 64).T
    bre = inputs["s5_b_re"].reshape(32, 64, 16).transpose(1, 0, 2)
    bim = inputs["s5_b_im"].reshape(32, 64, 16).transpose(1, 0, 2)
    cre = inputs["s5_c_re"].reshape(32, 16, 64).transpose(2, 0, 1)
    cim = inputs["s5_c_im"].reshape(32, 16, 64).transpose(2, 0, 1)
    glw = inputs["s5_glu_w"].reshape(4, 8, 16, 32)
    WA = np.zeros((4, 128, 128), np.float32)
    WB = np.zeros((4, 128, 128), np.float32)
    for u in range(4):
        for g in range(8):
            WA[u, g * 16:(g + 1) * 16, g * 16:(g + 1) * 16] = glw[u, g, :, 0:16]
            WB[u, g * 16:(g + 1) * 16, g * 16:(g + 1) * 16] = glw[u, g, :, 16:32]
    glb = inputs["s5_glu_b"].reshape(4, 8, 32)
    common.update({
        "lam2_re": f(np.concatenate([lre, lre], 0)), "lam2_im": f(np.concatenate([lim, lim], 0)),
        "logdt": f(inputs["s5_log_dt"].reshape(1, 32)),
        "X1d": f(np.concatenate([bre, bim], 0)), "X2d": f(np.concatenate([bim, bre], 0)),
        "CY1d": f(np.concatenate([cre, cim], 0)), "CY2d": f(np.concatenate([cim, cre], 0)),
        "s5d": f(inputs["s5_d"].reshape(1, 512)),
        "WAd": WA, "WBd": WB,
        "w_out": f(inputs["w_out"].reshape(D, D)), "g_ffn": f(inputs["norm_ffn_g"].reshape(1, D)),
        "w_rt": f(np.concatenate([inputs["router_group_w"].reshape(D, 4), inputs["router_expert_w"].reshape(D, 32)], 1)),
        "b_rt": f(np.concatenate([inputs["router_group_b"].reshape(1, 4), inputs["router_expert_b"].reshape(1, 32)], 1)),
        "w_gate": f(inputs["expert_w_gate"].reshape(32, D, 512)), "w_up": f(inputs["expert_w_up"].reshape(32, D, 512)),
        "w_down": f(inputs["expert_w_down"].reshape(32, 512, D)), "g_fin": f(inputs["norm_final_g"].reshape(1, D)),
        "gbad": f(glb[:, :, 0:16].reshape(4, 128).T), "gbbd": f(glb[:, :, 16:32].reshape(4, 128).T),
    })
    maps = []
    for c in range(NCORES):
        b, p = c // 4, c % 4
        m = dict(common)
        m["x"] = f(x[b, p * NTOK:(p + 1) * NTOK])
        if p == 0:
            m["xh"] = np.zeros((HALO, D), np.float32)
        else:
            m["xh"] = f(x[b, p * NTOK - HALO:p * NTOK])
        pmk = np.zeros((128, 4), np.float32)
        pmk[:, :p] = 1.0
        m["pmask"] = pmk
        maps.append(m)
    return maps


_CACHE = {}


def kernel(**inputs):
    if "nc" not in _CACHE:
        _CACHE["nc"] = build()[0]
    nc = _CACHE["nc"]
    in_maps = make_in_maps(inputs)
    res = run_bass_kernel_spmd(nc, in_maps, core_ids=list(range(NCORES)))
    out = np.empty((2, 4 * NTOK, D), np.float32)
    for c in range(NCORES):
        b, p = c // 4, c % 4
        out[b, p * NTOK:(p + 1) * NTOK] = np.asarray(res.results[c]["y"], dtype=np.float32)
    return out
```

```python
import numpy as np
from contextlib import ExitStack
import concourse.bass as bass
import concourse.mybir as mybir
from concourse.bass_utils import run_bass_kernel_spmd

F32 = mybir.dt.float32
BF16 = mybir.dt.bfloat16
AF = mybir.ActivationFunctionType
ALU = mybir.AluOpType

NTOK = 2048
NT = 16
NCH = 16
CL = 128
HALO = 32
D = 1024
EPS = 1e-6
NCORES = 8
GROUPS4 = [[0, 1, 2, 3], [4, 5, 6, 7]]
L5 = 32
LL = 5
NC5 = NTOK // L5
KC5 = L5 // 8
NLEV = 7


class Prog:
    def __init__(self, nc, es):
        self.nc = nc
        self.es = es
        self.queues = {e: [] for e in ("pe", "act", "dve", "pool", "sp")}
        self.esem = {}
        self.ecnt = {}
        for e in ("pe", "act", "dve", "pool"):
            self.esem[e] = es.enter_context(nc.semaphore("sem_" + e))
            self.ecnt[e] = 0
        self.dsem = {}
        self.dcnt = {}
        self.lastw = {}
        self.readers = {}
        self.known = {e: {} for e in self.queues}

    def _deps(self, reads, writes):
        deps = []
        for r in reads:
            if r in self.lastw:
                deps.append(self.lastw[r])
        for w in writes:
            if w in self.lastw:
                deps.append(self.lastw[w])
            deps.extend(self.readers.get(w, ()))
        return deps

    def _prune(self, eng, deps):
        need = {}
        for (s, v) in deps:
            if eng == "pe" and s is self.esem["pe"]:
                continue
            if v > need.get(s, 0):
                need[s] = v
        out = []
        kn = self.known[eng]
        for s, v in need.items():
            if kn.get(s, 0) >= v:
                continue
            kn[s] = v
            out.append((s, v))
        return out

    def _record(self, tok, reads, writes):
        for r in reads:
            self.readers.setdefault(r, []).append(tok)
        for w in writes:
            self.lastw[w] = tok
            self.readers[w] = []

    def op(self, eng, fn, reads=(), writes=()):
        reads = tuple(reads)
        writes = tuple(writes)
        waits = self._prune(eng, self._deps(reads, writes))
        self.ecnt[eng] += 1
        tok = (self.esem[eng], self.ecnt[eng])
        self.queues[eng].append((waits, fn, (self.esem[eng], 1)))
        self._record(tok, reads, writes)

    def seq(self, eng, fns, reads=(), writes=()):
        self._chain = getattr(self, "_chain", 0) + 1
        ck = f"__chain{self._chain}"
        for fn in fns:
            self.op(eng, fn, reads=tuple(reads) + (ck,), writes=tuple(writes) + (ck,))

    def dma(self, q, fn, stream, reads=(), writes=(), inc=16):
        reads = tuple(reads)
        writes = tuple(writes)
        if stream not in self.dsem:
            self.dsem[stream] = self.es.enter_context(self.nc.semaphore("dsem_" + stream))
            self.dcnt[stream] = 0
        waits = self._prune(q, self._deps(reads, writes))
        self.dcnt[stream] += inc
        tok = (self.dsem[stream], self.dcnt[stream])
        self.queues[q].append((waits, fn, (self.dsem[stream], inc)))
        self._record(tok, reads, writes)

    def bulk_done(self, stream):
        s = self.dsem[stream]
        fin = self.dcnt[stream]
        for k, (ss, v) in list(self.lastw.items()):
            if ss is s:
                self.lastw[k] = (s, fin)

    def barrier(self):
        allw = [(self.esem[e], self.ecnt[e]) for e in self.esem if self.ecnt[e] > 0]
        allw += [(self.dsem[s], self.dcnt[s]) for s in self.dsem]
        for e in self.queues:
            w = self._prune(e, allw)
            if w:
                self.queues[e].append((w, None, None))

    def final_wait(self, q, streams):
        waits = [(self.dsem[st], self.dcnt[st]) for st in streams if st in self.dsem]
        self.queues[q].append((waits, None, None))

    def replay(self, eng, h):
        for waits, fn, inc in self.queues[eng]:
            for (s, v) in waits:
                h.wait_ge(s, v)
            if fn is None:
                continue
            inst = fn(h)
            inst.then_inc(inc[0], inc[1])


def build(stage=99, dbg=()):
    nc = bass.Bass("TRN2", target_bir_lowering=False)
    din = lambda name, shape, dt=F32: nc.dram_tensor(name, shape, dt, kind="ExternalInput")
    x = din("x", [NTOK, D])
    xh = din("xh", [HALO, D])
    g_mix = din("g_mix", [1, D])
    w_in = din("w_in", [D, 2568])
    conv_wT = din("conv_wT", [128, 8, 4])
    conv_b = din("conv_b", [128, 8])
    i_bias = din("i_bias", [1, 4])
    f_bias = din("f_bias", [1, 4])
    g_mlc = din("g_mlc", [128, 4])
    pmask = din("pmask", [128, 4])
    lam2_re = din("lam2_re", [128, 32])
    lam2_im = din("lam2_im", [128, 32])
    logdt = din("logdt", [1, 32])
    X1d = din("X1d", [128, 32, 16])
    X2d = din("X2d", [128, 32, 16])
    CY1d = din("CY1d", [128, 32, 16])
    CY2d = din("CY2d", [128, 32, 16])
    s5d = din("s5d", [1, 512])
    WAd = din("WAd", [4, 128, 128])
    WBd = din("WBd", [4, 128, 128])
    gbad = din("gbad", [128, 4])
    gbbd = din("gbbd", [128, 4])
    w_out = din("w_out", [D, D])
    g_ffn = din("g_ffn", [1, D])
    w_rt = din("w_rt", [D, 36])
    b_rt = din("b_rt", [1, 36])
    if stage >= 5:
        w_gate = din("w_gate", [32, D, 512])
        w_up = din("w_up", [32, D, 512])
        w_down = din("w_down", [32, 512, D])
    g_fin = din("g_fin", [1, D])
    y = nc.dram_tensor("y", [NTOK, D], F32, kind="ExternalOutput")
    dbg_out = {}
    def dbgt(name, shape, dt=F32):
        dbg_out[name] = nc.dram_tensor("dbg_" + name, shape, dt, kind="ExternalOutput")
        return dbg_out[name]

    mergedT_scr = nc.dram_tensor("mergedT_scr", [1024, NTOK], BF16)
    cc_in = [nc.dram_tensor(f"cc_in{h}", [128, 130], F32) for h in range(4)]
    cc_out = [nc.dram_tensor(f"cc_out{h}", [512, 130], F32) for h in range(4)]
    u_scr = nc.dram_tensor("u_scr", [NTOK, 512], BF16)
    xmid_scr = nc.dram_tensor("xmid_scr", [NTOK, D], F32)
    prep_scr = {n: nc.dram_tensor("prep_" + n, [128, 32, k], F32) for n, k in (("PWrr", L5), ("PWri", L5), ("PWbr", 8), ("PWbi", 8), ("BB1", 16), ("BB2", 16),
                                                                             ("PRs", L5 + 1), ("nPI", L5 + 1), ("sqr", 12), ("sqi", 12), ("PLr", NC5), ("PLi", NC5))}
    ccs_in = [nc.dram_tensor(f"ccs_in{h}", [128, 8], F32) for h in range(4)]
    ccs_out = [nc.dram_tensor(f"ccs_out{h}", [512, 8], F32) for h in range(4)]

    w_in_v = w_in.ap().rearrange("(kc p) c -> p kc c", p=128)

    es = ExitStack()
    with es:
        P = Prog(nc, es)
        sb = lambda name, shape, dt: es.enter_context(nc.sbuf_tensor(name, shape, dt))
        ps = lambda name, shape, dt: es.enter_context(nc.psum_tensor(name, shape, dt))

        g_bc = sb("g_bc", [128, D], F32)
        ident = sb("ident", [128, 128], BF16)
        identf = sb("identf", [128, 128], F32)
        mslab = sb("mslab", [128, NTOK], BF16)
        pm = sb("pm", [128, 4], F32)
        esH = ExitStack()
        esH.__enter__()
        hT = esH.enter_context(nc.sbuf_tensor("hT", [128, 8, HALO + NTOK], BF16))
        esB = ExitStack()
        esB.__enter__()
        sb = lambda name, shape, dt: esB.enter_context(nc.sbuf_tensor(name, shape, dt))
        ps = lambda name, shape, dt: esB.enter_context(nc.psum_tensor(name, shape, dt))
        xt = [sb(f"xt{i}", [128, D], F32) for i in range(2)]
        junk = sb("junk", [128, D], BF16)
        hb = sb("hb", [128, D], BF16)
        ssq = sb("ssq", [128, NT + 1], F32)
        rstd = sb("rstd", [128, NT + 1], F32)
        wh = sb("wh", [128, 8, 512], BF16)
        wg = sb("wg", [128, 8, 33], BF16)
        raw = sb("raw", [128, NTOK + 3], F32)
        ctmp = sb("ctmp", [128, NTOK], F32)
        qsT = sb("qsT", [128, NTOK], BF16)
        ksT = sb("ksT", [128, NTOK], BF16)
        cwt = sb("cwt", [128, 8, 4], F32)
        cbt = sb("cbt", [128, 8], F32)
        fbt = sb("fbt", [33, 4], F32)
        nfb = sb("nfb", [33, 4], F32)
        Gt = sb("Gt", [33, NTOK], F32)
        Gt2 = sb("Gt2", [33, NTOK], F32)
        cmask = sb("cmask", [33, NTOK], F32)
        sel0 = sb("sel0", [33, 128], F32)
        sel032 = sb("sel032", [33, 128], F32)
        expb = sb("expb", [128, NTOK], F32)
        e2bc = sb("e2bc", [128, NTOK], F32)
        v_ext = sb("v_ext", [CL, NCH, 130], BF16)
        vT = sb("vT", [128, NTOK], BF16)
        sigoT = sb("sigoT", [128, NTOK], BF16)
        kstok = sb("kstok", [CL, NCH, 128], BF16)
        Us = sb("Us", [128, 4, 130], F32)
        gAc = sb("gAc", [128, 4], F32)
        CTloc = sb("CTloc", [128, NCH + 1, 130], F32)
        CTball = sb("CTball", [128, NCH, 130], BF16)
        CTt = sb("CTt", [128, 130], F32)
        cg = sb("cg", [128, 4, 130], F32)
        hacc = sb("hacc", [128, 130], F32)
        hacc2 = sb("hacc2", [128, 130], F32)
        maskST = sb("maskST", [CL, CL], F32)
        STb = sb("STb", [CL, 4, CL], BF16)
        dnb = sb("dnb", [CL, 4, 4], F32)
        hnb = sb("hnb", [CL, 4, 128], F32)
        oab = sb("oab", [CL, 4, 128], BF16)
        junkb = sb("junkb", [CL, 4, 128], BF16)

        Bk = [ps(f"bank{k}", [128, 512], F32) for k in range(8)]
        bfv = lambda k: Bk[k][:, :].bitcast(BF16)
        ps_big = [Bk[0], Bk[1]]
        tp = bfv(2).rearrange("p (a b) -> p a b", b=128)
        pvb = [Bk[3][0:64, 0:256], Bk[4][0:64, 0:256]]
        pab = [Bk[0][0:CL, 0:CL], Bk[1][0:CL, 0:CL]]
        pcb = [Bk[3][0:CL, 0:130], Bk[4][0:CL, 0:130]]
        pub = [Bk[5][:, 0:130], Bk[6][:, 0:130]]
        ptk = [bfv(2)[0:CL, 0:128], bfv(7)[0:CL, 0:128]]
        pto = [bfv(6)[:, 0:CL], bfv(7)[:, 0:CL]]
        KPV = ["bank3", "bank4"]; KPA = ["bank0", "bank1"]; KPC = ["bank3", "bank4"]; KPU = ["bank5", "bank6"]
        KPTK = ["bank2", "bank7"]; KPTO = ["bank6", "bank7"]

        P.dma("sp", lambda e: e.dma_start(out=g_bc[:, :], in_=g_mix[0:1, :].partition_broadcast(128)), "setup", writes=["g_bc"])
        P.dma("sp", lambda e: e.dma_start(out=cwt[:, :, :], in_=conv_wT[:, :, :]), "setup", writes=["cwt"])
        P.dma("sp", lambda e: e.dma_start(out=cbt[:, :], in_=conv_b[:, :]), "setup", writes=["cbt"])
        P.dma("sp", lambda e: e.dma_start(out=fbt[0:1, :], in_=f_bias[0:1, :]), "setup", writes=["fbt0"])
        P.dma("sp", lambda e: e.dma_start(out=fbt[32:33, :], in_=i_bias[0:1, :]), "setup", writes=["fbt32"])
        P.dma("sp", lambda e: e.dma_start(out=gAc[:, :], in_=g_mlc[:, :]), "setup", writes=["gAc"])
        P.dma("sp", lambda e: e.dma_start(out=pm[:, :], in_=pmask[:, :]), "setup", writes=["pm"])
        P.bulk_done("setup")

        P.seq("pool", [lambda e: e.memset(identf[:, :], 0.0),
                       lambda e: e.affine_select(out=identf[:, :], in_=identf[:, :], pattern=[[-1, 128]], compare_op=ALU.not_equal,
                                                 fill=1.0, base=0, channel_multiplier=1)], writes=["identf"])
        P.op("dve", lambda e: e.tensor_copy(out=ident[:, :], in_=identf[:, :]), reads=["identf"], writes=["ident"])

        P.seq("pool", [lambda e: e.memset(cmask[:, :], 1.0),
                       lambda e: e.memset(cmask[:, 0:NTOK:CL], 0.0),
                       lambda e: e.memset(sel0[:, :], 0.0),
                       lambda e: e.memset(sel0[0:1, :], 1.0),
                       lambda e: e.memset(sel032[:, :], 0.0),
                       lambda e: e.memset(sel032[0:1, :], 1.0),
                       lambda e: e.memset(sel032[32:33, :], 1.0),
                       lambda e: e.memset(Gt2[:, :], 0.0),
                       lambda e: e.memset(v_ext[:, :, 128:129], 1.0),
                       lambda e: e.memset(v_ext[:, :, 129:130], 0.0),
                       lambda e: e.memset(maskST[:, :], 1.0),
                       lambda e: e.affine_select(out=maskST[:, :], in_=maskST[:, :], pattern=[[1, CL]], compare_op=ALU.is_ge,
                                                 fill=0.0, base=0, channel_multiplier=-1)],
              writes=["cmask", "sel0", "sel032", "Gt2", "v_ext_c", "maskST"])
        P.op("pool", lambda e: e.tensor_scalar(out=nfb[0:1, :], in0=fbt[0:1, :], scalar1=-1.0, scalar2=None, op0=ALU.mult),
             reads=["fbt0"], writes=["nfb"])

        def norm_tile(src_ap, rows, slot, col0, idx, tag):
            s = slot
            P.dma("sp", lambda e: e.dma_start(out=xt[s][0:rows, :], in_=src_ap), f"xt{s}", writes=[f"xt{s}"])
            P.op("act", lambda e: e.activation(out=junk[0:rows, :], in_=xt[s][0:rows, :], func=AF.Square, accum_out=ssq[0:rows, idx:idx + 1]),
                 reads=[f"xt{s}"], writes=["junk", f"ssq{idx}"])
            P.op("act", lambda e: e.activation(out=ssq[0:rows, idx:idx + 1], in_=ssq[0:rows, idx:idx + 1], func=AF.Sqrt, scale=1.0 / D, bias=EPS),
                 reads=[f"ssq{idx}"], writes=[f"ssq{idx}"])
            P.op("dve", lambda e: e.reciprocal(out=rstd[0:rows, idx:idx + 1], in_=ssq[0:rows, idx:idx + 1]), reads=[f"ssq{idx}"], writes=[f"rstd{idx}"])
            P.op("dve", lambda e: e.scalar_tensor_tensor(out=hb[0:rows, :], in0=xt[s][0:rows, :], scalar=rstd[0:rows, idx:idx + 1], in1=g_bc[0:rows, :],
                                                         op0=ALU.mult, op1=ALU.mult),
                 reads=[f"xt{s}", f"rstd{idx}", "g_bc"], writes=["hb"])
            def f_tp(e):
                for k in range(8):
                    i = e.transpose(out=tp[:, k, 0:rows], in_=hb[0:rows, k * 128:(k + 1) * 128], identity=ident[0:rows, 0:rows])
                return i
            P.op("pe", f_tp, reads=["hb", "ident"], writes=["bank2"])
            P.op("act", lambda e: e.copy(out=hT[:, :, col0:col0 + rows], in_=tp[:, :, 0:rows]), reads=["bank2"], writes=[tag])

        norm_tile(xh[:, :], HALO, 0, 0, NT, "hT_h")
        for t in range(NT):
            norm_tile(x[t * 128:(t + 1) * 128, :], 128, (t + 1) % 2, HALO + t * 128, t, f"hT_{t // 4}")
        hT_all = ["hT_h"] + [f"hT_{i}" for i in range(4)]

        SC = float(128 ** -0.5)
        for hd in range(4 if stage >= 2 else 0):
            for j, c0 in enumerate((hd * 128, 512 + hd * 128, 1024 + hd * 128, 1536 + hd * 128)):
                P.dma("pool", lambda e, j=j, c0=c0: e.dma_start(out=wh[:, :, j * 128:(j + 1) * 128], in_=w_in_v[:, :, c0:c0 + 128]),
                      "wh", writes=[f"wh{j}"])
            P.bulk_done("wh")
            P.op("pool", lambda e: e.memset(wg[:, :, :], 0.0), writes=["wg", "wg_a", "wg_b"])
            def ld_wg(e, dst, col):
                with nc.allow_non_contiguous_dma(reason="gate columns"):
                    return e.dma_start(out=wg[:, :, dst:dst + 1], in_=w_in_v[:, :, col:col + 1])
            P.dma("pool", lambda e, hd=hd: ld_wg(e, 0, 2052 + hd), "wg", reads=["wg"], writes=["wg_a"])
            P.dma("pool", lambda e, hd=hd: ld_wg(e, 32, 2048 + hd), "wg", reads=["wg"], writes=["wg_b"])
            P.bulk_done("wg")

            for tb in range(4):
                pgt = ps_big[tb % 2]
                def f_g(e, tb=tb, pgt=pgt):
                    for k in range(8):
                        i = e.matmul(out=pgt[0:33, :], lhsT=wg[:, k, :], rhs=hT[:, k, HALO + tb * 512:HALO + (tb + 1) * 512], start=(k == 0), stop=(k == 7))
                    return i
                P.op("pe", f_g, reads=["wg", "wg_a", "wg_b", f"hT_{tb}"], writes=[f"bank{tb % 2}"])
                P.op("act", lambda e, tb=tb, pgt=pgt: e.copy(out=Gt[0:33, tb * 512:(tb + 1) * 512], in_=pgt[0:33, :]),
                     reads=[f"bank{tb % 2}"], writes=[f"Gt_{tb}"])
            Gt_all = [f"Gt_{tb}" for tb in range(4)]
            P.op("act", lambda e, hd=hd: e.activation(out=Gt[0:1, :], in_=Gt[0:1, :], func=AF.Exp, scale=-1.0, bias=nfb[0:1, hd:hd + 1]),
                 reads=Gt_all + ["nfb"], writes=["Gt_f"])
            P.op("act", lambda e: e.activation(out=Gt[0:1, :], in_=Gt[0:1, :], func=AF.Ln, bias=1.0), reads=["Gt_f"], writes=["Gt_f"])
            P.op("dve", lambda e: e.tensor_tensor_scan(out=Gt2[0:1, :], data0=cmask[0:1, :], data1=Gt[0:1, :], initial=0.0, op0=ALU.mult, op1=ALU.add),
                 reads=["Gt_f", "cmask"], writes=["Gt2_0"])
            P.op("act", lambda e, hd=hd: e.activation(out=Gt2[32:33, :], in_=Gt[32:33, :], func=AF.Identity, bias=fbt[32:33, hd:hd + 1]),
                 reads=Gt_all + ["fbt32", "Gt2"], writes=["Gt2_32"])
            for tb in range(4):
                pgt = ps_big[tb % 2]
                P.op("pe", lambda e, tb=tb, pgt=pgt: e.matmul(out=pgt[:, :], lhsT=sel0[0:33, :], rhs=Gt2[0:33, tb * 512:(tb + 1) * 512], start=True, stop=True),
                     reads=["sel0", "Gt2", "Gt2_0", "Gt2_32"], writes=[f"bank{tb % 2}"])
                P.op("act", lambda e, tb=tb, pgt=pgt: e.activation(out=expb[:, tb * 512:(tb + 1) * 512], in_=pgt[:, :], func=AF.Exp, scale=-1.0),
                     reads=[f"bank{tb % 2}"], writes=[f"expb_{tb}"])
            for tb in range(4):
                pgt = ps_big[tb % 2]
                P.op("pe", lambda e, tb=tb, pgt=pgt: e.matmul(out=pgt[:, :], lhsT=sel032[0:33, :], rhs=Gt2[0:33, tb * 512:(tb + 1) * 512], start=True, stop=True),
                     reads=["sel032", "Gt2", "Gt2_0", "Gt2_32"], writes=[f"bank{tb % 2}"])
                P.op("act", lambda e, tb=tb, pgt=pgt: e.activation(out=e2bc[:, tb * 512:(tb + 1) * 512], in_=pgt[:, :], func=AF.Exp),
                     reads=[f"bank{tb % 2}"], writes=[f"e2bc_{tb}"])
            expb_all = [f"expb_{tb}" for tb in range(4)]
            e2_all = [f"e2bc_{tb}" for tb in range(4)]

            for qi in range(2):
                cidx = qi * 4 + hd
                def f_halo(e, qi=qi):
                    for k in range(8):
                        i = e.matmul(out=ps_big[0][:, 0:HALO], lhsT=wh[:, k, qi * 128:(qi + 1) * 128], rhs=hT[:, k, 0:HALO], start=(k == 0), stop=(k == 7))
                    return i
                P.op("pe", f_halo, reads=[f"wh{qi}", "hT_h"], writes=["bank0"])
                P.op("act", lambda e: e.copy(out=raw[:, 0:3], in_=ps_big[0][:, HALO - 3:HALO]), reads=["bank0"], writes=["raw_h"])
                for tb in range(4):
                    pgt = ps_big[(tb + 1) % 2]
                    def f_q(e, tb=tb, qi=qi, pgt=pgt):
                        for k in range(8):
                            i = e.matmul(out=pgt[:, :], lhsT=wh[:, k, qi * 128:(qi + 1) * 128], rhs=hT[:, k, HALO + tb * 512:HALO + (tb + 1) * 512],
                                         start=(k == 0), stop=(k == 7))
                        return i
                    P.op("pe", f_q, reads=[f"wh{qi}", f"hT_{tb}"], writes=[f"bank{(tb + 1) % 2}"])
                    P.op("act", lambda e, tb=tb, pgt=pgt: e.copy(out=raw[:, 3 + tb * 512:3 + (tb + 1) * 512], in_=pgt[:, :]),
                         reads=[f"bank{(tb + 1) % 2}"], writes=[f"raw_{tb}"])
                raw_all = ["raw_h"] + [f"raw_{tb}" for tb in range(4)]
                fl = [lambda e, cidx=cidx: e.tensor_scalar(out=ctmp[:, :], in0=raw[:, 0:NTOK], scalar1=cwt[:, cidx, 0:1], scalar2=cbt[:, cidx:cidx + 1],
                                                           op0=ALU.mult, op1=ALU.add)]
                for j in (1, 2, 3):
                    fl.append(lambda e, cidx=cidx, j=j: e.scalar_tensor_tensor(out=ctmp[:, :], in0=raw[:, j:j + NTOK], scalar=cwt[:, cidx, j:j + 1],
                                                                               in1=ctmp[:, :], op0=ALU.mult, op1=ALU.add))
                P.seq("dve", fl, reads=raw_all + ["cwt", "cbt"], writes=["ctmp"])
                P.op("act", lambda e: e.activation(out=ctmp[:, :], in_=ctmp[:, :], func=AF.Silu), reads=["ctmp"], writes=["ctmp"])
                if qi == 0:
                    P.op("dve", lambda e: e.tensor_tensor(out=qsT[:, :], in0=ctmp[:, :], in1=expb[:, :], op=ALU.mult),
                         reads=["ctmp"] + expb_all, writes=["qsT"])
                else:
                    P.op("dve", lambda e: e.scalar_tensor_tensor(out=ksT[:, :], in0=ctmp[:, :], scalar=SC, in1=e2bc[:, :], op0=ALU.mult, op1=ALU.mult),
                         reads=["ctmp"] + e2_all, writes=["ksT"])

            for vi in range(2):
                for tb in range(4):
                    pgt = ps_big[tb % 2]
                    def f_vo(e, tb=tb, vi=vi, pgt=pgt):
                        for k in range(8):
                            i = e.matmul(out=pgt[:, :], lhsT=wh[:, k, 256 + vi * 128:384 + vi * 128], rhs=hT[:, k, HALO + tb * 512:HALO + (tb + 1) * 512],
                                         start=(k == 0), stop=(k == 7))
                        return i
                    P.op("pe", f_vo, reads=[f"wh{2 + vi}", f"hT_{tb}"], writes=[f"bank{tb % 2}"])
                    if vi == 0:
                        P.op("act", lambda e, tb=tb, pgt=pgt: e.copy(out=vT[:, tb * 512:(tb + 1) * 512], in_=pgt[:, :]), reads=[f"bank{tb % 2}"], writes=[f"vT_{tb}"])
                    else:
                        P.op("act", lambda e, tb=tb, pgt=pgt: e.activation(out=ctmp[:, tb * 512:(tb + 1) * 512], in_=pgt[:, :], func=AF.Sigmoid),
                             reads=[f"bank{tb % 2}"], writes=["ctmp"])
            P.op("dve", lambda e, hd=hd: e.tensor_scalar(out=sigoT[:, :], in0=ctmp[:, :], scalar1=gAc[:, hd:hd + 1], scalar2=None, op0=ALU.mult),
                 reads=["ctmp", "gAc"], writes=["sigoT"])

            for c in range(NCH):
                P.op("pe", lambda e, c=c: e.transpose(out=ptk[0], in_=vT[:, c * CL:(c + 1) * CL], identity=ident[:, :]),
                     reads=[f"vT_{c // 4}", "ident"], writes=[KPTK[0]])
                P.op("act", lambda e, c=c: e.copy(out=v_ext[:, c, 0:128], in_=ptk[0]), reads=[KPTK[0]], writes=[f"v_{c}"])
                P.op("pe", lambda e, c=c: e.transpose(out=ptk[1], in_=ksT[:, c * CL:(c + 1) * CL], identity=ident[:, :]),
                     reads=["ksT", "ident"], writes=[KPTK[1]])
                P.op("act", lambda e, c=c: e.copy(out=kstok[:, c, :], in_=ptk[1]), reads=[KPTK[1]], writes=[f"kstok_{c}"])

            P.seq("pool", [lambda e: e.memset(CTloc[:, 0, :], 0.0), lambda e: e.memset(CTloc[:, 0, 129:130], 1.0)], writes=["CTloc_0"])
            for c in range(NCH):
                b = c % 2
                ub = c % 4
                wc = expb[:, c * CL + CL - 1:c * CL + CL]
                P.op("pe", lambda e, c=c, b=b: e.matmul(out=pub[b], lhsT=kstok[:, c, :], rhs=v_ext[:, c, :], start=True, stop=True),
                     reads=[f"kstok_{c}", f"v_{c}", "v_ext_c"], writes=[KPU[b]])
                P.op("act", lambda e, b=b, ub=ub, wc=wc: e.activation(out=Us[:, ub, :], in_=pub[b], func=AF.Copy, scale=wc),
                     reads=[KPU[b], f"expb_{c // 4}"], writes=[f"Us{ub}"])
                P.op("dve", lambda e, c=c, ub=ub, wc=wc: e.scalar_tensor_tensor(out=CTloc[:, c + 1, :], in0=CTloc[:, c, :], scalar=wc, in1=Us[:, ub, :],
                                                                              op0=ALU.mult, op1=ALU.add),
                     reads=[f"Us{ub}", f"CTloc_{c}", f"expb_{c // 4}"], writes=[f"CTloc_{c + 1}"])

            P.dma("sp", lambda e, hd=hd: e.dma_start(out=cc_in[hd][:, :], in_=CTloc[:, NCH, :]), f"ccin{hd}", reads=[f"CTloc_{NCH}"], writes=[f"cc_in{hd}"])
            def f_cc(e, hd=hd):
                return e.collective_compute("AllGather", ALU.bypass, replica_groups=GROUPS4,
                                            ins=[cc_in[hd].ap().opt()], outs=[cc_out[hd].ap().opt()])
            P.dma("pool", f_cc, f"cc{hd}", reads=[f"cc_in{hd}"], writes=[f"cc_out{hd}"], inc=1)
            P.dma("sp", lambda e, hd=hd: e.dma_start(out=cg[:, :, :], in_=cc_out[hd].ap().rearrange("(r p) c -> p r c", p=128)),
                  f"ccld{hd}", reads=[f"cc_out{hd}"], writes=["cg"])
            P.op("pool", lambda e: e.memset(hacc[:, :], 0.0), writes=["hacc"])
            for pp in range(3):
                P.seq("dve", [
                    lambda e, pp=pp: e.scalar_tensor_tensor(out=hacc2[:, :], in0=hacc[:, :], scalar=cg[:, pp, 129:130], in1=cg[:, pp, :], op0=ALU.mult, op1=ALU.add),
                    lambda e: e.tensor_tensor(out=hacc2[:, :], in0=hacc2[:, :], in1=hacc[:, :], op=ALU.subtract),
                    lambda e, pp=pp: e.scalar_tensor_tensor(out=hacc[:, :], in0=hacc2[:, :], scalar=pm[:, pp:pp + 1], in1=hacc[:, :], op0=ALU.mult, op1=ALU.add)],
                    reads=["hacc", "cg", "pm"], writes=["hacc", "hacc2"])
            for c in range(NCH):
                eng = "dve"
                P.op(eng, lambda e, c=c: e.scalar_tensor_tensor(out=CTball[:, c, :], in0=hacc[:, :], scalar=CTloc[:, c, 129:130], in1=CTloc[:, c, :],
                                                                op0=ALU.mult, op1=ALU.add),
                     reads=["hacc", f"CTloc_{c}"], writes=[f"CTb_{c}"])

            NB = 2
            for c0 in range(0, NCH, NB):
                cl = list(range(c0, c0 + NB))
                for c in cl:
                    b = c % NB
                    cs = slice(c * CL, (c + 1) * CL)
                    P.op("pe", lambda e, cs=cs, b=b: e.matmul(out=pab[b], lhsT=ksT[:, cs], rhs=qsT[:, cs], start=True, stop=True),
                         reads=["ksT", "qsT"], writes=[KPA[b]])
                for c in cl:
                    b = c % NB
                    P.op("dve", lambda e, b=b: e.tensor_tensor(out=STb[:, b, :], in0=pab[b], in1=maskST[:, :], op=ALU.mult), reads=[KPA[b], "maskST"], writes=[f"ST{b}"])
                for c in cl:
                    b = c % NB
                    cs = slice(c * CL, (c + 1) * CL)
                    def f_cn(e, c=c, cs=cs, b=b):
                        e.matmul(out=pcb[b], lhsT=STb[:, b, :], rhs=v_ext[:, c, :], start=True, stop=False)
                        return e.matmul(out=pcb[b], lhsT=qsT[:, cs], rhs=CTball[:, c, :], start=False, stop=True)
                    P.op("pe", f_cn, reads=[f"ST{b}", f"v_{c}", "v_ext_c", "qsT", f"CTb_{c}"], writes=[KPC[b]])
                for c in cl:
                    b = c % NB
                    P.seq("dve", [
                        lambda e, b=b: e.tensor_copy(out=dnb[:, b, 1:2], in_=pcb[b][:, 128:129]),
                        lambda e, b=b: e.scalar_tensor_tensor(out=dnb[:, b, 0:1], in0=dnb[:, b, 1:2], scalar=-1.0, in1=dnb[:, b, 1:2], op0=ALU.mult, op1=ALU.max),
                        lambda e, b=b: e.tensor_scalar(out=dnb[:, b, 0:1], in0=dnb[:, b, 0:1], scalar1=1.0, scalar2=None, op0=ALU.max),
                        lambda e, b=b: e.reciprocal(out=dnb[:, b, 1:2], in_=dnb[:, b, 0:1]),
                        lambda e, b=b: e.tensor_scalar(out=hnb[:, b, :], in0=pcb[b][:, 0:128], scalar1=dnb[:, b, 1:2], scalar2=None, op0=ALU.mult)],
                        reads=[KPC[b]], writes=[f"dn{b}", f"hn{b}"])
                for c in cl:
                    b = c % NB
                    P.seq("act", [
                        lambda e, b=b: e.activation(out=junkb[:, b, :], in_=hnb[:, b, :], func=AF.Square, accum_out=dnb[:, b, 2:3]),
                        lambda e, b=b: e.activation(out=dnb[:, b, 2:3], in_=dnb[:, b, 2:3], func=AF.Sqrt, scale=1.0 / 128, bias=EPS)],
                        reads=[f"hn{b}", f"dn{b}"], writes=[f"junk{b}", f"dn2_{b}"])
                for c in cl:
                    b = c % NB
                    P.seq("dve", [
                        lambda e, b=b: e.reciprocal(out=dnb[:, b, 3:4], in_=dnb[:, b, 2:3]),
                        lambda e, c=c, b=b: e.tensor_scalar(out=oab[:, b, :], in0=hnb[:, b, :], scalar1=dnb[:, b, 3:4], scalar2=None, op0=ALU.mult)],
                        reads=[f"dn2_{b}", f"hn{b}"], writes=[f"oa{b}", f"dn{b}"])
                for c in cl:
                    b = c % NB
                    P.op("pe", lambda e, b=b: e.transpose(out=pto[b], in_=oab[:, b, :], identity=ident[0:CL, 0:CL]), reads=[f"oa{b}", "ident"], writes=[KPTO[b]])
                for c in cl:
                    b = c % NB
                    cs = slice(c * CL, (c + 1) * CL)
                    P.op("dve", lambda e, cs=cs, b=b: e.tensor_tensor(out=mslab[:, cs], in0=pto[b], in1=sigoT[:, cs], op=ALU.mult), reads=[KPTO[b], "sigoT"], writes=["mslab"])
            P.dma("sp", lambda e, hd=hd: e.dma_start(out=mergedT_scr[hd * 128:(hd + 1) * 128, :], in_=mslab[:, :]), "mscr", reads=["mslab"], writes=[f"mscr{hd}"])

        esB.close()
        P.barrier()
        if stage >= 3:
            esC1 = ExitStack()
            esC1.__enter__()
            sb = lambda name, shape, dt: esC1.enter_context(nc.sbuf_tensor(name, shape, dt))
            ps = lambda name, shape, dt: esC1.enter_context(nc.psum_tensor(name, shape, dt))
            wu = sb("wu", [128, 8, 512], BF16)
            utok = [sb(f"utok{i}", [128, 512], BF16) for i in range(2)]
            pu1 = [ps(f"pu1_{i}", [128, 512], F32) for i in range(2)]
            P.dma("pool", lambda e: e.dma_start(out=wu[:, :, :], in_=w_in_v[:, :, 2056:2568]), "wu", writes=["wu"])
            for t in range(NT):
                def f_u1(e, t=t):
                    for k in range(8):
                        i = e.matmul(out=pu1[t % 2][:, :], lhsT=hT[:, k, HALO + t * 128:HALO + (t + 1) * 128], rhs=wu[:, k, :], start=(k == 0), stop=(k == 7))
                    return i
                P.op("pe", f_u1, reads=["wu", f"hT_{t // 4}"], writes=[f"pu1_{t % 2}"])
                P.op("act", lambda e, t=t: e.copy(out=utok[t % 2][:, :], in_=pu1[t % 2][:, :]), reads=[f"pu1_{t % 2}"], writes=[f"utok{t % 2}"])
                P.dma("sp", lambda e, t=t: e.dma_start(out=u_scr[t * 128:(t + 1) * 128, :], in_=utok[t % 2][:, :]), f"uscr{t % 2}",
                      reads=[f"utok{t % 2}"], writes=[f"u_scr{t}"])
            esC1.close()
            esH.close()
            P.barrier()

            TT = lambda o, a, b, op: (lambda e: e.tensor_tensor(out=o, in0=a, in1=b, op=op))
            TS = lambda o, a, s1, s2, op0, op1=None: ((lambda e: e.tensor_scalar(out=o, in0=a, scalar1=s1, scalar2=s2, op0=op0, op1=op1)) if op1 is not None
                                                      else (lambda e: e.tensor_scalar(out=o, in0=a, scalar1=s1, scalar2=None, op0=op0)))
            ACT = lambda o, a, f, **kw: (lambda e: e.activation(out=o, in_=a, func=f, **kw))
            MUL, ADD, SUB = ALU.mult, ALU.add, ALU.subtract

            def cmul(o_r, o_i, a_r, a_i, s_r, s_i, t1, t2):
                return [TT(t1, a_r, s_r, MUL), TT(t2, a_i, s_i, MUL), TT(o_r, t1, t2, SUB),
                        TT(t1, a_r, s_i, MUL), TT(t2, a_i, s_r, MUL), TT(o_i, t1, t2, ADD)]

            def emit_prep(GP, gs, lam_re_t, lam_im_t, logdt_t, X1t, X2t, sgn, hpi, sc, sqr, sqi, bqr, bqi, a5r, a5i, PWfr, PWfi, PWrr, PWri, PWbr, PWbi, cta, ctb, BB1, BB2, bbt, PRs, nPI, PLr, PLi):
                ops = []
                S = {k: v[:, :] for k, v in sc.items()}
                ops = []
                ops.append(TS(S["lr"], lam_re_t[:, gs], -1e-4, None, ALU.min))
                P.seq("dve", ops, reads=["lam_re_t"], writes=["prep"]); ops = []
                P.op("act", ACT(S["dt"], logdt_t[:, gs], AF.Exp), reads=["logdt_t", "prep"], writes=["prep_dt"])
                ops += [TT(S["lrdt"], S["lr"], S["dt"], MUL), TT(S["th"], lam_im_t[:, gs], S["dt"], MUL)]
                P.seq("dve", ops, reads=["prep", "prep_dt", "lam_im_t"], writes=["prep"]); ops = []
                P.seq("act", [ACT(S["sn"], S["th"], AF.Sin, scale=1.0 / 32), ACT(S["cs"], S["th"], AF.Sin, scale=1.0 / 32, bias=hpi[:, 0:1]),
                              ACT(S["mag"], S["lrdt"], AF.Exp, scale=1.0 / 32), ACT(S["im2"], S["lrdt"], AF.Exp, scale=-2.0)],
                      reads=["prep", "hpi"], writes=["prep_cs"])
                li = lam_im_t[:, gs]
                ops += [TT(a5r[:, :, 0], S["mag"], S["cs"], MUL), TT(a5i[:, :, 0], S["mag"], S["sn"], MUL)]
                for e_ in range(5):
                    ops += cmul(a5r[:, :, e_ + 1], a5i[:, :, e_ + 1], a5r[:, :, e_], a5i[:, :, e_], a5r[:, :, e_], a5i[:, :, e_], S["ta"], S["nsq"])
                ops += [(lambda e: e.tensor_copy(out=S["ar"], in_=a5r[:, :, 5])), (lambda e: e.tensor_copy(out=S["ai"], in_=a5i[:, :, 5])),
                        TT(S["den"], S["lr"], S["lr"], MUL), TT(S["ta"], li, li, MUL), TT(S["den"], S["den"], S["ta"], ADD),
                        (lambda e: e.reciprocal(out=S["den"], in_=S["den"])),
                        TS(S["am1"], S["ar"], -1.0, None, ADD),
                        TT(S["ta"], S["am1"], S["lr"], MUL), TT(S["tb"], S["ai"], li, MUL), TT(S["ta"], S["ta"], S["tb"], ADD), TT(S["cr"], S["ta"], S["den"], MUL),
                        TT(S["ta"], S["ai"], S["lr"], MUL), TT(S["tb"], S["am1"], li, MUL), TT(S["ta"], S["ta"], S["tb"], SUB), TT(S["ci"], S["ta"], S["den"], MUL),
                        TS(S["scr"], S["cr"], sgn[:, 0:1], None, MUL),
                        TS(S["tb"], S["ci"], sgn[:, 0:1], None, MUL),
                        TS(S["nci"], S["ci"], -1.0, None, MUL),
                        TT(S["bir"], S["ar"], S["im2"], MUL), TT(S["bii"], S["ai"], S["im2"], MUL), TS(S["bii"], S["bii"], -1.0, None, MUL),
                        (lambda e: e.tensor_copy(out=sqr[:, :, 0], in_=S["ar"])), (lambda e: e.tensor_copy(out=sqi[:, :, 0], in_=S["ai"])),
                        (lambda e: e.tensor_copy(out=bqr[:, :, 0], in_=S["bir"])), (lambda e: e.tensor_copy(out=bqi[:, :, 0], in_=S["bii"]))]
                for e_ in range(11):
                    ops += cmul(sqr[:, :, e_ + 1], sqi[:, :, e_ + 1], sqr[:, :, e_], sqi[:, :, e_], sqr[:, :, e_], sqi[:, :, e_], S["ta"], S["nsq"])
                for e_ in range(2):
                    ops += cmul(bqr[:, :, e_ + 1], bqi[:, :, e_ + 1], bqr[:, :, e_], bqi[:, :, e_], bqr[:, :, e_], bqi[:, :, e_], S["ta"], S["nsq"])
                bc16 = lambda a: a.unsqueeze(2).to_broadcast([128, GP, 16])
                ops += [TT(BB1[:, :, :], X1t[:, gs, :], bc16(S["cr"]), MUL), TT(bbt[:, :, :], X2t[:, gs, :], bc16(S["tb"]), MUL), TT(BB1[:, :, :], BB1[:, :, :], bbt[:, :, :], ADD),
                        TT(BB2[:, :, :], X2t[:, gs, :], bc16(S["scr"]), MUL), TT(bbt[:, :, :], X1t[:, gs, :], bc16(S["nci"]), MUL), TT(BB2[:, :, :], BB2[:, :, :], bbt[:, :, :], ADD)]
                ops += [(lambda e: e.memset(PWfr[:, :, 0:1], 1.0)), (lambda e: e.memset(PWfi[:, :, 0:1], 0.0))]
                for k in range(LL):
                    n = 1 << k
                    bcn = lambda a, n=n: a.to_broadcast([128, GP, n])
                    ops += cmul(PWfr[:, :, n:2 * n], PWfi[:, :, n:2 * n], PWfr[:, :, 0:n], PWfi[:, :, 0:n],
                                bcn(sqr[:, :, k:k + 1]), bcn(sqi[:, :, k:k + 1]), cta[:, :, 0:n], ctb[:, :, 0:n])
                ops += [(lambda e: e.tensor_copy(out=PWfr[:, :, L5:L5 + 1], in_=sqr[:, :, LL:LL + 1])), (lambda e: e.tensor_copy(out=PWfi[:, :, L5:L5 + 1], in_=sqi[:, :, LL:LL + 1]))]
                ops += [(lambda e: e.memset(PWrr[:, :, L5 - 1:L5], 1.0)), (lambda e: e.memset(PWri[:, :, L5 - 1:L5], 0.0))]
                for k in range(LL):
                    n = 1 << k
                    bcn = lambda a, n=n: a.to_broadcast([128, GP, n])
                    ops += cmul(PWrr[:, :, L5 - 2 * n:L5 - n], PWri[:, :, L5 - 2 * n:L5 - n], PWrr[:, :, L5 - n:L5], PWri[:, :, L5 - n:L5],
                                bcn(sqr[:, :, k:k + 1]), bcn(sqi[:, :, k:k + 1]), cta[:, :, 0:n], ctb[:, :, 0:n])
                ops += [(lambda e: e.memset(PWbr[:, :, 0:1], 1.0)), (lambda e: e.memset(PWbi[:, :, 0:1], 0.0))]
                for k in range(3):
                    n = 1 << k
                    bcn = lambda a, n=n: a.to_broadcast([128, GP, n])
                    ops += cmul(PWbr[:, :, n:2 * n], PWbi[:, :, n:2 * n], PWbr[:, :, 0:n], PWbi[:, :, 0:n],
                                bcn(bqr[:, :, k:k + 1]), bcn(bqi[:, :, k:k + 1]), cta[:, :, 0:n], ctb[:, :, 0:n])
                ops += [(lambda e: e.memset(PLr[:, :, 0:1], 1.0)), (lambda e: e.memset(PLi[:, :, 0:1], 0.0))]
                for k in range(6):
                    n = 1 << k
                    bcn = lambda a, n=n: a.to_broadcast([128, GP, n])
                    ops += cmul(PLr[:, :, n:2 * n], PLi[:, :, n:2 * n], PLr[:, :, 0:n], PLi[:, :, 0:n],
                                bcn(sqr[:, :, LL + k:LL + k + 1]), bcn(sqi[:, :, LL + k:LL + k + 1]), cta[:, :, 0:n], ctb[:, :, 0:n])
                ops += [TS(PRs[:, :, :], PWfr[:, :, :], sgn[:, 0:1], None, MUL), TS(PRs[:, :, :], PRs[:, :, :], -1.0, None, MUL), TS(nPI[:, :, :], PWfi[:, :, :], -1.0, None, MUL)]
                P.seq("dve", ops, reads=["prep", "prep_sn", "prep_cs", "sgn", "X1t", "X2t", "lam_im_t", "Rk_use", "tab_use"], writes=["prep", "prepT"]); ops = []

            esC0 = ExitStack()
            esC0.__enter__()
            sb0 = lambda name, shape, dt: esC0.enter_context(nc.sbuf_tensor(name, shape, dt))
            GA = 32
            i_lre = sb0("i_lre", [128, 32], F32); i_lim = sb0("i_lim", [128, 32], F32); i_ldt = sb0("i_ldt", [128, 32], F32)
            i_X1 = sb0("i_X1", [128, 32, 16], F32); i_X2 = sb0("i_X2", [128, 32, 16], F32)
            i_sgn = sb0("i_sgn", [128, 1], F32); i_hpi = sb0("i_hpi", [128, 1], F32)
            for dst_, src_ in ((i_lre[:, :], lam2_re[:, :]), (i_lim[:, :], lam2_im[:, :]), (i_ldt[:, :], logdt[0:1, :].partition_broadcast(128)),
                               (i_X1[:, :, :], X1d[:, :, :]), (i_X2[:, :, :], X2d[:, :, :])):
                P.dma("sp", lambda e, dst_=dst_, src_=src_: e.dma_start(out=dst_, in_=src_), "setupC0", writes=["c0in"])
            P.bulk_done("setupC0")
            P.seq("pool", [lambda e: e.memset(i_sgn[0:64, :], -1.0), lambda e: e.memset(i_sgn[64:128, :], 1.0), lambda e: e.memset(i_hpi[:, :], float(np.pi / 2))],
                  writes=["sgn", "hpi", "lam_re_t", "lam_im_t", "logdt_t", "X1t", "X2t"], reads=["c0in"])
            sc_names = ("lr", "dt", "lrdt", "th", "mag", "t1", "sn", "cs", "ar", "ai", "den", "am1", "cr", "ci", "ta", "tb", "scr", "nci", "im2", "bir", "bii", "nsq")
            sc0 = {n: sb0("sc0_" + n, [128, GA], F32) for n in sc_names}
            T0 = {n: sb0("p0_" + n, [128, GA, k], F32) for n, k in (("sqr", 12), ("sqi", 12), ("bqr", 3), ("bqi", 3), ("a5r", 6), ("a5i", 6), ("PWfr", L5 + 1), ("PWfi", L5 + 1),
                                                                     ("PWrr", L5), ("PWri", L5), ("PWbr", 8), ("PWbi", 8), ("cta", 64), ("ctb", 64), ("BB1", 16), ("BB2", 16),
                                                                     ("bbt", 16), ("PRs", L5 + 1), ("nPI", L5 + 1), ("PLr", NC5), ("PLi", NC5))}
            emit_prep(GA, slice(0, 32), i_lre, i_lim, i_ldt, i_X1, i_X2, i_sgn, i_hpi, sc0, *[T0[n] for n in ("sqr", "sqi", "bqr", "bqi", "a5r", "a5i", "PWfr", "PWfi", "PWrr", "PWri", "PWbr", "PWbi",
                                                                 "cta", "ctb", "BB1", "BB2", "bbt", "PRs", "nPI", "PLr", "PLi")])
            for nm_ in ("PWrr", "PWri", "PWbr", "PWbi", "BB1", "BB2", "PRs", "nPI", "sqr", "sqi", "PLr", "PLi"):
                P.dma("sp", lambda e, nm_=nm_: e.dma_start(out=prep_scr[nm_][:, :, :], in_=T0[nm_][:, :, :]), "prepst", reads=["prepT"], writes=["prep_scr"])
            P.bulk_done("prepst")
            esC0.close()
            P.barrier()
            esC = ExitStack()
            esC.__enter__()
            sb = lambda name, shape, dt: esC.enter_context(nc.sbuf_tensor(name, shape, dt))
            ps = lambda name, shape, dt: esC.enter_context(nc.psum_tensor(name, shape, dt))
            G8 = 8
            lam_re_t = sb("lam_re_t", [128, 32], F32)
            lam_im_t = sb("lam_im_t", [128, 32], F32)
            logdt_t = sb("logdt_t", [128, 32], F32)
            X1t = sb("X1t", [128, 32, 16], F32)
            X2t = sb("X2t", [128, 32, 16], F32)
            CY1t = sb("CY1t", [128, 32, 16], F32)
            CY2t = sb("CY2t", [128, 32, 16], F32)
            sgn = sb("sgn", [128, 1], F32)
            d_bc = sb("d_bc", [NC5, 512], F32)
            WAt = sb("WAt", [128, 4, 128], BF16)
            WBt = sb("WBt", [128, 4, 128], BF16)
            gba = sb("gba", [128, 4], F32)
            gbb = sb("gbb", [128, 4], F32)
            SW = sb("SW", [128, 128], F32)
            rkt8 = sb("rkt8", [128, G8, 128], F32)
            send = sb("send", [128, G8], F32)
            sc = {n: sb("sc_" + n, [128, G8], F32) for n in
                  ("lr", "dt", "lrdt", "th", "mag", "t1", "sn", "cs", "ar", "ai", "den", "am1", "cr", "ci", "ta", "tb", "scr", "nci", "im2", "bir", "bii", "nsq")}
            a5r = sb("a5r", [128, G8, 6], F32)
            a5i = sb("a5i", [128, G8, 6], F32)
            hpi = sb("hpi", [128, 1], F32)
            sqr2 = [sb(f"sqr_{i}", [128, G8, 12], F32) for i in range(2)]
            sqi2 = [sb(f"sqi_{i}", [128, G8, 12], F32) for i in range(2)]
            bqr = sb("bqr", [128, G8, 3], F32)
            bqi = sb("bqi", [128, G8, 3], F32)
            PWrr2 = [sb(f"PWrr_{i}", [128, G8, L5], F32) for i in range(2)]
            PWri2 = [sb(f"PWri_{i}", [128, G8, L5], F32) for i in range(2)]
            PWbr2 = [sb(f"PWbr_{i}", [128, G8, 8], F32) for i in range(2)]
            PWbi2 = [sb(f"PWbi_{i}", [128, G8, 8], F32) for i in range(2)]
            BB12 = [sb(f"BB1_{i}", [128, G8, 16], F32) for i in range(2)]
            BB22 = [sb(f"BB2_{i}", [128, G8, 16], F32) for i in range(2)]
            bbt = sb("bbt", [128, G8, 16], F32)
            PRs2 = [sb(f"PRs_{i}", [128, G8, L5 + 1], F32) for i in range(2)]
            nPI2 = [sb(f"nPI_{i}", [128, G8, L5 + 1], F32) for i in range(2)]
            PLr2 = [sb(f"PLr_{i}", [128, G8, NC5], F32) for i in range(2)]
            PLi2 = [sb(f"PLi_{i}", [128, G8, NC5], F32) for i in range(2)]
            Wc1 = sb("Wc1", [128, G8, NC5], F32)
            Wc2 = sb("Wc2", [128, G8, NC5], F32)
            hsw = sb("hsw", [128, G8], F32)
            tA = sb("tA", [128, L5 + 1, 16], F32)
            tB = sb("tB", [128, L5 + 1, 16], F32)
            tC = sb("tC", [128, L5 + 1, 16], F32)
            tD = sb("tD", [128, L5 + 1, 16], F32)
            ZTb = sb("ZTb", [128, L5, 16], BF16)
            XTb = sb("XTb", [128, 8, 16], BF16)
            Zt2 = [sb(f"Zt_{i}", [128, G8, KC5, 128], BF16) for i in range(2)]
            Gtab = sb("Gtab", [128, G8, L5 * 16], BF16)
            Yt = sb("Yt", [128, G8, L5 + 1, 16], BF16)
            Rk2 = [sb(f"Rk_{i}", [128, G8, NLEV, 128], F32) for i in range(2)]
            ucj = sb("ucj", [NC5, G8, L5, 16], BF16)
            ustack = sb("ustack", [128, G8, KC5, NC5], BF16)
            E32 = sb("E32", [128, G8, NC5], F32)
            Xs = sb("Xs", [128, G8, NC5 + 1], F32)
            Sprevb = sb("Sprevb", [128, G8, NC5], BF16)
            sg_t = sb("sg_t", [128, 4, G8], F32)
            hs = sb("hs", [128, G8], F32)
            hs2 = sb("hs2", [128, G8], F32)
            du = sb("du", [NC5, L5, 16], F32)
            yv = sb("yv", [NC5, L5 * 16], F32)
            y2 = sb("y2", [NC5, L5 * 16], F32)
            ysg = sb("ysg", [NC5, L5 * 16], F32)
            ygel = sb("ygel", [NC5, L5, 128], BF16)
            ygT = sb("ygT", [128, L5, NC5], BF16)
            sbt = sb("sbt", [128, 512], F32)
            ps_z = [ps(f"ps_z{i}", [128, 512], F32) for i in range(2)]
            pT = ps("pT", [128, 2048], BF16)
            pE = ps("pE", [128, G8, NC5], F32)
            pD = ps("pD", [128, G8, NC5], F32)
            pY = ps("pY", [128, 512], F32)

            def ld(dst, src, key, q="sp"):
                P.dma(q, lambda e: e.dma_start(out=dst, in_=src), "setupC" if q == "sp" else "setupCp", writes=[key])
            ld(lam_re_t[:, :], lam2_re[:, :], "lam_re_t")
            ld(lam_im_t[:, :], lam2_im[:, :], "lam_im_t")
            ld(logdt_t[:, :], logdt[0:1, :].partition_broadcast(128), "logdt_t")
            ld(X1t[:, :, :], X1d[:, :, :], "X1t")
            ld(X2t[:, :, :], X2d[:, :, :], "X2t")
            ld(CY1t[:, :, :], CY1d[:, :, :], "CY1t")
            ld(CY2t[:, :, :], CY2d[:, :, :], "CY2t")
            ld(d_bc[:, :], s5d[0:1, :].partition_broadcast(NC5), "d_bc")
            ld(gba[:, :], gbad[:, :], "gba")
            ld(gbb[:, :], gbbd[:, :], "gbb")
            ld(WAt[:, :, :], WAd.ap().rearrange("u p c -> p u c"), "WAt", q="pool")
            ld(WBt[:, :, :], WBd.ap().rearrange("u p c -> p u c"), "WBt", q="pool")
            P.bulk_done("setupC")
            P.bulk_done("setupCp")
            P.seq("pool", [lambda e: e.memset(sgn[0:64, :], -1.0), lambda e: e.memset(sgn[64:128, :], 1.0)], writes=["sgn"])
            P.op("pool", lambda e: e.memset(hpi[:, :], float(np.pi / 2)), writes=["hpi"])
            P.seq("pool", [lambda e: e.memset(SW[:, :], 0.0),
                           lambda e: e.affine_select(out=SW[:, :], in_=SW[:, :], pattern=[[-1, 128]], compare_op=ALU.not_equal, fill=1.0, base=64, channel_multiplier=1),
                           lambda e: e.affine_select(out=SW[:, :], in_=SW[:, :], pattern=[[-1, 128]], compare_op=ALU.not_equal, fill=1.0, base=-64, channel_multiplier=1)],
                  writes=["SW"])
            P.op("pool", lambda e: e.tensor_scalar(out=SW[:, :], in0=SW[:, :], scalar1=sgn[:, 0:1], scalar2=None, op0=ALU.mult), reads=["SW", "sgn"], writes=["SW"])

            PI = float(np.pi)
            bh = lambda a, n: a.unsqueeze(1).to_broadcast([128, n, 16])
            def u_loads(un):
                par = un % 2
                gs = slice(un * 8, (un + 1) * 8)
                PWrr, PWri, PWbr, PWbi, BB1, BB2, PRs, nPI, sqr, sqi, PLr, PLi = [t2[par] for t2 in (PWrr2, PWri2, PWbr2, PWbi2, BB12, BB22, PRs2, nPI2, sqr2, sqi2, PLr2, PLi2)]
                Zt = Zt2[par]; Rk = Rk2[par]
                KP = f"prepT{par}"; KZ = f"Zt{par}"; KR = [f"Rk{par}_{g}" for g in range(G8)]
                for nm_, dst_ in (("PWrr", PWrr), ("PWri", PWri), ("PWbr", PWbr), ("PWbi", PWbi), ("BB1", BB1), ("BB2", BB2),
                                  ("PRs", PRs), ("nPI", nPI), ("sqr", sqr), ("sqi", sqi), ("PLr", PLr), ("PLi", PLi)):
                    P.dma("sp", lambda e, nm_=nm_, dst_=dst_, gs=gs: e.dma_start(out=dst_[:, :, :], in_=prep_scr[nm_][:, gs, :]), "prepld",
                          reads=["prep_scr", "Rk_use", "tab_use"], writes=[KP])
                P.bulk_done("prepld")


            def u_Z(un):
                par = un % 2
                gs = slice(un * 8, (un + 1) * 8)
                PWrr, PWri, PWbr, PWbi, BB1, BB2, PRs, nPI, sqr, sqi, PLr, PLi = [t2[par] for t2 in (PWrr2, PWri2, PWbr2, PWbi2, BB12, BB22, PRs2, nPI2, sqr2, sqi2, PLr2, PLi2)]
                Zt = Zt2[par]; Rk = Rk2[par]
                KP = f"prepT{par}"; KZ = f"Zt{par}"; KR = [f"Rk{par}_{g}" for g in range(G8)]
                bh = lambda a, n: a.unsqueeze(1).to_broadcast([128, n, 16])
                for g in range(G8):
                    bq = lambda a: a.unsqueeze(2).to_broadcast([128, L5, 16])
                    P.seq("dve", [TT(tA[:, 0:L5, :], bq(PWrr[:, g, :]), bh(BB1[:, g, :], L5), MUL),
                                  TT(tB[:, 0:L5, :], bq(PWri[:, g, :]), bh(BB2[:, g, :], L5), MUL),
                                  TT(ZTb[:, :, :], tA[:, 0:L5, :], tB[:, 0:L5, :], ADD)],
                          reads=[KP, "ZTb_use"], writes=["tA", "tB", "ZTb"])
                    def f_zt(e):
                        for kc in range(KC5):
                            i = e.transpose(out=pT[:, kc * 128:(kc + 1) * 128], in_=ZTb[:, kc * 8:(kc + 1) * 8, :], identity=ident[:, :])
                        return i
                    P.op("pe", f_zt, reads=["ZTb", "ident"], writes=["pT"])
                    P.op("act", lambda e, g=g: e.copy(out=Zt[:, g, :, :], in_=pT[:, 0:KC5 * 128].rearrange("p (a b) -> p a b", b=128)), reads=["pT"], writes=[KZ, "ZTb_use"])

            def u_rk(un):
                par = un % 2
                gs = slice(un * 8, (un + 1) * 8)
                PWrr, PWri, PWbr, PWbi, BB1, BB2, PRs, nPI, sqr, sqi, PLr, PLi = [t2[par] for t2 in (PWrr2, PWri2, PWbr2, PWbi2, BB12, BB22, PRs2, nPI2, sqr2, sqi2, PLr2, PLi2)]
                Zt = Zt2[par]; Rk = Rk2[par]
                KP = f"prepT{par}"; KZ = f"Zt{par}"; KR = [f"Rk{par}_{g}" for g in range(G8)]
                for k in range(NLEV):
                    P.seq("pool", [lambda e, k=k: e.tensor_tensor(out=Rk[:, :, k, :], in0=identf[:, :].unsqueeze(1).to_broadcast([128, G8, 128]),
                                                                  in1=sqr[:, :, LL + k:LL + k + 1].to_broadcast([128, G8, 128]), op=MUL),
                                   lambda e, k=k: e.tensor_tensor(out=rkt8[:, :, :], in0=SW[:, :].unsqueeze(1).to_broadcast([128, G8, 128]),
                                                                  in1=sqi[:, :, LL + k:LL + k + 1].to_broadcast([128, G8, 128]), op=MUL),
                                   lambda e, k=k: e.tensor_tensor(out=Rk[:, :, k, :], in0=Rk[:, :, k, :], in1=rkt8[:, :, :], op=SUB)],
                          reads=[KP, "identf", "SW", "sgn", "Rk_use"], writes=KR + ["rkt"])

            def u_pre(un):
                par = un % 2
                gs = slice(un * 8, (un + 1) * 8)
                PWrr, PWri, PWbr, PWbi, BB1, BB2, PRs, nPI, sqr, sqi, PLr, PLi = [t2[par] for t2 in (PWrr2, PWri2, PWbr2, PWbi2, BB12, BB22, PRs2, nPI2, sqr2, sqi2, PLr2, PLi2)]
                Zt = Zt2[par]; Rk = Rk2[par]
                KP = f"prepT{par}"; KZ = f"Zt{par}"; KR = [f"Rk{par}_{g}" for g in range(G8)]
                for g in range(G8):
                    P.dma("sp", lambda e, un=un, g=g: e.dma_start(out=ucj[:, g, :, :],
                                                                   in_=u_scr.ap().rearrange("(c j) n -> c j n", j=L5)[:, :, un * 128 + g * 16:un * 128 + (g + 1) * 16]),
                          "ucj", reads=[f"u_scr{t}" for t in range(NT)] + ["ucj_use"], writes=[f"ucj{g}"])
                P.bulk_done("ucj")
                US0 = 1024
                for g in range(G8):
                    def f_us(e, g=g):
                        for kc in range(KC5):
                            i = e.transpose(out=pT[:, US0 + kc * NC5:US0 + (kc + 1) * NC5], in_=ucj[:, g, kc * 8:(kc + 1) * 8, :], identity=ident[0:NC5, 0:NC5])
                        return i
                    P.op("pe", f_us, reads=[f"ucj{g}", "ident"], writes=["pT2"])
                    P.op("act", lambda e, g=g: e.copy(out=ustack[:, g, :, :], in_=pT[:, US0:US0 + KC5 * NC5].rearrange("p (a b) -> p a b", b=NC5)), reads=["pT2"], writes=[f"ustack{g}"])
                    def f_E(e, g=g):
                        for kc in range(KC5):
                            i = e.matmul(out=pE[:, g, :], lhsT=Zt[:, g, kc, :], rhs=ustack[:, g, kc, :], start=(kc == 0), stop=(kc == KC5 - 1))
                        return i
                    P.op("pe", f_E, reads=[KZ, f"ustack{g}"], writes=["pE"])
                P.op("act", lambda e: e.copy(out=E32[:, :, :], in_=pE[:, :, :]), reads=["pE"], writes=["E32"])

                NX = NC5 + 1
                def doubling(tag):
                    for k in range(NLEV):
                        n = NX - (1 << k)
                        def f_d(e, k=k, n=n):
                            for g in range(G8):
                                i = e.matmul(out=pD[:, g, 0:n], lhsT=Rk[:, g, k, :], rhs=Xs[:, g, 0:n], start=True, stop=True)
                            return i
                        P.op("pe", f_d, reads=["Xs"] + KR, writes=["pD"])
                        P.op("dve", lambda e, k=k, n=n: e.tensor_tensor(out=Xs[:, :, 1 << k:NX], in0=Xs[:, :, 1 << k:NX], in1=pD[:, :, 0:n], op=ADD),
                             reads=["pD", "Xs"], writes=["Xs"])
                P.seq("dve", [lambda e: e.memset(Xs[:, :, 0:1], 0.0), lambda e: e.tensor_copy(out=Xs[:, :, 1:NX], in_=E32[:, :, :])], reads=["E32"], writes=["Xs"])
                doubling("loc")
                P.op("dve", lambda e: e.tensor_copy(out=send[:, :], in_=Xs[:, :, NC5]), reads=["Xs"], writes=["send"])
                P.dma("sp", lambda e, un=un: e.dma_start(out=ccs_in[un][:, :], in_=send[:, :]), f"ccsin{un}", reads=["send"], writes=[f"ccs_in{un}"])
                P.dma("pool", lambda e, un=un: e.collective_compute("AllGather", ALU.bypass, replica_groups=GROUPS4,
                                                                     ins=[ccs_in[un].ap().opt()], outs=[ccs_out[un].ap().opt()]),
                      f"ccs{un}", reads=[f"ccs_in{un}"], writes=[f"ccs_out{un}"], inc=1)
                P.dma("sp", lambda e, un=un: e.dma_start(out=sg_t[:, :, :], in_=ccs_out[un].ap().rearrange("(r p) c -> p r c", p=128)),
                      f"ccsld{un}", reads=[f"ccs_out{un}"], writes=["sg_t"])

            def u_xyg(un):
                par = un % 2
                gs = slice(un * 8, (un + 1) * 8)
                PWrr, PWri, PWbr, PWbi, BB1, BB2, PRs, nPI, sqr, sqi, PLr, PLi = [t2[par] for t2 in (PWrr2, PWri2, PWbr2, PWbi2, BB12, BB22, PRs2, nPI2, sqr2, sqi2, PLr2, PLi2)]
                Zt = Zt2[par]; Rk = Rk2[par]
                KP = f"prepT{par}"; KZ = f"Zt{par}"; KR = [f"Rk{par}_{g}" for g in range(G8)]
                for g in range(G8):
                    ga = un * 8 + g
                    bq8 = lambda a: a.unsqueeze(2).to_broadcast([128, 8, 16])
                    P.seq("dve", [TT(tA[:, 0:8, :], bq8(PWbr[:, g, :]), bh(BB1[:, g, :], 8), MUL),
                                  TT(tB[:, 0:8, :], bq8(PWbi[:, g, :]), bh(BB2[:, g, :], 8), MUL),
                                  TT(XTb[:, :, :], tA[:, 0:8, :], tB[:, 0:8, :], ADD)],
                          reads=[KP, "tA", "tB", "XTb_use"], writes=["tA", "tB", "XTb"])
                    bqY = lambda a: a.unsqueeze(2).to_broadcast([128, L5 + 1, 16])
                    P.seq("pool", [TT(tC[:, :, :], bqY(PRs[:, g, :]), bh(CY1t[:, ga, :], L5 + 1), MUL),
                                   TT(tD[:, :, :], bqY(nPI[:, g, :]), bh(CY2t[:, ga, :], L5 + 1), MUL),
                                   TT(Yt[:, g, :, :], tC[:, :, :], tD[:, :, :], ADD)],
                          reads=[KP, "CY1t", "CY2t", "tab_use"], writes=["tC", "tD", f"Yt{g}"])
                    P.op("pe", lambda e, g=g: e.matmul(out=pY[:, :], lhsT=XTb[:, :, :], rhs=Yt[:, g, 0:L5, :], start=True, stop=True), reads=["XTb", f"Yt{g}"], writes=["pY"])
                    P.op("act", lambda e, g=g: e.copy(out=Gtab[:, g, :], in_=pY[:, :]), reads=["pY", "tab_use"], writes=[f"Gtab{g}", "XTb_use"])
                    P.op("pool", lambda e, g=g: e.affine_select(out=Gtab[:, g, :].rearrange("p (m h) -> p m h", h=16), in_=Gtab[:, g, :].rearrange("p (m h) -> p m h", h=16),
                                                                pattern=[[16, L5], [0, 16]], compare_op=ALU.is_ge, fill=0.0, base=15, channel_multiplier=-1),
                         reads=[f"Gtab{g}"], writes=[f"Gtab{g}"])


            def u_post(un):
                par = un % 2
                gs = slice(un * 8, (un + 1) * 8)
                PWrr, PWri, PWbr, PWbi, BB1, BB2, PRs, nPI, sqr, sqi, PLr, PLi = [t2[par] for t2 in (PWrr2, PWri2, PWbr2, PWbi2, BB12, BB22, PRs2, nPI2, sqr2, sqi2, PLr2, PLi2)]
                Zt = Zt2[par]; Rk = Rk2[par]
                KP = f"prepT{par}"; KZ = f"Zt{par}"; KR = [f"Rk{par}_{g}" for g in range(G8)]
                P.op("dve", lambda e: e.memset(hs[:, :], 0.0), writes=["hs"])
                for pp in range(3):
                    def f_hr(e):
                        for g in range(G8):
                            i = e.matmul(out=pD[:, g, 0:1], lhsT=Rk[:, g, NLEV - 1, :], rhs=hs[:, g:g + 1], start=True, stop=True)
                        return i
                    P.op("pe", f_hr, reads=["hs"] + KR, writes=["pD"])
                    P.seq("dve", [lambda e, pp=pp: e.tensor_tensor(out=hs2[:, :], in0=pD[:, :, 0], in1=sg_t[:, pp, :], op=ADD),
                                  lambda e: e.tensor_tensor(out=hs2[:, :], in0=hs2[:, :], in1=hs[:, :], op=SUB),
                                  lambda e, pp=pp: e.scalar_tensor_tensor(out=hs[:, :], in0=hs2[:, :], scalar=pm[:, pp:pp + 1], in1=hs[:, :], op0=MUL, op1=ADD)],
                          reads=["pD", "sg_t", "hs", "pm"], writes=["hs", "hs2"])
                P.op("pe", lambda e: e.matmul(out=pD[:, 0, 0:G8], lhsT=SW[:, :], rhs=hs[:, :], start=True, stop=True), reads=["hs", "SW"], writes=["pD"])
                bcc = lambda a: a.unsqueeze(2).to_broadcast([128, G8, NC5])
                P.seq("dve", [lambda e: e.tensor_copy(out=hsw[:, :], in_=pD[:, 0, 0:G8]),
                              lambda e: e.tensor_tensor(out=Wc1[:, :, :], in0=PLr[:, :, :], in1=bcc(hs[:, :]), op=MUL),
                              lambda e: e.tensor_tensor(out=Wc2[:, :, :], in0=PLi[:, :, :], in1=bcc(hsw[:, :]), op=MUL),
                              lambda e: e.tensor_tensor(out=Wc1[:, :, :], in0=Wc1[:, :, :], in1=Wc2[:, :, :], op=SUB),
                              lambda e: e.tensor_tensor(out=Sprevb[:, :, :], in0=Wc1[:, :, :], in1=Xs[:, :, 0:NC5], op=ADD)],
                      reads=["pD", "hs", KP, "Xs"], writes=["hsw", "Wc", "Sprevb"])

                for g in range(G8):
                    def f_y(e, g=g):
                        e.matmul(out=pY[0:NC5, :], lhsT=Sprevb[:, g, :], rhs=Yt[:, g, 1:L5 + 1, :], start=True, stop=False)
                        for kc in range(KC5):
                            lo = 128 * kc
                            i = e.matmul(out=pY[0:NC5, lo:512], lhsT=ustack[:, g, kc, :], rhs=Gtab[:, g, 0:512 - lo], start=False, stop=(kc == KC5 - 1))
                        return i
                    P.op("pe", f_y, reads=["Sprevb", f"Yt{g}", f"Gtab{g}", f"ustack{g}"], writes=["pY"])
                    ga = un * 8 + g
                    P.op("pool", lambda e, g=g, ga=ga: e.tensor_tensor(out=du[:, :, :], in0=ucj[:, g, :, :],
                                                                       in1=d_bc[:, ga * 16:(ga + 1) * 16].unsqueeze(1).to_broadcast([NC5, L5, 16]), op=MUL),
                         reads=[f"ucj{g}", "d_bc"], writes=["du"])
                    yv_ = yv[:, :]
                    P.seq("dve", [lambda e: e.tensor_tensor(out=yv[:, :], in0=pY[0:NC5, :], in1=du[:, :, :].rearrange("p a b -> p (a b)"), op=ADD),
                                  TT(y2[:, :], yv_, yv_, MUL), TS(y2[:, :], y2[:, :], 0.044715, 1.0, MUL, ADD), TT(y2[:, :], y2[:, :], yv_, MUL)],
                          reads=["pY", "du", "ysg"], writes=["yv", "y2"])
                    P.op("act", ACT(ysg[:, :], y2[:, :], AF.Sigmoid, scale=1.5957691216057308), reads=["y2"], writes=["ysg"])
                    P.op("dve", lambda e, g=g: e.tensor_tensor(out=ygel[:, :, g * 16:(g + 1) * 16], in0=yv[:, :].rearrange("p (a b) -> p a b", b=16),
                                                               in1=ysg[:, :].rearrange("p (a b) -> p a b", b=16), op=MUL),
                         reads=["yv", "ysg", "ygel_use"], writes=[f"ygel{g}"])
                def f_gt(e):
                    for j in range(L5):
                        i = e.transpose(out=pT[:, j * NC5:(j + 1) * NC5], in_=ygel[:, j, :], identity=ident[0:NC5, 0:NC5])
                    return i
                P.op("pe", f_gt, reads=[f"ygel{g}" for g in range(G8)] + ["ident"], writes=["pT", "pT2"])
                P.op("act", lambda e: e.copy(out=ygT[:, :, :], in_=pT[:, :].rearrange("p (a b) -> p a b", b=NC5)), reads=["pT", "pT2"], writes=["ygT", "ygel_use"])
                ms_v = mslab[:, :].rearrange("p (c j) -> p j c", j=L5)
                JB = 512 // NC5
                for nb in range(4):
                    P.op("pe", lambda e, nb=nb, un=un: e.matmul(out=ps_z[0][:, :], lhsT=WAt[:, un, :], rhs=ygT[:, nb * JB:(nb + 1) * JB, :], start=True, stop=True),
                         reads=["WAt", "ygT"], writes=["ps_z0"])
                    P.op("pe", lambda e, nb=nb, un=un: e.matmul(out=ps_z[1][:, :], lhsT=WBt[:, un, :], rhs=ygT[:, nb * JB:(nb + 1) * JB, :], start=True, stop=True),
                         reads=["WBt", "ygT"], writes=["ps_z1"])
                    P.op("act", lambda e, un=un: e.activation(out=sbt[:, :], in_=ps_z[1][:, :], func=AF.Sigmoid, bias=gbb[:, un:un + 1]),
                         reads=["ps_z1", "gbb"], writes=["sbt"])
                    P.op("dve", lambda e, nb=nb, un=un: e.scalar_tensor_tensor(out=ms_v[:, nb * JB:(nb + 1) * JB, :], in0=ps_z[0][:, :].rearrange("p (j c) -> p j c", c=NC5), scalar=gba[:, un:un + 1],
                                                                               in1=sbt[:, :].rearrange("p (j c) -> p j c", c=NC5), op0=ADD, op1=MUL),
                         reads=["ps_z0", "sbt", "gba"], writes=["mslab"])
                P.dma("sp", lambda e, un=un: e.dma_start(out=mergedT_scr[512 + un * 128:512 + (un + 1) * 128, :], in_=mslab[:, :]), "mscr",
                      reads=["mslab"], writes=[f"mscr{4 + un}"])

            u_loads(0); u_Z(0); u_rk(0)
            for un in range(4):
                u_pre(un)
                u_xyg(un)
                if un + 1 < 4:
                    u_loads(un + 1); u_Z(un + 1); u_rk(un + 1)
                u_post(un)
            esC.close()
            P.barrier()
        else:
            esH.close()

        if stage >= 4:
            AX = mybir.AxisListType
            MUL, ADD, SUB = ALU.mult, ALU.add, ALU.subtract
            esDE = ExitStack()
            esDE.__enter__()
            sbp = lambda name, shape, dt: esDE.enter_context(nc.sbuf_tensor(name, shape, dt))
            h2T = sbp("h2T", [128, 8, NTOK], BF16)
            cw = sbp("cw", [128, NT, 32], F32)
            xt2 = [sbp(f"xt2_{i}", [128, D], F32) for i in range(2)]
            ssq2 = sbp("ssq2", [128, 2 * NT], F32)
            rstd2 = sbp("rstd2", [128, 2 * NT], F32)
            junk2 = sbp("junk2", [128, D], BF16)
            esD = ExitStack()
            esD.__enter__()
            sb = lambda name, shape, dt: esD.enter_context(nc.sbuf_tensor(name, shape, dt))
            ps = lambda name, shape, dt: esD.enter_context(nc.psum_tensor(name, shape, dt))
            mT = sb("mT", [128, 8, NTOK], BF16)
            Wout = sb("Wout", [128, 8, D], BF16)
            Wr = sb("Wr", [128, 8, 36], BF16)
            rb_bc = sb("rb_bc", [128, 36], F32)
            xm = [sb(f"xm{i}", [128, D], F32) for i in range(2)]
            hb2 = [sb(f"hb2_{i}", [128, D], BF16) for i in range(2)]
            lg = sb("lg", [128, NT, 36], F32)
            r_a = sb("r_a", [128, NT, 4], F32)
            r_b = sb("r_b", [128, NT, 4], F32)
            r_gmax = sb("r_gmax", [128, NT], F32)
            r_gw = sb("r_gw", [128, NT], F32)
            r_el = sb("r_el", [128, NT, 32], F32)
            r_t = sb("r_t", [128, NT, 32], F32)
            r_oh1 = sb("r_oh1", [128, NT, 32], F32)
            r_oh2 = sb("r_oh2", [128, NT, 32], F32)
            r_m1 = sb("r_m1", [128, NT], F32)
            r_m2 = sb("r_m2", [128, NT], F32)
            r_w1 = sb("r_w1", [128, NT], F32)
            r_w2 = sb("r_w2", [128, NT], F32)
            po4 = [[ps(f"po{i}{j}", [128, 512], F32) for j in range(2)] for i in range(2)]
            tp2 = [ps(f"tp2_{i}", [128, 8, 128], BF16) for i in range(2)]
            pr2 = [ps(f"pr{i}", [128, 36], F32) for i in range(2)]

            P.dma("sp", lambda e: e.dma_start(out=mT[:, :, :], in_=mergedT_scr.ap().rearrange("(kc p) t -> p kc t", p=128)), "mT",
                  reads=[f"mscr{i}" for i in range(8)], writes=["mT"])
            P.dma("pool", lambda e: e.dma_start(out=Wout[:, :, :], in_=w_out.ap().rearrange("(kc p) c -> p kc c", p=128)), "setupDp", writes=["Wout"])
            P.dma("pool", lambda e: e.dma_start(out=Wr[:, :, :], in_=w_rt.ap().rearrange("(kc p) c -> p kc c", p=128)), "setupDp", writes=["Wr"])
            P.dma("sp", lambda e: e.dma_start(out=rb_bc[:, :], in_=b_rt[0:1, :].partition_broadcast(128)), "setupD", writes=["rb_bc"])
            P.dma("sp", lambda e: e.dma_start(out=g_bc[:, :], in_=g_ffn[0:1, :].partition_broadcast(128)), "setupD", writes=["g_bc"])
            P.bulk_done("setupD")
            P.bulk_done("setupDp")

            for t0 in range(0, NT, 2):
                tl = (t0, t0 + 1)
                for t in tl:
                    b = t % 2
                    P.dma("sp", lambda e, t=t, b=b: e.dma_start(out=xt2[b][:, :], in_=x[t * 128:(t + 1) * 128, :]), f"xt2_{b}", writes=[f"xt2_{b}"])
                for t in tl:
                    b = t % 2
                    ts_ = slice(t * 128, (t + 1) * 128)
                    for hf in range(2):
                        def f_o(e, ts_=ts_, hf=hf, b=b):
                            for k in range(8):
                                i = e.matmul(out=po4[b][hf][:, :], lhsT=mT[:, k, ts_], rhs=Wout[:, k, hf * 512:(hf + 1) * 512], start=(k == 0), stop=(k == 7))
                            return i
                        P.op("pe", f_o, reads=["mT", "Wout"], writes=[f"po{b}{hf}"])
                for t in tl:
                    b = t % 2
                    for hf in range(2):
                        P.op("dve", lambda e, hf=hf, b=b: e.tensor_tensor(out=xm[b][:, hf * 512:(hf + 1) * 512], in0=po4[b][hf][:, :], in1=xt2[b][:, hf * 512:(hf + 1) * 512], op=ADD),
                             reads=[f"po{b}{hf}", f"xt2_{b}"], writes=[f"xm{b}_{hf}"])
                for t in tl:
                    b = t % 2
                    xk = [f"xm{b}_0", f"xm{b}_1"]
                    P.dma("sp", lambda e, t=t, b=b: e.dma_start(out=xmid_scr[t * 128:(t + 1) * 128, :], in_=xm[b][:, :]), f"xmid{b}", reads=xk, writes=[f"xmid{t}"])
                    P.seq("act", [lambda e, b=b, t=t: e.activation(out=junk2[:, :], in_=xm[b][:, :], func=AF.Square, accum_out=ssq2[:, t:t + 1]),
                                  lambda e, t=t: e.activation(out=ssq2[:, t:t + 1], in_=ssq2[:, t:t + 1], func=AF.Sqrt, scale=1.0 / D, bias=EPS)],
                          reads=xk, writes=["junk2", f"ssq2_{t}"])
                for t in tl:
                    b = t % 2
                    xk = [f"xm{b}_0", f"xm{b}_1"]
                    P.op("dve", lambda e, t=t: e.reciprocal(out=rstd2[:, t:t + 1], in_=ssq2[:, t:t + 1]), reads=[f"ssq2_{t}"], writes=[f"rstd2_{t}"])
                    P.op("dve", lambda e, b=b, t=t: e.scalar_tensor_tensor(out=hb2[b][:, :], in0=xm[b][:, :], scalar=rstd2[:, t:t + 1], in1=g_bc[:, :], op0=MUL, op1=MUL),
                         reads=xk + [f"rstd2_{t}", "g_bc"], writes=[f"hb2_{b}"])
                for t in tl:
                    b = t % 2
                    def f_tp2(e, b=b):
                        for k in range(8):
                            i = e.transpose(out=tp2[b][:, k, :], in_=hb2[b][:, k * 128:(k + 1) * 128], identity=ident[:, :])
                        return i
                    P.op("pe", f_tp2, reads=[f"hb2_{b}", "ident"], writes=[f"tp2_{b}"])
                for t in tl:
                    b = t % 2
                    ts_ = slice(t * 128, (t + 1) * 128)
                    P.op("act", lambda e, ts_=ts_, b=b: e.copy(out=h2T[:, :, ts_], in_=tp2[b][:, :, :]), reads=[f"tp2_{b}"], writes=[f"h2T_{t // 4}"])
                for t in tl:
                    b = t % 2
                    ts_ = slice(t * 128, (t + 1) * 128)
                    def f_r(e, ts_=ts_, b=b):
                        for k in range(8):
                            i = e.matmul(out=pr2[b][:, :], lhsT=h2T[:, k, ts_], rhs=Wr[:, k, :], start=(k == 0), stop=(k == 7))
                        return i
                    P.op("pe", f_r, reads=[f"h2T_{t // 4}", "Wr"], writes=[f"pr{b}"])
                for t in tl:
                    b = t % 2
                    P.op("dve", lambda e, t=t, b=b: e.tensor_tensor(out=lg[:, t, :], in0=pr2[b][:, :], in1=rb_bc[:, :], op=ADD), reads=[f"pr{b}", "rb_bc"], writes=["lg"])

            BIG = 1.0e9
            bc4 = lambda a: a.unsqueeze(2).to_broadcast([128, NT, 4])
            bc32 = lambda a: a.unsqueeze(2).to_broadcast([128, NT, 32])
            gl = lg[:, :, 0:4]
            el = lg[:, :, 4:36]
            P.seq("dve", [
                lambda e: e.tensor_reduce(out=r_gmax[:, :], in_=gl, axis=AX.X, op=ALU.max),
                lambda e: e.tensor_tensor(out=r_a[:, :, :], in0=gl, in1=bc4(r_gmax[:, :]), op=SUB)], reads=["lg"], writes=["r_a", "r_gmax"])
            P.op("act", lambda e: e.activation(out=r_b[:, :, :], in_=r_a[:, :, :], func=AF.Exp), reads=["r_a"], writes=["r_b"])
            P.seq("dve", [
                lambda e: e.tensor_reduce(out=r_gw[:, :], in_=r_b[:, :, :], axis=AX.X, op=ADD),
                lambda e: e.reciprocal(out=r_gw[:, :], in_=r_gw[:, :]),
                lambda e: e.tensor_tensor(out=r_a[:, :, :], in0=gl, in1=bc4(r_gmax[:, :]), op=ALU.is_equal),
                lambda e: e.tensor_scalar(out=r_a[:, :, :], in0=r_a[:, :, :], scalar1=-1.0, scalar2=BIG, op0=ADD, op1=MUL),
                lambda e: e.tensor_tensor(out=r_el[:, :, :].rearrange("p t (g k) -> p t g k", k=8), in0=el.rearrange("p t (g k) -> p t g k", k=8),
                                          in1=r_a[:, :, :].unsqueeze(3).to_broadcast([128, NT, 4, 8]), op=ADD),
                lambda e: e.tensor_reduce(out=r_m1[:, :], in_=r_el[:, :, :], axis=AX.X, op=ALU.max),
                lambda e: e.tensor_tensor(out=r_oh1[:, :, :], in0=r_el[:, :, :], in1=bc32(r_m1[:, :]), op=ALU.is_equal),
                lambda e: e.scalar_tensor_tensor(out=r_t[:, :, :], in0=r_oh1[:, :, :], scalar=-BIG, in1=r_el[:, :, :], op0=MUL, op1=ADD),
                lambda e: e.tensor_reduce(out=r_m2[:, :], in_=r_t[:, :, :], axis=AX.X, op=ALU.max),
                lambda e: e.tensor_tensor(out=r_oh2[:, :, :], in0=r_t[:, :, :], in1=bc32(r_m2[:, :]), op=ALU.is_equal),
                lambda e: e.tensor_tensor(out=r_w1[:, :], in0=r_m1[:, :], in1=r_m2[:, :], op=SUB)],
                reads=["lg", "r_b", "r_a"], writes=["r_a", "router1"])
            P.op("act", lambda e: e.activation(out=r_w1[:, :], in_=r_w1[:, :], func=AF.Sigmoid), reads=["router1"], writes=["r_w1"])
            P.seq("dve", [
                lambda e: e.tensor_scalar(out=r_w2[:, :], in0=r_w1[:, :], scalar1=-1.0, scalar2=1.0, op0=MUL, op1=ADD),
                lambda e: e.tensor_tensor(out=r_w1[:, :], in0=r_w1[:, :], in1=r_gw[:, :], op=MUL),
                lambda e: e.tensor_tensor(out=r_w2[:, :], in0=r_w2[:, :], in1=r_gw[:, :], op=MUL),
                lambda e: e.tensor_tensor(out=r_oh1[:, :, :], in0=r_oh1[:, :, :], in1=bc32(r_w1[:, :]), op=MUL),
                lambda e: e.tensor_tensor(out=r_oh2[:, :, :], in0=r_oh2[:, :, :], in1=bc32(r_w2[:, :]), op=MUL),
                lambda e: e.tensor_tensor(out=cw[:, :, :], in0=r_oh1[:, :, :], in1=r_oh2[:, :, :], op=ADD)],
                reads=["router1", "r_w1"], writes=["cw", "router1"])
            if "cw" in dbg:
                tcw = dbgt("cw", [128, NT, 32])
                P.dma("sp", lambda e: e.dma_start(out=tcw[:, :, :], in_=cw[:, :, :]), "out", reads=["cw"])
            esD.close()
            P.barrier()

            NE = 32 if stage >= 5 else 0
            esE = ExitStack()
            esE.__enter__()
            sb = lambda name, shape, dt: esE.enter_context(nc.sbuf_tensor(name, shape, dt))
            ps = lambda name, shape, dt: esE.enter_context(nc.psum_tensor(name, shape, dt))
            yacc = sb("yacc", [128, NT, D], F32)
            Wg = [sb(f"Wg{i}", [128, 8, 512], BF16) for i in range(2)]
            Wu = [sb(f"Wu{i}", [128, 8, 512], BF16) for i in range(2)]
            Wd = [sb(f"Wd{i}", [128, 4, D], BF16) for i in range(2)]
            actT = sb("actT", [128, 4, NTOK], BF16)
            sgt = [sb(f"sgt{i}", [128, 512], F32) for i in range(2)]
            pg = [ps(f"pg{i}", [128, 512], F32) for i in range(2)]
            pu2 = [ps(f"pu2_{i}", [128, 512], F32) for i in range(2)]
            pd = [ps(f"pd{i}", [128, 512], F32) for i in range(2)]
            P.op("pool", lambda e: e.memset(yacc[:, :, :], 0.0), writes=[f"yacc{t}_{hf}" for t in range(NT) for hf in range(2)])
            h2T_all = [f"h2T_{i}" for i in range(4)]
            for ex in range(NE):
                s_ = ex % 2
                P.dma("pool", lambda e, ex=ex, s_=s_: e.dma_start(out=Wg[s_][:, :, :], in_=w_gate[ex].rearrange("(kc p) c -> p kc c", p=128)), f"Wg{s_}", writes=[f"Wg{s_}"])
                P.dma("pool", lambda e, ex=ex, s_=s_: e.dma_start(out=Wu[s_][:, :, :], in_=w_up[ex].rearrange("(kc p) c -> p kc c", p=128)), f"Wu{s_}", writes=[f"Wu{s_}"])
                P.dma("pool", lambda e, ex=ex, s_=s_: e.dma_start(out=Wd[s_][:, :, :], in_=w_down[ex].rearrange("(kc p) c -> p kc c", p=128)), f"Wd{s_}", writes=[f"Wd{s_}"])
                it = 0
                for tb in range(4):
                    tbs = slice(tb * 512, (tb + 1) * 512)
                    for m in range(4):
                        b_ = it % 2
                        it += 1
                        def f_gu(e, s_=s_, m=m, tbs=tbs, b_=b_):
                            for k in range(8):
                                e.matmul(out=pg[b_][:, :], lhsT=Wg[s_][:, k, m * 128:(m + 1) * 128], rhs=h2T[:, k, tbs], start=(k == 0), stop=(k == 7))
                            for k in range(8):
                                i = e.matmul(out=pu2[b_][:, :], lhsT=Wu[s_][:, k, m * 128:(m + 1) * 128], rhs=h2T[:, k, tbs], start=(k == 0), stop=(k == 7))
                            return i
                        P.op("pe", f_gu, reads=[f"Wg{s_}", f"Wu{s_}", f"h2T_{tb}"], writes=[f"pg{b_}", f"pu2_{b_}"])
                        P.op("act", lambda e, b_=b_: e.activation(out=sgt[b_][:, :], in_=pg[b_][:, :], func=AF.Silu), reads=[f"pg{b_}"], writes=[f"sgt{b_}"])
                        P.op("dve", lambda e, b_=b_, m=m, tbs=tbs: e.tensor_tensor(out=actT[:, m, tbs], in0=sgt[b_][:, :], in1=pu2[b_][:, :], op=MUL),
                             reads=[f"sgt{b_}", f"pu2_{b_}"], writes=[f"actT_{tb}"])
                it = 0
                for t in range(NT):
                    ts_ = slice(t * 128, (t + 1) * 128)
                    for hf in range(2):
                        b_ = it % 2
                        it += 1
                        def f_d(e, s_=s_, ts_=ts_, hf=hf, b_=b_):
                            for m in range(4):
                                i = e.matmul(out=pd[b_][:, :], lhsT=actT[:, m, ts_], rhs=Wd[s_][:, m, hf * 512:(hf + 1) * 512], start=(m == 0), stop=(m == 3))
                            return i
                        P.op("pe", f_d, reads=[f"Wd{s_}", f"actT_{t // 4}"], writes=[f"pd{b_}"])
                        P.op("dve", lambda e, t=t, hf=hf, b_=b_, ex=ex: e.scalar_tensor_tensor(out=yacc[:, t, hf * 512:(hf + 1) * 512], in0=pd[b_][:, :], scalar=cw[:, t, ex:ex + 1],
                                                                                                in1=yacc[:, t, hf * 512:(hf + 1) * 512], op0=MUL, op1=ADD),
                             reads=[f"pd{b_}", "cw", f"yacc{t}_{hf}"], writes=[f"yacc{t}_{hf}"])

            P.dma("sp", lambda e: e.dma_start(out=g_bc[:, :], in_=g_fin[0:1, :].partition_broadcast(128)), "setupF", writes=["g_bc"])
            for t in range(NT):
                s_ = t % 2
                P.dma("sp", lambda e, t=t, s_=s_: e.dma_start(out=xt2[s_][:, :], in_=xmid_scr[t * 128:(t + 1) * 128, :]), f"xt2_{s_}", reads=[f"xmid{t}"], writes=[f"xt2_{s_}"])
                P.op("dve", lambda e, t=t, s_=s_: e.tensor_tensor(out=xt2[s_][:, :], in0=xt2[s_][:, :], in1=yacc[:, t, :], op=ADD),
                     reads=[f"xt2_{s_}", f"yacc{t}_0", f"yacc{t}_1"], writes=[f"xt2_{s_}"])
                P.seq("act", [lambda e, s_=s_, t=t: e.activation(out=junk2[:, :], in_=xt2[s_][:, :], func=AF.Square, accum_out=ssq2[:, NT + t:NT + t + 1]),
                              lambda e, t=t: e.activation(out=ssq2[:, NT + t:NT + t + 1], in_=ssq2[:, NT + t:NT + t + 1], func=AF.Sqrt, scale=1.0 / D, bias=EPS)],
                      reads=[f"xt2_{s_}"], writes=["junk2", f"ssq2_{NT + t}"])
                P.op("dve", lambda e, t=t: e.reciprocal(out=rstd2[:, NT + t:NT + t + 1], in_=ssq2[:, NT + t:NT + t + 1]), reads=[f"ssq2_{NT + t}"], writes=[f"rstd2_{NT + t}"])
                P.op("dve", lambda e, s_=s_, t=t: e.scalar_tensor_tensor(out=xt2[s_][:, :], in0=xt2[s_][:, :], scalar=rstd2[:, NT + t:NT + t + 1], in1=g_bc[:, :], op0=MUL, op1=MUL),
                     reads=[f"xt2_{s_}", f"rstd2_{NT + t}", "g_bc"], writes=[f"xt2_{s_}"])
                P.dma("sp", lambda e, t=t, s_=s_: e.dma_start(out=y[t * 128:(t + 1) * 128, :], in_=xt2[s_][:, :]), "yout", reads=[f"xt2_{s_}"], writes=[f"y{t}"])
            esE.close()
            esDE.close()

        if "merged" in dbg:
            t = dbgt("merged", [1024, NTOK], BF16)
            nrow = 1024 if stage >= 3 else 512
            P.dma("sp", lambda e: e.dma_start(out=t[0:nrow, :], in_=mergedT_scr[0:nrow, :]), "out", reads=[f"mscr{h}" for h in range(nrow // 128)])
        if "hT" in dbg:
            t2 = dbgt("hT", [128, 8, HALO + NTOK], BF16)
            P.dma("sp", lambda e: e.dma_start(out=t2[:, :, :], in_=hT[:, :, :]), "out", reads=hT_all)
        if "qk" in dbg:
            t3 = dbgt("qs", [128, NTOK], BF16)
            t4 = dbgt("ks", [128, NTOK], BF16)
            t5 = dbgt("expb", [128, NTOK], F32)
            t6 = dbgt("e2", [128, NTOK], F32)
            P.dma("sp", lambda e: e.dma_start(out=t3[:, :], in_=qsT[:, :]), "out", reads=["qsT"])
            P.dma("sp", lambda e: e.dma_start(out=t4[:, :], in_=ksT[:, :]), "out", reads=["ksT"])
            P.dma("sp", lambda e: e.dma_start(out=t5[:, :], in_=expb[:, :]), "out", reads=expb_all)
            P.dma("sp", lambda e: e.dma_start(out=t6[:, :], in_=e2bc[:, :]), "out", reads=e2_all)
        if "xmid" in dbg:
            txm = dbgt("xmid", [NTOK, D])
            P.dma("sp", lambda e: e.dma_start(out=txm[:, :], in_=xmid_scr[:, :]), "out", reads=[f"xmid{t}" for t in range(NT)])
        P.final_wait("sp", ["out", "yout"])

        with nc.Block() as block:
            @block.tensor
            def _(e): P.replay("pe", e)
            @block.scalar
            def _(e): P.replay("act", e)
            @block.vector
            def _(e): P.replay("dve", e)
            @block.gpsimd
            def _(e): P.replay("pool", e)
            @block.sync
            def _(e): P.replay("sp", e)
    return nc, dbg_out


def make_in_maps(inputs):
    f = lambda a: np.ascontiguousarray(a, dtype=np.float32)
    x = inputs["x"]
    common = {
        "g_mix": f(inputs["norm_mix_g"].reshape(1, D)),
        "w_in": f(inputs["w_in"].reshape(D, 2568)),
        "conv_wT": f(inputs["conv_w"].reshape(4, 8, 128).transpose(2, 1, 0)),
        "conv_b": f(inputs["conv_b"].reshape(8, 128).T),
        "i_bias": f(inputs["i_bias"].reshape(1, 4)),
        "f_bias": f(inputs["f_bias"].reshape(1, 4)),
        "g_mlc": f(inputs["mlstm_norm_g"].reshape(4, 128).T),
    }
    lre = inputs["s5_lambda_re"].reshape(32, 64).T
    lim = inputs["s5_lambda_im"].reshape(32, 64).T
    bre = inputs["s5_b_re"].reshape(32, 64, 16).transpose(1, 0, 2)
    bim = inputs["s5_b_im"].reshape(32, 64, 16).transpose(1, 0, 2)
    cre = inputs["s5_c_re"].reshape(32, 16, 64).transpose(2, 0, 1)
    cim = inputs["s5_c_im"].reshape(32, 16, 64).transpose(2, 0, 1)
    glw = inputs["s5_glu_w"].reshape(4, 8, 16, 32)
    WA = np.zeros((4, 128, 128), np.float32)
    WB = np.zeros((4, 128, 128), np.float32)
    for u in range(4):
        for g in range(8):
            WA[u, g * 16:(g + 1) * 16, g * 16:(g + 1) * 16] = glw[u, g, :, 0:16]
            WB[u, g * 16:(g + 1) * 16, g * 16:(g + 1) * 16] = glw[u, g, :, 16:32]
    glb = inputs["s5_glu_b"].reshape(4, 8, 32)
    common.update({
        "lam2_re": f(np.concatenate([lre, lre], 0)), "lam2_im": f(np.concatenate([lim, lim], 0)),
        "logdt": f(inputs["s5_log_dt"].reshape(1, 32)),
        "X1d": f(np.concatenate([bre, bim], 0)), "X2d": f(np.concatenate([bim, bre], 0)),
        "CY1d": f(np.concatenate([cre, cim], 0)), "CY2d": f(np.concatenate([cim, cre], 0)),
        "s5d": f(inputs["s5_d"].reshape(1, 512)),
        "WAd": WA, "WBd": WB,
        "w_out": f(inputs["w_out"].reshape(D, D)), "g_ffn": f(inputs["norm_ffn_g"].reshape(1, D)),
        "w_rt": f(np.concatenate([inputs["router_group_w"].reshape(D, 4), inputs["router_expert_w"].reshape(D, 32)], 1)),
        "b_rt": f(np.concatenate([inputs["router_group_b"].reshape(1, 4), inputs["router_expert_b"].reshape(1, 32)], 1)),
        "w_gate": f(inputs["expert_w_gate"].reshape(32, D, 512)), "w_up": f(inputs["expert_w_up"].reshape(32, D, 512)),
        "w_down": f(inputs["expert_w_down"].reshape(32, 512, D)), "g_fin": f(inputs["norm_final_g"].reshape(1, D)),
        "gbad": f(glb[:, :, 0:16].reshape(4, 128).T), "gbbd": f(glb[:, :, 16:32].reshape(4, 128).T),
    })
    maps = []
    for c in range(NCORES):
        b, p = c // 4, c % 4
        m = dict(common)
        m["x"] = f(x[b, p * NTOK:(p + 1) * NTOK])
        if p == 0:
            m["xh"] = np.zeros((HALO, D), np.float32)
        else:
            m["xh"] = f(x[b, p * NTOK - HALO:p * NTOK])
        pmk = np.zeros((128, 4), np.float32)
        pmk[:, :p] = 1.0
        m["pmask"] = pmk
        maps.append(m)
    return maps


_CACHE = {}


def kernel(**inputs):
    if "nc" not in _CACHE:
        _CACHE["nc"] = build()[0]
    nc = _CACHE["nc"]
    in_maps = make_in_maps(inputs)
    res = run_bass_kernel_spmd(nc, in_maps, core_ids=list(range(NCORES)))
    out = np.empty((2, 4 * NTOK, D), np.float32)
    for c in range(NCORES):
        b, p = c // 4, c % 4
        out[b, p * NTOK:(p + 1) * NTOK] = np.asarray(res.results[c]["y"], dtype=np.float32)
    return out
```

```python
import numpy as np
from contextlib import ExitStack
import concourse.bass as bass
import concourse.mybir as mybir
from concourse.bass_utils import run_bass_kernel_spmd

F32 = mybir.dt.float32
BF16 = mybir.dt.bfloat16
AF = mybir.ActivationFunctionType
ALU = mybir.AluOpType

NTOK = 2048
NT = 16
NCH = 16
CL = 128
HALO = 32
D = 1024
EPS = 1e-6
NCORES = 8
GROUPS4 = [[0, 1, 2, 3], [4, 5, 6, 7]]
L5 = 32
LL = 5
NC5 = NTOK // L5
KC5 = L5 // 8
NLEV = 7


class Prog:
    def __init__(self, nc, es):
        self.nc = nc
        self.es = es
        self.queues = {e: [] for e in ("pe", "act", "dve", "pool", "sp")}
        self.esem = {}
        self.ecnt = {}
        for e in ("pe", "act", "dve", "pool"):
            self.esem[e] = es.enter_context(nc.semaphore("sem_" + e))
            self.ecnt[e] = 0
        self.dsem = {}
        self.dcnt = {}
        self.lastw = {}
        self.readers = {}
        self.known = {e: {} for e in self.queues}

    def _deps(self, reads, writes):
        deps = []
        for r in reads:
            if r in self.lastw:
                deps.append(self.lastw[r])
        for w in writes:
            if w in self.lastw:
                deps.append(self.lastw[w])
            deps.extend(self.readers.get(w, ()))
        return deps

    def _prune(self, eng, deps):
        need = {}
        for (s, v) in deps:
            if eng == "pe" and s is self.esem["pe"]:
                continue
            if v > need.get(s, 0):
                need[s] = v
        out = []
        kn = self.known[eng]
        for s, v in need.items():
            if kn.get(s, 0) >= v:
                continue
            kn[s] = v
            out.append((s, v))
        return out

    def _record(self, tok, reads, writes):
        for r in reads:
            self.readers.setdefault(r, []).append(tok)
        for w in writes:
            self.lastw[w] = tok
            self.readers[w] = []

    def op(self, eng, fn, reads=(), writes=()):
        reads = tuple(reads)
        writes = tuple(writes)
        waits = self._prune(eng, self._deps(reads, writes))
        self.ecnt[eng] += 1
        tok = (self.esem[eng], self.ecnt[eng])
        self.queues[eng].append((waits, fn, (self.esem[eng], 1)))
        self._record(tok, reads, writes)

    def seq(self, eng, fns, reads=(), writes=()):
        self._chain = getattr(self, "_chain", 0) + 1
        ck = f"__chain{self._chain}"
        for fn in fns:
            self.op(eng, fn, reads=tuple(reads) + (ck,), writes=tuple(writes) + (ck,))

    def dma(self, q, fn, stream, reads=(), writes=(), inc=16):
        reads = tuple(reads)
        writes = tuple(writes)
        if stream not in self.dsem:
            self.dsem[stream] = self.es.enter_context(self.nc.semaphore("dsem_" + stream))
            self.dcnt[stream] = 0
        waits = self._prune(q, self._deps(reads, writes))
        self.dcnt[stream] += inc
        tok = (self.dsem[stream], self.dcnt[stream])
        self.queues[q].append((waits, fn, (self.dsem[stream], inc)))
        self._record(tok, reads, writes)

    def bulk_done(self, stream):
        s = self.dsem[stream]
        fin = self.dcnt[stream]
        for k, (ss, v) in list(self.lastw.items()):
            if ss is s:
                self.lastw[k] = (s, fin)

    def barrier(self):
        allw = [(self.esem[e], self.ecnt[e]) for e in self.esem if self.ecnt[e] > 0]
        allw += [(self.dsem[s], self.dcnt[s]) for s in self.dsem]
        for e in self.queues:
            w = self._prune(e, allw)
            if w:
                self.queues[e].append((w, None, None))

    def final_wait(self, q, streams):
        waits = [(self.dsem[st], self.dcnt[st]) for st in streams if st in self.dsem]
        self.queues[q].append((waits, None, None))

    def replay(self, eng, h):
        for waits, fn, inc in self.queues[eng]:
            for (s, v) in waits:
                h.wait_ge(s, v)
            if fn is None:
                continue
            inst = fn(h)
            inst.then_inc(inc[0], inc[1])


def build(stage=99, dbg=()):
    nc = bass.Bass("TRN2", target_bir_lowering=False)
    din = lambda name, shape, dt=F32: nc.dram_tensor(name, shape, dt, kind="ExternalInput")
    x = din("x", [NTOK, D])
    xh = din("xh", [HALO, D])
    g_mix = din("g_mix", [1, D])
    w_in = din("w_in", [D, 2568])
    conv_wT = din("conv_wT", [128, 8, 4])
    conv_b = din("conv_b", [128, 8])
    i_bias = din("i_bias", [1, 4])
    f_bias = din("f_bias", [1, 4])
    g_mlc = din("g_mlc", [128, 4])
    pmask = din("pmask", [128, 4])
    lam2_re = din("lam2_re", [128, 32])
    lam2_im = din("lam2_im", [128, 32])
    logdt = din("logdt", [1, 32])
    X1d = din("X1d", [128, 32, 16])
    X2d = din("X2d", [128, 32, 16])
    CY1d = din("CY1d", [128, 32, 16])
    CY2d = din("CY2d", [128, 32, 16])
    s5d = din("s5d", [1, 512])
    WAd = din("WAd", [4, 128, 128])
    WBd = din("WBd", [4, 128, 128])
    gbad = din("gbad", [128, 4])
    gbbd = din("gbbd", [128, 4])
    w_out = din("w_out", [D, D])
    g_ffn = din("g_ffn", [1, D])
    w_rt = din("w_rt", [D, 36])
    b_rt = din("b_rt", [1, 36])
    if stage >= 5:
        w_gate = din("w_gate", [32, D, 512])
        w_up = din("w_up", [32, D, 512])
        w_down = din("w_down", [32, 512, D])
    g_fin = din("g_fin", [1, D])
    y = nc.dram_tensor("y", [NTOK, D], F32, kind="ExternalOutput")
    dbg_out = {}
    def dbgt(name, shape, dt=F32):
        dbg_out[name] = nc.dram_tensor("dbg_" + name, shape, dt, kind="ExternalOutput")
        return dbg_out[name]

    mergedT_scr = nc.dram_tensor("mergedT_scr", [1024, NTOK], BF16)
    cc_in = [nc.dram_tensor(f"cc_in{h}", [128, 130], F32) for h in range(4)]
    cc_out = [nc.dram_tensor(f"cc_out{h}", [512, 130], F32) for h in range(4)]
    u_scr = nc.dram_tensor("u_scr", [NTOK, 512], BF16)
    xmid_scr = nc.dram_tensor("xmid_scr", [NTOK, D], F32)
    prep_scr = {n: nc.dram_tensor("prep_" + n, [128, 32, k], F32) for n, k in (("PWrr", L5), ("PWri", L5), ("PWbr", 8), ("PWbi", 8), ("BB1", 16), ("BB2", 16),
                                                                             ("PRs", L5 + 1), ("nPI", L5 + 1), ("sqr", 12), ("sqi", 12), ("PLr", NC5), ("PLi", NC5))}
    ccs_in = [nc.dram_tensor(f"ccs_in{h}", [128, 8], F32) for h in range(4)]
    ccs_out = [nc.dram_tensor(f"ccs_out{h}", [512, 8], F32) for h in range(4)]

    w_in_v = w_in.ap().rearrange("(kc p) c -> p kc c", p=128)

    es = ExitStack()
    with es:
        P = Prog(nc, es)
        sb = lambda name, shape, dt: es.enter_context(nc.sbuf_tensor(name, shape, dt))
        ps = lambda name, shape, dt: es.enter_context(nc.psum_tensor(name, shape, dt))

        g_bc = sb("g_bc", [128, D], F32)
        ident = sb("ident", [128, 128], BF16)
        identf = sb("identf", [128, 128], F32)
        mslab = sb("mslab", [128, NTOK], BF16)
        pm = sb("pm", [128, 4], F32)
        esH = ExitStack()
        esH.__enter__()
        hT = esH.enter_context(nc.sbuf_tensor("hT", [128, 8, HALO + NTOK], BF16))
        esB = ExitStack()
        esB.__enter__()
        sb = lambda name, shape, dt: esB.enter_context(nc.sbuf_tensor(name, shape, dt))
        ps = lambda name, shape, dt: esB.enter_context(nc.psum_tensor(name, shape, dt))
        xt = [sb(f"xt{i}", [128, D], F32) for i in range(2)]
        junk = sb("junk", [128, D], BF16)
        hb = sb("hb", [128, D], BF16)
        ssq = sb("ssq", [128, NT + 1], F32)
        rstd = sb("rstd", [128, NT + 1], F32)
        wh = sb("wh", [128, 8, 512], BF16)
        wg = sb("wg", [128, 8, 33], BF16)
        raw = sb("raw", [128, NTOK + 3], F32)
        ctmp = sb("ctmp", [128, NTOK], F32)
        qsT = sb("qsT", [128, NTOK], BF16)
        ksT = sb("ksT", [128, NTOK], BF16)
        cwt = sb("cwt", [128, 8, 4], F32)
        cbt = sb("cbt", [128, 8], F32)
        fbt = sb("fbt", [33, 4], F32)
        nfb = sb("nfb", [33, 4], F32)
        Gt = sb("Gt", [33, NTOK], F32)
        Gt2 = sb("Gt2", [33, NTOK], F32)
        cmask = sb("cmask", [33, NTOK], F32)
        sel0 = sb("sel0", [33, 128], F32)
        sel032 = sb("sel032", [33, 128], F32)
        expb = sb("expb", [128, NTOK], F32)
        e2bc = sb("e2bc", [128, NTOK], F32)
        v_ext = sb("v_ext", [CL, NCH, 130], BF16)
        vT = sb("vT", [128, NTOK], BF16)
        sigoT = sb("sigoT", [128, NTOK], BF16)
        kstok = sb("kstok", [CL, NCH, 128], BF16)
        Us = sb("Us", [128, 4, 130], F32)
        gAc = sb("gAc", [128, 4], F32)
        CTloc = sb("CTloc", [128, NCH + 1, 130], F32)
        CTball = sb("CTball", [128, NCH, 130], BF16)
        CTt = sb("CTt", [128, 130], F32)
        cg = sb("cg", [128, 4, 130], F32)
        hacc = sb("hacc", [128, 130], F32)
        hacc2 = sb("hacc2", [128, 130], F32)
        maskST = sb("maskST", [CL, CL], F32)
        STb = sb("STb", [CL, 4, CL], BF16)
        dnb = sb("dnb", [CL, 4, 4], F32)
        hnb = sb("hnb", [CL, 4, 128], F32)
        oab = sb("oab", [CL, 4, 128], BF16)
        junkb = sb("junkb", [CL, 4, 128], BF16)

        Bk = [ps(f"bank{k}", [128, 512], F32) for k in range(8)]
        bfv = lambda k: Bk[k][:, :].bitcast(BF16)
        ps_big = [Bk[0], Bk[1]]
        tp = bfv(2).rearrange("p (a b) -> p a b", b=128)
        pvb = [Bk[3][0:64, 0:256], Bk[4][0:64, 0:256]]
        pab = [Bk[0][0:CL, 0:CL], Bk[1][0:CL, 0:CL]]
        pcb = [Bk[3][0:CL, 0:130], Bk[4][0:CL, 0:130]]
        pub = [Bk[5][:, 0:130], Bk[6][:, 0:130]]
        ptk = [bfv(2)[0:CL, 0:128], bfv(7)[0:CL, 0:128]]
        pto = [bfv(6)[:, 0:CL], bfv(7)[:, 0:CL]]
        KPV = ["bank3", "bank4"]; KPA = ["bank0", "bank1"]; KPC = ["bank3", "bank4"]; KPU = ["bank5", "bank6"]
        KPTK = ["bank2", "bank7"]; KPTO = ["bank6", "bank7"]

        P.dma("sp", lambda e: e.dma_start(out=g_bc[:, :], in_=g_mix[0:1, :].partition_broadcast(128)), "setup", writes=["g_bc"])
        P.dma("sp", lambda e: e.dma_start(out=cwt[:, :, :], in_=conv_wT[:, :, :]), "setup", writes=["cwt"])
        P.dma("sp", lambda e: e.dma_start(out=cbt[:, :], in_=conv_b[:, :]), "setup", writes=["cbt"])
        P.dma("sp", lambda e: e.dma_start(out=fbt[0:1, :], in_=f_bias[0:1, :]), "setup", writes=["fbt0"])
        P.dma("sp", lambda e: e.dma_start(out=fbt[32:33, :], in_=i_bias[0:1, :]), "setup", writes=["fbt32"])
        P.dma("sp", lambda e: e.dma_start(out=gAc[:, :], in_=g_mlc[:, :]), "setup", writes=["gAc"])
        P.dma("sp", lambda e: e.dma_start(out=pm[:, :], in_=pmask[:, :]), "setup", writes=["pm"])
        P.bulk_done("setup")

        P.seq("pool", [lambda e: e.memset(identf[:, :], 0.0),
                       lambda e: e.affine_select(out=identf[:, :], in_=identf[:, :], pattern=[[-1, 128]], compare_op=ALU.not_equal,
                                                 fill=1.0, base=0, channel_multiplier=1)], writes=["identf"])
        P.op("dve", lambda e: e.tensor_copy(out=ident[:, :], in_=identf[:, :]), reads=["identf"], writes=["ident"])

        P.seq("pool", [lambda e: e.memset(cmask[:, :], 1.0),
                       lambda e: e.memset(cmask[:, 0:NTOK:CL], 0.0),
                       lambda e: e.memset(sel0[:, :], 0.0),
                       lambda e: e.memset(sel0[0:1, :], 1.0),
                       lambda e: e.memset(sel032[:, :], 0.0),
                       lambda e: e.memset(sel032[0:1, :], 1.0),
                       lambda e: e.memset(sel032[32:33, :], 1.0),
                       lambda e: e.memset(Gt2[:, :], 0.0),
                       lambda e: e.memset(v_ext[:, :, 128:129], 1.0),
                       lambda e: e.memset(v_ext[:, :, 129:130], 0.0),
                       lambda e: e.memset(maskST[:, :], 1.0),
                       lambda e: e.affine_select(out=maskST[:, :], in_=maskST[:, :], pattern=[[1, CL]], compare_op=ALU.is_ge,
                                                 fill=0.0, base=0, channel_multiplier=-1)],
              writes=["cmask", "sel0", "sel032", "Gt2", "v_ext_c", "maskST"])
        P.op("pool", lambda e: e.tensor_scalar(out=nfb[0:1, :], in0=fbt[0:1, :], scalar1=-1.0, scalar2=None, op0=ALU.mult),
             reads=["fbt0"], writes=["nfb"])

        def norm_tile(src_ap, rows, slot, col0, idx, tag):
            s = slot
            P.dma("sp", lambda e: e.dma_start(out=xt[s][0:rows, :], in_=src_ap), f"xt{s}", writes=[f"xt{s}"])
            P.op("act", lambda e: e.activation(out=junk[0:rows, :], in_=xt[s][0:rows, :], func=AF.Square, accum_out=ssq[0:rows, idx:idx + 1]),
                 reads=[f"xt{s}"], writes=["junk", f"ssq{idx}"])
            P.op("act", lambda e: e.activation(out=ssq[0:rows, idx:idx + 1], in_=ssq[0:rows, idx:idx + 1], func=AF.Sqrt, scale=1.0 / D, bias=EPS),
                 reads=[f"ssq{idx}"], writes=[f"ssq{idx}"])
            P.op("dve", lambda e: e.reciprocal(out=rstd[0:rows, idx:idx + 1], in_=ssq[0:rows, idx:idx + 1]), reads=[f"ssq{idx}"], writes=[f"rstd{idx}"])
            P.op("dve", lambda e: e.scalar_tensor_tensor(out=hb[0:rows, :], in0=xt[s][0:rows, :], scalar=rstd[0:rows, idx:idx + 1], in1=g_bc[0:rows, :],
                                                         op0=ALU.mult, op1=ALU.mult),
                 reads=[f"xt{s}", f"rstd{idx}", "g_bc"], writes=["hb"])
            def f_tp(e):
                for k in range(8):
                    i = e.transpose(out=tp[:, k, 0:rows], in_=hb[0:rows, k * 128:(k + 1) * 128], identity=ident[0:rows, 0:rows])
                return i
            P.op("pe", f_tp, reads=["hb", "ident"], writes=["bank2"])
            P.op("act", lambda e: e.copy(out=hT[:, :, col0:col0 + rows], in_=tp[:, :, 0:rows]), reads=["bank2"], writes=[tag])

        norm_tile(xh[:, :], HALO, 0, 0, NT, "hT_h")
        for t in range(NT):
            norm_tile(x[t * 128:(t + 1) * 128, :], 128, (t + 1) % 2, HALO + t * 128, t, f"hT_{t // 4}")
        hT_all = ["hT_h"] + [f"hT_{i}" for i in range(4)]

        SC = float(128 ** -0.5)
        for hd in range(4 if stage >= 2 else 0):
            for j, c0 in enumerate((hd * 128, 512 + hd * 128, 1024 + hd * 128, 1536 + hd * 128)):
                P.dma("pool", lambda e, j=j, c0=c0: e.dma_start(out=wh[:, :, j * 128:(j + 1) * 128], in_=w_in_v[:, :, c0:c0 + 128]),
                      "wh", writes=[f"wh{j}"])
            P.bulk_done("wh")
            P.op("pool", lambda e: e.memset(wg[:, :, :], 0.0), writes=["wg", "wg_a", "wg_b"])
            def ld_wg(e, dst, col):
                with nc.allow_non_contiguous_dma(reason="gate columns"):
                    return e.dma_start(out=wg[:, :, dst:dst + 1], in_=w_in_v[:, :, col:col + 1])
            P.dma("pool", lambda e, hd=hd: ld_wg(e, 0, 2052 + hd), "wg", reads=["wg"], writes=["wg_a"])
            P.dma("pool", lambda e, hd=hd: ld_wg(e, 32, 2048 + hd), "wg", reads=["wg"], writes=["wg_b"])
            P.bulk_done("wg")

            for tb in range(4):
                pgt = ps_big[tb % 2]
                def f_g(e, tb=tb, pgt=pgt):
                    for k in range(8):
                        i = e.matmul(out=pgt[0:33, :], lhsT=wg[:, k, :], rhs=hT[:, k, HALO + tb * 512:HALO + (tb + 1) * 512], start=(k == 0), stop=(k == 7))
                    return i
                P.op("pe", f_g, reads=["wg", "wg_a", "wg_b", f"hT_{tb}"], writes=[f"bank{tb % 2}"])
                P.op("act", lambda e, tb=tb, pgt=pgt: e.copy(out=Gt[0:33, tb * 512:(tb + 1) * 512], in_=pgt[0:33, :]),
                     reads=[f"bank{tb % 2}"], writes=[f"Gt_{tb}"])
            Gt_all = [f"Gt_{tb}" for tb in range(4)]
            P.op("act", lambda e, hd=hd: e.activation(out=Gt[0:1, :], in_=Gt[0:1, :], func=AF.Exp, scale=-1.0, bias=nfb[0:1, hd:hd + 1]),
                 reads=Gt_all + ["nfb"], writes=["Gt_f"])
            P.op("act", lambda e: e.activation(out=Gt[0:1, :], in_=Gt[0:1, :], func=AF.Ln, bias=1.0), reads=["Gt_f"], writes=["Gt_f"])
            P.op("dve", lambda e: e.tensor_tensor_scan(out=Gt2[0:1, :], data0=cmask[0:1, :], data1=Gt[0:1, :], initial=0.0, op0=ALU.mult, op1=ALU.add),
                 reads=["Gt_f", "cmask"], writes=["Gt2_0"])
            P.op("act", lambda e, hd=hd: e.activation(out=Gt2[32:33, :], in_=Gt[32:33, :], func=AF.Identity, bias=fbt[32:33, hd:hd + 1]),
                 reads=Gt_all + ["fbt32", "Gt2"], writes=["Gt2_32"])
            for tb in range(4):
                pgt = ps_big[tb % 2]
                P.op("pe", lambda e, tb=tb, pgt=pgt: e.matmul(out=pgt[:, :], lhsT=sel0[0:33, :], rhs=Gt2[0:33, tb * 512:(tb + 1) * 512], start=True, stop=True),
                     reads=["sel0", "Gt2", "Gt2_0", "Gt2_32"], writes=[f"bank{tb % 2}"])
                P.op("act", lambda e, tb=tb, pgt=pgt: e.activation(out=expb[:, tb * 512:(tb + 1) * 512], in_=pgt[:, :], func=AF.Exp, scale=-1.0),
                     reads=[f"bank{tb % 2}"], writes=[f"expb_{tb}"])
            for tb in range(4):
                pgt = ps_big[tb % 2]
                P.op("pe", lambda e, tb=tb, pgt=pgt: e.matmul(out=pgt[:, :], lhsT=sel032[0:33, :], rhs=Gt2[0:33, tb * 512:(tb + 1) * 512], start=True, stop=True),
                     reads=["sel032", "Gt2", "Gt2_0", "Gt2_32"], writes=[f"bank{tb % 2}"])
                P.op("act", lambda e, tb=tb, pgt=pgt: e.activation(out=e2bc[:, tb * 512:(tb + 1) * 512], in_=pgt[:, :], func=AF.Exp),
                     reads=[f"bank{tb % 2}"], writes=[f"e2bc_{tb}"])
            expb_all = [f"expb_{tb}" for tb in range(4)]
            e2_all = [f"e2bc_{tb}" for tb in range(4)]

            for qi in range(2):
                cidx = qi * 4 + hd
                def f_halo(e, qi=qi):
                    for k in range(8):
                        i = e.matmul(out=ps_big[0][:, 0:HALO], lhsT=wh[:, k, qi * 128:(qi + 1) * 128], rhs=hT[:, k, 0:HALO], start=(k == 0), stop=(k == 7))
                    return i
                P.op("pe", f_halo, reads=[f"wh{qi}", "hT_h"], writes=["bank0"])
                P.op("act", lambda e: e.copy(out=raw[:, 0:3], in_=ps_big[0][:, HALO - 3:HALO]), reads=["bank0"], writes=["raw_h"])
                for tb in range(4):
                    pgt = ps_big[(tb + 1) % 2]
                    def f_q(e, tb=tb, qi=qi, pgt=pgt):
                        for k in range(8):
                            i = e.matmul(out=pgt[:, :], lhsT=wh[:, k, qi * 128:(qi + 1) * 128], rhs=hT[:, k, HALO + tb * 512:HALO + (tb + 1) * 512],
                                         start=(k == 0), stop=(k == 7))
                        return i
                    P.op("pe", f_q, reads=[f"wh{qi}", f"hT_{tb}"], writes=[f"bank{(tb + 1) % 2}"])
                    P.op("act", lambda e, tb=tb, pgt=pgt: e.copy(out=raw[:, 3 + tb * 512:3 + (tb + 1) * 512], in_=pgt[:, :]),
                         reads=[f"bank{(tb + 1) % 2}"], writes=[f"raw_{tb}"])
                raw_all = ["raw_h"] + [f"raw_{tb}" for tb in range(4)]
                fl = [lambda e, cidx=cidx: e.tensor_scalar(out=ctmp[:, :], in0=raw[:, 0:NTOK], scalar1=cwt[:, cidx, 0:1], scalar2=cbt[:, cidx:cidx + 1],
                                                           op0=ALU.mult, op1=ALU.add)]
                for j in (1, 2, 3):
                    fl.append(lambda e, cidx=cidx, j=j: e.scalar_tensor_tensor(out=ctmp[:, :], in0=raw[:, j:j + NTOK], scalar=cwt[:, cidx, j:j + 1],
                                                                               in1=ctmp[:, :], op0=ALU.mult, op1=ALU.add))
                P.seq("dve", fl, reads=raw_all + ["cwt", "cbt"], writes=["ctmp"])
                P.op("act", lambda e: e.activation(out=ctmp[:, :], in_=ctmp[:, :], func=AF.Silu), reads=["ctmp"], writes=["ctmp"])
                if qi == 0:
                    P.op("dve", lambda e: e.tensor_tensor(out=qsT[:, :], in0=ctmp[:, :], in1=expb[:, :], op=ALU.mult),
                         reads=["ctmp"] + expb_all, writes=["qsT"])
                else:
                    P.op("dve", lambda e: e.scalar_tensor_tensor(out=ksT[:, :], in0=ctmp[:, :], scalar=SC, in1=e2bc[:, :], op0=ALU.mult, op1=ALU.mult),
                         reads=["ctmp"] + e2_all, writes=["ksT"])

            for vi in range(2):
                for tb in range(4):
                    pgt = ps_big[tb % 2]
                    def f_vo(e, tb=tb, vi=vi, pgt=pgt):
                        for k in range(8):
                            i = e.matmul(out=pgt[:, :], lhsT=wh[:, k, 256 + vi * 128:384 + vi * 128], rhs=hT[:, k, HALO + tb * 512:HALO + (tb + 1) * 512],
                                         start=(k == 0), stop=(k == 7))
                        return i
                    P.op("pe", f_vo, reads=[f"wh{2 + vi}", f"hT_{tb}"], writes=[f"bank{tb % 2}"])
                    if vi == 0:
                        P.op("act", lambda e, tb=tb, pgt=pgt: e.copy(out=vT[:, tb * 512:(tb + 1) * 512], in_=pgt[:, :]), reads=[f"bank{tb % 2}"], writes=[f"vT_{tb}"])
                    else:
                        P.op("act", lambda e, tb=tb, pgt=pgt: e.activation(out=ctmp[:, tb * 512:(tb + 1) * 512], in_=pgt[:, :], func=AF.Sigmoid),
                             reads=[f"bank{tb % 2}"], writes=["ctmp"])
            P.op("dve", lambda e, hd=hd: e.tensor_scalar(out=sigoT[:, :], in0=ctmp[:, :], scalar1=gAc[:, hd:hd + 1], scalar2=None, op0=ALU.mult),
                 reads=["ctmp", "gAc"], writes=["sigoT"])

            for c in range(NCH):
                P.op("pe", lambda e, c=c: e.transpose(out=ptk[0], in_=vT[:, c * CL:(c + 1) * CL], identity=ident[:, :]),
                     reads=[f"vT_{c // 4}", "ident"], writes=[KPTK[0]])
                P.op("act", lambda e, c=c: e.copy(out=v_ext[:, c, 0:128], in_=ptk[0]), reads=[KPTK[0]], writes=[f"v_{c}"])
                P.op("pe", lambda e, c=c: e.transpose(out=ptk[1], in_=ksT[:, c * CL:(c + 1) * CL], identity=ident[:, :]),
                     reads=["ksT", "ident"], writes=[KPTK[1]])
                P.op("act", lambda e, c=c: e.copy(out=kstok[:, c, :], in_=ptk[1]), reads=[KPTK[1]], writes=[f"kstok_{c}"])

            P.seq("pool", [lambda e: e.memset(CTloc[:, 0, :], 0.0), lambda e: e.memset(CTloc[:, 0, 129:130], 1.0)], writes=["CTloc_0"])
            for c in range(NCH):
                b = c % 2
                ub = c % 4
                wc = expb[:, c * CL + CL - 1:c * CL + CL]
                P.op("pe", lambda e, c=c, b=b: e.matmul(out=pub[b], lhsT=kstok[:, c, :], rhs=v_ext[:, c, :], start=True, stop=True),
                     reads=[f"kstok_{c}", f"v_{c}", "v_ext_c"], writes=[KPU[b]])
                P.op("act", lambda e, b=b, ub=ub, wc=wc: e.activation(out=Us[:, ub, :], in_=pub[b], func=AF.Copy, scale=wc),
                     reads=[KPU[b], f"expb_{c // 4}"], writes=[f"Us{ub}"])
                P.op("dve", lambda e, c=c, ub=ub, wc=wc: e.scalar_tensor_tensor(out=CTloc[:, c + 1, :], in0=CTloc[:, c, :], scalar=wc, in1=Us[:, ub, :],
                                                                              op0=ALU.mult, op1=ALU.add),
                     reads=[f"Us{ub}", f"CTloc_{c}", f"expb_{c // 4}"], writes=[f"CTloc_{c + 1}"])

            P.dma("sp", lambda e, hd=hd: e.dma_start(out=cc_in[hd][:, :], in_=CTloc[:, NCH, :]), f"ccin{hd}", reads=[f"CTloc_{NCH}"], writes=[f"cc_in{hd}"])
            def f_cc(e, hd=hd):
                return e.collective_compute("AllGather", ALU.bypass, replica_groups=GROUPS4,
                                            ins=[cc_in[hd].ap().opt()], outs=[cc_out[hd].ap().opt()])
            P.dma("pool", f_cc, f"cc{hd}", reads=[f"cc_in{hd}"], writes=[f"cc_out{hd}"], inc=1)
            P.dma("sp", lambda e, hd=hd: e.dma_start(out=cg[:, :, :], in_=cc_out[hd].ap().rearrange("(r p) c -> p r c", p=128)),
                  f"ccld{hd}", reads=[f"cc_out{hd}"], writes=["cg"])
            P.op("pool", lambda e: e.memset(hacc[:, :], 0.0), writes=["hacc"])
            for pp in range(3):
                P.seq("dve", [
                    lambda e, pp=pp: e.scalar_tensor_tensor(out=hacc2[:, :], in0=hacc[:, :], scalar=cg[:, pp, 129:130], in1=cg[:, pp, :], op0=ALU.mult, op1=ALU.add),
                    lambda e: e.tensor_tensor(out=hacc2[:, :], in0=hacc2[:, :], in1=hacc[:, :], op=ALU.subtract),
                    lambda e, pp=pp: e.scalar_tensor_tensor(out=hacc[:, :], in0=hacc2[:, :], scalar=pm[:, pp:pp + 1], in1=hacc[:, :], op0=ALU.mult, op1=ALU.add)],
                    reads=["hacc", "cg", "pm"], writes=["hacc", "hacc2"])
            for c in range(NCH):
                eng = "dve"
                P.op(eng, lambda e, c=c: e.scalar_tensor_tensor(out=CTball[:, c, :], in0=hacc[:, :], scalar=CTloc[:, c, 129:130], in1=CTloc[:, c, :],
                                                                op0=ALU.mult, op1=ALU.add),
                     reads=["hacc", f"CTloc_{c}"], writes=[f"CTb_{c}"])

            NB = 2
            for c0 in range(0, NCH, NB):
                cl = list(range(c0, c0 + NB))
                for c in cl:
                    b = c % NB
                    cs = slice(c * CL, (c + 1) * CL)
                    P.op("pe", lambda e, cs=cs, b=b: e.matmul(out=pab[b], lhsT=ksT[:, cs], rhs=qsT[:, cs], start=True, stop=True),
                         reads=["ksT", "qsT"], writes=[KPA[b]])
                for c in cl:
                    b = c % NB
                    P.op("dve", lambda e, b=b: e.tensor_tensor(out=STb[:, b, :], in0=pab[b], in1=maskST[:, :], op=ALU.mult), reads=[KPA[b], "maskST"], writes=[f"ST{b}"])
                for c in cl:
                    b = c % NB
                    cs = slice(c * CL, (c + 1) * CL)
                    def f_cn(e, c=c, cs=cs, b=b):
                        e.matmul(out=pcb[b], lhsT=STb[:, b, :], rhs=v_ext[:, c, :], start=True, stop=False)
                        return e.matmul(out=pcb[b], lhsT=qsT[:, cs], rhs=CTball[:, c, :], start=False, stop=True)
                    P.op("pe", f_cn, reads=[f"ST{b}", f"v_{c}", "v_ext_c", "qsT", f"CTb_{c}"], writes=[KPC[b]])
                for c in cl:
                    b = c % NB
                    P.seq("dve", [
                        lambda e, b=b: e.tensor_copy(out=dnb[:, b, 1:2], in_=pcb[b][:, 128:129]),
                        lambda e, b=b: e.scalar_tensor_tensor(out=dnb[:, b, 0:1], in0=dnb[:, b, 1:2], scalar=-1.0, in1=dnb[:, b, 1:2], op0=ALU.mult, op1=ALU.max),
                        lambda e, b=b: e.tensor_scalar(out=dnb[:, b, 0:1], in0=dnb[:, b, 0:1], scalar1=1.0, scalar2=None, op0=ALU.max),
                        lambda e, b=b: e.reciprocal(out=dnb[:, b, 1:2], in_=dnb[:, b, 0:1]),
                        lambda e, b=b: e.tensor_scalar(out=hnb[:, b, :], in0=pcb[b][:, 0:128], scalar1=dnb[:, b, 1:2], scalar2=None, op0=ALU.mult)],
                        reads=[KPC[b]], writes=[f"dn{b}", f"hn{b}"])
                for c in cl:
                    b = c % NB
                    P.seq("act", [
                        lambda e, b=b: e.activation(out=junkb[:, b, :], in_=hnb[:, b, :], func=AF.Square, accum_out=dnb[:, b, 2:3]),
                        lambda e, b=b: e.activation(out=dnb[:, b, 2:3], in_=dnb[:, b, 2:3], func=AF.Sqrt, scale=1.0 / 128, bias=EPS)],
                        reads=[f"hn{b}", f"dn{b}"], writes=[f"junk{b}", f"dn2_{b}"])
                for c in cl:
                    b = c % NB
                    P.seq("dve", [
                        lambda e, b=b: e.reciprocal(out=dnb[:, b, 3:4], in_=dnb[:, b, 2:3]),
                        lambda e, c=c, b=b: e.tensor_scalar(out=oab[:, b, :], in0=hnb[:, b, :], scalar1=dnb[:, b, 3:4], scalar2=None, op0=ALU.mult)],
                        reads=[f"dn2_{b}", f"hn{b}"], writes=[f"oa{b}", f"dn{b}"])
                for c in cl:
                    b = c % NB
                    P.op("pe", lambda e, b=b: e.transpose(out=pto[b], in_=oab[:, b, :], identity=ident[0:CL, 0:CL]), reads=[f"oa{b}", "ident"], writes=[KPTO[b]])
                for c in cl:
                    b = c % NB
                    cs = slice(c * CL, (c + 1) * CL)
                    P.op("dve", lambda e, cs=cs, b=b: e.tensor_tensor(out=mslab[:, cs], in0=pto[b], in1=sigoT[:, cs], op=ALU.mult), reads=[KPTO[b], "sigoT"], writes=["mslab"])
            P.dma("sp", lambda e, hd=hd: e.dma_start(out=mergedT_scr[hd * 128:(hd + 1) * 128, :], in_=mslab[:, :]), "mscr", reads=["mslab"], writes=[f"mscr{hd}"])

        esB.close()
        P.barrier()
        if stage >= 3:
            esC1 = ExitStack()
            esC1.__enter__()
            sb = lambda name, shape, dt: esC1.enter_context(nc.sbuf_tensor(name, shape, dt))
            ps = lambda name, shape, dt: esC1.enter_context(nc.psum_tensor(name, shape, dt))
            wu = sb("wu", [128, 8, 512], BF16)
            utok = [sb(f"utok{i}", [128, 512], BF16) for i in range(2)]
            pu1 = [ps(f"pu1_{i}", [128, 512], F32) for i in range(2)]
            P.dma("pool", lambda e: e.dma_start(out=wu[:, :, :], in_=w_in_v[:, :, 2056:2568]), "wu", writes=["wu"])
            for t in range(NT):
                def f_u1(e, t=t):
                    for k in range(8):
                        i = e.matmul(out=pu1[t % 2][:, :], lhsT=hT[:, k, HALO + t * 128:HALO + (t + 1) * 128], rhs=wu[:, k, :], start=(k == 0), stop=(k == 7))
                    return i
                P.op("pe", f_u1, reads=["wu", f"hT_{t // 4}"], writes=[f"pu1_{t % 2}"])
                P.op("act", lambda e, t=t: e.copy(out=utok[t % 2][:, :], in_=pu1[t % 2][:, :]), reads=[f"pu1_{t % 2}"], writes=[f"utok{t % 2}"])
                P.dma("sp", lambda e, t=t: e.dma_start(out=u_scr[t * 128:(t + 1) * 128, :], in_=utok[t % 2][:, :]), f"uscr{t % 2}",
                      reads=[f"utok{t % 2}"], writes=[f"u_scr{t}"])
            esC1.close()
            esH.close()
            P.barrier()

            TT = lambda o, a, b, op: (lambda e: e.tensor_tensor(out=o, in0=a, in1=b, op=op))
            TS = lambda o, a, s1, s2, op0, op1=None: ((lambda e: e.tensor_scalar(out=o, in0=a, scalar1=s1, scalar2=s2, op0=op0, op1=op1)) if op1 is not None
                                                      else (lambda e: e.tensor_scalar(out=o, in0=a, scalar1=s1, scalar2=None, op0=op0)))
            ACT = lambda o, a, f, **kw: (lambda e: e.activation(out=o, in_=a, func=f, **kw))
            MUL, ADD, SUB = ALU.mult, ALU.add, ALU.subtract

            def cmul(o_r, o_i, a_r, a_i, s_r, s_i, t1, t2):
                return [TT(t1, a_r, s_r, MUL), TT(t2, a_i, s_i, MUL), TT(o_r, t1, t2, SUB),
                        TT(t1, a_r, s_i, MUL), TT(t2, a_i, s_r, MUL), TT(o_i, t1, t2, ADD)]

            def emit_prep(GP, gs, lam_re_t, lam_im_t, logdt_t, X1t, X2t, sgn, hpi, sc, sqr, sqi, bqr, bqi, a5r, a5i, PWfr, PWfi, PWrr, PWri, PWbr, PWbi, cta, ctb, BB1, BB2, bbt, PRs, nPI, PLr, PLi):
                ops = []
                S = {k: v[:, :] for k, v in sc.items()}
                ops = []
                ops.append(TS(S["lr"], lam_re_t[:, gs], -1e-4, None, ALU.min))
                P.seq("dve", ops, reads=["lam_re_t"], writes=["prep"]); ops = []
                P.op("act", ACT(S["dt"], logdt_t[:, gs], AF.Exp), reads=["logdt_t", "prep"], writes=["prep_dt"])
                ops += [TT(S["lrdt"], S["lr"], S["dt"], MUL), TT(S["th"], lam_im_t[:, gs], S["dt"], MUL)]
                P.seq("dve", ops, reads=["prep", "prep_dt", "lam_im_t"], writes=["prep"]); ops = []
                P.seq("act", [ACT(S["sn"], S["th"], AF.Sin, scale=1.0 / 32), ACT(S["cs"], S["th"], AF.Sin, scale=1.0 / 32, bias=hpi[:, 0:1]),
                              ACT(S["mag"], S["lrdt"], AF.Exp, scale=1.0 / 32), ACT(S["im2"], S["lrdt"], AF.Exp, scale=-2.0)],
                      reads=["prep", "hpi"], writes=["prep_cs"])
                li = lam_im_t[:, gs]
                ops += [TT(a5r[:, :, 0], S["mag"], S["cs"], MUL), TT(a5i[:, :, 0], S["mag"], S["sn"], MUL)]
                for e_ in range(5):
                    ops += cmul(a5r[:, :, e_ + 1], a5i[:, :, e_ + 1], a5r[:, :, e_], a5i[:, :, e_], a5r[:, :, e_], a5i[:, :, e_], S["ta"], S["nsq"])
                ops += [(lambda e: e.tensor_copy(out=S["ar"], in_=a5r[:, :, 5])), (lambda e: e.tensor_copy(out=S["ai"], in_=a5i[:, :, 5])),
                        TT(S["den"], S["lr"], S["lr"], MUL), TT(S["ta"], li, li, MUL), TT(S["den"], S["den"], S["ta"], ADD),
                        (lambda e: e.reciprocal(out=S["den"], in_=S["den"])),
                        TS(S["am1"], S["ar"], -1.0, None, ADD),
                        TT(S["ta"], S["am1"], S["lr"], MUL), TT(S["tb"], S["ai"], li, MUL), TT(S["ta"], S["ta"], S["tb"], ADD), TT(S["cr"], S["ta"], S["den"], MUL),
                        TT(S["ta"], S["ai"], S["lr"], MUL), TT(S["tb"], S["am1"], li, MUL), TT(S["ta"], S["ta"], S["tb"], SUB), TT(S["ci"], S["ta"], S["den"], MUL),
                        TS(S["scr"], S["cr"], sgn[:, 0:1], None, MUL),
                        TS(S["tb"], S["ci"], sgn[:, 0:1], None, MUL),
                        TS(S["nci"], S["ci"], -1.0, None, MUL),
                        TT(S["bir"], S["ar"], S["im2"], MUL), TT(S["bii"], S["ai"], S["im2"], MUL), TS(S["bii"], S["bii"], -1.0, None, MUL),
                        (lambda e: e.tensor_copy(out=sqr[:, :, 0], in_=S["ar"])), (lambda e: e.tensor_copy(out=sqi[:, :, 0], in_=S["ai"])),
                        (lambda e: e.tensor_copy(out=bqr[:, :, 0], in_=S["bir"])), (lambda e: e.tensor_copy(out=bqi[:, :, 0], in_=S["bii"]))]
                for e_ in range(11):
                    ops += cmul(sqr[:, :, e_ + 1], sqi[:, :, e_ + 1], sqr[:, :, e_], sqi[:, :, e_], sqr[:, :, e_], sqi[:, :, e_], S["ta"], S["nsq"])
                for e_ in range(2):
                    ops += cmul(bqr[:, :, e_ + 1], bqi[:, :, e_ + 1], bqr[:, :, e_], bqi[:, :, e_], bqr[:, :, e_], bqi[:, :, e_], S["ta"], S["nsq"])
                bc16 = lambda a: a.unsqueeze(2).to_broadcast([128, GP, 16])
                ops += [TT(BB1[:, :, :], X1t[:, gs, :], bc16(S["cr"]), MUL), TT(bbt[:, :, :], X2t[:, gs, :], bc16(S["tb"]), MUL), TT(BB1[:, :, :], BB1[:, :, :], bbt[:, :, :], ADD),
                        TT(BB2[:, :, :], X2t[:, gs, :], bc16(S["scr"]), MUL), TT(bbt[:, :, :], X1t[:, gs, :], bc16(S["nci"]), MUL), TT(BB2[:, :, :], BB2[:, :, :], bbt[:, :, :], ADD)]
                ops += [(lambda e: e.memset(PWfr[:, :, 0:1], 1.0)), (lambda e: e.memset(PWfi[:, :, 0:1], 0.0))]
                for k in range(LL):
                    n = 1 << k
                    bcn = lambda a, n=n: a.to_broadcast([128, GP, n])
                    ops += cmul(PWfr[:, :, n:2 * n], PWfi[:, :, n:2 * n], PWfr[:, :, 0:n], PWfi[:, :, 0:n],
                                bcn(sqr[:, :, k:k + 1]), bcn(sqi[:, :, k:k + 1]), cta[:, :, 0:n], ctb[:, :, 0:n])
                ops += [(lambda e: e.tensor_copy(out=PWfr[:, :, L5:L5 + 1], in_=sqr[:, :, LL:LL + 1])), (lambda e: e.tensor_copy(out=PWfi[:, :, L5:L5 + 1], in_=sqi[:, :, LL:LL + 1]))]
                ops += [(lambda e: e.memset(PWrr[:, :, L5 - 1:L5], 1.0)), (lambda e: e.memset(PWri[:, :, L5 - 1:L5], 0.0))]
                for k in range(LL):
                    n = 1 << k
                    bcn = lambda a, n=n: a.to_broadcast([128, GP, n])
                    ops += cmul(PWrr[:, :, L5 - 2 * n:L5 - n], PWri[:, :, L5 - 2 * n:L5 - n], PWrr[:, :, L5 - n:L5], PWri[:, :, L5 - n:L5],
                                bcn(sqr[:, :, k:k + 1]), bcn(sqi[:, :, k:k + 1]), cta[:, :, 0:n], ctb[:, :, 0:n])
                ops += [(lambda e: e.memset(PWbr[:, :, 0:1], 1.0)), (lambda e: e.memset(PWbi[:, :, 0:1], 0.0))]
                for k in range(3):
                    n = 1 << k
                    bcn = lambda a, n=n: a.to_broadcast([128, GP, n])
                    ops += cmul(PWbr[:, :, n:2 * n], PWbi[:, :, n:2 * n], PWbr[:, :, 0:n], PWbi[:, :, 0:n],
                                bcn(bqr[:, :, k:k + 1]), bcn(bqi[:, :, k:k + 1]), cta[:, :, 0:n], ctb[:, :, 0:n])
                ops += [(lambda e: e.memset(PLr[:, :, 0:1], 1.0)), (lambda e: e.memset(PLi[:, :, 0:1], 0.0))]
                for k in range(6):
                    n = 1 << k
                    bcn = lambda a, n=n: a.to_broadcast([128, GP, n])
                    ops += cmul(PLr[:, :, n:2 * n], PLi[:, :, n:2 * n], PLr[:, :, 0:n], PLi[:, :, 0:n],
                                bcn(sqr[:, :, LL + k:LL + k + 1]), bcn(sqi[:, :, LL + k:LL + k + 1]), cta[:, :, 0:n], ctb[:, :, 0:n])
                ops += [TS(PRs[:, :, :], PWfr[:, :, :], sgn[:, 0:1], None, MUL), TS(PRs[:, :, :], PRs[:, :, :], -1.0, None, MUL), TS(nPI[:, :, :], PWfi[:, :, :], -1.0, None, MUL)]
                P.seq("dve", ops, reads=["prep", "prep_sn", "prep_cs", "sgn", "X1t", "X2t", "lam_im_t", "Rk_use", "tab_use"], writes=["prep", "prepT"]); ops = []

            esC0 = ExitStack()
            esC0.__enter__()
            sb0 = lambda name, shape, dt: esC0.enter_context(nc.sbuf_tensor(name, shape, dt))
            GA = 32
            i_lre = sb0("i_lre", [128, 32], F32); i_lim = sb0("i_lim", [128, 32], F32); i_ldt = sb0("i_ldt", [128, 32], F32)
            i_X1 = sb0("i_X1", [128, 32, 16], F32); i_X2 = sb0("i_X2", [128, 32, 16], F32)
            i_sgn = sb0("i_sgn", [128, 1], F32); i_hpi = sb0("i_hpi", [128, 1], F32)
            for dst_, src_ in ((i_lre[:, :], lam2_re[:, :]), (i_lim[:, :], lam2_im[:, :]), (i_ldt[:, :], logdt[0:1, :].partition_broadcast(128)),
                               (i_X1[:, :, :], X1d[:, :, :]), (i_X2[:, :, :], X2d[:, :, :])):
                P.dma("sp", lambda e, dst_=dst_, src_=src_: e.dma_start(out=dst_, in_=src_), "setupC0", writes=["c0in"])
            P.bulk_done("setupC0")
            P.seq("pool", [lambda e: e.memset(i_sgn[0:64, :], -1.0), lambda e: e.memset(i_sgn[64:128, :], 1.0), lambda e: e.memset(i_hpi[:, :], float(np.pi / 2))],
                  writes=["sgn", "hpi", "lam_re_t", "lam_im_t", "logdt_t", "X1t", "X2t"], reads=["c0in"])
            sc_names = ("lr", "dt", "lrdt", "th", "mag", "t1", "sn", "cs", "ar", "ai", "den", "am1", "cr", "ci", "ta", "tb", "scr", "nci", "im2", "bir", "bii", "nsq")
            sc0 = {n: sb0("sc0_" + n, [128, GA], F32) for n in sc_names}
            T0 = {n: sb0("p0_" + n, [128, GA, k], F32) for n, k in (("sqr", 12), ("sqi", 12), ("bqr", 3), ("bqi", 3), ("a5r", 6), ("a5i", 6), ("PWfr", L5 + 1), ("PWfi", L5 + 1),
                                                                     ("PWrr", L5), ("PWri", L5), ("PWbr", 8), ("PWbi", 8), ("cta", 64), ("ctb", 64), ("BB1", 16), ("BB2", 16),
                                                                     ("bbt", 16), ("PRs", L5 + 1), ("nPI", L5 + 1), ("PLr", NC5), ("PLi", NC5))}
            emit_prep(GA, slice(0, 32), i_lre, i_lim, i_ldt, i_X1, i_X2, i_sgn, i_hpi, sc0, *[T0[n] for n in ("sqr", "sqi", "bqr", "bqi", "a5r", "a5i", "PWfr", "PWfi", "PWrr", "PWri", "PWbr", "PWbi",
                                                                 "cta", "ctb", "BB1", "BB2", "bbt", "PRs", "nPI", "PLr", "PLi")])
            for nm_ in ("PWrr", "PWri", "PWbr", "PWbi", "BB1", "BB2", "PRs", "nPI", "sqr", "sqi", "PLr", "PLi"):
                P.dma("sp", lambda e, nm_=nm_: e.dma_start(out=prep_scr[nm_][:, :, :], in_=T0[nm_][:, :, :]), "prepst", reads=["prepT"], writes=["prep_scr"])
            P.bulk_done("prepst")
            esC0.close()
            P.barrier()
            esC = ExitStack()
            esC.__enter__()
            sb = lambda name, shape, dt: esC.enter_context(nc.sbuf_tensor(name, shape, dt))
            ps = lambda name, shape, dt: esC.enter_context(nc.psum_tensor(name, shape, dt))
            G8 = 8
            lam_re_t = sb("lam_re_t", [128, 32], F32)
            lam_im_t = sb("lam_im_t", [128, 32], F32)
            logdt_t = sb("logdt_t", [128, 32], F32)
            X1t = sb("X1t", [128, 32, 16], F32)
            X2t = sb("X2t", [128, 32, 16], F32)
            CY1t = sb("CY1t", [128, 32, 16], F32)
            CY2t = sb("CY2t", [128, 32, 16], F32)
            sgn = sb("sgn", [128, 1], F32)
            d_bc = sb("d_bc", [NC5, 512], F32)
            WAt = sb("WAt", [128, 4, 128], BF16)
            WBt = sb("WBt", [128, 4, 128], BF16)
            gba = sb("gba", [128, 4], F32)
            gbb = sb("gbb", [128, 4], F32)
            SW = sb("SW", [128, 128], F32)
            rkt8 = sb("rkt8", [128, G8, 128], F32)
            send = sb("send", [128, G8], F32)
            sc = {n: sb("sc_" + n, [128, G8], F32) for n in
                  ("lr", "dt", "lrdt", "th", "mag", "t1", "sn", "cs", "ar", "ai", "den", "am1", "cr", "ci", "ta", "tb", "scr", "nci", "im2", "bir", "bii", "nsq")}
            a5r = sb("a5r", [128, G8, 6], F32)
            a5i = sb("a5i", [128, G8, 6], F32)
            hpi = sb("hpi", [128, 1], F32)
            sqr2 = [sb(f"sqr_{i}", [128, G8, 12], F32) for i in range(2)]
            sqi2 = [sb(f"sqi_{i}", [128, G8, 12], F32) for i in range(2)]
            bqr = sb("bqr", [128, G8, 3], F32)
            bqi = sb("bqi", [128, G8, 3], F32)
            PWrr2 = [sb(f"PWrr_{i}", [128, G8, L5], F32) for i in range(2)]
            PWri2 = [sb(f"PWri_{i}", [128, G8, L5], F32) for i in range(2)]
            PWbr2 = [sb(f"PWbr_{i}", [128, G8, 8], F32) for i in range(2)]
            PWbi2 = [sb(f"PWbi_{i}", [128, G8, 8], F32) for i in range(2)]
            BB12 = [sb(f"BB1_{i}", [128, G8, 16], F32) for i in range(2)]
            BB22 = [sb(f"BB2_{i}", [128, G8, 16], F32) for i in range(2)]
            bbt = sb("bbt", [128, G8, 16], F32)
            PRs2 = [sb(f"PRs_{i}", [128, G8, L5 + 1], F32) for i in range(2)]
            nPI2 = [sb(f"nPI_{i}", [128, G8, L5 + 1], F32) for i in range(2)]
            PLr2 = [sb(f"PLr_{i}", [128, G8, NC5], F32) for i in range(2)]
            PLi2 = [sb(f"PLi_{i}", [128, G8, NC5], F32) for i in range(2)]
            Wc1 = sb("Wc1", [128, G8, NC5], F32)
            Wc2 = sb("Wc2", [128, G8, NC5], F32)
            hsw = sb("hsw", [128, G8], F32)
            tA = sb("tA", [128, L5 + 1, 16], F32)
            tB = sb("tB", [128, L5 + 1, 16], F32)
            tC = sb("tC", [128, L5 + 1, 16], F32)
            tD = sb("tD", [128, L5 + 1, 16], F32)
            ZTb = sb("ZTb", [128, L5, 16], BF16)
            XTb = sb("XTb", [128, 8, 16], BF16)
            Zt2 = [sb(f"Zt_{i}", [128, G8, KC5, 128], BF16) for i in range(2)]
            Gtab = sb("Gtab", [128, G8, L5 * 16], BF16)
            Yt = sb("Yt", [128, G8, L5 + 1, 16], BF16)
            Rk2 = [sb(f"Rk_{i}", [128, G8, NLEV, 128], F32) for i in range(2)]
            ucj = sb("ucj", [NC5, G8, L5, 16], BF16)
            ustack = sb("ustack", [128, G8, KC5, NC5], BF16)
            E32 = sb("E32", [128, G8, NC5], F32)
            Xs = sb("Xs", [128, G8, NC5 + 1], F32)
            Sprevb = sb("Sprevb", [128, G8, NC5], BF16)
            sg_t = sb("sg_t", [128, 4, G8], F32)
            hs = sb("hs", [128, G8], F32)
            hs2 = sb("hs2", [128, G8], F32)
            du = sb("du", [NC5, L5, 16], F32)
            yv = sb("yv", [NC5, L5 * 16], F32)
            y2 = sb("y2", [NC5, L5 * 16], F32)
            ysg = sb("ysg", [NC5, L5 * 16], F32)
            ygel = sb("ygel", [NC5, L5, 128], BF16)
            ygT = sb("ygT", [128, L5, NC5], BF16)
            sbt = sb("sbt", [128, 512], F32)
            ps_z = [ps(f"ps_z{i}", [128, 512], F32) for i in range(2)]
            pT = ps("pT", [128, 2048], BF16)
            pE = ps("pE", [128, G8, NC5], F32)
            pD = ps("pD", [128, G8, NC5], F32)
            pY = ps("pY", [128, 512], F32)

            def ld(dst, src, key, q="sp"):
                P.dma(q, lambda e: e.dma_start(out=dst, in_=src), "setupC" if q == "sp" else "setupCp", writes=[key])
            ld(lam_re_t[:, :], lam2_re[:, :], "lam_re_t")
            ld(lam_im_t[:, :], lam2_im[:, :], "lam_im_t")
            ld(logdt_t[:, :], logdt[0:1, :].partition_broadcast(128), "logdt_t")
            ld(X1t[:, :, :], X1d[:, :, :], "X1t")
            ld(X2t[:, :, :], X2d[:, :, :], "X2t")
            ld(CY1t[:, :, :], CY1d[:, :, :], "CY1t")
            ld(CY2t[:, :, :], CY2d[:, :, :], "CY2t")
            ld(d_bc[:, :], s5d[0:1, :].partition_broadcast(NC5), "d_bc")
            ld(gba[:, :], gbad[:, :], "gba")
            ld(gbb[:, :], gbbd[:, :], "gbb")
            ld(WAt[:, :, :], WAd.ap().rearrange("u p c -> p u c"), "WAt", q="pool")
            ld(WBt[:, :, :], WBd.ap().rearrange("u p c -> p u c"), "WBt", q="pool")
            P.bulk_done("setupC")
            P.bulk_done("setupCp")
            P.seq("pool", [lambda e: e.memset(sgn[0:64, :], -1.0), lambda e: e.memset(sgn[64:128, :], 1.0)], writes=["sgn"])
            P.op("pool", lambda e: e.memset(hpi[:, :], float(np.pi / 2)), writes=["hpi"])
            P.seq("pool", [lambda e: e.memset(SW[:, :], 0.0),
                           lambda e: e.affine_select(out=SW[:, :], in_=SW[:, :], pattern=[[-1, 128]], compare_op=ALU.not_equal, fill=1.0, base=64, channel_multiplier=1),
                           lambda e: e.affine_select(out=SW[:, :], in_=SW[:, :], pattern=[[-1, 128]], compare_op=ALU.not_equal, fill=1.0, base=-64, channel_multiplier=1)],
                  writes=["SW"])
            P.op("pool", lambda e: e.tensor_scalar(out=SW[:, :], in0=SW[:, :], scalar1=sgn[:, 0:1], scalar2=None, op0=ALU.mult), reads=["SW", "sgn"], writes=["SW"])

            PI = float(np.pi)
            bh = lambda a, n: a.unsqueeze(1).to_broadcast([128, n, 16])
            def u_loads(un):
                par = un % 2
                gs = slice(un * 8, (un + 1) * 8)
                PWrr, PWri, PWbr, PWbi, BB1, BB2, PRs, nPI, sqr, sqi, PLr, PLi = [t2[par] for t2 in (PWrr2, PWri2, PWbr2, PWbi2, BB12, BB22, PRs2, nPI2, sqr2, sqi2, PLr2, PLi2)]
                Zt = Zt2[par]; Rk = Rk2[par]
                KP = f"prepT{par}"; KZ = f"Zt{par}"; KR = [f"Rk{par}_{g}" for g in range(G8)]
                for nm_, dst_ in (("PWrr", PWrr), ("PWri", PWri), ("PWbr", PWbr), ("PWbi", PWbi), ("BB1", BB1), ("BB2", BB2),
                                  ("PRs", PRs), ("nPI", nPI), ("sqr", sqr), ("sqi", sqi), ("PLr", PLr), ("PLi", PLi)):
                    P.dma("sp", lambda e, nm_=nm_, dst_=dst_, gs=gs: e.dma_start(out=dst_[:, :, :], in_=prep_scr[nm_][:, gs, :]), "prepld",
                          reads=["prep_scr", "Rk_use", "tab_use"], writes=[KP])
                P.bulk_done("prepld")


            def u_Z(un):
                par = un % 2
                gs = slice(un * 8, (un + 1) * 8)
                PWrr, PWri, PWbr, PWbi, BB1, BB2, PRs, nPI, sqr, sqi, PLr, PLi = [t2[par] for t2 in (PWrr2, PWri2, PWbr2, PWbi2, BB12, BB22, PRs2, nPI2, sqr2, sqi2, PLr2, PLi2)]
                Zt = Zt2[par]; Rk = Rk2[par]
                KP = f"prepT{par}"; KZ = f"Zt{par}"; KR = [f"Rk{par}_{g}" for g in range(G8)]
                bh = lambda a, n: a.unsqueeze(1).to_broadcast([128, n, 16])
                for g in range(G8):
                    bq = lambda a: a.unsqueeze(2).to_broadcast([128, L5, 16])
                    P.seq("dve", [TT(tA[:, 0:L5, :], bq(PWrr[:, g, :]), bh(BB1[:, g, :], L5), MUL),
                                  TT(tB[:, 0:L5, :], bq(PWri[:, g, :]), bh(BB2[:, g, :], L5), MUL),
                                  TT(ZTb[:, :, :], tA[:, 0:L5, :], tB[:, 0:L5, :], ADD)],
                          reads=[KP, "ZTb_use"], writes=["tA", "tB", "ZTb"])
                    def f_zt(e):
                        for kc in range(KC5):
                            i = e.transpose(out=pT[:, kc * 128:(kc + 1) * 128], in_=ZTb[:, kc * 8:(kc + 1) * 8, :], identity=ident[:, :])
                        return i
                    P.op("pe", f_zt, reads=["ZTb", "ident"], writes=["pT"])
                    P.op("act", lambda e, g=g: e.copy(out=Zt[:, g, :, :], in_=pT[:, 0:KC5 * 128].rearrange("p (a b) -> p a b", b=128)), reads=["pT"], writes=[KZ, "ZTb_use"])

            def u_rk(un):
                par = un % 2
                gs = slice(un * 8, (un + 1) * 8)
                PWrr, PWri, PWbr, PWbi, BB1, BB2, PRs, nPI, sqr, sqi, PLr, PLi = [t2[par] for t2 in (PWrr2, PWri2, PWbr2, PWbi2, BB12, BB22, PRs2, nPI2, sqr2, sqi2, PLr2, PLi2)]
                Zt = Zt2[par]; Rk = Rk2[par]
                KP = f"prepT{par}"; KZ = f"Zt{par}"; KR = [f"Rk{par}_{g}" for g in range(G8)]
                for k in range(NLEV):
                    P.seq("pool", [lambda e, k=k: e.tensor_tensor(out=Rk[:, :, k, :], in0=identf[:, :].unsqueeze(1).to_broadcast([128, G8, 128]),
                                                                  in1=sqr[:, :, LL + k:LL + k + 1].to_broadcast([128, G8, 128]), op=MUL),
                                   lambda e, k=k: e.tensor_tensor(out=rkt8[:, :, :], in0=SW[:, :].unsqueeze(1).to_broadcast([128, G8, 128]),
                                                                  in1=sqi[:, :, LL + k:LL + k + 1].to_broadcast([128, G8, 128]), op=MUL),
                                   lambda e, k=k: e.tensor_tensor(out=Rk[:, :, k, :], in0=Rk[:, :, k, :], in1=rkt8[:, :, :], op=SUB)],
                          reads=[KP, "identf", "SW", "sgn", "Rk_use"], writes=KR + ["rkt"])

            def u_pre(un):
                par = un % 2
                gs = slice(un * 8, (un + 1) * 8)
                PWrr, PWri, PWbr, PWbi, BB1, BB2, PRs, nPI, sqr, sqi, PLr, PLi = [t2[par] for t2 in (PWrr2, PWri2, PWbr2, PWbi2, BB12, BB22, PRs2, nPI2, sqr2, sqi2, PLr2, PLi2)]
                Zt = Zt2[par]; Rk = Rk2[par]
                KP = f"prepT{par}"; KZ = f"Zt{par}"; KR = [f"Rk{par}_{g}" for g in range(G8)]
                for g in range(G8):
                    P.dma("sp", lambda e, un=un, g=g: e.dma_start(out=ucj[:, g, :, :],
                                                                   in_=u_scr.ap().rearrange("(c j) n -> c j n", j=L5)[:, :, un * 128 + g * 16:un * 128 + (g + 1) * 16]),
                          "ucj", reads=[f"u_scr{t}" for t in range(NT)] + ["ucj_use"], writes=[f"ucj{g}"])
                P.bulk_done("ucj")
                US0 = 1024
                for g in range(G8):
                    def f_us(e, g=g):
                        for kc in range(KC5):
                            i = e.transpose(out=pT[:, US0 + kc * NC5:US0 + (kc + 1) * NC5], in_=ucj[:, g, kc * 8:(kc + 1) * 8, :], identity=ident[0:NC5, 0:NC5])
                        return i
                    P.op("pe", f_us, reads=[f"ucj{g}", "ident"], writes=["pT2"])
                    P.op("act", lambda e, g=g: e.copy(out=ustack[:, g, :, :], in_=pT[:, US0:US0 + KC5 * NC5].rearrange("p (a b) -> p a b", b=NC5)), reads=["pT2"], writes=[f"ustack{g}"])
                    def f_E(e, g=g):
                        for kc in range(KC5):
                            i = e.matmul(out=pE[:, g, :], lhsT=Zt[:, g, kc, :], rhs=ustack[:, g, kc, :], start=(kc == 0), stop=(kc == KC5 - 1))
                        return i
                    P.op("pe", f_E, reads=[KZ, f"ustack{g}"], writes=["pE"])
                P.op("act", lambda e: e.copy(out=E32[:, :, :], in_=pE[:, :, :]), reads=["pE"], writes=["E32"])

                NX = NC5 + 1
                def doubling(tag):
                    for k in range(NLEV):
                        n = NX - (1 << k)
                        def f_d(e, k=k, n=n):
                            for g in range(G8):
                                i = e.matmul(out=pD[:, g, 0:n], lhsT=Rk[:, g, k, :], rhs=Xs[:, g, 0:n], start=True, stop=True)
                            return i
                        P.op("pe", f_d, reads=["Xs"] + KR, writes=["pD"])
                        P.op("dve", lambda e, k=k, n=n: e.tensor_tensor(out=Xs[:, :, 1 << k:NX], in0=Xs[:, :, 1 << k:NX], in1=pD[:, :, 0:n], op=ADD),
                             reads=["pD", "Xs"], writes=["Xs"])
                P.seq("dve", [lambda e: e.memset(Xs[:, :, 0:1], 0.0), lambda e: e.tensor_copy(out=Xs[:, :, 1:NX], in_=E32[:, :, :])], reads=["E32"], writes=["Xs"])
                doubling("loc")
                P.op("dve", lambda e: e.tensor_copy(out=send[:, :], in_=Xs[:, :, NC5]), reads=["Xs"], writes=["send"])
                P.dma("sp", lambda e, un=un: e.dma_start(out=ccs_in[un][:, :], in_=send[:, :]), f"ccsin{un}", reads=["send"], writes=[f"ccs_in{un}"])
                P.dma("pool", lambda e, un=un: e.collective_compute("AllGather", ALU.bypass, replica_groups=GROUPS4,
                                                                     ins=[ccs_in[un].ap().opt()], outs=[ccs_out[un].ap().opt()]),
                      f"ccs{un}", reads=[f"ccs_in{un}"], writes=[f"ccs_out{un}"], inc=1)
                P.dma("sp", lambda e, un=un: e.dma_start(out=sg_t[:, :, :], in_=ccs_out[un].ap().rearrange("(r p) c -> p r c", p=128)),
                      f"ccsld{un}", reads=[f"ccs_out{un}"], writes=["sg_t"])

            def u_xyg(un):
                par = un % 2
                gs = slice(un * 8, (un + 1) * 8)
                PWrr, PWri, PWbr, PWbi, BB1, BB2, PRs, nPI, sqr, sqi, PLr, PLi = [t2[par] for t2 in (PWrr2, PWri2, PWbr2, PWbi2, BB12, BB22, PRs2, nPI2, sqr2, sqi2, PLr2, PLi2)]
                Zt = Zt2[par]; Rk = Rk2[par]
                KP = f"prepT{par}"; KZ = f"Zt{par}"; KR = [f"Rk{par}_{g}" for g in range(G8)]
                for g in range(G8):
                    ga = un * 8 + g
                    bq8 = lambda a: a.unsqueeze(2).to_broadcast([128, 8, 16])
                    P.seq("dve", [TT(tA[:, 0:8, :], bq8(PWbr[:, g, :]), bh(BB1[:, g, :], 8), MUL),
                                  TT(tB[:, 0:8, :], bq8(PWbi[:, g, :]), bh(BB2[:, g, :], 8), MUL),
                                  TT(XTb[:, :, :], tA[:, 0:8, :], tB[:, 0:8, :], ADD)],
                          reads=[KP, "tA", "tB", "XTb_use"], writes=["tA", "tB", "XTb"])
                    bqY = lambda a: a.unsqueeze(2).to_broadcast([128, L5 + 1, 16])
                    P.seq("pool", [TT(tC[:, :, :], bqY(PRs[:, g, :]), bh(CY1t[:, ga, :], L5 + 1), MUL),
                                   TT(tD[:, :, :], bqY(nPI[:, g, :]), bh(CY2t[:, ga, :], L5 + 1), MUL),
                                   TT(Yt[:, g, :, :], tC[:, :, :], tD[:, :, :], ADD)],
                          reads=[KP, "CY1t", "CY2t", "tab_use"], writes=["tC", "tD", f"Yt{g}"])
                    P.op("pe", lambda e, g=g: e.matmul(out=pY[:, :], lhsT=XTb[:, :, :], rhs=Yt[:, g, 0:L5, :], start=True, stop=True), reads=["XTb", f"Yt{g}"], writes=["pY"])
                    P.op("act", lambda e, g=g: e.copy(out=Gtab[:, g, :], in_=pY[:, :]), reads=["pY", "tab_use"], writes=[f"Gtab{g}", "XTb_use"])
                    P.op("pool", lambda e, g=g: e.affine_select(out=Gtab[:, g, :].rearrange("p (m h) -> p m h", h=16), in_=Gtab[:, g, :].rearrange("p (m h) -> p m h", h=16),
                                                                pattern=[[16, L5], [0, 16]], compare_op=ALU.is_ge, fill=0.0, base=15, channel_multiplier=-1),
                         reads=[f"Gtab{g}"], writes=[f"Gtab{g}"])


            def u_post(un):
                par = un % 2
                gs = slice(un * 8, (un + 1) * 8)
                PWrr, PWri, PWbr, PWbi, BB1, BB2, PRs, nPI, sqr, sqi, PLr, PLi = [t2[par] for t2 in (PWrr2, PWri2, PWbr2, PWbi2, BB12, BB22, PRs2, nPI2, sqr2, sqi2, PLr2, PLi2)]
                Zt = Zt2[par]; Rk = Rk2[par]
                KP = f"prepT{par}"; KZ = f"Zt{par}"; KR = [f"Rk{par}_{g}" for g in range(G8)]
                P.op("dve", lambda e: e.memset(hs[:, :], 0.0), writes=["hs"])
                for pp in range(3):
                    def f_hr(e):
                        for g in range(G8):
                            i = e.matmul(out=pD[:, g, 0:1], lhsT=Rk[:, g, NLEV - 1, :], rhs=hs[:, g:g + 1], start=True, stop=True)
                        return i
                    P.op("pe", f_hr, reads=["hs"] + KR, writes=["pD"])
                    P.seq("dve", [lambda e, pp=pp: e.tensor_tensor(out=hs2[:, :], in0=pD[:, :, 0], in1=sg_t[:, pp, :], op=ADD),
                                  lambda e: e.tensor_tensor(out=hs2[:, :], in0=hs2[:, :], in1=hs[:, :], op=SUB),
                                  lambda e, pp=pp: e.scalar_tensor_tensor(out=hs[:, :], in0=hs2[:, :], scalar=pm[:, pp:pp + 1], in1=hs[:, :], op0=MUL, op1=ADD)],
                          reads=["pD", "sg_t", "hs", "pm"], writes=["hs", "hs2"])
                P.op("pe", lambda e: e.matmul(out=pD[:, 0, 0:G8], lhsT=SW[:, :], rhs=hs[:, :], start=True, stop=True), reads=["hs", "SW"], writes=["pD"])
                bcc = lambda a: a.unsqueeze(2).to_broadcast([128, G8, NC5])
                P.seq("dve", [lambda e: e.tensor_copy(out=hsw[:, :], in_=pD[:, 0, 0:G8]),
                              lambda e: e.tensor_tensor(out=Wc1[:, :, :], in0=PLr[:, :, :], in1=bcc(hs[:, :]), op=MUL),
                              lambda e: e.tensor_tensor(out=Wc2[:, :, :], in0=PLi[:, :, :], in1=bcc(hsw[:, :]), op=MUL),
                              lambda e: e.tensor_tensor(out=Wc1[:, :, :], in0=Wc1[:, :, :], in1=Wc2[:, :, :], op=SUB),
                              lambda e: e.tensor_tensor(out=Sprevb[:, :, :], in0=Wc1[:, :, :], in1=Xs[:, :, 0:NC5], op=ADD)],
                      reads=["pD", "hs", KP, "Xs"], writes=["hsw", "Wc", "Sprevb"])

                for g in range(G8):
                    def f_y(e, g=g):
                        e.matmul(out=pY[0:NC5, :], lhsT=Sprevb[:, g, :], rhs=Yt[:, g, 1:L5 + 1, :], start=True, stop=False)
                        for kc in range(KC5):
                            lo = 128 * kc
                            i = e.matmul(out=pY[0:NC5, lo:512], lhsT=ustack[:, g, kc, :], rhs=Gtab[:, g, 0:512 - lo], start=False, stop=(kc == KC5 - 1))
                        return i
                    P.op("pe", f_y, reads=["Sprevb", f"Yt{g}", f"Gtab{g}", f"ustack{g}"], writes=["pY"])
                    ga = un * 8 + g
                    P.op("pool", lambda e, g=g, ga=ga: e.tensor_tensor(out=du[:, :, :], in0=ucj[:, g, :, :],
                                                                       in1=d_bc[:, ga * 16:(ga + 1) * 16].unsqueeze(1).to_broadcast([NC5, L5, 16]), op=MUL),
                         reads=[f"ucj{g}", "d_bc"], writes=["du"])
                    yv_ = yv[:, :]
                    P.seq("dve", [lambda e: e.tensor_tensor(out=yv[:, :], in0=pY[0:NC5, :], in1=du[:, :, :].rearrange("p a b -> p (a b)"), op=ADD),
                                  TT(y2[:, :], yv_, yv_, MUL), TS(y2[:, :], y2[:, :], 0.044715, 1.0, MUL, ADD), TT(y2[:, :], y2[:, :], yv_, MUL)],
                          reads=["pY", "du", "ysg"], writes=["yv", "y2"])
                    P.op("act", ACT(ysg[:, :], y2[:, :], AF.Sigmoid, scale=1.5957691216057308), reads=["y2"], writes=["ysg"])
                    P.op("dve", lambda e, g=g: e.tensor_tensor(out=ygel[:, :, g * 16:(g + 1) * 16], in0=yv[:, :].rearrange("p (a b) -> p a b", b=16),
                                                               in1=ysg[:, :].rearrange("p (a b) -> p a b", b=16), op=MUL),
                         reads=["yv", "ysg", "ygel_use"], writes=[f"ygel{g}"])
                def f_gt(e):
                    for j in range(L5):
                        i = e.transpose(out=pT[:, j * NC5:(j + 1) * NC5], in_=ygel[:, j, :], identity=ident[0:NC5, 0:NC5])
                    return i
                P.op("pe", f_gt, reads=[f"ygel{g}" for g in range(G8)] + ["ident"], writes=["pT", "pT2"])
                P.op("act", lambda e: e.copy(out=ygT[:, :, :], in_=pT[:, :].rearrange("p (a b) -> p a b", b=NC5)), reads=["pT", "pT2"], writes=["ygT", "ygel_use"])
                ms_v = mslab[:, :].rearrange("p (c j) -> p j c", j=L5)
                JB = 512 // NC5
                for nb in range(4):
                    P.op("pe", lambda e, nb=nb, un=un: e.matmul(out=ps_z[0][:, :], lhsT=WAt[:, un, :], rhs=ygT[:, nb * JB:(nb + 1) * JB, :], start=True, stop=True),
                         reads=["WAt", "ygT"], writes=["ps_z0"])
                    P.op("pe", lambda e, nb=nb, un=un: e.matmul(out=ps_z[1][:, :], lhsT=WBt[:, un, :], rhs=ygT[:, nb * JB:(nb + 1) * JB, :], start=True, stop=True),
                         reads=["WBt", "ygT"], writes=["ps_z1"])
                    P.op("act", lambda e, un=un: e.activation(out=sbt[:, :], in_=ps_z[1][:, :], func=AF.Sigmoid, bias=gbb[:, un:un + 1]),
                         reads=["ps_z1", "gbb"], writes=["sbt"])
                    P.op("dve", lambda e, nb=nb, un=un: e.scalar_tensor_tensor(out=ms_v[:, nb * JB:(nb + 1) * JB, :], in0=ps_z[0][:, :].rearrange("p (j c) -> p j c", c=NC5), scalar=gba[:, un:un + 1],
                                                                               in1=sbt[:, :].rearrange("p (j c) -> p j c", c=NC5), op0=ADD, op1=MUL),
                         reads=["ps_z0", "sbt", "gba"], writes=["mslab"])
                P.dma("sp", lambda e, un=un: e.dma_start(out=mergedT_scr[512 + un * 128:512 + (un + 1) * 128, :], in_=mslab[:, :]), "mscr",
                      reads=["mslab"], writes=[f"mscr{4 + un}"])

            u_loads(0); u_Z(0); u_rk(0)
            for un in range(4):
                u_pre(un)
                u_xyg(un)
                if un + 1 < 4:
                    u_loads(un + 1); u_Z(un + 1); u_rk(un + 1)
                u_post(un)
            esC.close()
            P.barrier()
        else:
            esH.close()

        if stage >= 4:
            AX = mybir.AxisListType
            MUL, ADD, SUB = ALU.mult, ALU.add, ALU.subtract
            esDE = ExitStack()
            esDE.__enter__()
            sbp = lambda name, shape, dt: esDE.enter_context(nc.sbuf_tensor(name, shape, dt))
            h2T = sbp("h2T", [128, 8, NTOK], BF16)
            cw = sbp("cw", [128, NT, 32], F32)
            xt2 = [sbp(f"xt2_{i}", [128, D], F32) for i in range(2)]
            ssq2 = sbp("ssq2", [128, 2 * NT], F32)
            rstd2 = sbp("rstd2", [128, 2 * NT], F32)
            junk2 = sbp("junk2", [128, D], BF16)
            esD = ExitStack()
            esD.__enter__()
            sb = lambda name, shape, dt: esD.enter_context(nc.sbuf_tensor(name, shape, dt))
            ps = lambda name, shape, dt: esD.enter_context(nc.psum_tensor(name, shape, dt))
            mT = sb("mT", [128, 8, NTOK], BF16)
            Wout = sb("Wout", [128, 8, D], BF16)
            Wr = sb("Wr", [128, 8, 36], BF16)
            rb_bc = sb("rb_bc", [128, 36], F32)
            xm = [sb(f"xm{i}", [128, D], F32) for i in range(2)]
            hb2 = [sb(f"hb2_{i}", [128, D], BF16) for i in range(2)]
            lg = sb("lg", [128, NT, 36], F32)
            r_a = sb("r_a", [128, NT, 4], F32)
            r_b = sb("r_b", [128, NT, 4], F32)
            r_gmax = sb("r_gmax", [128, NT], F32)
            r_gw = sb("r_gw", [128, NT], F32)
            r_el = sb("r_el", [128, NT, 32], F32)
            r_t = sb("r_t", [128, NT, 32], F32)
            r_oh1 = sb("r_oh1", [128, NT, 32], F32)
            r_oh2 = sb("r_oh2", [128, NT, 32], F32)
            r_m1 = sb("r_m1", [128, NT], F32)
            r_m2 = sb("r_m2", [128, NT], F32)
            r_w1 = sb("r_w1", [128, NT], F32)
            r_w2 = sb("r_w2", [128, NT], F32)
            po4 = [[ps(f"po{i}{j}", [128, 512], F32) for j in range(2)] for i in range(2)]
            tp2 = [ps(f"tp2_{i}", [128, 8, 128], BF16) for i in range(2)]
            pr2 = [ps(f"pr{i}", [128, 36], F32) for i in range(2)]

            P.dma("sp", lambda e: e.dma_start(out=mT[:, :, :], in_=mergedT_scr.ap().rearrange("(kc p) t -> p kc t", p=128)), "mT",
                  reads=[f"mscr{i}" for i in range(8)], writes=["mT"])
            P.dma("pool", lambda e: e.dma_start(out=Wout[:, :, :], in_=w_out.ap().rearrange("(kc p) c -> p kc c", p=128)), "setupDp", writes=["Wout"])
            P.dma("pool", lambda e: e.dma_start(out=Wr[:, :, :], in_=w_rt.ap().rearrange("(kc p) c -> p kc c", p=128)), "setupDp", writes=["Wr"])
            P.dma("sp", lambda e: e.dma_start(out=rb_bc[:, :], in_=b_rt[0:1, :].partition_broadcast(128)), "setupD", writes=["rb_bc"])
            P.dma("sp", lambda e: e.dma_start(out=g_bc[:, :], in_=g_ffn[0:1, :].partition_broadcast(128)), "setupD", writes=["g_bc"])
            P.bulk_done("setupD")
            P.bulk_done("setupDp")

            for t0 in range(0, NT, 2):
                tl = (t0, t0 + 1)
                for t in tl:
                    b = t % 2
                    P.dma("sp", lambda e, t=t, b=b: e.dma_start(out=xt2[b][:, :], in_=x[t * 128:(t + 1) * 128, :]), f"xt2_{b}", writes=[f"xt2_{b}"])
                for t in tl:
                    b = t % 2
                    ts_ = slice(t * 128, (t + 1) * 128)
                    for hf in range(2):
                        def f_o(e, ts_=ts_, hf=hf, b=b):
                            for k in range(8):
                                i = e.matmul(out=po4[b][hf][:, :], lhsT=mT[:, k, ts_], rhs=Wout[:, k, hf * 512:(hf + 1) * 512], start=(k == 0), stop=(k == 7))
                            return i
                        P.op("pe", f_o, reads=["mT", "Wout"], writes=[f"po{b}{hf}"])
                for t in tl:
                    b = t % 2
                    for hf in range(2):
                        P.op("dve", lambda e, hf=hf, b=b: e.tensor_tensor(out=xm[b][:, hf * 512:(hf + 1) * 512], in0=po4[b][hf][:, :], in1=xt2[b][:, hf * 512:(hf + 1) * 512], op=ADD),
                             reads=[f"po{b}{hf}", f"xt2_{b}"], writes=[f"xm{b}_{hf}"])
                for t in tl:
                    b = t % 2
                    xk = [f"xm{b}_0", f"xm{b}_1"]
                    P.dma("sp", lambda e, t=t, b=b: e.dma_start(out=xmid_scr[t * 128:(t + 1) * 128, :], in_=xm[b][:, :]), f"xmid{b}", reads=xk, writes=[f"xmid{t}"])
                    P.seq("act", [lambda e, b=b, t=t: e.activation(out=junk2[:, :], in_=xm[b][:, :], func=AF.Square, accum_out=ssq2[:, t:t + 1]),
                                  lambda e, t=t: e.activation(out=ssq2[:, t:t + 1], in_=ssq2[:, t:t + 1], func=AF.Sqrt, scale=1.0 / D, bias=EPS)],
                          reads=xk, writes=["junk2", f"ssq2_{t}"])
                for t in tl:
                    b = t % 2
                    xk = [f"xm{b}_0", f"xm{b}_1"]
                    P.op("dve", lambda e, t=t: e.reciprocal(out=rstd2[:, t:t + 1], in_=ssq2[:, t:t + 1]), reads=[f"ssq2_{t}"], writes=[f"rstd2_{t}"])
                    P.op("dve", lambda e, b=b, t=t: e.scalar_tensor_tensor(out=hb2[b][:, :], in0=xm[b][:, :], scalar=rstd2[:, t:t + 1], in1=g_bc[:, :], op0=MUL, op1=MUL),
                         reads=xk + [f"rstd2_{t}", "g_bc"], writes=[f"hb2_{b}"])
                for t in tl:
                    b = t % 2
                    def f_tp2(e, b=b):
                        for k in range(8):
                            i = e.transpose(out=tp2[b][:, k, :], in_=hb2[b][:, k * 128:(k + 1) * 128], identity=ident[:, :])
                        return i
                    P.op("pe", f_tp2, reads=[f"hb2_{b}", "ident"], writes=[f"tp2_{b}"])
                for t in tl:
                    b = t % 2
                    ts_ = slice(t * 128, (t + 1) * 128)
                    P.op("act", lambda e, ts_=ts_, b=b: e.copy(out=h2T[:, :, ts_], in_=tp2[b][:, :, :]), reads=[f"tp2_{b}"], writes=[f"h2T_{t // 4}"])
                for t in tl:
                    b = t % 2
                    ts_ = slice(t * 128, (t + 1) * 128)
                    def f_r(e, ts_=ts_, b=b):
                        for k in range(8):
                            i = e.matmul(out=pr2[b][:, :], lhsT=h2T[:, k, ts_], rhs=Wr[:, k, :], start=(k == 0), stop=(k == 7))
                        return i
                    P.op("pe", f_r, reads=[f"h2T_{t // 4}", "Wr"], writes=[f"pr{b}"])
                for t in tl:
                    b = t % 2
                    P.op("dve", lambda e, t=t, b=b: e.tensor_tensor(out=lg[:, t, :], in0=pr2[b][:, :], in1=rb_bc[:, :], op=ADD), reads=[f"pr{b}", "rb_bc"], writes=["lg"])

            BIG = 1.0e9
            bc4 = lambda a: a.unsqueeze(2).to_broadcast([128, NT, 4])
            bc32 = lambda a: a.unsqueeze(2).to_broadcast([128, NT, 32])
            gl = lg[:, :, 0:4]
            el = lg[:, :, 4:36]
            P.seq("dve", [
                lambda e: e.tensor_reduce(out=r_gmax[:, :], in_=gl, axis=AX.X, op=ALU.max),
                lambda e: e.tensor_tensor(out=r_a[:, :, :], in0=gl, in1=bc4(r_gmax[:, :]), op=SUB)], reads=["lg"], writes=["r_a", "r_gmax"])
            P.op("act", lambda e: e.activation(out=r_b[:, :, :], in_=r_a[:, :, :], func=AF.Exp), reads=["r_a"], writes=["r_b"])
            P.seq("dve", [
                lambda e: e.tensor_reduce(out=r_gw[:, :], in_=r_b[:, :, :], axis=AX.X, op=ADD),
                lambda e: e.reciprocal(out=r_gw[:, :], in_=r_gw[:, :]),
                lambda e: e.tensor_tensor(out=r_a[:, :, :], in0=gl, in1=bc4(r_gmax[:, :]), op=ALU.is_equal),
                lambda e: e.tensor_scalar(out=r_a[:, :, :], in0=r_a[:, :, :], scalar1=-1.0, scalar2=BIG, op0=ADD, op1=MUL),
                lambda e: e.tensor_tensor(out=r_el[:, :, :].rearrange("p t (g k) -> p t g k", k=8), in0=el.rearrange("p t (g k) -> p t g k", k=8),
                                          in1=r_a[:, :, :].unsqueeze(3).to_broadcast([128, NT, 4, 8]), op=ADD),
                lambda e: e.tensor_reduce(out=r_m1[:, :], in_=r_el[:, :, :], axis=AX.X, op=ALU.max),
                lambda e: e.tensor_tensor(out=r_oh1[:, :, :], in0=r_el[:, :, :], in1=bc32(r_m1[:, :]), op=ALU.is_equal),
                lambda e: e.scalar_tensor_tensor(out=r_t[:, :, :], in0=r_oh1[:, :, :], scalar=-BIG, in1=r_el[:, :, :], op0=MUL, op1=ADD),
                lambda e: e.tensor_reduce(out=r_m2[:, :], in_=r_t[:, :, :], axis=AX.X, op=ALU.max),
                lambda e: e.tensor_tensor(out=r_oh2[:, :, :], in0=r_t[:, :, :], in1=bc32(r_m2[:, :]), op=ALU.is_equal),
                lambda e: e.tensor_tensor(out=r_w1[:, :], in0=r_m1[:, :], in1=r_m2[:, :], op=SUB)],
                reads=["lg", "r_b", "r_a"], writes=["r_a", "router1"])
            P.op("act", lambda e: e.activation(out=r_w1[:, :], in_=r_w1[:, :], func=AF.Sigmoid), reads=["router1"], writes=["r_w1"])
            P.seq("dve", [
                lambda e: e.tensor_scalar(out=r_w2[:, :], in0=r_w1[:, :], scalar1=-1.0, scalar2=1.0, op0=MUL, op1=ADD),
                lambda e: e.tensor_tensor(out=r_w1[:, :], in0=r_w1[:, :], in1=r_gw[:, :], op=MUL),
                lambda e: e.tensor_tensor(out=r_w2[:, :], in0=r_w2[:, :], in1=r_gw[:, :], op=MUL),
                lambda e: e.tensor_tensor(out=r_oh1[:, :, :], in0=r_oh1[:, :, :], in1=bc32(r_w1[:, :]), op=MUL),
                lambda e: e.tensor_tensor(out=r_oh2[:, :, :], in0=r_oh2[:, :, :], in1=bc32(r_w2[:, :]), op=MUL),
                lambda e: e.tensor_tensor(out=cw[:, :, :], in0=r_oh1[:, :, :], in1=r_oh2[:, :, :], op=ADD)],
                reads=["router1", "r_w1"], writes=["cw", "router1"])
            if "cw" in dbg:
                tcw = dbgt("cw", [128, NT, 32])
                P.dma("sp", lambda e: e.dma_start(out=tcw[:, :, :], in_=cw[:, :, :]), "out", reads=["cw"])
            esD.close()
            P.barrier()

            NE = 32 if stage >= 5 else 0
            esE = ExitStack()
            esE.__enter__()
            sb = lambda name, shape, dt: esE.enter_context(nc.sbuf_tensor(name, shape, dt))
            ps = lambda name, shape, dt: esE.enter_context(nc.psum_tensor(name, shape, dt))
            yacc = sb("yacc", [128, NT, D], F32)
            Wg = [sb(f"Wg{i}", [128, 8, 512], BF16) for i in range(2)]
            Wu = [sb(f"Wu{i}", [128, 8, 512], BF16) for i in range(2)]
            Wd = [sb(f"Wd{i}", [128, 4, D], BF16) for i in range(2)]
            actT = sb("actT", [128, 4, NTOK], BF16)
            sgt = [sb(f"sgt{i}", [128, 512], F32) for i in range(2)]
            pg = [ps(f"pg{i}", [128, 512], F32) for i in range(2)]
            pu2 = [ps(f"pu2_{i}", [128, 512], F32) for i in range(2)]
            pd = [ps(f"pd{i}", [128, 512], F32) for i in range(2)]
            P.op("pool", lambda e: e.memset(yacc[:, :, :], 0.0), writes=[f"yacc{t}_{hf}" for t in range(NT) for hf in range(2)])
            h2T_all = [f"h2T_{i}" for i in range(4)]
            for ex in range(NE):
                s_ = ex % 2
                P.dma("pool", lambda e, ex=ex, s_=s_: e.dma_start(out=Wg[s_][:, :, :], in_=w_gate[ex].rearrange("(kc p) c -> p kc c", p=128)), f"Wg{s_}", writes=[f"Wg{s_}"])
                P.dma("pool", lambda e, ex=ex, s_=s_: e.dma_start(out=Wu[s_][:, :, :], in_=w_up[ex].rearrange("(kc p) c -> p kc c", p=128)), f"Wu{s_}", writes=[f"Wu{s_}"])
                P.dma("pool", lambda e, ex=ex, s_=s_: e.dma_start(out=Wd[s_][:, :, :], in_=w_down[ex].rearrange("(kc p) c -> p kc c", p=128)), f"Wd{s_}", writes=[f"Wd{s_}"])
                it = 0
                for tb in range(4):
                    tbs = slice(tb * 512, (tb + 1) * 512)
                    for m in range(4):
                        b_ = it % 2
                        it += 1
                        def f_gu(e, s_=s_, m=m, tbs=tbs, b_=b_):
                            for k in range(8):
                                e.matmul(out=pg[b_][:, :], lhsT=Wg[s_][:, k, m * 128:(m + 1) * 128], rhs=h2T[:, k, tbs], start=(k == 0), stop=(k == 7))
                            for k in range(8):
                                i = e.matmul(out=pu2[b_][:, :], lhsT=Wu[s_][:, k, m * 128:(m + 1) * 128], rhs=h2T[:, k, tbs], start=(k == 0), stop=(k == 7))
                            return i
                        P.op("pe", f_gu, reads=[f"Wg{s_}", f"Wu{s_}", f"h2T_{tb}"], writes=[f"pg{b_}", f"pu2_{b_}"])
                        P.op("act", lambda e, b_=b_: e.activation(out=sgt[b_][:, :], in_=pg[b_][:, :], func=AF.Silu), reads=[f"pg{b_}"], writes=[f"sgt{b_}"])
                        P.op("dve", lambda e, b_=b_, m=m, tbs=tbs: e.tensor_tensor(out=actT[:, m, tbs], in0=sgt[b_][:, :], in1=pu2[b_][:, :], op=MUL),
                             reads=[f"sgt{b_}", f"pu2_{b_}"], writes=[f"actT_{tb}"])
                it = 0
                for t in range(NT):
                    ts_ = slice(t * 128, (t + 1) * 128)
                    for hf in range(2):
                        b_ = it % 2
                        it += 1
                        def f_d(e, s_=s_, ts_=ts_, hf=hf, b_=b_):
                            for m in range(4):
                                i = e.matmul(out=pd[b_][:, :], lhsT=actT[:, m, ts_], rhs=Wd[s_][:, m, hf * 512:(hf + 1) * 512], start=(m == 0), stop=(m == 3))
                            return i
                        P.op("pe", f_d, reads=[f"Wd{s_}", f"actT_{t // 4}"], writes=[f"pd{b_}"])
                        P.op("dve", lambda e, t=t, hf=hf, b_=b_, ex=ex: e.scalar_tensor_tensor(out=yacc[:, t, hf * 512:(hf + 1) * 512], in0=pd[b_][:, :], scalar=cw[:, t, ex:ex + 1],
                                                                                                in1=yacc[:, t, hf * 512:(hf + 1) * 512], op0=MUL, op1=ADD),
                             reads=[f"pd{b_}", "cw", f"yacc{t}_{hf}"], writes=[f"yacc{t}_{hf}"])

            P.dma("sp", lambda e: e.dma_start(out=g_bc[:, :], in_=g_fin[0:1, :].partition_broadcast(128)), "setupF", writes=["g_bc"])
            for t in range(NT):
                s_ = t % 2
                P.dma("sp", lambda e, t=t, s_=s_: e.dma_start(out=xt2[s_][:, :], in_=xmid_scr[t * 128:(t + 1) * 128, :]), f"xt2_{s_}", reads=[f"xmid{t}"], writes=[f"xt2_{s_}"])
                P.op("dve", lambda e, t=t, s_=s_: e.tensor_tensor(out=xt2[s_][:, :], in0=xt2[s_][:, :], in1=yacc[:, t, :], op=ADD),
                     reads=[f"xt2_{s_}", f"yacc{t}_0", f"yacc{t}_1"], writes=[f"xt2_{s_}"])
                P.seq("act", [lambda e, s_=s_, t=t: e.activation(out=junk2[:, :], in_=xt2[s_][:, :], func=AF.Square, accum_out=ssq2[:, NT + t:NT + t + 1]),
                              lambda e, t=t: e.activation(out=ssq2[:, NT + t:NT + t + 1], in_=ssq2[:, NT + t:NT + t + 1], func=AF.Sqrt, scale=1.0 / D, bias=EPS)],
                      reads=[f"xt2_{s_}"], writes=["junk2", f"ssq2_{NT + t}"])
                P.op("dve", lambda e, t=t: e.reciprocal(out=rstd2[:, NT + t:NT + t + 1], in_=ssq2[:, NT + t:NT + t + 1]), reads=[f"ssq2_{NT + t}"], writes=[f"rstd2_{NT + t}"])
                P.op("dve", lambda e, s_=s_, t=t: e.scalar_tensor_tensor(out=xt2[s_][:, :], in0=xt2[s_][:, :], scalar=rstd2[:, NT + t:NT + t + 1], in1=g_bc[:, :], op0=MUL, op1=MUL),
                     reads=[f"xt2_{s_}", f"rstd2_{NT + t}", "g_bc"], writes=[f"xt2_{s_}"])
                P.dma("sp", lambda e, t=t, s_=s_: e.dma_start(out=y[t * 128:(t + 1) * 128, :], in_=xt2[s_][:, :]), f"yout{s_}", reads=[f"xt2_{s_}"], writes=[f"y{t}"])
            esE.close()
            esDE.close()

        if "merged" in dbg:
            t = dbgt("merged", [1024, NTOK], BF16)
            nrow = 1024 if stage >= 3 else 512
            P.dma("sp", lambda e: e.dma_start(out=t[0:nrow, :], in_=mergedT_scr[0:nrow, :]), "out", reads=[f"mscr{h}" for h in range(nrow // 128)])
        if "hT" in dbg:
            t2 = dbgt("hT", [128, 8, HALO + NTOK], BF16)
            P.dma("sp", lambda e: e.dma_start(out=t2[:, :, :], in_=hT[:, :, :]), "out", reads=hT_all)
        if "qk" in dbg:
            t3 = dbgt("qs", [128, NTOK], BF16)
            t4 = dbgt("ks", [128, NTOK], BF16)
            t5 = dbgt("expb", [128, NTOK], F32)
            t6 = dbgt("e2", [128, NTOK], F32)
            P.dma("sp", lambda e: e.dma_start(out=t3[:, :], in_=qsT[:, :]), "out", reads=["qsT"])
            P.dma("sp", lambda e: e.dma_start(out=t4[:, :], in_=ksT[:, :]), "out", reads=["ksT"])
            P.dma("sp", lambda e: e.dma_start(out=t5[:, :], in_=expb[:, :]), "out", reads=expb_all)
            P.dma("sp", lambda e: e.dma_start(out=t6[:, :], in_=e2bc[:, :]), "out", reads=e2_all)
        if "xmid" in dbg:
            txm = dbgt("xmid", [NTOK, D])
            P.dma("sp", lambda e: e.dma_start(out=txm[:, :], in_=xmid_scr[:, :]), "out", reads=[f"xmid{t}" for t in range(NT)])
        P.final_wait("sp", ["out", "yout0", "yout1"])

        with nc.Block() as block:
            @block.tensor
            def _(e): P.replay("pe", e)
            @block.scalar
            def _(e): P.replay("act", e)
            @block.vector
            def _(e): P.replay("dve", e)
            @block.gpsimd
            def _(e): P.replay("pool", e)
            @block.sync
            def _(e): P.replay("sp", e)
    return nc, dbg_out


def make_in_maps(inputs):
    f = lambda a: np.ascontiguousarray(a, dtype=np.float32)
    x = inputs["x"]
    common = {
        "g_mix": f(inputs["norm_mix_g"].reshape(1, D)),
        "w_in": f(inputs["w_in"].reshape(D, 2568)),
        "conv_wT": f(inputs["conv_w"].reshape(4, 8, 128).transpose(2, 1, 0)),
        "conv_b": f(inputs["conv_b"].reshape(8, 128).T),
        "i_bias": f(inputs["i_bias"].reshape(1, 4)),
        "f_bias": f(inputs["f_bias"].reshape(1, 4)),
        "g_mlc": f(inputs["mlstm_norm_g"].reshape(4, 128).T),
    }
    lre = inputs["s5_lambda_re"].reshape(32, 64).T
    lim = inputs["s5_lambda_im"].reshape(32, 64).T
    bre = inputs["s5_b_re"].reshape(32, 64, 16).transpose(1, 0, 2)
    bim = inputs["s5_b_im"].reshape(32, 64, 16).transpose(1, 0, 2)
    cre = inputs["s5_c_re"].reshape(32, 16, 64).transpose(2, 0, 1)
    cim = inputs["s5_c_im"].reshape(32, 16, 64).transpose(2, 0, 1)
    glw = inputs["s5_glu_w"].reshape(4, 8, 16, 32)
    WA = np.zeros((4, 128, 128), np.float32)
    WB = np.zeros((4, 128, 128), np.float32)
    for u in range(4):
        for g in range(8):
            WA[u, g * 16:(g + 1) * 16, g * 16:(g + 1) * 16] = glw[u, g, :, 0:16]
            WB[u, g * 16:(g + 1) * 16, g * 16:(g + 1) * 16] = glw[u, g, :, 16:32]
    glb = inputs["s5_glu_b"].reshape(4, 8, 32)
    common.update({
        "lam2_re": f(np.concatenate([lre, lre], 0)), "lam2_im": f(np.concatenate([lim, lim], 0)),
        "logdt": f(inputs["s5_log_dt"].reshape(1, 32)),
        "X1d": f(np.concatenate([bre, bim], 0)), "X2d": f(np.concatenate([bim, bre], 0)),
        "CY1d": f(np.concatenate([cre, cim], 0)), "CY2d": f(np.concatenate([cim, cre], 0)),
        "s5d": f(inputs["s5_d"].reshape(1, 512)),
        "WAd": WA, "WBd": WB,
        "w_out": f(inputs["w_out"].reshape(D, D)), "g_ffn": f(inputs["norm_ffn_g"].reshape(1, D)),
        "w_rt": f(np.concatenate([inputs["router_group_w"].reshape(D, 4), inputs["router_expert_w"].reshape(D, 32)], 1)),
        "b_rt": f(np.concatenate([inputs["router_group_b"].reshape(1, 4), inputs["router_expert_b"].reshape(1, 32)], 1)),
        "w_gate": f(inputs["expert_w_gate"].reshape(32, D, 512)), "w_up": f(inputs["expert_w_up"].reshape(32, D, 512)),
        "w_down": f(inputs["expert_w_down"].reshape(32, 512, D)), "g_fin": f(inputs["norm_final_g"].reshape(1, D)),
        "gbad": f(glb[:, :, 0:16].reshape(4, 128).T), "gbbd": f(glb[:, :, 16:32].reshape(4, 128).T),
    })
    maps = []
    for c in range(NCORES):
        b, p = c // 4, c % 4
        m = dict(common)
        m["x"] = f(x[b, p * NTOK:(p + 1) * NTOK])
        if p == 0:
            m["xh"] = np.zeros((HALO, D), np.float32)
        else:
            m["xh"] = f(x[b, p * NTOK - HALO:p * NTOK])
        pmk = np.zeros((128, 4), np.float32)
        pmk[:, :p] = 1.0
        m["pmask"] = pmk
        maps.append(m)
    return maps


_CACHE = {}


def kernel(**inputs):
    if "nc" not in _CACHE:
        _CACHE["nc"] = build()[0]
    nc = _CACHE["nc"]
    in_maps = make_in_maps(inputs)
    res = run_bass_kernel_spmd(nc, in_maps, core_ids=list(range(NCORES)))
    out = np.empty((2, 4 * NTOK, D), np.float32)
    for c in range(NCORES):
        b, p = c // 4, c % 4
        out[b, p * NTOK:(p + 1) * NTOK] = np.asarray(res.results[c]["y"], dtype=np.float32)
    return out
```

```python
import numpy as np
from contextlib import ExitStack
import concourse.bass as bass
import concourse.mybir as mybir
from concourse.bass_utils import run_bass_kernel_spmd

F32 = mybir.dt.float32
BF16 = mybir.dt.bfloat16
AF = mybir.ActivationFunctionType
ALU = mybir.AluOpType

NTOK = 2048
NT = 16
NCH = 16
CL = 128
HALO = 32
D = 1024
EPS = 1e-6
NCORES = 8
GROUPS4 = [[0, 1, 2, 3], [4, 5, 6, 7]]
L5 = 32
LL = 5
NC5 = NTOK // L5
KC5 = L5 // 8
NLEV = 7


class Prog:
    def __init__(self, nc, es):
        self.nc = nc
        self.es = es
        self.queues = {e: [] for e in ("pe", "act", "dve", "pool", "sp")}
        self.esem = {}
        self.ecnt = {}
        for e in ("pe", "act", "dve", "pool"):
            self.esem[e] = es.enter_context(nc.semaphore("sem_" + e))
            self.ecnt[e] = 0
        self.dsem = {}
        self.dcnt = {}
        self.lastw = {}
        self.readers = {}
        self.known = {e: {} for e in self.queues}

    def _deps(self, reads, writes):
        deps = []
        for r in reads:
            if r in self.lastw:
                deps.append(self.lastw[r])
        for w in writes:
            if w in self.lastw:
                deps.append(self.lastw[w])
            deps.extend(self.readers.get(w, ()))
        return deps

    def _prune(self, eng, deps):
        need = {}
        for (s, v) in deps:
            if eng == "pe" and s is self.esem["pe"]:
                continue
            if v > need.get(s, 0):
                need[s] = v
        out = []
        kn = self.known[eng]
        for s, v in need.items():
            if kn.get(s, 0) >= v:
                continue
            kn[s] = v
            out.append((s, v))
        return out

    def _record(self, tok, reads, writes):
        for r in reads:
            self.readers.setdefault(r, []).append(tok)
        for w in writes:
            self.lastw[w] = tok
            self.readers[w] = []

    def op(self, eng, fn, reads=(), writes=()):
        reads = tuple(reads)
        writes = tuple(writes)
        waits = self._prune(eng, self._deps(reads, writes))
        self.ecnt[eng] += 1
        tok = (self.esem[eng], self.ecnt[eng])
        self.queues[eng].append((waits, fn, (self.esem[eng], 1)))
        self._record(tok, reads, writes)

    def seq(self, eng, fns, reads=(), writes=()):
        self._chain = getattr(self, "_chain", 0) + 1
        ck = f"__chain{self._chain}"
        for fn in fns:
            self.op(eng, fn, reads=tuple(reads) + (ck,), writes=tuple(writes) + (ck,))

    def dma(self, q, fn, stream, reads=(), writes=(), inc=16):
        reads = tuple(reads)
        writes = tuple(writes)
        if stream not in self.dsem:
            self.dsem[stream] = self.es.enter_context(self.nc.semaphore("dsem_" + stream))
            self.dcnt[stream] = 0
        waits = self._prune(q, self._deps(reads, writes))
        self.dcnt[stream] += inc
        tok = (self.dsem[stream], self.dcnt[stream])
        self.queues[q].append((waits, fn, (self.dsem[stream], inc)))
        self._record(tok, reads, writes)

    def bulk_done(self, stream):
        s = self.dsem[stream]
        fin = self.dcnt[stream]
        for k, (ss, v) in list(self.lastw.items()):
            if ss is s:
                self.lastw[k] = (s, fin)

    def barrier(self):
        allw = [(self.esem[e], self.ecnt[e]) for e in self.esem if self.ecnt[e] > 0]
        allw += [(self.dsem[s], self.dcnt[s]) for s in self.dsem]
        for e in self.queues:
            w = self._prune(e, allw)
            if w:
                self.queues[e].append((w, None, None))

    def final_wait(self, q, streams):
        waits = [(self.dsem[st], self.dcnt[st]) for st in streams if st in self.dsem]
        self.queues[q].append((waits, None, None))

    def replay(self, eng, h):
        for waits, fn, inc in self.queues[eng]:
            for (s, v) in waits:
                h.wait_ge(s, v)
            if fn is None:
                continue
            inst = fn(h)
            inst.then_inc(inc[0], inc[1])


def build(stage=99, dbg=()):
    nc = bass.Bass("TRN2", target_bir_lowering=False)
    din = lambda name, shape, dt=F32: nc.dram_tensor(name, shape, dt, kind="ExternalInput")
    x = din("x", [NTOK, D])
    xh = din("xh", [HALO, D])
    g_mix = din("g_mix", [1, D])
    w_in = din("w_in", [D, 2568])
    conv_wT = din("conv_wT", [128, 8, 4])
    conv_b = din("conv_b", [128, 8])
    i_bias = din("i_bias", [1, 4])
    f_bias = din("f_bias", [1, 4])
    g_mlc = din("g_mlc", [128, 4])
    pmask = din("pmask", [128, 4])
    lam2_re = din("lam2_re", [128, 32])
    lam2_im = din("lam2_im", [128, 32])
    logdt = din("logdt", [1, 32])
    X1d = din("X1d", [128, 32, 16])
    X2d = din("X2d", [128, 32, 16])
    CY1d = din("CY1d", [128, 32, 16])
    CY2d = din("CY2d", [128, 32, 16])
    s5d = din("s5d", [1, 512])
    WAd = din("WAd", [4, 128, 128])
    WBd = din("WBd", [4, 128, 128])
    gbad = din("gbad", [128, 4])
    gbbd = din("gbbd", [128, 4])
    w_out = din("w_out", [D, D])
    g_ffn = din("g_ffn", [1, D])
    w_rt = din("w_rt", [D, 36])
    b_rt = din("b_rt", [1, 36])
    if stage >= 5:
        w_gate = din("w_gate", [32, D, 512])
        w_up = din("w_up", [32, D, 512])
        w_down = din("w_down", [32, 512, D])
    g_fin = din("g_fin", [1, D])
    y = nc.dram_tensor("y", [NTOK, D], F32, kind="ExternalOutput")
    dbg_out = {}
    def dbgt(name, shape, dt=F32):
        dbg_out[name] = nc.dram_tensor("dbg_" + name, shape, dt, kind="ExternalOutput")
        return dbg_out[name]

    mergedT_scr = nc.dram_tensor("mergedT_scr", [1024, NTOK], BF16)
    cc_in = [nc.dram_tensor(f"cc_in{h}", [128, 130], F32) for h in range(4)]
    cc_out = [nc.dram_tensor(f"cc_out{h}", [512, 130], F32) for h in range(4)]
    u_scr = nc.dram_tensor("u_scr", [NTOK, 512], BF16)
    xmid_scr = nc.dram_tensor("xmid_scr", [NTOK, D], F32)
    prep_scr = {n: nc.dram_tensor("prep_" + n, [128, 32, k], F32) for n, k in (("PWrr", L5), ("PWri", L5), ("PWbr", 8), ("PWbi", 8), ("BB1", 16), ("BB2", 16),
                                                                             ("PRs", L5 + 1), ("nPI", L5 + 1), ("sqr", 12), ("sqi", 12), ("PLr", NC5), ("PLi", NC5))}
    ccs_in = [nc.dram_tensor(f"ccs_in{h}", [128, 8], F32) for h in range(4)]
    ccs_out = [nc.dram_tensor(f"ccs_out{h}", [512, 8], F32) for h in range(4)]

    w_in_v = w_in.ap().rearrange("(kc p) c -> p kc c", p=128)

    es = ExitStack()
    with es:
        P = Prog(nc, es)
        sb = lambda name, shape, dt: es.enter_context(nc.sbuf_tensor(name, shape, dt))
        ps = lambda name, shape, dt: es.enter_context(nc.psum_tensor(name, shape, dt))

        g_bc = sb("g_bc", [128, D], F32)
        ident = sb("ident", [128, 128], BF16)
        identf = sb("identf", [128, 128], F32)
        mslab = sb("mslab", [128, NTOK], BF16)
        pm = sb("pm", [128, 4], F32)
        esH = ExitStack()
        esH.__enter__()
        hT = esH.enter_context(nc.sbuf_tensor("hT", [128, 8, HALO + NTOK], BF16))
        esB = ExitStack()
        esB.__enter__()
        sb = lambda name, shape, dt: esB.enter_context(nc.sbuf_tensor(name, shape, dt))
        ps = lambda name, shape, dt: esB.enter_context(nc.psum_tensor(name, shape, dt))
        xt = [sb(f"xt{i}", [128, D], F32) for i in range(2)]
        junk = sb("junk", [128, D], BF16)
        hb = sb("hb", [128, D], BF16)
        ssq = sb("ssq", [128, NT + 1], F32)
        rstd = sb("rstd", [128, NT + 1], F32)
        wh = sb("wh", [128, 8, 512], BF16)
        wg = sb("wg", [128, 8, 33], BF16)
        raw = sb("raw", [128, NTOK + 3], F32)
        ctmp = sb("ctmp", [128, NTOK], F32)
        qsT2 = [sb(f"qsT{i}", [128, NTOK], BF16) for i in range(2)]
        ksT2 = [sb(f"ksT{i}", [128, NTOK], BF16) for i in range(2)]
        cwt = sb("cwt", [128, 8, 4], F32)
        cbt = sb("cbt", [128, 8], F32)
        fbt = sb("fbt", [33, 4], F32)
        nfb = sb("nfb", [33, 4], F32)
        Gt = sb("Gt", [33, NTOK], F32)
        Gt2 = sb("Gt2", [33, NTOK], F32)
        cmask = sb("cmask", [33, NTOK], F32)
        sel0 = sb("sel0", [33, 128], F32)
        sel032 = sb("sel032", [33, 128], F32)
        expb = sb("expb", [128, NTOK], F32)
        e2bc = sb("e2bc", [128, NTOK], F32)
        v_ext2 = [sb(f"v_ext{i}", [CL, NCH, 130], BF16) for i in range(2)]
        vT = sb("vT", [128, NTOK], BF16)
        sigoT2 = [sb(f"sigoT{i}", [128, NTOK], BF16) for i in range(2)]
        kstok = sb("kstok", [CL, NCH, 128], BF16)
        Us = sb("Us", [128, 4, 130], F32)
        gAc = sb("gAc", [128, 4], F32)
        CTloc2 = [sb(f"CTloc{i}", [128, NCH + 1, 130], F32) for i in range(2)]
        CTball = sb("CTball", [128, NCH, 130], BF16)
        CTt = sb("CTt", [128, 130], F32)
        cg = sb("cg", [128, 4, 130], F32)
        hacc = sb("hacc", [128, 130], F32)
        hacc2 = sb("hacc2", [128, 130], F32)
        maskST = sb("maskST", [CL, CL], F32)
        STb = sb("STb", [CL, 4, CL], BF16)
        dnb = sb("dnb", [CL, 4, 4], F32)
        hnb = sb("hnb", [CL, 4, 128], F32)
        oab = sb("oab", [CL, 4, 128], BF16)
        junkb = sb("junkb", [CL, 4, 128], BF16)

        Bk = [ps(f"bank{k}", [128, 512], F32) for k in range(8)]
        bfv = lambda k: Bk[k][:, :].bitcast(BF16)
        ps_big = [Bk[0], Bk[1]]
        tp = bfv(2).rearrange("p (a b) -> p a b", b=128)
        pvb = [Bk[3][0:64, 0:256], Bk[4][0:64, 0:256]]
        pab = [Bk[0][0:CL, 0:CL], Bk[1][0:CL, 0:CL]]
        pcb = [Bk[3][0:CL, 0:130], Bk[4][0:CL, 0:130]]
        pub = [Bk[5][:, 0:130], Bk[6][:, 0:130]]
        ptk = [bfv(2)[0:CL, 0:128], bfv(7)[0:CL, 0:128]]
        pto = [bfv(6)[:, 0:CL], bfv(7)[:, 0:CL]]
        KPV = ["bank3", "bank4"]; KPA = ["bank0", "bank1"]; KPC = ["bank3", "bank4"]; KPU = ["bank5", "bank6"]
        KPTK = ["bank2", "bank7"]; KPTO = ["bank6", "bank7"]

        P.dma("sp", lambda e: e.dma_start(out=g_bc[:, :], in_=g_mix[0:1, :].partition_broadcast(128)), "setup", writes=["g_bc"])
        P.dma("sp", lambda e: e.dma_start(out=cwt[:, :, :], in_=conv_wT[:, :, :]), "setup", writes=["cwt"])
        P.dma("sp", lambda e: e.dma_start(out=cbt[:, :], in_=conv_b[:, :]), "setup", writes=["cbt"])
        P.dma("sp", lambda e: e.dma_start(out=fbt[0:1, :], in_=f_bias[0:1, :]), "setup", writes=["fbt0"])
        P.dma("sp", lambda e: e.dma_start(out=fbt[32:33, :], in_=i_bias[0:1, :]), "setup", writes=["fbt32"])
        P.dma("sp", lambda e: e.dma_start(out=gAc[:, :], in_=g_mlc[:, :]), "setup", writes=["gAc"])
        P.dma("sp", lambda e: e.dma_start(out=pm[:, :], in_=pmask[:, :]), "setup", writes=["pm"])
        P.bulk_done("setup")

        P.seq("pool", [lambda e: e.memset(identf[:, :], 0.0),
                       lambda e: e.affine_select(out=identf[:, :], in_=identf[:, :], pattern=[[-1, 128]], compare_op=ALU.not_equal,
                                                 fill=1.0, base=0, channel_multiplier=1)], writes=["identf"])
        P.op("dve", lambda e: e.tensor_copy(out=ident[:, :], in_=identf[:, :]), reads=["identf"], writes=["ident"])

        P.seq("pool", [lambda e: e.memset(cmask[:, :], 1.0),
                       lambda e: e.memset(cmask[:, 0:NTOK:CL], 0.0),
                       lambda e: e.memset(sel0[:, :], 0.0),
                       lambda e: e.memset(sel0[0:1, :], 1.0),
                       lambda e: e.memset(sel032[:, :], 0.0),
                       lambda e: e.memset(sel032[0:1, :], 1.0),
                       lambda e: e.memset(sel032[32:33, :], 1.0),
                       lambda e: e.memset(Gt2[:, :], 0.0),
                       lambda e: e.memset(v_ext2[0][:, :, 128:129], 1.0),
                       lambda e: e.memset(v_ext2[0][:, :, 129:130], 0.0),
                       lambda e: e.memset(v_ext2[1][:, :, 128:129], 1.0),
                       lambda e: e.memset(v_ext2[1][:, :, 129:130], 0.0),
                       lambda e: e.memset(maskST[:, :], 1.0),
                       lambda e: e.affine_select(out=maskST[:, :], in_=maskST[:, :], pattern=[[1, CL]], compare_op=ALU.is_ge,
                                                 fill=0.0, base=0, channel_multiplier=-1)],
              writes=["cmask", "sel0", "sel032", "Gt2", "v_ext_c0", "v_ext_c1", "maskST"])
        P.op("pool", lambda e: e.tensor_scalar(out=nfb[0:1, :], in0=fbt[0:1, :], scalar1=-1.0, scalar2=None, op0=ALU.mult),
             reads=["fbt0"], writes=["nfb"])

        def norm_tile(src_ap, rows, slot, col0, idx, tag):
            s = slot
            P.dma("sp", lambda e: e.dma_start(out=xt[s][0:rows, :], in_=src_ap), f"xt{s}", writes=[f"xt{s}"])
            P.op("act", lambda e: e.activation(out=junk[0:rows, :], in_=xt[s][0:rows, :], func=AF.Square, accum_out=ssq[0:rows, idx:idx + 1]),
                 reads=[f"xt{s}"], writes=["junk", f"ssq{idx}"])
            P.op("act", lambda e: e.activation(out=ssq[0:rows, idx:idx + 1], in_=ssq[0:rows, idx:idx + 1], func=AF.Sqrt, scale=1.0 / D, bias=EPS),
                 reads=[f"ssq{idx}"], writes=[f"ssq{idx}"])
            P.op("dve", lambda e: e.reciprocal(out=rstd[0:rows, idx:idx + 1], in_=ssq[0:rows, idx:idx + 1]), reads=[f"ssq{idx}"], writes=[f"rstd{idx}"])
            P.op("dve", lambda e: e.scalar_tensor_tensor(out=hb[0:rows, :], in0=xt[s][0:rows, :], scalar=rstd[0:rows, idx:idx + 1], in1=g_bc[0:rows, :],
                                                         op0=ALU.mult, op1=ALU.mult),
                 reads=[f"xt{s}", f"rstd{idx}", "g_bc"], writes=["hb"])
            def f_tp(e):
                for k in range(8):
                    i = e.transpose(out=tp[:, k, 0:rows], in_=hb[0:rows, k * 128:(k + 1) * 128], identity=ident[0:rows, 0:rows])
                return i
            P.op("pe", f_tp, reads=["hb", "ident"], writes=["bank2"])
            P.op("act", lambda e: e.copy(out=hT[:, :, col0:col0 + rows], in_=tp[:, :, 0:rows]), reads=["bank2"], writes=[tag])

        norm_tile(xh[:, :], HALO, 0, 0, NT, "hT_h")
        for t in range(NT):
            norm_tile(x[t * 128:(t + 1) * 128, :], 128, (t + 1) % 2, HALO + t * 128, t, f"hT_{t // 4}")
        hT_all = ["hT_h"] + [f"hT_{i}" for i in range(4)]

        SC = float(128 ** -0.5)
        def head_s1(hd):
            par = hd % 2
            qsT, ksT, v_ext, sigoT, CTloc = qsT2[par], ksT2[par], v_ext2[par], sigoT2[par], CTloc2[par]
            KQ = f"qsT{par}"; KK = f"ksT{par}"; KS = f"sigoT{par}"; KVC = f"v_ext_c{par}"
            for j, c0 in enumerate((hd * 128, 512 + hd * 128, 1024 + hd * 128, 1536 + hd * 128)):
                P.dma("pool", lambda e, j=j, c0=c0: e.dma_start(out=wh[:, :, j * 128:(j + 1) * 128], in_=w_in_v[:, :, c0:c0 + 128]),
                      "wh", writes=[f"wh{j}"])
            P.bulk_done("wh")
            P.op("pool", lambda e: e.memset(wg[:, :, :], 0.0), writes=["wg", "wg_a", "wg_b"])
            def ld_wg(e, dst, col):
                with nc.allow_non_contiguous_dma(reason="gate columns"):
                    return e.dma_start(out=wg[:, :, dst:dst + 1], in_=w_in_v[:, :, col:col + 1])
            P.dma("pool", lambda e, hd=hd: ld_wg(e, 0, 2052 + hd), "wg", reads=["wg"], writes=["wg_a"])
            P.dma("pool", lambda e, hd=hd: ld_wg(e, 32, 2048 + hd), "wg", reads=["wg"], writes=["wg_b"])
            P.bulk_done("wg")

            for tb in range(4):
                pgt = ps_big[tb % 2]
                def f_g(e, tb=tb, pgt=pgt):
                    for k in range(8):
                        i = e.matmul(out=pgt[0:33, :], lhsT=wg[:, k, :], rhs=hT[:, k, HALO + tb * 512:HALO + (tb + 1) * 512], start=(k == 0), stop=(k == 7))
                    return i
                P.op("pe", f_g, reads=["wg", "wg_a", "wg_b", f"hT_{tb}"], writes=[f"bank{tb % 2}"])
                P.op("act", lambda e, tb=tb, pgt=pgt: e.copy(out=Gt[0:33, tb * 512:(tb + 1) * 512], in_=pgt[0:33, :]),
                     reads=[f"bank{tb % 2}"], writes=[f"Gt_{tb}"])
            Gt_all = [f"Gt_{tb}" for tb in range(4)]
            P.op("act", lambda e, hd=hd: e.activation(out=Gt[0:1, :], in_=Gt[0:1, :], func=AF.Exp, scale=-1.0, bias=nfb[0:1, hd:hd + 1]),
                 reads=Gt_all + ["nfb"], writes=["Gt_f"])
            P.op("act", lambda e: e.activation(out=Gt[0:1, :], in_=Gt[0:1, :], func=AF.Ln, bias=1.0), reads=["Gt_f"], writes=["Gt_f"])
            P.op("dve", lambda e: e.tensor_tensor_scan(out=Gt2[0:1, :], data0=cmask[0:1, :], data1=Gt[0:1, :], initial=0.0, op0=ALU.mult, op1=ALU.add),
                 reads=["Gt_f", "cmask"], writes=["Gt2_0"])
            P.op("act", lambda e, hd=hd: e.activation(out=Gt2[32:33, :], in_=Gt[32:33, :], func=AF.Identity, bias=fbt[32:33, hd:hd + 1]),
                 reads=Gt_all + ["fbt32", "Gt2"], writes=["Gt2_32"])
            for tb in range(4):
                pgt = ps_big[tb % 2]
                P.op("pe", lambda e, tb=tb, pgt=pgt: e.matmul(out=pgt[:, :], lhsT=sel0[0:33, :], rhs=Gt2[0:33, tb * 512:(tb + 1) * 512], start=True, stop=True),
                     reads=["sel0", "Gt2", "Gt2_0", "Gt2_32"], writes=[f"bank{tb % 2}"])
                P.op("act", lambda e, tb=tb, pgt=pgt: e.activation(out=expb[:, tb * 512:(tb + 1) * 512], in_=pgt[:, :], func=AF.Exp, scale=-1.0),
                     reads=[f"bank{tb % 2}"], writes=[f"expb_{tb}"])
            for tb in range(4):
                pgt = ps_big[tb % 2]
                P.op("pe", lambda e, tb=tb, pgt=pgt: e.matmul(out=pgt[:, :], lhsT=sel032[0:33, :], rhs=Gt2[0:33, tb * 512:(tb + 1) * 512], start=True, stop=True),
                     reads=["sel032", "Gt2", "Gt2_0", "Gt2_32"], writes=[f"bank{tb % 2}"])
                P.op("act", lambda e, tb=tb, pgt=pgt: e.activation(out=e2bc[:, tb * 512:(tb + 1) * 512], in_=pgt[:, :], func=AF.Exp),
                     reads=[f"bank{tb % 2}"], writes=[f"e2bc_{tb}"])
            expb_all = [f"expb_{tb}" for tb in range(4)]
            e2_all = [f"e2bc_{tb}" for tb in range(4)]

            for qi in range(2):
                cidx = qi * 4 + hd
                def f_halo(e, qi=qi):
                    for k in range(8):
                        i = e.matmul(out=ps_big[0][:, 0:HALO], lhsT=wh[:, k, qi * 128:(qi + 1) * 128], rhs=hT[:, k, 0:HALO], start=(k == 0), stop=(k == 7))
                    return i
                P.op("pe", f_halo, reads=[f"wh{qi}", "hT_h"], writes=["bank0"])
                P.op("act", lambda e: e.copy(out=raw[:, 0:3], in_=ps_big[0][:, HALO - 3:HALO]), reads=["bank0"], writes=["raw_h"])
                for tb in range(4):
                    pgt = ps_big[(tb + 1) % 2]
                    def f_q(e, tb=tb, qi=qi, pgt=pgt):
                        for k in range(8):
                            i = e.matmul(out=pgt[:, :], lhsT=wh[:, k, qi * 128:(qi + 1) * 128], rhs=hT[:, k, HALO + tb * 512:HALO + (tb + 1) * 512],
                                         start=(k == 0), stop=(k == 7))
                        return i
                    P.op("pe", f_q, reads=[f"wh{qi}", f"hT_{tb}"], writes=[f"bank{(tb + 1) % 2}"])
                    P.op("act", lambda e, tb=tb, pgt=pgt: e.copy(out=raw[:, 3 + tb * 512:3 + (tb + 1) * 512], in_=pgt[:, :]),
                         reads=[f"bank{(tb + 1) % 2}"], writes=[f"raw_{tb}"])
                raw_all = ["raw_h"] + [f"raw_{tb}" for tb in range(4)]
                fl = [lambda e, cidx=cidx: e.tensor_scalar(out=ctmp[:, :], in0=raw[:, 0:NTOK], scalar1=cwt[:, cidx, 0:1], scalar2=cbt[:, cidx:cidx + 1],
                                                           op0=ALU.mult, op1=ALU.add)]
                for j in (1, 2, 3):
                    fl.append(lambda e, cidx=cidx, j=j: e.scalar_tensor_tensor(out=ctmp[:, :], in0=raw[:, j:j + NTOK], scalar=cwt[:, cidx, j:j + 1],
                                                                               in1=ctmp[:, :], op0=ALU.mult, op1=ALU.add))
                P.seq("dve", fl, reads=raw_all + ["cwt", "cbt"], writes=["ctmp"])
                P.op("act", lambda e: e.activation(out=ctmp[:, :], in_=ctmp[:, :], func=AF.Silu), reads=["ctmp"], writes=["ctmp"])
                if qi == 0:
                    P.op("dve", lambda e: e.tensor_tensor(out=qsT[:, :], in0=ctmp[:, :], in1=expb[:, :], op=ALU.mult),
                         reads=["ctmp"] + expb_all, writes=[KQ])
                else:
                    P.op("dve", lambda e: e.scalar_tensor_tensor(out=ksT[:, :], in0=ctmp[:, :], scalar=SC, in1=e2bc[:, :], op0=ALU.mult, op1=ALU.mult),
                         reads=["ctmp"] + e2_all, writes=[KK])

            for vi in range(2):
                for tb in range(4):
                    pgt = ps_big[tb % 2]
                    def f_vo(e, tb=tb, vi=vi, pgt=pgt):
                        for k in range(8):
                            i = e.matmul(out=pgt[:, :], lhsT=wh[:, k, 256 + vi * 128:384 + vi * 128], rhs=hT[:, k, HALO + tb * 512:HALO + (tb + 1) * 512],
                                         start=(k == 0), stop=(k == 7))
                        return i
                    P.op("pe", f_vo, reads=[f"wh{2 + vi}", f"hT_{tb}"], writes=[f"bank{tb % 2}"])
                    if vi == 0:
                        P.op("act", lambda e, tb=tb, pgt=pgt: e.copy(out=vT[:, tb * 512:(tb + 1) * 512], in_=pgt[:, :]), reads=[f"bank{tb % 2}"], writes=[f"vT_{tb}"])
                    else:
                        P.op("act", lambda e, tb=tb, pgt=pgt: e.activation(out=ctmp[:, tb * 512:(tb + 1) * 512], in_=pgt[:, :], func=AF.Sigmoid),
                             reads=[f"bank{tb % 2}"], writes=["ctmp"])
            P.op("dve", lambda e, hd=hd: e.tensor_scalar(out=sigoT[:, :], in0=ctmp[:, :], scalar1=gAc[:, hd:hd + 1], scalar2=None, op0=ALU.mult),
                 reads=["ctmp", "gAc"], writes=[KS])

            for c in range(NCH):
                P.op("pe", lambda e, c=c: e.transpose(out=ptk[0], in_=vT[:, c * CL:(c + 1) * CL], identity=ident[:, :]),
                     reads=[f"vT_{c // 4}", "ident"], writes=[KPTK[0]])
                P.op("act", lambda e, c=c: e.copy(out=v_ext[:, c, 0:128], in_=ptk[0]), reads=[KPTK[0]], writes=[f"v{par}_{c}"])
                P.op("pe", lambda e, c=c: e.transpose(out=ptk[1], in_=ksT[:, c * CL:(c + 1) * CL], identity=ident[:, :]),
                     reads=[KK, "ident"], writes=[KPTK[1]])
                P.op("act", lambda e, c=c: e.copy(out=kstok[:, c, :], in_=ptk[1]), reads=[KPTK[1]], writes=[f"kstok_{c}"])

            P.seq("pool", [lambda e: e.memset(CTloc[:, 0, :], 0.0), lambda e: e.memset(CTloc[:, 0, 129:130], 1.0)], writes=[f"CTloc{par}_0"])
            for c in range(NCH):
                b = c % 2
                ub = c % 4
                wc = expb[:, c * CL + CL - 1:c * CL + CL]
                P.op("pe", lambda e, c=c, b=b: e.matmul(out=pub[b], lhsT=kstok[:, c, :], rhs=v_ext[:, c, :], start=True, stop=True),
                     reads=[f"kstok_{c}", f"v{par}_{c}", KVC], writes=[KPU[b]])
                P.op("act", lambda e, b=b, ub=ub, wc=wc: e.activation(out=Us[:, ub, :], in_=pub[b], func=AF.Copy, scale=wc),
                     reads=[KPU[b], f"expb_{c // 4}"], writes=[f"Us{ub}"])
                P.op("dve", lambda e, c=c, ub=ub, wc=wc: e.scalar_tensor_tensor(out=CTloc[:, c + 1, :], in0=CTloc[:, c, :], scalar=wc, in1=Us[:, ub, :],
                                                                              op0=ALU.mult, op1=ALU.add),
                     reads=[f"Us{ub}", f"CTloc{par}_{c}", f"expb_{c // 4}"], writes=[f"CTloc{par}_{c + 1}"])

            P.dma("sp", lambda e, hd=hd: e.dma_start(out=cc_in[hd][:, :], in_=CTloc[:, NCH, :]), f"ccin{hd}", reads=[f"CTloc{par}_{NCH}"], writes=[f"cc_in{hd}"])
            def f_cc(e, hd=hd):
                return e.collective_compute("AllGather", ALU.bypass, replica_groups=GROUPS4,
                                            ins=[cc_in[hd].ap().opt()], outs=[cc_out[hd].ap().opt()])
            P.dma("pool", f_cc, f"cc{hd}", reads=[f"cc_in{hd}"], writes=[f"cc_out{hd}"], inc=1)

        def head_s2(hd):
            par = hd % 2
            qsT, ksT, v_ext, sigoT, CTloc = qsT2[par], ksT2[par], v_ext2[par], sigoT2[par], CTloc2[par]
            KQ = f"qsT{par}"; KK = f"ksT{par}"; KS = f"sigoT{par}"; KVC = f"v_ext_c{par}"
            P.dma("sp", lambda e, hd=hd: e.dma_start(out=cg[:, :, :], in_=cc_out[hd].ap().rearrange("(r p) c -> p r c", p=128)),
                  f"ccld{hd}", reads=[f"cc_out{hd}"], writes=["cg"])
            P.op("pool", lambda e: e.memset(hacc[:, :], 0.0), writes=["hacc"])
            for pp in range(3):
                P.seq("dve", [
                    lambda e, pp=pp: e.scalar_tensor_tensor(out=hacc2[:, :], in0=hacc[:, :], scalar=cg[:, pp, 129:130], in1=cg[:, pp, :], op0=ALU.mult, op1=ALU.add),
                    lambda e: e.tensor_tensor(out=hacc2[:, :], in0=hacc2[:, :], in1=hacc[:, :], op=ALU.subtract),
                    lambda e, pp=pp: e.scalar_tensor_tensor(out=hacc[:, :], in0=hacc2[:, :], scalar=pm[:, pp:pp + 1], in1=hacc[:, :], op0=ALU.mult, op1=ALU.add)],
                    reads=["hacc", "cg", "pm"], writes=["hacc", "hacc2"])
            for c in range(NCH):
                eng = "dve"
                P.op(eng, lambda e, c=c: e.scalar_tensor_tensor(out=CTball[:, c, :], in0=hacc[:, :], scalar=CTloc[:, c, 129:130], in1=CTloc[:, c, :],
                                                                op0=ALU.mult, op1=ALU.add),
                     reads=["hacc", f"CTloc{par}_{c}"], writes=[f"CTb_{c}"])

            NB = 2
            for c0 in range(0, NCH, NB):
                cl = list(range(c0, c0 + NB))
                for c in cl:
                    b = c % NB
                    cs = slice(c * CL, (c + 1) * CL)
                    P.op("pe", lambda e, cs=cs, b=b: e.matmul(out=pab[b], lhsT=ksT[:, cs], rhs=qsT[:, cs], start=True, stop=True),
                         reads=[KK, KQ], writes=[KPA[b]])
                for c in cl:
                    b = c % NB
                    P.op("dve", lambda e, b=b: e.tensor_tensor(out=STb[:, b, :], in0=pab[b], in1=maskST[:, :], op=ALU.mult), reads=[KPA[b], "maskST"], writes=[f"ST{b}"])
                for c in cl:
                    b = c % NB
                    cs = slice(c * CL, (c + 1) * CL)
                    def f_cn(e, c=c, cs=cs, b=b):
                        e.matmul(out=pcb[b], lhsT=STb[:, b, :], rhs=v_ext[:, c, :], start=True, stop=False)
                        return e.matmul(out=pcb[b], lhsT=qsT[:, cs], rhs=CTball[:, c, :], start=False, stop=True)
                    P.op("pe", f_cn, reads=[f"ST{b}", f"v{par}_{c}", KVC, KQ, f"CTb_{c}"], writes=[KPC[b]])
                for c in cl:
                    b = c % NB
                    P.seq("dve", [
                        lambda e, b=b: e.tensor_copy(out=dnb[:, b, 1:2], in_=pcb[b][:, 128:129]),
                        lambda e, b=b: e.scalar_tensor_tensor(out=dnb[:, b, 0:1], in0=dnb[:, b, 1:2], scalar=-1.0, in1=dnb[:, b, 1:2], op0=ALU.mult, op1=ALU.max),
                        lambda e, b=b: e.tensor_scalar(out=dnb[:, b, 0:1], in0=dnb[:, b, 0:1], scalar1=1.0, scalar2=None, op0=ALU.max),
                        lambda e, b=b: e.reciprocal(out=dnb[:, b, 1:2], in_=dnb[:, b, 0:1]),
                        lambda e, b=b: e.tensor_scalar(out=hnb[:, b, :], in0=pcb[b][:, 0:128], scalar1=dnb[:, b, 1:2], scalar2=None, op0=ALU.mult)],
                        reads=[KPC[b]], writes=[f"dn{b}", f"hn{b}"])
                for c in cl:
                    b = c % NB
                    P.seq("act", [
                        lambda e, b=b: e.activation(out=junkb[:, b, :], in_=hnb[:, b, :], func=AF.Square, accum_out=dnb[:, b, 2:3]),
                        lambda e, b=b: e.activation(out=dnb[:, b, 2:3], in_=dnb[:, b, 2:3], func=AF.Sqrt, scale=1.0 / 128, bias=EPS)],
                        reads=[f"hn{b}", f"dn{b}"], writes=[f"junk{b}", f"dn2_{b}"])
                for c in cl:
                    b = c % NB
                    P.seq("dve", [
                        lambda e, b=b: e.reciprocal(out=dnb[:, b, 3:4], in_=dnb[:, b, 2:3]),
                        lambda e, c=c, b=b: e.tensor_scalar(out=oab[:, b, :], in0=hnb[:, b, :], scalar1=dnb[:, b, 3:4], scalar2=None, op0=ALU.mult)],
                        reads=[f"dn2_{b}", f"hn{b}"], writes=[f"oa{b}", f"dn{b}"])
                for c in cl:
                    b = c % NB
                    P.op("pe", lambda e, b=b: e.transpose(out=pto[b], in_=oab[:, b, :], identity=ident[0:CL, 0:CL]), reads=[f"oa{b}", "ident"], writes=[KPTO[b]])
                for c in cl:
                    b = c % NB
                    cs = slice(c * CL, (c + 1) * CL)
                    P.op("dve", lambda e, cs=cs, b=b: e.tensor_tensor(out=mslab[:, cs], in0=pto[b], in1=sigoT[:, cs], op=ALU.mult), reads=[KPTO[b], KS], writes=["mslab"])
            P.dma("sp", lambda e, hd=hd: e.dma_start(out=mergedT_scr[hd * 128:(hd + 1) * 128, :], in_=mslab[:, :]), "mscr", reads=["mslab"], writes=[f"mscr{hd}"])


        NH = 4 if stage >= 2 else 0
        if NH:
            head_s1(0)
        for hd in range(NH):
            if hd + 1 < NH:
                head_s1(hd + 1)
            head_s2(hd)

        esB.close()
        P.barrier()
        if stage >= 3:
            esC1 = ExitStack()
            esC1.__enter__()
            sb = lambda name, shape, dt: esC1.enter_context(nc.sbuf_tensor(name, shape, dt))
            ps = lambda name, shape, dt: esC1.enter_context(nc.psum_tensor(name, shape, dt))
            wu = sb("wu", [128, 8, 512], BF16)
            utok = [sb(f"utok{i}", [128, 512], BF16) for i in range(2)]
            pu1 = [ps(f"pu1_{i}", [128, 512], F32) for i in range(2)]
            P.dma("pool", lambda e: e.dma_start(out=wu[:, :, :], in_=w_in_v[:, :, 2056:2568]), "wu", writes=["wu"])
            for t in range(NT):
                def f_u1(e, t=t):
                    for k in range(8):
                        i = e.matmul(out=pu1[t % 2][:, :], lhsT=hT[:, k, HALO + t * 128:HALO + (t + 1) * 128], rhs=wu[:, k, :], start=(k == 0), stop=(k == 7))
                    return i
                P.op("pe", f_u1, reads=["wu", f"hT_{t // 4}"], writes=[f"pu1_{t % 2}"])
                P.op("act", lambda e, t=t: e.copy(out=utok[t % 2][:, :], in_=pu1[t % 2][:, :]), reads=[f"pu1_{t % 2}"], writes=[f"utok{t % 2}"])
                P.dma("sp", lambda e, t=t: e.dma_start(out=u_scr[t * 128:(t + 1) * 128, :], in_=utok[t % 2][:, :]), f"uscr{t % 2}",
                      reads=[f"utok{t % 2}"], writes=[f"u_scr{t}"])
            esC1.close()
            esH.close()
            P.barrier()

            TT = lambda o, a, b, op: (lambda e: e.tensor_tensor(out=o, in0=a, in1=b, op=op))
            TS = lambda o, a, s1, s2, op0, op1=None: ((lambda e: e.tensor_scalar(out=o, in0=a, scalar1=s1, scalar2=s2, op0=op0, op1=op1)) if op1 is not None
                                                      else (lambda e: e.tensor_scalar(out=o, in0=a, scalar1=s1, scalar2=None, op0=op0)))
            ACT = lambda o, a, f, **kw: (lambda e: e.activation(out=o, in_=a, func=f, **kw))
            MUL, ADD, SUB = ALU.mult, ALU.add, ALU.subtract

            def cmul(o_r, o_i, a_r, a_i, s_r, s_i, t1, t2):
                return [TT(t1, a_r, s_r, MUL), TT(t2, a_i, s_i, MUL), TT(o_r, t1, t2, SUB),
                        TT(t1, a_r, s_i, MUL), TT(t2, a_i, s_r, MUL), TT(o_i, t1, t2, ADD)]

            def emit_prep(GP, gs, lam_re_t, lam_im_t, logdt_t, X1t, X2t, sgn, hpi, sc, sqr, sqi, bqr, bqi, a5r, a5i, PWfr, PWfi, PWrr, PWri, PWbr, PWbi, cta, ctb, BB1, BB2, bbt, PRs, nPI, PLr, PLi):
                ops = []
                S = {k: v[:, :] for k, v in sc.items()}
                ops = []
                ops.append(TS(S["lr"], lam_re_t[:, gs], -1e-4, None, ALU.min))
                P.seq("dve", ops, reads=["lam_re_t"], writes=["prep"]); ops = []
                P.op("act", ACT(S["dt"], logdt_t[:, gs], AF.Exp), reads=["logdt_t", "prep"], writes=["prep_dt"])
                ops += [TT(S["lrdt"], S["lr"], S["dt"], MUL), TT(S["th"], lam_im_t[:, gs], S["dt"], MUL)]
                P.seq("dve", ops, reads=["prep", "prep_dt", "lam_im_t"], writes=["prep"]); ops = []
                P.seq("act", [ACT(S["sn"], S["th"], AF.Sin, scale=1.0 / 32), ACT(S["cs"], S["th"], AF.Sin, scale=1.0 / 32, bias=hpi[:, 0:1]),
                              ACT(S["mag"], S["lrdt"], AF.Exp, scale=1.0 / 32), ACT(S["im2"], S["lrdt"], AF.Exp, scale=-2.0)],
                      reads=["prep", "hpi"], writes=["prep_cs"])
                li = lam_im_t[:, gs]
                ops += [TT(a5r[:, :, 0], S["mag"], S["cs"], MUL), TT(a5i[:, :, 0], S["mag"], S["sn"], MUL)]
                for e_ in range(5):
                    ops += cmul(a5r[:, :, e_ + 1], a5i[:, :, e_ + 1], a5r[:, :, e_], a5i[:, :, e_], a5r[:, :, e_], a5i[:, :, e_], S["ta"], S["nsq"])
                ops += [(lambda e: e.tensor_copy(out=S["ar"], in_=a5r[:, :, 5])), (lambda e: e.tensor_copy(out=S["ai"], in_=a5i[:, :, 5])),
                        TT(S["den"], S["lr"], S["lr"], MUL), TT(S["ta"], li, li, MUL), TT(S["den"], S["den"], S["ta"], ADD),
                        (lambda e: e.reciprocal(out=S["den"], in_=S["den"])),
                        TS(S["am1"], S["ar"], -1.0, None, ADD),
                        TT(S["ta"], S["am1"], S["lr"], MUL), TT(S["tb"], S["ai"], li, MUL), TT(S["ta"], S["ta"], S["tb"], ADD), TT(S["cr"], S["ta"], S["den"], MUL),
                        TT(S["ta"], S["ai"], S["lr"], MUL), TT(S["tb"], S["am1"], li, MUL), TT(S["ta"], S["ta"], S["tb"], SUB), TT(S["ci"], S["ta"], S["den"], MUL),
                        TS(S["scr"], S["cr"], sgn[:, 0:1], None, MUL),
                        TS(S["tb"], S["ci"], sgn[:, 0:1], None, MUL),
                        TS(S["nci"], S["ci"], -1.0, None, MUL),
                        TT(S["bir"], S["ar"], S["im2"], MUL), TT(S["bii"], S["ai"], S["im2"], MUL), TS(S["bii"], S["bii"], -1.0, None, MUL),
                        (lambda e: e.tensor_copy(out=sqr[:, :, 0], in_=S["ar"])), (lambda e: e.tensor_copy(out=sqi[:, :, 0], in_=S["ai"])),
                        (lambda e: e.tensor_copy(out=bqr[:, :, 0], in_=S["bir"])), (lambda e: e.tensor_copy(out=bqi[:, :, 0], in_=S["bii"]))]
                for e_ in range(11):
                    ops += cmul(sqr[:, :, e_ + 1], sqi[:, :, e_ + 1], sqr[:, :, e_], sqi[:, :, e_], sqr[:, :, e_], sqi[:, :, e_], S["ta"], S["nsq"])
                for e_ in range(2):
                    ops += cmul(bqr[:, :, e_ + 1], bqi[:, :, e_ + 1], bqr[:, :, e_], bqi[:, :, e_], bqr[:, :, e_], bqi[:, :, e_], S["ta"], S["nsq"])
                bc16 = lambda a: a.unsqueeze(2).to_broadcast([128, GP, 16])
                ops += [TT(BB1[:, :, :], X1t[:, gs, :], bc16(S["cr"]), MUL), TT(bbt[:, :, :], X2t[:, gs, :], bc16(S["tb"]), MUL), TT(BB1[:, :, :], BB1[:, :, :], bbt[:, :, :], ADD),
                        TT(BB2[:, :, :], X2t[:, gs, :], bc16(S["scr"]), MUL), TT(bbt[:, :, :], X1t[:, gs, :], bc16(S["nci"]), MUL), TT(BB2[:, :, :], BB2[:, :, :], bbt[:, :, :], ADD)]
                ops += [(lambda e: e.memset(PWfr[:, :, 0:1], 1.0)), (lambda e: e.memset(PWfi[:, :, 0:1], 0.0))]
                for k in range(LL):
                    n = 1 << k
                    bcn = lambda a, n=n: a.to_broadcast([128, GP, n])
                    ops += cmul(PWfr[:, :, n:2 * n], PWfi[:, :, n:2 * n], PWfr[:, :, 0:n], PWfi[:, :, 0:n],
                                bcn(sqr[:, :, k:k + 1]), bcn(sqi[:, :, k:k + 1]), cta[:, :, 0:n], ctb[:, :, 0:n])
                ops += [(lambda e: e.tensor_copy(out=PWfr[:, :, L5:L5 + 1], in_=sqr[:, :, LL:LL + 1])), (lambda e: e.tensor_copy(out=PWfi[:, :, L5:L5 + 1], in_=sqi[:, :, LL:LL + 1]))]
                ops += [(lambda e: e.memset(PWrr[:, :, L5 - 1:L5], 1.0)), (lambda e: e.memset(PWri[:, :, L5 - 1:L5], 0.0))]
                for k in range(LL):
                    n = 1 << k
                    bcn = lambda a, n=n: a.to_broadcast([128, GP, n])
                    ops += cmul(PWrr[:, :, L5 - 2 * n:L5 - n], PWri[:, :, L5 - 2 * n:L5 - n], PWrr[:, :, L5 - n:L5], PWri[:, :, L5 - n:L5],
                                bcn(sqr[:, :, k:k + 1]), bcn(sqi[:, :, k:k + 1]), cta[:, :, 0:n], ctb[:, :, 0:n])
                ops += [(lambda e: e.memset(PWbr[:, :, 0:1], 1.0)), (lambda e: e.memset(PWbi[:, :, 0:1], 0.0))]
                for k in range(3):
                    n = 1 << k
                    bcn = lambda a, n=n: a.to_broadcast([128, GP, n])
                    ops += cmul(PWbr[:, :, n:2 * n], PWbi[:, :, n:2 * n], PWbr[:, :, 0:n], PWbi[:, :, 0:n],
                                bcn(bqr[:, :, k:k + 1]), bcn(bqi[:, :, k:k + 1]), cta[:, :, 0:n], ctb[:, :, 0:n])
                ops += [(lambda e: e.memset(PLr[:, :, 0:1], 1.0)), (lambda e: e.memset(PLi[:, :, 0:1], 0.0))]
                for k in range(6):
                    n = 1 << k
                    bcn = lambda a, n=n: a.to_broadcast([128, GP, n])
                    ops += cmul(PLr[:, :, n:2 * n], PLi[:, :, n:2 * n], PLr[:, :, 0:n], PLi[:, :, 0:n],
                                bcn(sqr[:, :, LL + k:LL + k + 1]), bcn(sqi[:, :, LL + k:LL + k + 1]), cta[:, :, 0:n], ctb[:, :, 0:n])
                ops += [TS(PRs[:, :, :], PWfr[:, :, :], sgn[:, 0:1], None, MUL), TS(PRs[:, :, :], PRs[:, :, :], -1.0, None, MUL), TS(nPI[:, :, :], PWfi[:, :, :], -1.0, None, MUL)]
                P.seq("dve", ops, reads=["prep", "prep_sn", "prep_cs", "sgn", "X1t", "X2t", "lam_im_t", "Rk_use", "tab_use"], writes=["prep", "prepT"]); ops = []

            esC0 = ExitStack()
            esC0.__enter__()
            sb0 = lambda name, shape, dt: esC0.enter_context(nc.sbuf_tensor(name, shape, dt))
            GA = 32
            i_lre = sb0("i_lre", [128, 32], F32); i_lim = sb0("i_lim", [128, 32], F32); i_ldt = sb0("i_ldt", [128, 32], F32)
            i_X1 = sb0("i_X1", [128, 32, 16], F32); i_X2 = sb0("i_X2", [128, 32, 16], F32)
            i_sgn = sb0("i_sgn", [128, 1], F32); i_hpi = sb0("i_hpi", [128, 1], F32)
            for dst_, src_ in ((i_lre[:, :], lam2_re[:, :]), (i_lim[:, :], lam2_im[:, :]), (i_ldt[:, :], logdt[0:1, :].partition_broadcast(128)),
                               (i_X1[:, :, :], X1d[:, :, :]), (i_X2[:, :, :], X2d[:, :, :])):
                P.dma("sp", lambda e, dst_=dst_, src_=src_: e.dma_start(out=dst_, in_=src_), "setupC0", writes=["c0in"])
            P.bulk_done("setupC0")
            P.seq("pool", [lambda e: e.memset(i_sgn[0:64, :], -1.0), lambda e: e.memset(i_sgn[64:128, :], 1.0), lambda e: e.memset(i_hpi[:, :], float(np.pi / 2))],
                  writes=["sgn", "hpi", "lam_re_t", "lam_im_t", "logdt_t", "X1t", "X2t"], reads=["c0in"])
            sc_names = ("lr", "dt", "lrdt", "th", "mag", "t1", "sn", "cs", "ar", "ai", "den", "am1", "cr", "ci", "ta", "tb", "scr", "nci", "im2", "bir", "bii", "nsq")
            sc0 = {n: sb0("sc0_" + n, [128, GA], F32) for n in sc_names}
            T0 = {n: sb0("p0_" + n, [128, GA, k], F32) for n, k in (("sqr", 12), ("sqi", 12), ("bqr", 3), ("bqi", 3), ("a5r", 6), ("a5i", 6), ("PWfr", L5 + 1), ("PWfi", L5 + 1),
                                                                     ("PWrr", L5), ("PWri", L5), ("PWbr", 8), ("PWbi", 8), ("cta", 64), ("ctb", 64), ("BB1", 16), ("BB2", 16),
                                                                     ("bbt", 16), ("PRs", L5 + 1), ("nPI", L5 + 1), ("PLr", NC5), ("PLi", NC5))}
            emit_prep(GA, slice(0, 32), i_lre, i_lim, i_ldt, i_X1, i_X2, i_sgn, i_hpi, sc0, *[T0[n] for n in ("sqr", "sqi", "bqr", "bqi", "a5r", "a5i", "PWfr", "PWfi", "PWrr", "PWri", "PWbr", "PWbi",
                                                                 "cta", "ctb", "BB1", "BB2", "bbt", "PRs", "nPI", "PLr", "PLi")])
            for nm_ in ("PWrr", "PWri", "PWbr", "PWbi", "BB1", "BB2", "PRs", "nPI", "sqr", "sqi", "PLr", "PLi"):
                P.dma("sp", lambda e, nm_=nm_: e.dma_start(out=prep_scr[nm_][:, :, :], in_=T0[nm_][:, :, :]), "prepst", reads=["prepT"], writes=["prep_scr"])
            P.bulk_done("prepst")
            esC0.close()
            P.barrier()
            esC = ExitStack()
            esC.__enter__()
            sb = lambda name, shape, dt: esC.enter_context(nc.sbuf_tensor(name, shape, dt))
            ps = lambda name, shape, dt: esC.enter_context(nc.psum_tensor(name, shape, dt))
            G8 = 8
            lam_re_t = sb("lam_re_t", [128, 32], F32)
            lam_im_t = sb("lam_im_t", [128, 32], F32)
            logdt_t = sb("logdt_t", [128, 32], F32)
            X1t = sb("X1t", [128, 32, 16], F32)
            X2t = sb("X2t", [128, 32, 16], F32)
            CY1t = sb("CY1t", [128, 32, 16], F32)
            CY2t = sb("CY2t", [128, 32, 16], F32)
            sgn = sb("sgn", [128, 1], F32)
            d_bc = sb("d_bc", [NC5, 512], F32)
            WAt = sb("WAt", [128, 4, 128], BF16)
            WBt = sb("WBt", [128, 4, 128], BF16)
            gba = sb("gba", [128, 4], F32)
            gbb = sb("gbb", [128, 4], F32)
            SW = sb("SW", [128, 128], F32)
            rkt8 = sb("rkt8", [128, G8, 128], F32)
            send = sb("send", [128, G8], F32)
            sc = {n: sb("sc_" + n, [128, G8], F32) for n in
                  ("lr", "dt", "lrdt", "th", "mag", "t1", "sn", "cs", "ar", "ai", "den", "am1", "cr", "ci", "ta", "tb", "scr", "nci", "im2", "bir", "bii", "nsq")}
            a5r = sb("a5r", [128, G8, 6], F32)
            a5i = sb("a5i", [128, G8, 6], F32)
            hpi = sb("hpi", [128, 1], F32)
            sqr2 = [sb(f"sqr_{i}", [128, G8, 12], F32) for i in range(2)]
            sqi2 = [sb(f"sqi_{i}", [128, G8, 12], F32) for i in range(2)]
            bqr = sb("bqr", [128, G8, 3], F32)
            bqi = sb("bqi", [128, G8, 3], F32)
            PWrr2 = [sb(f"PWrr_{i}", [128, G8, L5], F32) for i in range(2)]
            PWri2 = [sb(f"PWri_{i}", [128, G8, L5], F32) for i in range(2)]
            PWbr2 = [sb(f"PWbr_{i}", [128, G8, 8], F32) for i in range(2)]
            PWbi2 = [sb(f"PWbi_{i}", [128, G8, 8], F32) for i in range(2)]
            BB12 = [sb(f"BB1_{i}", [128, G8, 16], F32) for i in range(2)]
            BB22 = [sb(f"BB2_{i}", [128, G8, 16], F32) for i in range(2)]
            bbt = sb("bbt", [128, G8, 16], F32)
            PRs2 = [sb(f"PRs_{i}", [128, G8, L5 + 1], F32) for i in range(2)]
            nPI2 = [sb(f"nPI_{i}", [128, G8, L5 + 1], F32) for i in range(2)]
            PLr2 = [sb(f"PLr_{i}", [128, G8, NC5], F32) for i in range(2)]
            PLi2 = [sb(f"PLi_{i}", [128, G8, NC5], F32) for i in range(2)]
            Wc1 = sb("Wc1", [128, G8, NC5], F32)
            Wc2 = sb("Wc2", [128, G8, NC5], F32)
            hsw = sb("hsw", [128, G8], F32)
            tA = sb("tA", [128, L5 + 1, 16], F32)
            tB = sb("tB", [128, L5 + 1, 16], F32)
            tC = sb("tC", [128, L5 + 1, 16], F32)
            tD = sb("tD", [128, L5 + 1, 16], F32)
            ZTb = sb("ZTb", [128, L5, 16], BF16)
            XTb = sb("XTb", [128, 8, 16], BF16)
            Zt2 = [sb(f"Zt_{i}", [128, G8, KC5, 128], BF16) for i in range(2)]
            Gtab = sb("Gtab", [128, G8, L5 * 16], BF16)
            Yt = sb("Yt", [128, G8, L5 + 1, 16], BF16)
            Rk2 = [sb(f"Rk_{i}", [128, G8, NLEV, 128], F32) for i in range(2)]
            ucj = sb("ucj", [NC5, G8, L5, 16], BF16)
            ustack = sb("ustack", [128, G8, KC5, NC5], BF16)
            E32 = sb("E32", [128, G8, NC5], F32)
            Xs = sb("Xs", [128, G8, NC5 + 1], F32)
            Sprevb = sb("Sprevb", [128, G8, NC5], BF16)
            sg_t = sb("sg_t", [128, 4, G8], F32)
            hs = sb("hs", [128, G8], F32)
            hs2 = sb("hs2", [128, G8], F32)
            du = sb("du", [NC5, L5, 16], F32)
            yv = sb("yv", [NC5, L5 * 16], F32)
            y2 = sb("y2", [NC5, L5 * 16], F32)
            ysg = sb("ysg", [NC5, L5 * 16], F32)
            ygel = sb("ygel", [NC5, L5, 128], BF16)
            ygT = sb("ygT", [128, L5, NC5], BF16)
            sbt = sb("sbt", [128, 512], F32)
            ps_z = [ps(f"ps_z{i}", [128, 512], F32) for i in range(2)]
            pT = ps("pT", [128, 2048], BF16)
            pE = ps("pE", [128, G8, NC5], F32)
            pD = ps("pD", [128, G8, NC5], F32)
            pY = ps("pY", [128, 512], F32)

            def ld(dst, src, key, q="sp"):
                P.dma(q, lambda e: e.dma_start(out=dst, in_=src), "setupC" if q == "sp" else "setupCp", writes=[key])
            ld(lam_re_t[:, :], lam2_re[:, :], "lam_re_t")
            ld(lam_im_t[:, :], lam2_im[:, :], "lam_im_t")
            ld(logdt_t[:, :], logdt[0:1, :].partition_broadcast(128), "logdt_t")
            ld(X1t[:, :, :], X1d[:, :, :], "X1t")
            ld(X2t[:, :, :], X2d[:, :, :], "X2t")
            ld(CY1t[:, :, :], CY1d[:, :, :], "CY1t")
            ld(CY2t[:, :, :], CY2d[:, :, :], "CY2t")
            ld(d_bc[:, :], s5d[0:1, :].partition_broadcast(NC5), "d_bc")
            ld(gba[:, :], gbad[:, :], "gba")
            ld(gbb[:, :], gbbd[:, :], "gbb")
            ld(WAt[:, :, :], WAd.ap().rearrange("u p c -> p u c"), "WAt", q="pool")
            ld(WBt[:, :, :], WBd.ap().rearrange("u p c -> p u c"), "WBt", q="pool")
            P.bulk_done("setupC")
            P.bulk_done("setupCp")
            P.seq("pool", [lambda e: e.memset(sgn[0:64, :], -1.0), lambda e: e.memset(sgn[64:128, :], 1.0)], writes=["sgn"])
            P.op("pool", lambda e: e.memset(hpi[:, :], float(np.pi / 2)), writes=["hpi"])
            P.seq("pool", [lambda e: e.memset(SW[:, :], 0.0),
                           lambda e: e.affine_select(out=SW[:, :], in_=SW[:, :], pattern=[[-1, 128]], compare_op=ALU.not_equal, fill=1.0, base=64, channel_multiplier=1),
                           lambda e: e.affine_select(out=SW[:, :], in_=SW[:, :], pattern=[[-1, 128]], compare_op=ALU.not_equal, fill=1.0, base=-64, channel_multiplier=1)],
                  writes=["SW"])
            P.op("pool", lambda e: e.tensor_scalar(out=SW[:, :], in0=SW[:, :], scalar1=sgn[:, 0:1], scalar2=None, op0=ALU.mult), reads=["SW", "sgn"], writes=["SW"])

            PI = float(np.pi)
            bh = lambda a, n: a.unsqueeze(1).to_broadcast([128, n, 16])
            def u_loads(un):
                par = un % 2
                gs = slice(un * 8, (un + 1) * 8)
                PWrr, PWri, PWbr, PWbi, BB1, BB2, PRs, nPI, sqr, sqi, PLr, PLi = [t2[par] for t2 in (PWrr2, PWri2, PWbr2, PWbi2, BB12, BB22, PRs2, nPI2, sqr2, sqi2, PLr2, PLi2)]
                Zt = Zt2[par]; Rk = Rk2[par]
                KP = f"prepT{par}"; KZ = f"Zt{par}"; KR = [f"Rk{par}_{g}" for g in range(G8)]
                for nm_, dst_ in (("PWrr", PWrr), ("PWri", PWri), ("PWbr", PWbr), ("PWbi", PWbi), ("BB1", BB1), ("BB2", BB2),
                                  ("PRs", PRs), ("nPI", nPI), ("sqr", sqr), ("sqi", sqi), ("PLr", PLr), ("PLi", PLi)):
                    P.dma("sp", lambda e, nm_=nm_, dst_=dst_, gs=gs: e.dma_start(out=dst_[:, :, :], in_=prep_scr[nm_][:, gs, :]), "prepld",
                          reads=["prep_scr", "Rk_use", "tab_use"], writes=[KP])
                P.bulk_done("prepld")


            def u_Z(un):
                par = un % 2
                gs = slice(un * 8, (un + 1) * 8)
                PWrr, PWri, PWbr, PWbi, BB1, BB2, PRs, nPI, sqr, sqi, PLr, PLi = [t2[par] for t2 in (PWrr2, PWri2, PWbr2, PWbi2, BB12, BB22, PRs2, nPI2, sqr2, sqi2, PLr2, PLi2)]
                Zt = Zt2[par]; Rk = Rk2[par]
                KP = f"prepT{par}"; KZ = f"Zt{par}"; KR = [f"Rk{par}_{g}" for g in range(G8)]
                bh = lambda a, n: a.unsqueeze(1).to_broadcast([128, n, 16])
                for g in range(G8):
                    bq = lambda a: a.unsqueeze(2).to_broadcast([128, L5, 16])
                    P.seq("dve", [TT(tA[:, 0:L5, :], bq(PWrr[:, g, :]), bh(BB1[:, g, :], L5), MUL),
                                  TT(tB[:, 0:L5, :], bq(PWri[:, g, :]), bh(BB2[:, g, :], L5), MUL),
                                  TT(ZTb[:, :, :], tA[:, 0:L5, :], tB[:, 0:L5, :], ADD)],
                          reads=[KP, "ZTb_use"], writes=["tA", "tB", "ZTb"])
                    def f_zt(e):
                        for kc in range(KC5):
                            i = e.transpose(out=pT[:, kc * 128:(kc + 1) * 128], in_=ZTb[:, kc * 8:(kc + 1) * 8, :], identity=ident[:, :])
                        return i
                    P.op("pe", f_zt, reads=["ZTb", "ident"], writes=["pT"])
                    P.op("act", lambda e, g=g: e.copy(out=Zt[:, g, :, :], in_=pT[:, 0:KC5 * 128].rearrange("p (a b) -> p a b", b=128)), reads=["pT"], writes=[KZ, "ZTb_use"])

            def u_rk(un):
                par = un % 2
                gs = slice(un * 8, (un + 1) * 8)
                PWrr, PWri, PWbr, PWbi, BB1, BB2, PRs, nPI, sqr, sqi, PLr, PLi = [t2[par] for t2 in (PWrr2, PWri2, PWbr2, PWbi2, BB12, BB22, PRs2, nPI2, sqr2, sqi2, PLr2, PLi2)]
                Zt = Zt2[par]; Rk = Rk2[par]
                KP = f"prepT{par}"; KZ = f"Zt{par}"; KR = [f"Rk{par}_{g}" for g in range(G8)]
                for k in range(NLEV):
                    P.seq("pool", [lambda e, k=k: e.tensor_tensor(out=Rk[:, :, k, :], in0=identf[:, :].unsqueeze(1).to_broadcast([128, G8, 128]),
                                                                  in1=sqr[:, :, LL + k:LL + k + 1].to_broadcast([128, G8, 128]), op=MUL),
                                   lambda e, k=k: e.tensor_tensor(out=rkt8[:, :, :], in0=SW[:, :].unsqueeze(1).to_broadcast([128, G8, 128]),
                                                                  in1=sqi[:, :, LL + k:LL + k + 1].to_broadcast([128, G8, 128]), op=MUL),
                                   lambda e, k=k: e.tensor_tensor(out=Rk[:, :, k, :], in0=Rk[:, :, k, :], in1=rkt8[:, :, :], op=SUB)],
                          reads=[KP, "identf", "SW", "sgn", "Rk_use"], writes=KR + ["rkt"])

            def u_pre(un):
                par = un % 2
                gs = slice(un * 8, (un + 1) * 8)
                PWrr, PWri, PWbr, PWbi, BB1, BB2, PRs, nPI, sqr, sqi, PLr, PLi = [t2[par] for t2 in (PWrr2, PWri2, PWbr2, PWbi2, BB12, BB22, PRs2, nPI2, sqr2, sqi2, PLr2, PLi2)]
                Zt = Zt2[par]; Rk = Rk2[par]
                KP = f"prepT{par}"; KZ = f"Zt{par}"; KR = [f"Rk{par}_{g}" for g in range(G8)]
                for g in range(G8):
                    P.dma("sp", lambda e, un=un, g=g: e.dma_start(out=ucj[:, g, :, :],
                                                                   in_=u_scr.ap().rearrange("(c j) n -> c j n", j=L5)[:, :, un * 128 + g * 16:un * 128 + (g + 1) * 16]),
                          "ucj", reads=[f"u_scr{t}" for t in range(NT)] + ["ucj_use"], writes=[f"ucj{g}"])
                P.bulk_done("ucj")
                US0 = 1024
                for g in range(G8):
                    def f_us(e, g=g):
                        for kc in range(KC5):
                            i = e.transpose(out=pT[:, US0 + kc * NC5:US0 + (kc + 1) * NC5], in_=ucj[:, g, kc * 8:(kc + 1) * 8, :], identity=ident[0:NC5, 0:NC5])
                        return i
                    P.op("pe", f_us, reads=[f"ucj{g}", "ident"], writes=["pT2"])
                    P.op("act", lambda e, g=g: e.copy(out=ustack[:, g, :, :], in_=pT[:, US0:US0 + KC5 * NC5].rearrange("p (a b) -> p a b", b=NC5)), reads=["pT2"], writes=[f"ustack{g}"])
                    def f_E(e, g=g):
                        for kc in range(KC5):
                            i = e.matmul(out=pE[:, g, :], lhsT=Zt[:, g, kc, :], rhs=ustack[:, g, kc, :], start=(kc == 0), stop=(kc == KC5 - 1))
                        return i
                    P.op("pe", f_E, reads=[KZ, f"ustack{g}"], writes=["pE"])
                P.op("act", lambda e: e.copy(out=E32[:, :, :], in_=pE[:, :, :]), reads=["pE"], writes=["E32"])

                NX = NC5 + 1
                def doubling(tag):
                    for k in range(NLEV):
                        n = NX - (1 << k)
                        def f_d(e, k=k, n=n):
                            for g in range(G8):
                                i = e.matmul(out=pD[:, g, 0:n], lhsT=Rk[:, g, k, :], rhs=Xs[:, g, 0:n], start=True, stop=True)
                            return i
                        P.op("pe", f_d, reads=["Xs"] + KR, writes=["pD"])
                        P.op("dve", lambda e, k=k, n=n: e.tensor_tensor(out=Xs[:, :, 1 << k:NX], in0=Xs[:, :, 1 << k:NX], in1=pD[:, :, 0:n], op=ADD),
                             reads=["pD", "Xs"], writes=["Xs"])
                P.seq("dve", [lambda e: e.memset(Xs[:, :, 0:1], 0.0), lambda e: e.tensor_copy(out=Xs[:, :, 1:NX], in_=E32[:, :, :])], reads=["E32"], writes=["Xs"])
                doubling("loc")
                P.op("dve", lambda e: e.tensor_copy(out=send[:, :], in_=Xs[:, :, NC5]), reads=["Xs"], writes=["send"])
                P.dma("sp", lambda e, un=un: e.dma_start(out=ccs_in[un][:, :], in_=send[:, :]), f"ccsin{un}", reads=["send"], writes=[f"ccs_in{un}"])
                P.dma("pool", lambda e, un=un: e.collective_compute("AllGather", ALU.bypass, replica_groups=GROUPS4,
                                                                     ins=[ccs_in[un].ap().opt()], outs=[ccs_out[un].ap().opt()]),
                      f"ccs{un}", reads=[f"ccs_in{un}"], writes=[f"ccs_out{un}"], inc=1)
                P.dma("sp", lambda e, un=un: e.dma_start(out=sg_t[:, :, :], in_=ccs_out[un].ap().rearrange("(r p) c -> p r c", p=128)),
                      f"ccsld{un}", reads=[f"ccs_out{un}"], writes=["sg_t"])

            def u_xyg(un):
                par = un % 2
                gs = slice(un * 8, (un + 1) * 8)
                PWrr, PWri, PWbr, PWbi, BB1, BB2, PRs, nPI, sqr, sqi, PLr, PLi = [t2[par] for t2 in (PWrr2, PWri2, PWbr2, PWbi2, BB12, BB22, PRs2, nPI2, sqr2, sqi2, PLr2, PLi2)]
                Zt = Zt2[par]; Rk = Rk2[par]
                KP = f"prepT{par}"; KZ = f"Zt{par}"; KR = [f"Rk{par}_{g}" for g in range(G8)]
                for g in range(G8):
                    ga = un * 8 + g
                    bq8 = lambda a: a.unsqueeze(2).to_broadcast([128, 8, 16])
                    P.seq("dve", [TT(tA[:, 0:8, :], bq8(PWbr[:, g, :]), bh(BB1[:, g, :], 8), MUL),
                                  TT(tB[:, 0:8, :], bq8(PWbi[:, g, :]), bh(BB2[:, g, :], 8), MUL),
                                  TT(XTb[:, :, :], tA[:, 0:8, :], tB[:, 0:8, :], ADD)],
                          reads=[KP, "tA", "tB", "XTb_use"], writes=["tA", "tB", "XTb"])
                    bqY = lambda a: a.unsqueeze(2).to_broadcast([128, L5 + 1, 16])
                    P.seq("pool", [TT(tC[:, :, :], bqY(PRs[:, g, :]), bh(CY1t[:, ga, :], L5 + 1), MUL),
                                   TT(tD[:, :, :], bqY(nPI[:, g, :]), bh(CY2t[:, ga, :], L5 + 1), MUL),
                                   TT(Yt[:, g, :, :], tC[:, :, :], tD[:, :, :], ADD)],
                          reads=[KP, "CY1t", "CY2t", "tab_use"], writes=["tC", "tD", f"Yt{g}"])
                    P.op("pe", lambda e, g=g: e.matmul(out=pY[:, :], lhsT=XTb[:, :, :], rhs=Yt[:, g, 0:L5, :], start=True, stop=True), reads=["XTb", f"Yt{g}"], writes=["pY"])
                    P.op("act", lambda e, g=g: e.copy(out=Gtab[:, g, :], in_=pY[:, :]), reads=["pY", "tab_use"], writes=[f"Gtab{g}", "XTb_use"])
                    P.op("pool", lambda e, g=g: e.affine_select(out=Gtab[:, g, :].rearrange("p (m h) -> p m h", h=16), in_=Gtab[:, g, :].rearrange("p (m h) -> p m h", h=16),
                                                                pattern=[[16, L5], [0, 16]], compare_op=ALU.is_ge, fill=0.0, base=15, channel_multiplier=-1),
                         reads=[f"Gtab{g}"], writes=[f"Gtab{g}"])


            def u_post(un):
                par = un % 2
                gs = slice(un * 8, (un + 1) * 8)
                PWrr, PWri, PWbr, PWbi, BB1, BB2, PRs, nPI, sqr, sqi, PLr, PLi = [t2[par] for t2 in (PWrr2, PWri2, PWbr2, PWbi2, BB12, BB22, PRs2, nPI2, sqr2, sqi2, PLr2, PLi2)]
                Zt = Zt2[par]; Rk = Rk2[par]
                KP = f"prepT{par}"; KZ = f"Zt{par}"; KR = [f"Rk{par}_{g}" for g in range(G8)]
                P.op("dve", lambda e: e.memset(hs[:, :], 0.0), writes=["hs"])
                for pp in range(3):
                    def f_hr(e):
                        for g in range(G8):
                            i = e.matmul(out=pD[:, g, 0:1], lhsT=Rk[:, g, NLEV - 1, :], rhs=hs[:, g:g + 1], start=True, stop=True)
                        return i
                    P.op("pe", f_hr, reads=["hs"] + KR, writes=["pD"])
                    P.seq("dve", [lambda e, pp=pp: e.tensor_tensor(out=hs2[:, :], in0=pD[:, :, 0], in1=sg_t[:, pp, :], op=ADD),
                                  lambda e: e.tensor_tensor(out=hs2[:, :], in0=hs2[:, :], in1=hs[:, :], op=SUB),
                                  lambda e, pp=pp: e.scalar_tensor_tensor(out=hs[:, :], in0=hs2[:, :], scalar=pm[:, pp:pp + 1], in1=hs[:, :], op0=MUL, op1=ADD)],
                          reads=["pD", "sg_t", "hs", "pm"], writes=["hs", "hs2"])
                P.op("pe", lambda e: e.matmul(out=pD[:, 0, 0:G8], lhsT=SW[:, :], rhs=hs[:, :], start=True, stop=True), reads=["hs", "SW"], writes=["pD"])
                bcc = lambda a: a.unsqueeze(2).to_broadcast([128, G8, NC5])
                P.seq("dve", [lambda e: e.tensor_copy(out=hsw[:, :], in_=pD[:, 0, 0:G8]),
                              lambda e: e.tensor_tensor(out=Wc1[:, :, :], in0=PLr[:, :, :], in1=bcc(hs[:, :]), op=MUL),
                              lambda e: e.tensor_tensor(out=Wc2[:, :, :], in0=PLi[:, :, :], in1=bcc(hsw[:, :]), op=MUL),
                              lambda e: e.tensor_tensor(out=Wc1[:, :, :], in0=Wc1[:, :, :], in1=Wc2[:, :, :], op=SUB),
                              lambda e: e.tensor_tensor(out=Sprevb[:, :, :], in0=Wc1[:, :, :], in1=Xs[:, :, 0:NC5], op=ADD)],
                      reads=["pD", "hs", KP, "Xs"], writes=["hsw", "Wc", "Sprevb"])

                for g in range(G8):
                    def f_y(e, g=g):
                        e.matmul(out=pY[0:NC5, :], lhsT=Sprevb[:, g, :], rhs=Yt[:, g, 1:L5 + 1, :], start=True, stop=False)
                        for kc in range(KC5):
                            lo = 128 * kc
                            i = e.matmul(out=pY[0:NC5, lo:512], lhsT=ustack[:, g, kc, :], rhs=Gtab[:, g, 0:512 - lo], start=False, stop=(kc == KC5 - 1))
                        return i
                    P.op("pe", f_y, reads=["Sprevb", f"Yt{g}", f"Gtab{g}", f"ustack{g}"], writes=["pY"])
                    ga = un * 8 + g
                    P.op("pool", lambda e, g=g, ga=ga: e.tensor_tensor(out=du[:, :, :], in0=ucj[:, g, :, :],
                                                                       in1=d_bc[:, ga * 16:(ga + 1) * 16].unsqueeze(1).to_broadcast([NC5, L5, 16]), op=MUL),
                         reads=[f"ucj{g}", "d_bc"], writes=["du"])
                    yv_ = yv[:, :]
                    P.seq("dve", [lambda e: e.tensor_tensor(out=yv[:, :], in0=pY[0:NC5, :], in1=du[:, :, :].rearrange("p a b -> p (a b)"), op=ADD),
                                  TT(y2[:, :], yv_, yv_, MUL), TS(y2[:, :], y2[:, :], 0.044715, 1.0, MUL, ADD), TT(y2[:, :], y2[:, :], yv_, MUL)],
                          reads=["pY", "du", "ysg"], writes=["yv", "y2"])
                    P.op("act", ACT(ysg[:, :], y2[:, :], AF.Sigmoid, scale=1.5957691216057308), reads=["y2"], writes=["ysg"])
                    P.op("dve", lambda e, g=g: e.tensor_tensor(out=ygel[:, :, g * 16:(g + 1) * 16], in0=yv[:, :].rearrange("p (a b) -> p a b", b=16),
                                                               in1=ysg[:, :].rearrange("p (a b) -> p a b", b=16), op=MUL),
                         reads=["yv", "ysg", "ygel_use"], writes=[f"ygel{g}"])
                def f_gt(e):
                    for j in range(L5):
                        i = e.transpose(out=pT[:, j * NC5:(j + 1) * NC5], in_=ygel[:, j, :], identity=ident[0:NC5, 0:NC5])
                    return i
                P.op("pe", f_gt, reads=[f"ygel{g}" for g in range(G8)] + ["ident"], writes=["pT", "pT2"])
                P.op("act", lambda e: e.copy(out=ygT[:, :, :], in_=pT[:, :].rearrange("p (a b) -> p a b", b=NC5)), reads=["pT", "pT2"], writes=["ygT", "ygel_use"])
                ms_v = mslab[:, :].rearrange("p (c j) -> p j c", j=L5)
                JB = 512 // NC5
                for nb in range(4):
                    P.op("pe", lambda e, nb=nb, un=un: e.matmul(out=ps_z[0][:, :], lhsT=WAt[:, un, :], rhs=ygT[:, nb * JB:(nb + 1) * JB, :], start=True, stop=True),
                         reads=["WAt", "ygT"], writes=["ps_z0"])
                    P.op("pe", lambda e, nb=nb, un=un: e.matmul(out=ps_z[1][:, :], lhsT=WBt[:, un, :], rhs=ygT[:, nb * JB:(nb + 1) * JB, :], start=True, stop=True),
                         reads=["WBt", "ygT"], writes=["ps_z1"])
                    P.op("act", lambda e, un=un: e.activation(out=sbt[:, :], in_=ps_z[1][:, :], func=AF.Sigmoid, bias=gbb[:, un:un + 1]),
                         reads=["ps_z1", "gbb"], writes=["sbt"])
                    P.op("dve", lambda e, nb=nb, un=un: e.scalar_tensor_tensor(out=ms_v[:, nb * JB:(nb + 1) * JB, :], in0=ps_z[0][:, :].rearrange("p (j c) -> p j c", c=NC5), scalar=gba[:, un:un + 1],
                                                                               in1=sbt[:, :].rearrange("p (j c) -> p j c", c=NC5), op0=ADD, op1=MUL),
                         reads=["ps_z0", "sbt", "gba"], writes=["mslab"])
                P.dma("sp", lambda e, un=un: e.dma_start(out=mergedT_scr[512 + un * 128:512 + (un + 1) * 128, :], in_=mslab[:, :]), "mscr",
                      reads=["mslab"], writes=[f"mscr{4 + un}"])

            u_loads(0); u_Z(0); u_rk(0)
            for un in range(4):
                u_pre(un)
                u_xyg(un)
                if un + 1 < 4:
                    u_loads(un + 1); u_Z(un + 1); u_rk(un + 1)
                u_post(un)
            esC.close()
            P.barrier()
        else:
            esH.close()

        if stage >= 4:
            AX = mybir.AxisListType
            MUL, ADD, SUB = ALU.mult, ALU.add, ALU.subtract
            esDE = ExitStack()
            esDE.__enter__()
            sbp = lambda name, shape, dt: esDE.enter_context(nc.sbuf_tensor(name, shape, dt))
            h2T = sbp("h2T", [128, 8, NTOK], BF16)
            cw = sbp("cw", [128, NT, 32], F32)
            xt2 = [sbp(f"xt2_{i}", [128, D], F32) for i in range(2)]
            ssq2 = sbp("ssq2", [128, 2 * NT], F32)
            rstd2 = sbp("rstd2", [128, 2 * NT], F32)
            junk2 = sbp("junk2", [128, D], BF16)
            esD = ExitStack()
            esD.__enter__()
            sb = lambda name, shape, dt: esD.enter_context(nc.sbuf_tensor(name, shape, dt))
            ps = lambda name, shape, dt: esD.enter_context(nc.psum_tensor(name, shape, dt))
            mT = sb("mT", [128, 8, NTOK], BF16)
            Wout = sb("Wout", [128, 8, D], BF16)
            Wr = sb("Wr", [128, 8, 36], BF16)
            rb_bc = sb("rb_bc", [128, 36], F32)
            xm = [sb(f"xm{i}", [128, D], F32) for i in range(2)]
            hb2 = [sb(f"hb2_{i}", [128, D], BF16) for i in range(2)]
            lg = sb("lg", [128, NT, 36], F32)
            r_a = sb("r_a", [128, NT, 4], F32)
            r_b = sb("r_b", [128, NT, 4], F32)
            r_gmax = sb("r_gmax", [128, NT], F32)
            r_gw = sb("r_gw", [128, NT], F32)
            r_el = sb("r_el", [128, NT, 32], F32)
            r_t = sb("r_t", [128, NT, 32], F32)
            r_oh1 = sb("r_oh1", [128, NT, 32], F32)
            r_oh2 = sb("r_oh2", [128, NT, 32], F32)
            r_m1 = sb("r_m1", [128, NT], F32)
            r_m2 = sb("r_m2", [128, NT], F32)
            r_w1 = sb("r_w1", [128, NT], F32)
            r_w2 = sb("r_w2", [128, NT], F32)
            po4 = [[ps(f"po{i}{j}", [128, 512], F32) for j in range(2)] for i in range(2)]
            tp2 = [ps(f"tp2_{i}", [128, 8, 128], BF16) for i in range(2)]
            pr2 = [ps(f"pr{i}", [128, 36], F32) for i in range(2)]

            P.dma("sp", lambda e: e.dma_start(out=mT[:, :, :], in_=mergedT_scr.ap().rearrange("(kc p) t -> p kc t", p=128)), "mT",
                  reads=[f"mscr{i}" for i in range(8)], writes=["mT"])
            P.dma("pool", lambda e: e.dma_start(out=Wout[:, :, :], in_=w_out.ap().rearrange("(kc p) c -> p kc c", p=128)), "setupDp", writes=["Wout"])
            P.dma("pool", lambda e: e.dma_start(out=Wr[:, :, :], in_=w_rt.ap().rearrange("(kc p) c -> p kc c", p=128)), "setupDp", writes=["Wr"])
            P.dma("sp", lambda e: e.dma_start(out=rb_bc[:, :], in_=b_rt[0:1, :].partition_broadcast(128)), "setupD", writes=["rb_bc"])
            P.dma("sp", lambda e: e.dma_start(out=g_bc[:, :], in_=g_ffn[0:1, :].partition_broadcast(128)), "setupD", writes=["g_bc"])
            P.bulk_done("setupD")
            P.bulk_done("setupDp")

            for t0 in range(0, NT, 2):
                tl = (t0, t0 + 1)
                for t in tl:
                    b = t % 2
                    P.dma("sp", lambda e, t=t, b=b: e.dma_start(out=xt2[b][:, :], in_=x[t * 128:(t + 1) * 128, :]), f"xt2_{b}", writes=[f"xt2_{b}"])
                for t in tl:
                    b = t % 2
                    ts_ = slice(t * 128, (t + 1) * 128)
                    for hf in range(2):
                        def f_o(e, ts_=ts_, hf=hf, b=b):
                            for k in range(8):
                                i = e.matmul(out=po4[b][hf][:, :], lhsT=mT[:, k, ts_], rhs=Wout[:, k, hf * 512:(hf + 1) * 512], start=(k == 0), stop=(k == 7))
                            return i
                        P.op("pe", f_o, reads=["mT", "Wout"], writes=[f"po{b}{hf}"])
                for t in tl:
                    b = t % 2
                    for hf in range(2):
                        P.op("dve", lambda e, hf=hf, b=b: e.tensor_tensor(out=xm[b][:, hf * 512:(hf + 1) * 512], in0=po4[b][hf][:, :], in1=xt2[b][:, hf * 512:(hf + 1) * 512], op=ADD),
                             reads=[f"po{b}{hf}", f"xt2_{b}"], writes=[f"xm{b}_{hf}"])
                for t in tl:
                    b = t % 2
                    xk = [f"xm{b}_0", f"xm{b}_1"]
                    P.dma("sp", lambda e, t=t, b=b: e.dma_start(out=xmid_scr[t * 128:(t + 1) * 128, :], in_=xm[b][:, :]), f"xmid{b}", reads=xk, writes=[f"xmid{t}"])
                    P.seq("act", [lambda e, b=b, t=t: e.activation(out=junk2[:, :], in_=xm[b][:, :], func=AF.Square, accum_out=ssq2[:, t:t + 1]),
                                  lambda e, t=t: e.activation(out=ssq2[:, t:t + 1], in_=ssq2[:, t:t + 1], func=AF.Sqrt, scale=1.0 / D, bias=EPS)],
                          reads=xk, writes=["junk2", f"ssq2_{t}"])
                for t in tl:
                    b = t % 2
                    xk = [f"xm{b}_0", f"xm{b}_1"]
                    P.op("dve", lambda e, t=t: e.reciprocal(out=rstd2[:, t:t + 1], in_=ssq2[:, t:t + 1]), reads=[f"ssq2_{t}"], writes=[f"rstd2_{t}"])
                    P.op("dve", lambda e, b=b, t=t: e.scalar_tensor_tensor(out=hb2[b][:, :], in0=xm[b][:, :], scalar=rstd2[:, t:t + 1], in1=g_bc[:, :], op0=MUL, op1=MUL),
                         reads=xk + [f"rstd2_{t}", "g_bc"], writes=[f"hb2_{b}"])
                for t in tl:
                    b = t % 2
                    def f_tp2(e, b=b):
                        for k in range(8):
                            i = e.transpose(out=tp2[b][:, k, :], in_=hb2[b][:, k * 128:(k + 1) * 128], identity=ident[:, :])
                        return i
                    P.op("pe", f_tp2, reads=[f"hb2_{b}", "ident"], writes=[f"tp2_{b}"])
                for t in tl:
                    b = t % 2
                    ts_ = slice(t * 128, (t + 1) * 128)
                    P.op("act", lambda e, ts_=ts_, b=b: e.copy(out=h2T[:, :, ts_], in_=tp2[b][:, :, :]), reads=[f"tp2_{b}"], writes=[f"h2T_{t // 4}"])
                for t in tl:
                    b = t % 2
                    ts_ = slice(t * 128, (t + 1) * 128)
                    def f_r(e, ts_=ts_, b=b):
                        for k in range(8):
                            i = e.matmul(out=pr2[b][:, :], lhsT=h2T[:, k, ts_], rhs=Wr[:, k, :], start=(k == 0), stop=(k == 7))
                        return i
                    P.op("pe", f_r, reads=[f"h2T_{t // 4}", "Wr"], writes=[f"pr{b}"])
                for t in tl:
                    b = t % 2
                    P.op("dve", lambda e, t=t, b=b: e.tensor_tensor(out=lg[:, t, :], in0=pr2[b][:, :], in1=rb_bc[:, :], op=ADD), reads=[f"pr{b}", "rb_bc"], writes=["lg"])

            BIG = 1.0e9
            bc4 = lambda a: a.unsqueeze(2).to_broadcast([128, NT, 4])
            bc32 = lambda a: a.unsqueeze(2).to_broadcast([128, NT, 32])
            gl = lg[:, :, 0:4]
            el = lg[:, :, 4:36]
            P.seq("dve", [
                lambda e: e.tensor_reduce(out=r_gmax[:, :], in_=gl, axis=AX.X, op=ALU.max),
                lambda e: e.tensor_tensor(out=r_a[:, :, :], in0=gl, in1=bc4(r_gmax[:, :]), op=SUB)], reads=["lg"], writes=["r_a", "r_gmax"])
            P.op("act", lambda e: e.activation(out=r_b[:, :, :], in_=r_a[:, :, :], func=AF.Exp), reads=["r_a"], writes=["r_b"])
            P.seq("dve", [
                lambda e: e.tensor_reduce(out=r_gw[:, :], in_=r_b[:, :, :], axis=AX.X, op=ADD),
                lambda e: e.reciprocal(out=r_gw[:, :], in_=r_gw[:, :]),
                lambda e: e.tensor_tensor(out=r_a[:, :, :], in0=gl, in1=bc4(r_gmax[:, :]), op=ALU.is_equal),
                lambda e: e.tensor_scalar(out=r_a[:, :, :], in0=r_a[:, :, :], scalar1=-1.0, scalar2=BIG, op0=ADD, op1=MUL),
                lambda e: e.tensor_tensor(out=r_el[:, :, :].rearrange("p t (g k) -> p t g k", k=8), in0=el.rearrange("p t (g k) -> p t g k", k=8),
                                          in1=r_a[:, :, :].unsqueeze(3).to_broadcast([128, NT, 4, 8]), op=ADD),
                lambda e: e.tensor_reduce(out=r_m1[:, :], in_=r_el[:, :, :], axis=AX.X, op=ALU.max),
                lambda e: e.tensor_tensor(out=r_oh1[:, :, :], in0=r_el[:, :, :], in1=bc32(r_m1[:, :]), op=ALU.is_equal),
                lambda e: e.scalar_tensor_tensor(out=r_t[:, :, :], in0=r_oh1[:, :, :], scalar=-BIG, in1=r_el[:, :, :], op0=MUL, op1=ADD),
                lambda e: e.tensor_reduce(out=r_m2[:, :], in_=r_t[:, :, :], axis=AX.X, op=ALU.max),
                lambda e: e.tensor_tensor(out=r_oh2[:, :, :], in0=r_t[:, :, :], in1=bc32(r_m2[:, :]), op=ALU.is_equal),
                lambda e: e.tensor_tensor(out=r_w1[:, :], in0=r_m1[:, :], in1=r_m2[:, :], op=SUB)],
                reads=["lg", "r_b", "r_a"], writes=["r_a", "router1"])
            P.op("act", lambda e: e.activation(out=r_w1[:, :], in_=r_w1[:, :], func=AF.Sigmoid), reads=["router1"], writes=["r_w1"])
            P.seq("dve", [
                lambda e: e.tensor_scalar(out=r_w2[:, :], in0=r_w1[:, :], scalar1=-1.0, scalar2=1.0, op0=MUL, op1=ADD),
                lambda e: e.tensor_tensor(out=r_w1[:, :], in0=r_w1[:, :], in1=r_gw[:, :], op=MUL),
                lambda e: e.tensor_tensor(out=r_w2[:, :], in0=r_w2[:, :], in1=r_gw[:, :], op=MUL),
                lambda e: e.tensor_tensor(out=r_oh1[:, :, :], in0=r_oh1[:, :, :], in1=bc32(r_w1[:, :]), op=MUL),
                lambda e: e.tensor_tensor(out=r_oh2[:, :, :], in0=r_oh2[:, :, :], in1=bc32(r_w2[:, :]), op=MUL),
                lambda e: e.tensor_tensor(out=cw[:, :, :], in0=r_oh1[:, :, :], in1=r_oh2[:, :, :], op=ADD)],
                reads=["router1", "r_w1"], writes=["cw", "router1"])
            if "cw" in dbg:
                tcw = dbgt("cw", [128, NT, 32])
                P.dma("sp", lambda e: e.dma_start(out=tcw[:, :, :], in_=cw[:, :, :]), "out", reads=["cw"])
            esD.close()
            P.barrier()

            NE = 32 if stage >= 5 else 0
            esE = ExitStack()
            esE.__enter__()
            sb = lambda name, shape, dt: esE.enter_context(nc.sbuf_tensor(name, shape, dt))
            ps = lambda name, shape, dt: esE.enter_context(nc.psum_tensor(name, shape, dt))
            yacc = sb("yacc", [128, NT, D], F32)
            Wg = [sb(f"Wg{i}", [128, 8, 512], BF16) for i in range(2)]
            Wu = [sb(f"Wu{i}", [128, 8, 512], BF16) for i in range(2)]
            Wd = [sb(f"Wd{i}", [128, 4, D], BF16) for i in range(2)]
            actT = sb("actT", [128, 4, NTOK], BF16)
            sgt = [sb(f"sgt{i}", [128, 512], F32) for i in range(2)]
            pg = [ps(f"pg{i}", [128, 512], F32) for i in range(2)]
            pu2 = [ps(f"pu2_{i}", [128, 512], F32) for i in range(2)]
            pd = [ps(f"pd{i}", [128, 512], F32) for i in range(2)]
            P.op("pool", lambda e: e.memset(yacc[:, :, :], 0.0), writes=[f"yacc{t}_{hf}" for t in range(NT) for hf in range(2)])
            h2T_all = [f"h2T_{i}" for i in range(4)]
            for ex in range(NE):
                s_ = ex % 2
                P.dma("pool", lambda e, ex=ex, s_=s_: e.dma_start(out=Wg[s_][:, :, :], in_=w_gate[ex].rearrange("(kc p) c -> p kc c", p=128)), f"Wg{s_}", writes=[f"Wg{s_}"])
                P.dma("pool", lambda e, ex=ex, s_=s_: e.dma_start(out=Wu[s_][:, :, :], in_=w_up[ex].rearrange("(kc p) c -> p kc c", p=128)), f"Wu{s_}", writes=[f"Wu{s_}"])
                P.dma("pool", lambda e, ex=ex, s_=s_: e.dma_start(out=Wd[s_][:, :, :], in_=w_down[ex].rearrange("(kc p) c -> p kc c", p=128)), f"Wd{s_}", writes=[f"Wd{s_}"])
                it = 0
                for tb in range(4):
                    tbs = slice(tb * 512, (tb + 1) * 512)
                    for m in range(4):
                        b_ = it % 2
                        it += 1
                        def f_gu(e, s_=s_, m=m, tbs=tbs, b_=b_):
                            for k in range(8):
                                e.matmul(out=pg[b_][:, :], lhsT=Wg[s_][:, k, m * 128:(m + 1) * 128], rhs=h2T[:, k, tbs], start=(k == 0), stop=(k == 7))
                            for k in range(8):
                                i = e.matmul(out=pu2[b_][:, :], lhsT=Wu[s_][:, k, m * 128:(m + 1) * 128], rhs=h2T[:, k, tbs], start=(k == 0), stop=(k == 7))
                            return i
                        P.op("pe", f_gu, reads=[f"Wg{s_}", f"Wu{s_}", f"h2T_{tb}"], writes=[f"pg{b_}", f"pu2_{b_}"])
                        P.op("act", lambda e, b_=b_: e.activation(out=sgt[b_][:, :], in_=pg[b_][:, :], func=AF.Silu), reads=[f"pg{b_}"], writes=[f"sgt{b_}"])
                        P.op("dve", lambda e, b_=b_, m=m, tbs=tbs: e.tensor_tensor(out=actT[:, m, tbs], in0=sgt[b_][:, :], in1=pu2[b_][:, :], op=MUL),
                             reads=[f"sgt{b_}", f"pu2_{b_}"], writes=[f"actT_{tb}"])
                it = 0
                for t in range(NT):
                    ts_ = slice(t * 128, (t + 1) * 128)
                    for hf in range(2):
                        b_ = it % 2
                        it += 1
                        def f_d(e, s_=s_, ts_=ts_, hf=hf, b_=b_):
                            for m in range(4):
                                i = e.matmul(out=pd[b_][:, :], lhsT=actT[:, m, ts_], rhs=Wd[s_][:, m, hf * 512:(hf + 1) * 512], start=(m == 0), stop=(m == 3))
                            return i
                        P.op("pe", f_d, reads=[f"Wd{s_}", f"actT_{t // 4}"], writes=[f"pd{b_}"])
                        P.op("dve", lambda e, t=t, hf=hf, b_=b_, ex=ex: e.scalar_tensor_tensor(out=yacc[:, t, hf * 512:(hf + 1) * 512], in0=pd[b_][:, :], scalar=cw[:, t, ex:ex + 1],
                                                                                                in1=yacc[:, t, hf * 512:(hf + 1) * 512], op0=MUL, op1=ADD),
                             reads=[f"pd{b_}", "cw", f"yacc{t}_{hf}"], writes=[f"yacc{t}_{hf}"])

            P.dma("sp", lambda e: e.dma_start(out=g_bc[:, :], in_=g_fin[0:1, :].partition_broadcast(128)), "setupF", writes=["g_bc"])
            for t in range(NT):
                s_ = t % 2
                P.dma("sp", lambda e, t=t, s_=s_: e.dma_start(out=xt2[s_][:, :], in_=xmid_scr[t * 128:(t + 1) * 128, :]), f"xt2_{s_}", reads=[f"xmid{t}"], writes=[f"xt2_{s_}"])
                P.op("dve", lambda e, t=t, s_=s_: e.tensor_tensor(out=xt2[s_][:, :], in0=xt2[s_][:, :], in1=yacc[:, t, :], op=ADD),
                     reads=[f"xt2_{s_}", f"yacc{t}_0", f"yacc{t}_1"], writes=[f"xt2_{s_}"])
                P.seq("act", [lambda e, s_=s_, t=t: e.activation(out=junk2[:, :], in_=xt2[s_][:, :], func=AF.Square, accum_out=ssq2[:, NT + t:NT + t + 1]),
                              lambda e, t=t: e.activation(out=ssq2[:, NT + t:NT + t + 1], in_=ssq2[:, NT + t:NT + t + 1], func=AF.Sqrt, scale=1.0 / D, bias=EPS)],
                      reads=[f"xt2_{s_}"], writes=["junk2", f"ssq2_{NT + t}"])
                P.op("dve", lambda e, t=t: e.reciprocal(out=rstd2[:, NT + t:NT + t + 1], in_=ssq2[:, NT + t:NT + t + 1]), reads=[f"ssq2_{NT + t}"], writes=[f"rstd2_{NT + t}"])
                P.op("dve", lambda e, s_=s_, t=t: e.scalar_tensor_tensor(out=xt2[s_][:, :], in0=xt2[s_][:, :], scalar=rstd2[:, NT + t:NT + t + 1], in1=g_bc[:, :], op0=MUL, op1=MUL),
                     reads=[f"xt2_{s_}", f"rstd2_{NT + t}", "g_bc"], writes=[f"xt2_{s_}"])
                P.dma("sp", lambda e, t=t, s_=s_: e.dma_start(out=y[t * 128:(t + 1) * 128, :], in_=xt2[s_][:, :]), f"yout{s_}", reads=[f"xt2_{s_}"], writes=[f"y{t}"])
            esE.close()
            esDE.close()

        if "merged" in dbg:
            t = dbgt("merged", [1024, NTOK], BF16)
            nrow = 1024 if stage >= 3 else 512
            P.dma("sp", lambda e: e.dma_start(out=t[0:nrow, :], in_=mergedT_scr[0:nrow, :]), "out", reads=[f"mscr{h}" for h in range(nrow // 128)])
        if "hT" in dbg:
            t2 = dbgt("hT", [128, 8, HALO + NTOK], BF16)
            P.dma("sp", lambda e: e.dma_start(out=t2[:, :, :], in_=hT[:, :, :]), "out", reads=hT_all)
        if "qk" in dbg:
            t3 = dbgt("qs", [128, NTOK], BF16)
            t4 = dbgt("ks", [128, NTOK], BF16)
            t5 = dbgt("expb", [128, NTOK], F32)
            t6 = dbgt("e2", [128, NTOK], F32)
            P.dma("sp", lambda e: e.dma_start(out=t3[:, :], in_=qsT[:, :]), "out", reads=["qsT"])
            P.dma("sp", lambda e: e.dma_start(out=t4[:, :], in_=ksT[:, :]), "out", reads=["ksT"])
            P.dma("sp", lambda e: e.dma_start(out=t5[:, :], in_=expb[:, :]), "out", reads=expb_all)
            P.dma("sp", lambda e: e.dma_start(out=t6[:, :], in_=e2bc[:, :]), "out", reads=e2_all)
        if "xmid" in dbg:
            txm = dbgt("xmid", [NTOK, D])
            P.dma("sp", lambda e: e.dma_start(out=txm[:, :], in_=xmid_scr[:, :]), "out", reads=[f"xmid{t}" for t in range(NT)])
        P.final_wait("sp", ["out", "yout0", "yout1"])

        with nc.Block() as block:
            @block.tensor
            def _(e): P.replay("pe", e)
            @block.scalar
            def _(e): P.replay("act", e)
            @block.vector
            def _(e): P.replay("dve", e)
            @block.gpsimd
            def _(e): P.replay("pool", e)
            @block.sync
            def _(e): P.replay("sp", e)
    return nc, dbg_out


def make_in_maps(inputs):
    f = lambda a: np.ascontiguousarray(a, dtype=np.float32)
    x = inputs["x"]
    common = {
        "g_mix": f(inputs["norm_mix_g"].reshape(1, D)),
        "w_in": f(inputs["w_in"].reshape(D, 2568)),
        "conv_wT": f(inputs["conv_w"].reshape(4, 8, 128).transpose(2, 1, 0)),
        "conv_b": f(inputs["conv_b"].reshape(8, 128).T),
        "i_bias": f(inputs["i_bias"].reshape(1, 4)),
        "f_bias": f(inputs["f_bias"].reshape(1, 4)),
        "g_mlc": f(inputs["mlstm_norm_g"].reshape(4, 128).T),
    }
    lre = inputs["s5_lambda_re"].reshape(32, 64).T
    lim = inputs["s5_lambda_im"].reshape(32, 64).T
    bre = inputs["s5_b_re"].reshape(32, 64, 16).transpose(1, 0, 2)
    bim = inputs["s5_b_im"].reshape(32, 64, 16).transpose(1, 0, 2)
    cre = inputs["s5_c_re"].reshape(32, 16, 64).transpose(2, 0, 1)
    cim = inputs["s5_c_im"].reshape(32, 16, 64).transpose(2, 0, 1)
    glw = inputs["s5_glu_w"].reshape(4, 8, 16, 32)
    WA = np.zeros((4, 128, 128), np.float32)
    WB = np.zeros((4, 128, 128), np.float32)
    for u in range(4):
        for g in range(8):
            WA[u, g * 16:(g + 1) * 16, g * 16:(g + 1) * 16] = glw[u, g, :, 0:16]
            WB[u, g * 16:(g + 1) * 16, g * 16:(g + 1) * 16] = glw[u, g, :, 16:32]
    glb = inputs["s5_glu_b"].reshape(4, 8, 32)
    common.update({
        "lam2_re": f(np.concatenate([lre, lre], 0)), "lam2_im": f(np.concatenate([lim, lim], 0)),
        "logdt": f(inputs["s5_log_dt"].reshape(1, 32)),
        "X1d": f(np.concatenate([bre, bim], 0)), "X2d": f(np.concatenate([bim, bre], 0)),
        "CY1d": f(np.concatenate([cre, cim], 0)), "CY2d": f(np.concatenate([cim, cre], 0)),
        "s5d": f(inputs["s5_d"].reshape(1, 512)),
        "WAd": WA, "WBd": WB,
        "w_out": f(inputs["w_out"].reshape(D, D)), "g_ffn": f(inputs["norm_ffn_g"].reshape(1, D)),
        "w_rt": f(np.concatenate([inputs["router_group_w"].reshape(D, 4), inputs["router_expert_w"].reshape(D, 32)], 1)),
        "b_rt": f(np.concatenate([inputs["router_group_b"].reshape(1, 4), inputs["router_expert_b"].reshape(1, 32)], 1)),
        "w_gate": f(inputs["expert_w_gate"].reshape(32, D, 512)), "w_up": f(inputs["expert_w_up"].reshape(32, D, 512)),
        "w_down": f(inputs["expert_w_down"].reshape(32, 512, D)), "g_fin": f(inputs["norm_final_g"].reshape(1, D)),
        "gbad": f(glb[:, :, 0:16].reshape(4, 128).T), "gbbd": f(glb[:, :, 16:32].reshape(4, 128).T),
    })
    maps = []
    for c in range(NCORES):
        b, p = c // 4, c % 4
        m = dict(common)
        m["x"] = f(x[b, p * NTOK:(p + 1) * NTOK])
        if p == 0:
            m["xh"] = np.zeros((HALO, D), np.float32)
        else:
            m["xh"] = f(x[b, p * NTOK - HALO:p * NTOK])
        pmk = np.zeros((128, 4), np.float32)
        pmk[:, :p] = 1.0
        m["pmask"] = pmk
        maps.append(m)
    return maps


_CACHE = {}


def kernel(**inputs):
    if "nc" not in _CACHE:
        _CACHE["nc"] = build()[0]
    nc = _CACHE["nc"]
    in_maps = make_in_maps(inputs)
    res = run_bass_kernel_spmd(nc, in_maps, core_ids=list(range(NCORES)))
    out = np.empty((2, 4 * NTOK, D), np.float32)
    for c in range(NCORES):
        b, p = c // 4, c % 4
        out[b, p * NTOK:(p + 1) * NTOK] = np.asarray(res.results[c]["y"], dtype=np.float32)
    return out
```
